# Optimizing a Trainium2 kernel written in Bass

```python
import jax, jax.numpy as jnp
from jax import lax
import numpy as np

D_MODEL = 1024
BATCH = 32
SEQ = 2048
DEPTH = 1

HEAD_DIM = 64
ATTN_HEADS = 8
ATTN_KV_HEADS = 2
ATTN_GROUP = ATTN_HEADS // ATTN_KV_HEADS
ATTN_DIM = ATTN_HEADS * HEAD_DIM
KV_DIM = ATTN_KV_HEADS * HEAD_DIM
WINDOW = 128
BLOCK = WINDOW
ROPE_THETA = 10000.0

RWKV_HEADS = 8
RWKV_HEAD = 64
RWKV_DIM = RWKV_HEADS * RWKV_HEAD
DECAY_LORA = 64
ICLR_LORA = 64
GATE_LORA = 160
RWKV_GN_EPS = 64e-5
RWKV_IN = 3 * RWKV_DIM + DECAY_LORA + ICLR_LORA + GATE_LORA

IN_COLS = ATTN_DIM + 2 * KV_DIM + RWKV_IN + 2 * D_MODEL

PEER_HEADS = 8
N_KEYS = 128
N_EXPERTS = N_KEYS * N_KEYS
PEER_QDIM = 128
PEER_HALF = PEER_QDIM // 2
PEER_TOPK = 16
PEER_SEL = PEER_HEADS * PEER_TOPK
PEER_CHUNK = 128

NORM_EPS = 1e-5
NEG_INF = -1e30

kernel_name = 'hybrid_swa_rwkv7_peer_block'


def rms_norm(x, w):
    xf = x.astype(jnp.float32)
    y = xf * lax.rsqrt(jnp.mean(xf * xf, axis=-1, keepdims=True) + NORM_EPS)
    return (y * w.astype(jnp.float32)).astype(x.dtype)


def rope(x, pos):
    inv = ROPE_THETA ** (-jnp.arange(0, HEAD_DIM, 2, dtype=jnp.float32) / HEAD_DIM)
    ang = pos.astype(jnp.float32)[:, None] * inv[None, :]
    cos = jnp.cos(ang)[None, :, None, :]
    sin = jnp.sin(ang)[None, :, None, :]
    x1, x2 = jnp.split(x, 2, axis=-1)
    return jnp.concatenate([x1 * cos - x2 * sin, x2 * cos + x1 * sin], axis=-1)


def prev_block(t):
    return jnp.concatenate([jnp.zeros_like(t[:, :1]), t[:, :-1]], axis=1)


def sliding_window_attention(q, k, v, sinks):
    b, s = q.shape[0], q.shape[1]
    nb = s // BLOCK
    qb = q.reshape(b, nb, BLOCK, ATTN_KV_HEADS, ATTN_GROUP, HEAD_DIM)
    kb = k.reshape(b, nb, BLOCK, ATTN_KV_HEADS, HEAD_DIM)
    vb = v.reshape(b, nb, BLOCK, ATTN_KV_HEADS, HEAD_DIM)
    kb = jnp.concatenate([prev_block(kb), kb], axis=2)
    vb = jnp.concatenate([prev_block(vb), vb], axis=2)
    logits = jnp.einsum('bnqhgd,bnkhd->bnhgqk', qb, kb) * (HEAD_DIM ** -0.5)
    qi = jnp.arange(BLOCK)[:, None] + BLOCK
    kj = jnp.arange(2 * BLOCK)[None, :]
    diff = qi - kj
    band = (diff >= 0) & (diff < WINDOW)
    valid = (jnp.arange(nb)[:, None] * BLOCK + kj - BLOCK) >= 0
    mask = band[None] & valid[:, None, :]
    logits = jnp.where(mask[None, :, None, None], logits, NEG_INF)
    sink = sinks.astype(jnp.float32).reshape(ATTN_KV_HEADS, ATTN_GROUP)[None, None, :, :, None, None]
    m = jnp.maximum(jnp.max(logits, axis=-1, keepdims=True), sink)
    p = jnp.exp(logits - m)
    probs = p / (jnp.sum(p, axis=-1, keepdims=True) + jnp.exp(sink - m))
    out = jnp.einsum('bnhgqk,bnkhd->bnqhgd', probs, vb)
    return out.reshape(b, s, ATTN_DIM)


def token_shift(z, mu):
    z_prev = jnp.concatenate([jnp.zeros_like(z[:, :1]), z[:, :-1]], axis=1)
    return z + mu * (z_prev - z)


def to_heads(t):
    return t.reshape(t.shape[0], t.shape[1], RWKV_HEADS, RWKV_HEAD)


def rwkv7_time_mix(zr, w0, w2, a0, a2, g2, k_k, k_a, r_k, ln_w, ln_b):
    f32 = jnp.float32
    b, s = zr.shape[0], zr.shape[1]
    cuts = [RWKV_DIM, 2 * RWKV_DIM, 3 * RWKV_DIM, 3 * RWKV_DIM + DECAY_LORA,
            3 * RWKV_DIM + DECAY_LORA + ICLR_LORA]
    r, k, v, w_lo, a_lo, g_lo = jnp.split(zr, cuts, axis=-1)
    r, k, v = r.astype(f32), k.astype(f32), v.astype(f32)
    w = -jax.nn.softplus(-(w0 + jnp.tanh(w_lo) @ w2).astype(f32)) - 0.5
    decay = jnp.exp(-jnp.exp(w))
    a = jax.nn.sigmoid((a0 + a_lo @ a2).astype(f32))
    g = (jax.nn.sigmoid(g_lo) @ g2).astype(f32)
    kk = to_heads(k * k_k.astype(f32))
    kk = kk / jnp.maximum(jnp.sqrt(jnp.sum(kk * kk, axis=-1, keepdims=True)), 1e-12)
    k = k * (1.0 + (a - 1.0) * k_a.astype(f32))
    rh, kh, vh, wh, ah = to_heads(r), to_heads(k), to_heads(v), to_heads(decay), to_heads(a)

    def step(state, inp):
        r_t, w_t, k_t, v_t, kk_t, a_t = inp
        sa = jnp.einsum('bhvk,bhk->bhv', state, -kk_t)
        state = (state * w_t[:, :, None, :]
                 + sa[..., None] * (kk_t * a_t)[:, :, None, :]
                 + v_t[..., None] * k_t[:, :, None, :])
        return state, jnp.einsum('bhvk,bhk->bhv', state, r_t)

    tm = lambda t: jnp.swapaxes(t, 0, 1)
    state0 = jnp.zeros((b, RWKV_HEADS, RWKV_HEAD, RWKV_HEAD), f32)
    _, y = lax.scan(step, state0, (tm(rh), tm(wh), tm(kh), tm(vh), tm(kk), tm(ah)))
    y = tm(y)
    mean = jnp.mean(y, axis=-1, keepdims=True)
    var = jnp.mean(jnp.square(y - mean), axis=-1, keepdims=True)
    y = ((y - mean) * lax.rsqrt(var + RWKV_GN_EPS)).reshape(b, s, RWKV_DIM)
    y = y * ln_w.astype(f32) + ln_b.astype(f32)
    bonus = jnp.sum(rh * kh * r_k.astype(f32), axis=-1, keepdims=True) * vh
    y = y + bonus.reshape(b, s, RWKV_DIM)
    return (y * g).astype(zr.dtype)


def peer_ffn(xn, wq, subkeys, u_tab, v_tab):
    f32 = jnp.float32
    b, s, d = xn.shape
    t = b * s
    xt = xn.reshape(t, d)
    q = (xt @ wq).reshape(t, PEER_HEADS, 2, PEER_HALF)
    sc = jnp.einsum('thpd,hpnd->thpn', q, subkeys).astype(f32)
    s1, i1 = lax.top_k(sc[:, :, 0], PEER_TOPK)
    s2, i2 = lax.top_k(sc[:, :, 1], PEER_TOPK)
    cand = (s1[..., :, None] + s2[..., None, :]).reshape(t, PEER_HEADS, PEER_TOPK * PEER_TOPK)
    cidx = (i1[..., :, None] * N_KEYS + i2[..., None, :]).reshape(t, PEER_HEADS, PEER_TOPK * PEER_TOPK)
    top, pos = lax.top_k(cand, PEER_TOPK)
    idx = jnp.take_along_axis(cidx, pos, axis=-1)
    gate = jax.nn.softmax(top, axis=-1)
    nc = t // PEER_CHUNK

    def expert_block(args):
        xc, ic, gc = args
        hpre = jnp.einsum('ckd,cd->ck', u_tab[ic], xc).astype(f32)
        act = jax.nn.gelu(hpre, approximate=False) * gc
        return jnp.einsum('ck,ckd->cd', act.astype(v_tab.dtype), v_tab[ic])

    out = lax.map(expert_block, (xt.reshape(nc, PEER_CHUNK, d),
                                 idx.reshape(nc, PEER_CHUNK, PEER_SEL),
                                 gate.reshape(nc, PEER_CHUNK, PEER_SEL)))
    return out.reshape(b, s, d).astype(xn.dtype)


def setup_inputs(seed: int = 0) -> dict:
    key = jax.random.key(seed)
    ks = jax.random.split(key, 24)
    L = DEPTH
    f32 = jnp.float32
    nrm = lambda k, shape, scale: jax.random.normal(k, shape, f32) * scale
    return {
        'x': nrm(ks[0], (BATCH, SEQ, D_MODEL), 1.0),
        'norm_mix_w': 1.0 + nrm(ks[1], (L, D_MODEL), 0.02),
        'w_in': nrm(ks[2], (L, D_MODEL, IN_COLS), D_MODEL ** -0.5),
        'shift_mu': jax.random.uniform(ks[3], (L, RWKV_IN), f32),
        'attn_sinks': nrm(ks[4], (L, ATTN_HEADS), 0.5),
        'decay_w0': -1.0 + nrm(ks[5], (L, RWKV_DIM), 0.5),
        'decay_w2': nrm(ks[6], (L, DECAY_LORA, RWKV_DIM), 0.1 * DECAY_LORA ** -0.5),
        'iclr_a0': nrm(ks[7], (L, RWKV_DIM), 0.1),
        'iclr_a2': nrm(ks[8], (L, ICLR_LORA, RWKV_DIM), 0.5 * ICLR_LORA ** -0.5),
        'gate_g2': nrm(ks[9], (L, GATE_LORA, RWKV_DIM), GATE_LORA ** -0.5),
        'k_k': 0.85 + nrm(ks[10], (L, RWKV_DIM), 0.02),
        'k_a': 1.0 + nrm(ks[11], (L, RWKV_DIM), 0.02),
        'r_k': nrm(ks[12], (L, RWKV_HEADS, RWKV_HEAD), 0.1),
        'ln_x_w': 1.0 + nrm(ks[13], (L, RWKV_DIM), 0.02),
        'ln_x_b': nrm(ks[14], (L, RWKV_DIM), 0.02),
        'proj_attn': nrm(ks[15], (L, ATTN_DIM, D_MODEL), ATTN_DIM ** -0.5),
        'proj_rwkv': nrm(ks[16], (L, RWKV_DIM, D_MODEL), RWKV_DIM ** -0.5),
        'w_out': nrm(ks[17], (L, D_MODEL, D_MODEL), D_MODEL ** -0.5),
        'norm_ffn_w': 1.0 + nrm(ks[18], (L, D_MODEL), 0.02),
        'peer_wq': nrm(ks[19], (L, D_MODEL, PEER_HEADS * PEER_QDIM), D_MODEL ** -0.5),
        'peer_subkeys': nrm(ks[20], (L, PEER_HEADS, 2, N_KEYS, PEER_HALF), PEER_HALF ** -0.5),
        'peer_u': nrm(ks[21], (L, N_EXPERTS, D_MODEL), D_MODEL ** -0.5),
        'peer_v': nrm(ks[22], (L, N_EXPERTS, D_MODEL), PEER_HEADS ** -0.5),
        'norm_final_w': 1.0 + nrm(ks[23], (D_MODEL,), 0.02),
    }


def reference(x, norm_mix_w, w_in, shift_mu, attn_sinks, decay_w0, decay_w2, iclr_a0, iclr_a2,
              gate_g2, k_k, k_a, r_k, ln_x_w, ln_x_b, proj_attn, proj_rwkv, w_out, norm_ffn_w,
              peer_wq, peer_subkeys, peer_u, peer_v, norm_final_w):
    f32 = jnp.float32
    b, s = x.shape[0], x.shape[1]
    pos = jnp.arange(s, dtype=jnp.int32)
    o1 = ATTN_DIM
    o2 = o1 + KV_DIM
    o3 = o2 + KV_DIM
    o4 = o3 + RWKV_IN
    o5 = o4 + D_MODEL
    h = x
    for l in range(DEPTH):
        xn = rms_norm(h, norm_mix_w[l])
        z = xn @ w_in[l]
        q = rope(z[..., :o1].reshape(b, s, ATTN_HEADS, HEAD_DIM).astype(f32), pos)
        k = rope(z[..., o1:o2].reshape(b, s, ATTN_KV_HEADS, HEAD_DIM).astype(f32), pos)
        v = z[..., o2:o3].reshape(b, s, ATTN_KV_HEADS, HEAD_DIM).astype(f32)
        y_attn = sliding_window_attention(q, k, v, attn_sinks[l]).astype(h.dtype)
        zr = token_shift(z[..., o3:o4], shift_mu[l])
        y_rwkv = rwkv7_time_mix(zr, decay_w0[l], decay_w2[l], iclr_a0[l], iclr_a2[l], gate_g2[l],
                                k_k[l], k_a[l], r_k[l], ln_x_w[l], ln_x_b[l]).astype(h.dtype)
        g_attn = jax.nn.sigmoid(z[..., o4:o5])
        g_rwkv = jax.nn.sigmoid(z[..., o5:])
        merged = g_attn * (y_attn @ proj_attn[l]) + g_rwkv * (y_rwkv @ proj_rwkv[l])
        h = h + merged @ w_out[l]
        h = h + peer_ffn(rms_norm(h, norm_ffn_w[l]), peer_wq[l], peer_subkeys[l], peer_u[l], peer_v[l])
    return rms_norm(h, norm_final_w)
```

```python
import numpy as np
import concourse.bass as bass
import concourse.mybir as mybir

F32 = mybir.dt.float32
BF16 = mybir.dt.bfloat16
U32 = mybir.dt.uint32
I32 = mybir.dt.int32
ALU = mybir.AluOpType
AF = mybir.ActivationFunctionType
AX = mybir.AxisListType

NDMA_SEMS = 12


class T:
    def __init__(self, h, name):
        self.h = h
        self.name = name
        self.state = {}

    def __getitem__(self, k):
        return self.h[k]


class Prog:
    def __init__(self, nc):
        self.nc = nc
        self.ops = {e: [] for e in ("pe", "dve", "act", "pool", "sp")}
        self.cms = []
        self.ndma = {e: 0 for e in ("sp", "act", "pool")}
        self._defer = None

    def sb(self, name, shape, dtype):
        cm = self.nc.sbuf_tensor("sb_" + name, list(shape), dtype)
        h = cm.__enter__()
        self.cms.append(cm)
        return T(h, name)

    def ps(self, name, shape, dtype):
        cm = self.nc.psum_tensor("ps_" + name, list(shape), dtype)
        h = cm.__enter__()
        self.cms.append(cm)
        return T(h, name)

    def wrap(self, ap, name):
        return T(ap, name)

    def defer_begin(self):
        self._defer = []

    def defer_end(self):
        lst, self._defer = self._defer, None
        return lst

    def drain(self, lst, k):
        for _ in range(min(k, len(lst))):
            eng, fn, reads, writes, dma, extra = lst.pop(0)
            self.op(eng, fn, reads, writes, dma, extra)

    def mark(self):
        return len(self.cms)

    def release(self, mark):
        lasts = []
        for e in ("pe", "dve", "act", "pool", "sp"):
            for j in range(len(self.ops[e]) - 1, -1, -1):
                if self.ops[e][j][2][0] not in ("dma", "bar"):
                    lasts.append((e, j))
                    break
        dmat = []
        for q in ("sp", "act", "pool"):
            n = self.ndma[q]
            dmat += [("dma", q, i) for i in range(max(0, n - NDMA_SEMS), n)]
        for e in ("pe", "dve", "act", "pool", "sp"):
            self.ops[e].append((None, lasts + dmat, ("bar", e, len(self.ops[e]))))
        while len(self.cms) > mark:
            self.cms.pop().__exit__(None, None, None)

    def _collect(self, t, key, is_write, deps):
        if key is None:
            keys = list(t.state.keys())
        else:
            keys = [k for k in (key, None) if k in t.state]
        for k in keys:
            w, rs = t.state[k]
            if w is not None:
                deps.append(w)
            if is_write:
                deps.extend(rs)

    def _update(self, t, key, is_write, me):
        if is_write:
            if key is None:
                t.state = {None: [me, []]}
            else:
                t.state[key] = [me, []]
        else:
            st = t.state.setdefault(key, [None, []])
            if me[0] != "dma":
                st[1] = [r for r in st[1] if not (r[0] == me[0])]
            st[1].append(me)

    def op(self, eng, fn, reads=(), writes=(), dma=False, extra=()):
        if self._defer is not None:
            self._defer.append((eng, fn, reads, writes, dma, extra))
            return None
        deps = list(extra)
        norm = lambda x: x if isinstance(x, tuple) else (x, None)
        reads = [norm(r) for r in reads]
        writes = [norm(w) for w in writes]
        for t, k in reads:
            self._collect(t, k, False, deps)
        for t, k in writes:
            self._collect(t, k, True, deps)
        idx = len(self.ops[eng])
        if dma:
            n = self.ndma[eng]
            self.ndma[eng] += 1
            me = ("dma", eng, n)
        else:
            me = (eng, idx)
        for t, k in reads:
            self._update(t, k, False, me)
        for t, k in writes:
            self._update(t, k, True, me)
        self.ops[eng].append((fn, deps, me))
        return me

    def emit(self):
        nc = self.nc
        engs = ("pe", "dve", "act", "pool", "sp")
        sem_cms = {}
        sems = {}
        for e in engs:
            cm = nc.semaphore("s_" + e)
            sems[e] = cm.__enter__()
            self.cms.append(cm)
        dsems = {}
        for e in ("sp", "act", "pool"):
            if self.ndma[e]:
                lst = []
                for i in range(NDMA_SEMS):
                    cm = nc.semaphore("d_%s_%d" % (e, i))
                    lst.append(cm.__enter__())
                    self.cms.append(cm)
                dsems[e] = lst
        ops = self.ops

        def run(ename, engine):
            seen = {}
            cnt = 0
            for fn, deps, me in ops[ename]:
                need = {}
                for d in deps:
                    if d[0] == "dma":
                        s = dsems[d[1]][d[2] % NDMA_SEMS]
                        v = 16 * (d[2] // NDMA_SEMS + 1)
                    else:
                        if d[0] == ename:
                            if ename == "pe" or fn is None:
                                continue
                        s = sems[d[0]]
                        v = d[1] + 1 - self.dma_before[d[0]][d[1]]
                    key = s.num if hasattr(s, "num") else id(s)
                    if v > need.get(key, (None, 0))[1]:
                        need[key] = (s, v)
                if me[0] == "dma":
                    n = me[2]
                    s = dsems[ename][n % NDMA_SEMS]
                    if n >= NDMA_SEMS:
                        key = s.num if hasattr(s, "num") else id(s)
                        v = 16 * (n // NDMA_SEMS)
                        if v > need.get(key, (None, 0))[1]:
                            need[key] = (s, v)
                for key, (s, v) in need.items():
                    if seen.get(key, 0) >= v:
                        continue
                    engine.wait_ge(s, v)
                    seen[key] = v
                if fn is None:
                    continue
                ins = fn(engine)
                if me[0] == "dma":
                    ins.then_inc(dsems[ename][me[2] % NDMA_SEMS], 16)
                else:
                    ins.then_inc(sems[ename], 1)

        self.dma_before = {}
        for e in engs:
            c = 0
            lst = []
            for fn, deps, me in ops[e]:
                lst.append(c)
                if me[0] in ("dma", "bar"):
                    c += 1
            self.dma_before[e] = lst

        with nc.Block() as block:
            @block.tensor
            def _(eng):
                run("pe", eng)

            @block.vector
            def _(eng):
                run("dve", eng)

            @block.scalar
            def _(eng):
                run("act", eng)
                self._drain("act", eng, dsems)

            @block.gpsimd
            def _(eng):
                run("pool", eng)
                self._drain("pool", eng, dsems)

            @block.sync
            def _(eng):
                run("sp", eng)
                self._drain("sp", eng, dsems)

    def _drain(self, e, eng, dsems):
        n = self.ndma[e]
        if not n:
            return
        for j in range(min(n, NDMA_SEMS)):
            last = ((n - 1 - j) // NDMA_SEMS) * NDMA_SEMS + j
            eng.wait_ge(dsems[e][j], 16 * (last // NDMA_SEMS + 1))

    def close(self):
        for cm in reversed(self.cms):
            cm.__exit__(None, None, None)
from concourse.bass_utils import run_bass_kernel_spmd

D_MODEL = 1024
C_DEC = 0.6065306597126334
STAGE = 0
SKIP = ""
V_MU, V_W0, V_A0, V_KK, V_KA, V_LNW, V_LNB, V_RK, V_SINK, V_FIN = 0, 1824, 2336, 2848, 3360, 3872, 4384, 4896, 5408, 5416
NVEC = 5416 + 1024
C_ID, C_SU, C_IU, C_SL, C_BD, C_SH, C_CA, C_COS, C_SIN, C_ONE, C_IOTA = 0, 128, 256, 384, 512, 640, 768, 896, 1408, 1920, 1921
NCST = 1921 + 128


def make_cst():
    c = np.zeros((128, NCST), np.float32)
    i = np.arange(128)
    r, q = i[:, None], i[None, :]
    c[:, C_ID:C_ID + 128] = (r == q)
    c[:, C_SU:C_SU + 128] = (r < q)
    c[:, C_IU:C_IU + 128] = (r <= q)
    c[:, C_SL:C_SL + 128] = (r > q)
    c[:, C_BD:C_BD + 128] = ((r // 64) == (q // 64))
    c[:, C_SH:C_SH + 128] = (r == q - 1)
    c[127, C_CA] = 1.0
    inv = 10000.0 ** (-np.arange(0, 64, 2, dtype=np.float32) / 64)
    pos = (np.arange(16)[None, :] * 128 + i[:, None]).astype(np.float32)
    ang = pos[:, :, None] * inv[None, None, :]
    c[:, C_COS:C_COS + 512] = np.cos(ang).reshape(128, 512)
    c[:, C_SIN:C_SIN + 512] = np.sin(ang).reshape(128, 512)
    c[:, C_ONE] = 1.0
    c[:, C_IOTA:C_IOTA + 128] = q
    return c


class Ctx:
    pass


def helpers(p):
    h = Ctx()

    def tt(eng, out, in0, in1, op, r, w):
        p.op(eng, lambda e: e.tensor_tensor(out=out, in0=in0, in1=in1, op=op), r, w)

    def stt(eng, out, in0, scalar, in1, op0, op1, r, w):
        p.op(eng, lambda e: e.scalar_tensor_tensor(out=out, in0=in0, scalar=scalar, in1=in1, op0=op0, op1=op1), r, w)

    def ts(eng, out, in0, s1, s2, op0, op1, r, w):
        if s2 is None:
            p.op(eng, lambda e: e.tensor_scalar(out=out, in0=in0, scalar1=s1, scalar2=None, op0=op0), r, w)
        else:
            p.op(eng, lambda e: e.tensor_scalar(out=out, in0=in0, scalar1=s1, scalar2=s2, op0=op0, op1=op1), r, w)

    def act(out, in_, func, r, w, **kw):
        p.op("act", lambda e: e.activation(out=out, in_=in_, func=func, **kw), r, w)

    def cp(eng, out, in_, r, w):
        if eng == "act":
            p.op("act", lambda e: e.activation(out=out, in_=in_, func=AF.Copy), r, w)
        else:
            p.op(eng, lambda e: e.tensor_copy(out=out, in_=in_), r, w)

    def red(out, in_, r, w, op=ALU.add):
        p.op("dve", lambda e: e.tensor_reduce(out=out, in_=in_, axis=AX.X, op=op), r, w)

    def dma(eng, out, in_, r, w):
        p.op(eng, lambda e: e.dma_start(out=out, in_=in_), r, w, dma=True)

    def mms(fn, r, w):
        p.op("pe", fn, r, w)

    h.tt, h.stt, h.ts, h.act, h.cp, h.red, h.dma, h.mms = tt, stt, ts, act, cp, red, dma, mms
    return h


class Rot:
    def __init__(self, tiles):
        self.tiles = tiles
        self.i = 0

    def __call__(self):
        t = self.tiles[self.i % len(self.tiles)]
        self.i += 1
        return t


def load_w_bf16(p, h, name, dram, K, N, ncol0, ncols, stg, scale_t=None, scale_off=0, dt=BF16):
    kc = K // 128
    wt = p.sb(name, [128, kc, ncols], dt)
    for c in range(kc):
        s = stg()
        h.dma("sp", s[:, 0:ncols], dram[c * 128:(c + 1) * 128, ncol0:ncol0 + ncols], [], [s])
        if scale_t is not None:
            h.act(wt[:, c, :], s[:, 0:ncols], AF.Copy, [s, scale_t], [(wt, c)], scale=scale_t[:, scale_off + c:scale_off + c + 1])
        else:
            h.cp("pool", wt[:, c, :], s[:, 0:ncols], [s], [(wt, c)])
    return wt


def phase_a1(p, nc, D, n_seq, n_tiles, S_TOK):
    h = helpers(p)
    tt, stt, ts, act, cp, red, dma, mms = h.tt, h.stt, h.ts, h.act, h.cp, h.red, h.dma, h.mms
    NZ = 2592
    cst = p.sb("cst", [128, NCST], F32)
    dma("sp", cst[:], D["cst"], [], [cst])
    vec = p.sb("vec", [128, V_SINK + 8], F32)
    dma("sp", vec[:], D["vecs"][0:V_SINK + 8].partition_broadcast(128), [], [vec])
    nwp = p.sb("nwp", [128, 16], F32)
    dma("sp", nwp[:], D["nwp"], [], [nwp])
    ident = cst[:, C_ID:C_ID + 128]
    z = p.sb("z", [128, NZ], F32)
    Wb = load_w_bf16(p, h, "Wb1", D["w_in"], 1024, 4640, 0, NZ, (lambda: z), nwp, 0)
    w2 = p.sb("w2", [128, 512], F32)
    dma("sp", w2[0:64, :], D["decay_w2"], [], [w2])
    dma("sp", w2[64:128, :], D["iclr_a2"], [], [w2])
    g2 = p.sb("g2", [128, 2, 512], F32)
    dma("sp", g2[:, 0, :], D["gate_g2"][0:128, :], [], [g2])
    dma("sp", g2[0:32, 1, :], D["gate_g2"][128:160, :], [], [g2])
    negsink = p.sb("negsink", [128, 8], F32)
    ts("dve", negsink[:], vec[:, V_SINK:V_SINK + 8], -1.0, None, ALU.mult, None, [vec], [negsink])

    xt = Rot([p.sb("xt%d" % i, [128, 1024], F32) for i in range(2)])
    ss = p.sb("ss", [128, 1], F32)
    rstd = p.sb("rstd", [128, 1], F32)
    xs = p.sb("xs", [128, 1024], F32)
    xsT = p.sb("xsT", [128, 8, 128], BF16)
    carry = p.sb("carry", [128, 1824], F32)
    p.op("pool", lambda e: e.memset(carry[:], 0.0), [], [carry])
    zr = p.sb("zr", [128, 1824], F32)
    lin = p.sb("lin", [128, 288], F32)
    linT = p.sb("linT", [128, 3, 128], F32)
    T5 = Rot([p.sb("t5_%d" % i, [128, 512], F32) for i in range(12)])
    gv = p.sb("gv", [128, 512], F32)
    k2 = p.sb("k2", [128, 512], F32)
    M8 = Rot([p.sb("m8_%d" % i, [128, 8, 128], F32) for i in range(8)])
    M8d = [p.sb("m8d_%d" % i, [128, 8, 128], F32) for i in range(3)]
    Tq = Rot([p.sb("tq_%d" % i, [128, 4, 128], F32) for i in range(4)])
    sm = Rot([p.sb("sm_%d" % i, [128, 8], F32) for i in range(12)])
    PB = Rot([p.ps("pb%d" % i, [128, 512], F32) for i in range(6)])
    PV = [p.ps("pv%d" % i, [128, 512], F32) for i in range(2)]
    NDUM = 0
    if NDUM:
        dumb = p.ps("dumb", [128, 512], F32)
        mms0 = mms

        def mms(fn, r, w):
            def fn2(e):
                ins = fn(e)
                for _ in range(NDUM):
                    e.matmul(dumb[:], lhsT=Wb[:, 0, 0:128], rhs=Wb[:, 0, 0:512], start=True, stop=True)
                return ins
            mms0(fn2, r, w)
    ST = p.sb("ST", [128, 4, 128], F32)
    gC = p.sb("gC", [128, 4], F32)
    qk = p.sb("qk", [128, 10, 64], F32)
    kdup = p.sb("kdup", [128, 2, 2, 64], F32)
    qT = p.sb("qT", [128, 4, 128], BF16)
    kT = [p.sb("kT%d" % i, [128, 2, 128], BF16) for i in range(2)]
    va = [p.sb("va%d" % i, [128, 2, 65], BF16) for i in range(2)]
    for i in range(2):
        p.op("pool", lambda e, i=i: e.memset(va[i][:], 1.0), [], [va[i]])
    pT = Rot([p.sb("pT%d" % i, [128, 2, 128], BF16) for i in range(4)])
    yout = Rot([p.sb("yout%d" % i, [128, 1024], F32) for i in range(1)])

    def vb(off, n=512):
        return vec[:, off:off + n]

    order = [(b, n) for b in range(n_seq) for n in range(n_tiles)]
    xq = {}

    def prefetch(i):
        if i < len(order):
            b, n = order[i]
            t0 = b * S_TOK + n * 128
            xq[i] = xt()
            dma("sp", xq[i][:], D["x"][t0:t0 + 128, :], [], [xq[i]])

    prefetch(0)

    def tile_body(i):
            b, n = order[i]
            if n == 0:
                p.op("pool", lambda e: e.memset(ST[:], 0.0), [], [ST])
            tok0 = b * S_TOK + n * 128
            first = (n == 0)
            x_t = xq.pop(i)
            prefetch(i + 1)
            act(xs[:], x_t[:], AF.Square, [x_t], [xs, ss], accum_out=ss[:])
            act(rstd[:], ss[:], AF.Sqrt, [ss], [rstd], bias=1e-5, scale=1.0 / 1024)
            p.op("dve", lambda e: e.reciprocal(out=rstd[:], in_=rstd[:]), [rstd], [rstd])
            ts("dve", xs[:], x_t[:], rstd[:, 0:1], None, ALU.mult, None, [x_t, rstd], [xs])
            for hh in range(2):
                pb = PB()
                mms(lambda e, pb=pb, hh=hh: [e.transpose(out=pb[:, j * 128:(j + 1) * 128], in_=xs[:, (hh * 4 + j) * 128:(hh * 4 + j + 1) * 128], identity=ident) for j in range(4)][-1],
                    [xs, cst], [pb])
                cp("act" if hh else "dve", xsT[:, hh * 4:hh * 4 + 4, :], pb[:].rearrange("p (a b) -> p a b", a=4), [pb], [(xsT, hh)])
            for cc in range(6):
                c0 = cc * 512
                cw = min(512, NZ - c0)
                pb = PB()
                mms(lambda e, pb=pb, c0=c0, cw=cw: [e.matmul(pb[:, 0:cw], lhsT=xsT[:, c, :], rhs=Wb[:, c, c0:c0 + cw], start=(c == 0), stop=(c == 7)) for c in range(8)][-1],
                    [xsT, Wb], [pb])
                cp("act" if cc % 2 else "dve", z[:, c0:c0 + cw], pb[:, 0:cw], [pb], [(z, cc)])
            if STAGE == 1:
                yo = yout()
                cp("dve", yo[:], z[:, 0:1024], [z], [yo])
                dma("sp", D["y_scr"][tok0:tok0 + 128, :], yo[:], [yo], [])
                return
            cosb = cst[:, C_COS + n * 32:C_COS + n * 32 + 32].unsqueeze(1).to_broadcast([128, 10, 32])
            sinb = cst[:, C_SIN + n * 32:C_SIN + n * 32 + 32].unsqueeze(1).to_broadcast([128, 10, 32])
            zq = z[:, 0:640].rearrange("p (h d) -> p h d", h=10)
            x1, x2 = zq[:, :, 0:32], zq[:, :, 32:64]
            ta, tb_ = T5(), T5()
            tav = ta[:, 0:320].rearrange("p (h d) -> p h d", h=10)
            tbv = tb_[:, 0:320].rearrange("p (h d) -> p h d", h=10)
            zk = [(z, 0), (z, 1)]
            tt("dve", tav, x1, cosb, ALU.mult, zk + [cst], [ta])
            tt("pool", tbv, x2, sinb, ALU.mult, zk + [cst], [tb_])
            tt("dve", qk[:, :, 0:32], tav, tbv, ALU.subtract, [ta, tb_], [(qk, 0)])
            tc_, td = T5(), T5()
            tcv = tc_[:, 0:320].rearrange("p (h d) -> p h d", h=10)
            tdv = td[:, 0:320].rearrange("p (h d) -> p h d", h=10)
            tt("pool", tcv, x2, cosb, ALU.mult, zk + [cst], [tc_])
            tt("dve", tdv, x1, sinb, ALU.mult, zk + [cst], [td])
            tt("pool", qk[:, :, 32:64], tcv, tdv, ALU.add, [tc_, td], [(qk, 1)])
            cp("pool", kdup[:], qk[:, 8:10, :].unsqueeze(2).to_broadcast([128, 2, 2, 64]), [qk], [kdup])
            kTc, kTp = kT[n % 2], kT[(n + 1) % 2]
            vac, vap = va[n % 2], va[(n + 1) % 2]
            cp("pool", vac[:, :, 0:64], z[:, 640:768].rearrange("p (g d) -> p g d", g=2), [(z, 1)], [vac])
            pb = PB()
            mms(lambda e, pb=pb: [e.transpose(out=pb[:, j * 128:(j + 1) * 128], in_=qk[:, 2 * j:2 * j + 2, :].rearrange("p a d -> p (a d)"), identity=ident) for j in range(4)][-1],
                [qk, cst], [pb])
            cp("act", qT[:], pb[:].rearrange("p (a b) -> p a b", a=4), [pb], [qT])
            pb = PB()
            mms(lambda e, pb=pb: [e.transpose(out=pb[:, g * 128:(g + 1) * 128], in_=kdup[:, g, :, :].rearrange("p a d -> p (a d)"), identity=ident) for g in range(2)][-1],
                [kdup, cst], [pb])
            cp("dve", kTc[:], pb[:, 0:256].rearrange("p (a b) -> p a b", a=2), [pb], [kTc])
            yo = yout()
            pv = PV
            for hd in range(0 if "a" in SKIP else 8):
                m, base, g = hd // 2, 64 * (hd % 2), hd // 4
                pb = PB()
                if first:
                    mms(lambda e, pb=pb, m=m, base=base, g=g: e.matmul(pb[:, 128:256], lhsT=kTc[base:base + 64, g, :], rhs=qT[base:base + 64, m, :], start=True, stop=True),
                        [kTc, qT], [pb])
                else:
                    mms(lambda e, pb=pb, m=m, base=base, g=g: [e.matmul(pb[:, 0:128], lhsT=kTp[base:base + 64, g, :], rhs=qT[base:base + 64, m, :], start=True, stop=True),
                                                              e.matmul(pb[:, 128:256], lhsT=kTc[base:base + 64, g, :], rhs=qT[base:base + 64, m, :], start=True, stop=True)][-1],
                        [kTc, kTp, qT], [pb])
                pt = pT()
                lo = 1 if first else 0
                act(pt[:, lo:2, :], pb[:, lo * 128:256].rearrange("p (a b) -> p a b", a=2 - lo), AF.Exp, [pb, negsink], [pt],
                    scale=0.125, bias=negsink[:, hd:hd + 1])
                if not first:
                    tt("pool", pt[:, 0, :], pt[:, 0, :], cst[:, C_SL:C_SL + 128], ALU.mult, [pt, cst], [pt])
                tt("dve", pt[:, 1, :], pt[:, 1, :], cst[:, C_IU:C_IU + 128], ALU.mult, [pt, cst], [pt])
                pvb = pv[hd // 4]
                o0 = (hd % 4) * 65
                if first:
                    mms(lambda e, pvb=pvb, pt=pt, g=g, o0=o0: e.matmul(pvb[:, o0:o0 + 65], lhsT=pt[:, 1, :], rhs=vac[:, g, :], start=True, stop=True),
                        [pt, vac], [pvb])
                else:
                    mms(lambda e, pvb=pvb, pt=pt, g=g, o0=o0: [e.matmul(pvb[:, o0:o0 + 65], lhsT=pt[:, 0, :], rhs=vap[:, g, :], start=True, stop=False),
                                                              e.matmul(pvb[:, o0:o0 + 65], lhsT=pt[:, 1, :], rhs=vac[:, g, :], start=False, stop=True)][-1],
                        [pt, vac, vap], [pvb])
            for hf in range(2):
                pvb = pv[hf]
                pvv = pvb[:, 0:260].rearrange("p (h d) -> p h d", h=4)
                den = sm()
                ts("dve", den[:, 0:4], pvv[:, :, 64], 1.0, None, ALU.add, None, [pvb], [den])
                p.op("dve", lambda e, den=den: e.reciprocal(out=den[:, 0:4], in_=den[:, 0:4]), [den], [den])
                tt("dve", yo[:, hf * 256:(hf + 1) * 256].rearrange("p (h d) -> p h d", h=4), pvv[:, :, 0:64],
                   den[:, 0:4].unsqueeze(2).to_broadcast([128, 4, 64]), ALU.mult, [pvb, den], [(yo, hf)])
            if STAGE == 2:
                cp("dve", yo[:, 512:1024], z[:, 0:512], [z], [yo])
                dma("sp", D["y_scr"][tok0:tok0 + 128, :], yo[:], [yo], [])
                return
            if "r" in SKIP:
                dma("sp", D["y_scr"][tok0:tok0 + 128, :], yo[:], [yo], [(D["y_t"], tok0 // 128)])
                return
            for j in range(4):
                c0 = 768 + j * 512
                cw = min(512, NZ - c0)
                pb = PB()
                zkeys = [(z, 1), (z, 2), (z, 3), (z, 4), (z, 5)]
                if first:
                    mms(lambda e, pb=pb, c0=c0, cw=cw: e.matmul(pb[:, 0:cw], lhsT=cst[:, C_SH:C_SH + 128], rhs=z[:, c0:c0 + cw], start=True, stop=True),
                        zkeys + [cst], [pb])
                else:
                    mms(lambda e, pb=pb, c0=c0, cw=cw: [e.matmul(pb[:, 0:cw], lhsT=cst[:, C_SH:C_SH + 128], rhs=z[:, c0:c0 + cw], start=True, stop=False),
                                                       e.matmul(pb[:, 0:cw], lhsT=cst[:, C_CA:C_CA + 128], rhs=carry[:, c0 - 768:c0 - 768 + cw], start=False, stop=True)][-1],
                        zkeys + [cst, carry], [pb])
                r0 = c0 - 768
                tt("dve", zr[:, r0:r0 + cw], pb[:, 0:cw], z[:, c0:c0 + cw], ALU.subtract, [pb] + zkeys, [(zr, j)])
                tt("dve", zr[:, r0:r0 + cw], zr[:, r0:r0 + cw], vec[:, V_MU + r0:V_MU + r0 + cw], ALU.mult, [(zr, j), vec], [(zr, j)])
                tt("pool", zr[:, r0:r0 + cw], zr[:, r0:r0 + cw], z[:, c0:c0 + cw], ALU.add, [(zr, j)] + zkeys, [(zr, j)])
            cp("pool", carry[96:128, :], z[96:128, 768:NZ], [z], [carry])
            r_, k_, v_ = zr[:, 0:512], zr[:, 512:1024], zr[:, 1024:1536]
            if STAGE == 3:
                cp("dve", yo[:, 512:1024], zr[:, 0:512], [], [yo])
                dma("sp", D["y_scr"][tok0:tok0 + 128, :], yo[:], [yo], [])
                return
            act(lin[:, 0:64], zr[:, 1536:1600], AF.Tanh, [zr], [(lin, 0)])
            cp("pool", lin[:, 64:128], zr[:, 1600:1664], [zr], [(lin, 1)])
            act(lin[:, 128:288], zr[:, 1664:1824], AF.Sigmoid, [zr], [(lin, 2)])
            pb = PB()
            mms(lambda e, pb=pb: [e.transpose(out=pb[:, 0:128], in_=lin[:, 0:128], identity=ident),
                                  e.transpose(out=pb[:, 128:256], in_=lin[:, 128:256], identity=ident),
                                  e.transpose(out=pb[0:32, 256:384], in_=lin[:, 256:288], identity=ident)][-1], [lin, cst], [pb])
            cp("dve", linT[:, 0:2, :], pb[:, 0:256].rearrange("p (a b) -> p a b", a=2), [pb], [(linT, 0)])
            cp("dve", linT[0:32, 2, :], pb[0:32, 256:384], [pb], [(linT, 1)])
            pw, pa_, pg = PB(), PB(), PB()
            mms(lambda e, pw=pw: e.matmul(pw[:], lhsT=linT[0:64, 0, :], rhs=w2[0:64, :], start=True, stop=True), [linT, w2], [pw])
            mms(lambda e, pa_=pa_: e.matmul(pa_[:], lhsT=linT[64:128, 0, :], rhs=w2[64:128, :], start=True, stop=True), [linT, w2], [pa_])
            mms(lambda e, pg=pg: [e.matmul(pg[:], lhsT=linT[:, 1, :], rhs=g2[:, 0, :], start=True, stop=False),
                                  e.matmul(pg[:], lhsT=linT[0:32, 2, :], rhs=g2[0:32, 1, :], start=False, stop=True)][-1], [linT, g2], [pg])
            sg, av = T5(), T5()
            tt("dve", sg[:], pw[:], vb(V_W0), ALU.add, [pw, vec], [sg])
            act(sg[:], sg[:], AF.Sigmoid, [sg], [sg])
            tt("dve", av[:], pa_[:], vb(V_A0), ALU.add, [pa_, vec], [av])
            act(av[:], av[:], AF.Sigmoid, [av], [av])
            cp("act", gv[:], pg[:], [pg], [gv])
            if STAGE == 4:
                cp("dve", yo[:, 512:1024], sg[:], [], [yo])
                dma("sp", D["y_scr"][tok0:tok0 + 128, :], yo[:], [yo], [])
                return
            pc = PB()
            mms(lambda e, pc=pc, sg=sg: e.matmul(pc[:], lhsT=cst[:, C_IU:C_IU + 128], rhs=sg[:], start=True, stop=True), [cst, sg], [pc])
            gam, igam, gprev = T5(), T5(), T5()
            act(gam[:], pc[:], AF.Exp, [pc], [gam], scale=-C_DEC)
            act(igam[:], pc[:], AF.Exp, [pc], [igam], scale=C_DEC)
            tt("dve", gprev[:], pc[:], sg[:], ALU.subtract, [pc, sg], [gprev])
            act(gprev[:], gprev[:], AF.Exp, [gprev], [gprev], scale=-C_DEC)
            pgc = PB()
            mms(lambda e, pgc=pgc, sg=sg: [e.matmul(pgc[:, m:m + 1], lhsT=sg[:, m * 128:(m + 1) * 128], rhs=cst[:, C_ONE:C_ONE + 1], start=True, stop=True) for m in range(4)][-1],
                [cst, sg], [pgc])
            act(gC[:], pgc[:, 0:4], AF.Exp, [pgc], [gC], scale=-C_DEC)
            if STAGE == 5:
                cp("dve", yo[:, 512:1024], gprev[:], [], [yo])
                dma("sp", D["y_scr"][tok0:tok0 + 128, :], yo[:], [yo], [])
                return
            kk, sq = T5(), T5()
            tt("dve", kk[:], k_, vb(V_KK), ALU.mult, [zr, vec], [kk])
            tt("pool", sq[:], kk[:], kk[:], ALU.mult, [kk], [sq])
            if STAGE == 51:
                cp("dve", yo[:, 512:1024], sq[:], [], [yo])
                dma("sp", D["y_scr"][tok0:tok0 + 128, :], yo[:], [yo], [])
                return
            s8 = sm()
            red(s8[:], sq[:].rearrange("p (h d) -> p h d", h=8), [sq], [s8])
            if STAGE == 52:
                cp("dve", yo[:, 512:1024], sq[:], [], [yo])
                dma("sp", D["y_scr"][tok0:tok0 + 128, :], yo[:], [yo], [])
                return
            act(s8[:], s8[:], AF.Sqrt, [s8], [s8], bias=1e-24, scale=1.0)
            p.op("dve", lambda e, s8=s8: e.reciprocal(out=s8[:], in_=s8[:]), [s8], [s8])
            if STAGE == 53:
                cp("dve", yo[:, 512:1024], sq[:], [], [yo])
                dma("sp", D["y_scr"][tok0:tok0 + 128, :], yo[:], [yo], [])
                return
            tt("dve", kk[:].rearrange("p (h d) -> p h d", h=8), kk[:].rearrange("p (h d) -> p h d", h=8),
               s8[:].unsqueeze(2).to_broadcast([128, 8, 64]), ALU.mult, [kk, s8], [kk])
            if STAGE == 54:
                cp("dve", yo[:, 512:1024], kk[:], [], [yo])
                dma("sp", D["y_scr"][tok0:tok0 + 128, :], yo[:], [yo], [])
                return
            t1 = T5()
            stt("dve", t1[:], av[:], -1.0, vb(V_KA), ALU.add, ALU.mult, [av, vec], [t1])
            stt("dve", k2[:], t1[:], 1.0, k_, ALU.add, ALU.mult, [t1, zr], [k2])
            if STAGE == 55:
                cp("dve", yo[:, 512:1024], k2[:], [], [yo])
                dma("sp", D["y_scr"][tok0:tok0 + 128, :], yo[:], [yo], [])
                return
            At, Bt, Kt, Rt = T5(), T5(), T5(), T5()
            stt("dve", At[:], kk[:], -1.0, gprev[:], ALU.mult, ALU.mult, [kk, gprev], [At])
            if STAGE == 56:
                cp("dve", yo[:, 512:1024], At[:], [At], [yo])
                dma("sp", D["y_scr"][tok0:tok0 + 128, :], yo[:], [yo], [])
                return
            tt("pool", Bt[:], kk[:], av[:], ALU.mult, [kk, av], [Bt])
            tt("dve", Bt[:], Bt[:], igam[:], ALU.mult, [Bt, igam], [Bt])
            if STAGE == 57:
                cp("dve", yo[:, 512:1024], Bt[:], [Bt], [yo])
                dma("sp", D["y_scr"][tok0:tok0 + 128, :], yo[:], [yo], [])
                return
            tt("dve", Kt[:], k2[:], igam[:], ALU.mult, [k2, igam], [Kt])
            if STAGE == 58:
                cp("dve", yo[:, 512:1024], Kt[:], [Kt], [yo])
                dma("sp", D["y_scr"][tok0:tok0 + 128, :], yo[:], [yo], [])
                return
            tt("pool", Rt[:], r_, gam[:], ALU.mult, [zr, gam], [Rt])
            if STAGE == 6:
                cp("dve", yo[:, 512:1024], Rt[:], [Rt], [yo])
                dma("sp", D["y_scr"][tok0:tok0 + 128, :], yo[:], [yo], [])
                return
            XT = {}
            for nm, src in (("A", At), ("B", Bt), ("K", Kt), ("R", Rt)):
                pb = PB()
                mms(lambda e, pb=pb, src=src: [e.transpose(out=pb[:, j * 128:(j + 1) * 128], in_=src[:, j * 128:(j + 1) * 128], identity=ident) for j in range(4)][-1],
                    [src, cst], [pb])
                dst = Tq()
                cp("act" if nm in ("A", "K") else "dve", dst[:], pb[:].rearrange("p (a b) -> p a b", a=4), [pb], [dst])
                XT[nm] = dst
            AT, BT, KT, RT = XT["A"], XT["B"], XT["K"], XT["R"]

            def pairmat(l, r_op, mask_off, eng2, dst=None):
                dst = dst or M8()
                for par in range(2):
                    pb = PB()
                    mms(lambda e, pb=pb, par=par: [e.matmul(pb[:, j * 128:(j + 1) * 128],
                                                          lhsT=l[64 * par:64 * par + 64, j, :],
                                                          rhs=r_op[64 * par:64 * par + 64, j, :],
                                                          start=True, stop=True) for j in range(4)][-1], [l, r_op], [pb])
                    tt(eng2[par], dst[:, par:8:2, :], pb[:].rearrange("p (a b) -> p a b", a=4),
                       cst[:, mask_off:mask_off + 128].unsqueeze(1).to_broadcast([128, 4, 128]), ALU.mult, [pb, cst], [(dst, par)])
                return dst

            Nm = pairmat(BT, AT, C_SU, ("dve", "dve"))
            Am = pairmat(AT, BT, C_SL, ("dve", "dve"))
            AkT = pairmat(KT, AT, C_SU, ("dve", "dve"), M8d[0])
            RbT = pairmat(BT, RT, C_IU, ("dve", "dve"), M8d[1])
            RkT = pairmat(KT, RT, C_IU, ("dve", "dve"), M8d[2])
            if STAGE == 7:
                cp("dve", yo[:, 512:1024].rearrange("p (a b) -> p a b", a=4), RkT[:, 0:4, :], [RkT], [yo])
                dma("sp", D["y_scr"][tok0:tok0 + 128, :], yo[:], [yo], [])
                return
            X = M8()
            tt("dve", X[:], Nm[:], ident.unsqueeze(1).to_broadcast([128, 8, 128]), ALU.add, [Nm, cst], [X])
            for j in range(0 if "d" in SKIP else 6):
                Nn, An, Xn = M8(), M8(), M8()
                last = (j == 5)
                for hf in range(2):
                    pbn, pba = PB(), PB()
                    if not last:
                        mms(lambda e, pbn=pbn, hf=hf, Am=Am, Nm=Nm: [e.matmul(pbn[:, q * 128:(q + 1) * 128], lhsT=Am[:, hf * 4 + q, :], rhs=Nm[:, hf * 4 + q, :], start=True, stop=True) for q in range(4)][-1],
                            [Am, Nm], [pbn])
                        cp("act", Nn[:, hf * 4:hf * 4 + 4, :], pbn[:].rearrange("p (a b) -> p a b", a=4), [pbn], [(Nn, hf)])
                    mms(lambda e, pba=pba, hf=hf, Am=Am, Nm=Nm: [e.matmul(pba[:, q * 128:(q + 1) * 128], lhsT=Nm[:, hf * 4 + q, :], rhs=Am[:, hf * 4 + q, :], start=True, stop=True) for q in range(4)][-1],
                        [Am, Nm], [pba])
                    cp("dve", An[:, hf * 4:hf * 4 + 4, :], pba[:].rearrange("p (a b) -> p a b", a=4), [pba], [(An, hf)])
                for hf in range(2):
                    pbx = PB()
                    mms(lambda e, pbx=pbx, hf=hf, An=An, X=X: [e.matmul(pbx[:, q * 128:(q + 1) * 128], lhsT=An[:, hf * 4 + q, :], rhs=X[:, hf * 4 + q, :], start=True, stop=True) for q in range(4)][-1],
                        [An, X], [pbx])
                    tt("dve", Xn[:, hf * 4:hf * 4 + 4, :], pbx[:].rearrange("p (a b) -> p a b", a=4), X[:, hf * 4:hf * 4 + 4, :], ALU.add, [pbx, X], [(Xn, hf)])
                Nm, Am, X = Nn, An, Xn
            if STAGE == 8:
                cp("dve", yo[:, 512:1024].rearrange("p (a b) -> p a b", a=4), X[:, 0:4, :], [X], [yo])
                dma("sp", D["y_scr"][tok0:tok0 + 128, :], yo[:], [yo], [])
                return
            pr = PB()

            def f_rhs0(e, pr=pr, AT=AT, AkT=AkT):
                ins = None
                for m in range(4):
                    e.matmul(pr[:, m * 128:(m + 1) * 128], lhsT=AT[:, m, :], rhs=ST[:, m, :], start=True, stop=False)
                    for q in range(2):
                        hd = 2 * m + q
                        ins = e.matmul(pr[:, hd * 64:(hd + 1) * 64], lhsT=AkT[:, hd, :], rhs=zr[:, 1024 + hd * 64:1024 + (hd + 1) * 64], start=False, stop=(q == 1))
                return ins
            mms(f_rhs0, [AT, ST, AkT, zr], [pr])
            rhs0 = T5()
            cp("act", rhs0[:], pr[:], [pr], [rhs0])
            pu = PB()
            mms(lambda e, pu=pu, X=X, rhs0=rhs0: [e.matmul(pu[:, hd * 64:(hd + 1) * 64], lhsT=X[:, hd, :], rhs=rhs0[:, hd * 64:(hd + 1) * 64], start=True, stop=True) for hd in range(8)][-1],
                [X, rhs0], [pu])
            U = T5()
            cp("dve", U[:], pu[:], [pu], [U])
            py = PB()

            def f_y(e, py=py, RT=RT, RbT=RbT, RkT=RkT, U=U):
                ins = None
                for m in range(4):
                    e.matmul(py[:, m * 128:(m + 1) * 128], lhsT=RT[:, m, :], rhs=ST[:, m, :], start=True, stop=False)
                    for q in range(2):
                        hd = 2 * m + q
                        e.matmul(py[:, hd * 64:(hd + 1) * 64], lhsT=RbT[:, hd, :], rhs=U[:, hd * 64:(hd + 1) * 64], start=False, stop=False)
                        ins = e.matmul(py[:, hd * 64:(hd + 1) * 64], lhsT=RkT[:, hd, :], rhs=zr[:, 1024 + hd * 64:1024 + (hd + 1) * 64], start=False, stop=(q == 1))
                return ins
            mms(f_y, [RT, ST, RbT, RkT, U, zr], [py])
            yv = T5()
            cp("act", yv[:], py[:], [py], [yv])
            pst = PB()

            def f_s(e, pst=pst, Bt=Bt, Kt=Kt, U=U):
                ins = None
                for m in range(4):
                    e.matmul(pst[:, m * 128:(m + 1) * 128], lhsT=Bt[:, m * 128:(m + 1) * 128], rhs=U[:, m * 128:(m + 1) * 128], start=True, stop=False)
                    ins = e.matmul(pst[:, m * 128:(m + 1) * 128], lhsT=Kt[:, m * 128:(m + 1) * 128], rhs=zr[:, 1024 + m * 128:1024 + (m + 1) * 128], start=False, stop=True)
                return ins
            mms(f_s, [Bt, Kt, U, zr], [pst])
            tt("dve", ST[:], pst[:].rearrange("p (a b) -> p a b", a=4), ST[:], ALU.add, [pst, ST], [ST])
            tt("dve", ST[:], ST[:], gC[:].unsqueeze(2).to_broadcast([128, 4, 128]), ALU.mult, [ST, gC], [ST])
            tt("dve", ST[:], ST[:], cst[:, C_BD:C_BD + 128].unsqueeze(1).to_broadcast([128, 4, 128]), ALU.mult, [ST, cst], [ST])
            if STAGE == 9:
                cp("dve", yo[:, 512:1024], yv[:], [yv], [yo])
                dma("sp", D["y_scr"][tok0:tok0 + 128, :], yo[:], [yo], [])
                return
            y3 = yv[:].rearrange("p (h d) -> p h d", h=8)
            ysq = T5()
            tt("pool", ysq[:], yv[:], yv[:], ALU.mult, [yv], [ysq])
            s1, s2, mean, var = sm(), sm(), sm(), sm()
            red(s1[:], y3, [yv], [s1])
            red(s2[:], ysq[:].rearrange("p (h d) -> p h d", h=8), [ysq], [s2])
            ts("dve", mean[:], s1[:], 1.0 / 64, None, ALU.mult, None, [s1], [mean])
            tt("dve", var[:], mean[:], mean[:], ALU.mult, [mean], [var])
            stt("dve", var[:], s2[:], 1.0 / 64, var[:], ALU.mult, ALU.subtract, [s2, var], [var])
            act(var[:], var[:], AF.Sqrt, [var], [var], bias=64e-5, scale=1.0)
            p.op("dve", lambda e, var=var: e.reciprocal(out=var[:], in_=var[:]), [var], [var])
            yn = T5()
            yn3 = yn[:].rearrange("p (h d) -> p h d", h=8)
            tt("dve", yn3, y3, mean[:].unsqueeze(2).to_broadcast([128, 8, 64]), ALU.subtract, [yv, mean], [yn])
            tt("dve", yn3, yn3, var[:].unsqueeze(2).to_broadcast([128, 8, 64]), ALU.mult, [yn, var], [yn])
            tt("pool", yn[:], yn[:], vb(V_LNW), ALU.mult, [yn, vec], [yn])
            tt("pool", yn[:], yn[:], vb(V_LNB), ALU.add, [yn, vec], [yn])
            rk = T5()
            tt("pool", rk[:], r_, k2[:], ALU.mult, [zr, k2], [rk])
            tt("pool", rk[:], rk[:], vb(V_RK), ALU.mult, [rk, vec], [rk])
            sb_ = sm()
            red(sb_[:], rk[:].rearrange("p (h d) -> p h d", h=8), [rk], [sb_])
            tt("dve", rk[:].rearrange("p (h d) -> p h d", h=8), v_.rearrange("p (h d) -> p h d", h=8),
               sb_[:].unsqueeze(2).to_broadcast([128, 8, 64]), ALU.mult, [zr, sb_], [rk])
            tt("pool", yn[:], yn[:], rk[:], ALU.add, [yn, rk], [yn])
            tt("pool", yo[:, 512:1024], yn[:], gv[:], ALU.mult, [yn, gv], [(yo, 2)])
            dma("sp", D["y_scr"][tok0:tok0 + 128, :], yo[:], [yo], [(D["y_t"], tok0 // 128)])

    for i in range(len(order)):
        tile_body(i)


def phase_a2(p, nc, D, ntok, final=True, drip=None):
    h = helpers(p)
    tt, stt, ts, act, cp, red, dma, mms = h.tt, h.stt, h.ts, h.act, h.cp, h.red, h.dma, h.mms
    cst = p.sb("cstb", [128, 128], F32)
    dma("sp", cst[:], D["cst"][:, C_ID:C_ID + 128], [], [cst])
    ident = cst[:, 0:128]
    nwp = p.sb("nwpb", [128, 16], F32)
    dma("sp", nwp[:], D["nwp"], [], [nwp])
    fin = p.sb("finw", [128, 1024], F32)
    dma("sp", fin[:], D["vecs"][V_FIN:V_FIN + 1024].partition_broadcast(128), [], [fin])
    stg = Rot([p.sb("stgb%d" % i, [128, 2048], F32) for i in range(2)])
    Wg = load_w_bf16(p, h, "Wg", D["w_in"], 1024, 4640, 2592, 2048, stg, nwp, 0)
    PA = load_w_bf16(p, h, "PAw", D["proj_attn"], 512, 1024, 0, 1024, stg)
    PBw = load_w_bf16(p, h, "PBw", D["proj_rwkv"], 512, 1024, 0, 1024, stg)
    WO = load_w_bf16(p, h, "WOw", D["w_out"], 1024, 1024, 0, 1024, stg)
    xt = Rot([p.sb("xtb%d" % i, [128, 1024], F32) for i in range(2)])
    yt = Rot([p.sb("ytb%d" % i, [128, 1024], F32) for i in range(2)])
    ss = p.sb("ssb", [128, 1], F32)
    rstd = p.sb("rstdb", [128, 1], F32)
    xs = p.sb("xsb", [128, 1024], F32)
    xsT = p.sb("xsTb", [128, 8, 128], BF16)
    yT = p.sb("yTb", [128, 8, 128], BF16)
    sgt = p.sb("sgt", [128, 2048], BF16)
    mg = p.sb("mg", [128, 1024], F32)
    m2 = p.sb("m2", [128, 1024], F32)
    mgT = p.sb("mgT", [128, 8, 128], BF16)
    h1 = Rot([p.sb("h1b%d" % i, [128, 1024], F32) for i in range(2)])
    PB = Rot([p.ps("pq%d" % i, [128, 512], F32) for i in range(8)])
    nt = ntok // 128
    xq, yq = {}, {}

    def prefetch(i):
        if i < nt:
            xq[i] = xt()
            dma("sp", xq[i][:], D["x"][i * 128:(i + 1) * 128, :], [], [xq[i]])
            yq[i] = yt()
            dma("sp", yq[i][:], D["y_scr"][i * 128:(i + 1) * 128, :], [(D["y_t"], i)], [yq[i]])

    prefetch(0)

    def tp8(src, dst):
        for hh in range(2):
            pb = PB()
            mms(lambda e, pb=pb, hh=hh: [e.transpose(out=pb[:, j * 128:(j + 1) * 128], in_=src[:, (hh * 4 + j) * 128:(hh * 4 + j + 1) * 128], identity=ident) for j in range(4)][-1],
                [src, cst], [pb])
            cp("act" if hh else "dve", dst[:, hh * 4:hh * 4 + 4, :], pb[:].rearrange("p (a b) -> p a b", a=4), [pb], [(dst, hh)])

    def body(i):
        x_t, y_t = xq.pop(i), yq.pop(i)
        prefetch(i + 1)
        act(xs[:], x_t[:], AF.Square, [x_t], [xs, ss], accum_out=ss[:])
        act(rstd[:], ss[:], AF.Sqrt, [ss], [rstd], bias=1e-5, scale=1.0 / 1024)
        p.op("dve", lambda e: e.reciprocal(out=rstd[:], in_=rstd[:]), [rstd], [rstd])
        ts("dve", xs[:], x_t[:], rstd[:, 0:1], None, ALU.mult, None, [x_t, rstd], [xs])
        tp8(xs, xsT)
        for cc in range(4):
            pb = PB()
            mms(lambda e, pb=pb, cc=cc: [e.matmul(pb[:], lhsT=xsT[:, c, :], rhs=Wg[:, c, cc * 512:(cc + 1) * 512], start=(c == 0), stop=(c == 7)) for c in range(8)][-1],
                [xsT, Wg], [pb])
            act(sgt[:, cc * 512:(cc + 1) * 512], pb[:], AF.Sigmoid, [pb], [(sgt, cc)])
        tp8(y_t, yT)
        for br, (W, dstt) in enumerate(((PA, mg), (PBw, m2))):
            for hf in range(2):
                pb = PB()
                mms(lambda e, pb=pb, br=br, hf=hf, W=W: [e.matmul(pb[:], lhsT=yT[:, br * 4 + c, :], rhs=W[:, c, hf * 512:(hf + 1) * 512], start=(c == 0), stop=(c == 3)) for c in range(4)][-1],
                    [yT, W], [pb])
                tt("dve", dstt[:, hf * 512:(hf + 1) * 512], pb[:], sgt[:, br * 1024 + hf * 512:br * 1024 + (hf + 1) * 512], ALU.mult, [pb, sgt], [(dstt, hf)])
        tt("pool", mg[:], mg[:], m2[:], ALU.add, [mg, m2], [mg])
        tp8(mg, mgT)
        ho = h1()
        for hf in range(2):
            pb = PB()
            mms(lambda e, pb=pb, hf=hf: [e.matmul(pb[:], lhsT=mgT[:, c, :], rhs=WO[:, c, hf * 512:(hf + 1) * 512], start=(c == 0), stop=(c == 7)) for c in range(8)][-1],
                [mgT, WO], [pb])
            tt("dve", ho[:, hf * 512:(hf + 1) * 512], pb[:], x_t[:, hf * 512:(hf + 1) * 512], ALU.add, [pb, x_t], [(ho, hf)])
        if final:
            act(xs[:], ho[:], AF.Square, [ho], [xs, ss], accum_out=ss[:])
            act(rstd[:], ss[:], AF.Sqrt, [ss], [rstd], bias=1e-5, scale=1.0 / 1024)
            p.op("dve", lambda e: e.reciprocal(out=rstd[:], in_=rstd[:]), [rstd], [rstd])
            stt("dve", ho[:], ho[:], rstd[:, 0:1], fin[:], ALU.mult, ALU.mult, [ho, rstd, fin], [ho])
            dma("sp", D["out"][i * 128:(i + 1) * 128, :], ho[:], [ho], [])
        else:
            dma("sp", D["h1_scr"][i * 128:(i + 1) * 128, :], ho[:], [ho], [(D["h1_t"], i)])

    per = (len(drip) + max(nt - 2, 1) - 1) // max(nt - 2, 1) if drip else 0
    for i in range(nt):
        body(i)
        if drip:
            p.drain(drip, per)
    if drip:
        p.drain(drip, len(drip))


def phase_b0(p, nc, D, eng_rot=("dve", "pool")):
    h = helpers(p)
    dma, cp = h.dma, h.cp
    stg = Rot([p.sb("cs%d" % i, [128, 4096], F32) for i in range(2)])
    ob = Rot([p.sb("co%d" % i, [128, 4096], BF16) for i in range(2)])
    nwp0 = p.sb("nwp0", [128, 16], F32)
    dma("sp", nwp0[:], D["nwp"], [], [nwp0])
    k = 0
    for g in range(32):
        s, o = stg(), ob()
        dma("sp", s[:].rearrange("p (dc e) -> p dc e", dc=8), D["uT"][:, g * 512:(g + 1) * 512].rearrange("(dc p) e -> p dc e", p=128), [], [s])
        h.tt(eng_rot[k % 2], o[:].rearrange("p (i dc e) -> p dc i e", i=4, dc=8), s[:].rearrange("p (dc i e) -> p dc i e", dc=8, i=4),
             nwp0[:, 8:16].unsqueeze(2).unsqueeze(3).to_broadcast([128, 8, 4, 128]), ALU.mult, [s, nwp0], [o])
        k += 1
        dma("act", D["u2"][:, g * 4:(g + 1) * 4, :, :].rearrange("p i dc e -> p (i dc e)"), o[:], [o], [(D["u2_t"], g)])
        s, o = stg(), ob()
        dma("sp", s[:].rearrange("p (i d) -> p i d", i=4), D["v"][g * 512:(g + 1) * 512, :].rearrange("(i p) d -> p i d", p=128), [], [s])
        cp(eng_rot[k % 2], o[:], s[:], [s], [o])
        k += 1
        dma("act", D["vb"][g * 512:(g + 1) * 512, :].rearrange("(i p) d -> p i d", p=128), o[:].rearrange("p (i d) -> p i d", i=4), [o], [(D["vb_t"], g)])


def phase_b(p, nc, D, ntok):
    h = helpers(p)
    tt, stt, ts, act, cp, red, dma, mms = h.tt, h.stt, h.ts, h.act, h.cp, h.red, h.dma, h.mms
    TT = 256
    cst = p.sb("cstc", [128, 256], F32)
    dma("sp", cst[:, 0:128], D["cst"][:, C_ID:C_ID + 128], [], [cst])
    dma("sp", cst[:, 128:256], D["cst"][:, C_IOTA:C_IOTA + 128], [], [cst])
    ident = cst[:, 0:128]
    iota = cst[:, 128:256]
    iota_bf = p.sb("iota_bf", [128, 128], BF16)
    cp("dve", iota_bf[:], iota, [cst], [iota_bf])
    fin = p.sb("finc", [128, 1024], F32)
    dma("sp", fin[:], D["vecs"][V_FIN:V_FIN + 1024].partition_broadcast(128), [], [fin])
    nwpb = p.sb("nwpc", [128, 16], F32)
    dma("sp", nwpb[:], D["nwp"], [], [nwpb])
    skT = p.sb("skT", [128, 8, 128], F32)
    dma("sp", skT[:], D["skT"], [], [skT])
    G = p.sb("G", [128, 128, TT], BF16)
    xs = p.sb("xsc", [128, 1024], F32)
    Wq = load_w_bf16(p, h, "Wq", D["peer_wq"], 1024, 1024, 0, 1024, (lambda: xs), nwpb, 8)
    U2 = Rot([p.sb("u2t%d" % i, [128, 4, 8, 128], BF16) for i in range(3)])
    Vt = Rot([p.sb("vt%d" % i, [128, 4, 1024], BF16) for i in range(3)])
    h1 = [[p.sb("h1c%d_%d" % (b, i), [128, 1024], F32) for i in range(2)] for b in range(2)]
    ss = p.sb("ssc", [128, 1], F32)
    rstd = p.sb("rstdc", [128, 1], F32)
    xTb = [p.sb("xs2T%d" % b, [128, 8, TT], BF16) for b in range(2)]
    qT = p.sb("qTc", [128, 8, TT], F32)
    sc = p.sb("sc", [128, 16, 128], F32)
    v16 = p.sb("v16", [128, 16, 16], F32)
    i16u = p.sb("i16u", [128, 16, 16], U32)
    i16f = p.sb("i16f", [128, 16, 16], F32)
    cand = p.sb("cand", [128, 8, 256], F32)
    tv = p.sb("tv", [128, 8, 16], F32)
    posu = p.sb("posu", [128, 8, 16], U32)
    au = p.sb("au", [128, 8, 16], U32)
    bu = p.sb("bu", [128, 8, 16], U32)
    af = p.sb("af", [128, 8, 16], F32)
    bf_ = p.sb("bf", [128, 8, 16], F32)
    sel = p.sb("sel", [128, 3, 128], F32)
    sm = Rot([p.sb("smc%d" % i, [128, 8], F32) for i in range(4)])
    selTb = [p.sb("selT%d" % b, [128, 3, TT], BF16) for b in range(2)]
    OA = Rot([p.sb("oa%d" % i, [128, 16, 128], BF16) for i in range(2)])
    OB = Rot([p.sb("ob%d" % i, [128, 16, 128], BF16) for i in range(2)])
    gh = Rot([p.sb("gh%d" % i, [128, TT], BF16) for i in range(3)])
    ac = Rot([p.sb("ac%d" % i, [128, TT], BF16) for i in range(3)])
    pbs = [p.ps("pr%d" % i, [128, 512], F32) for i in range(4)]
    PBH = Rot(pbs[0:3])
    PBP = Rot(pbs[3:4])
    PBG = Rot(pbs)
    ACC = [p.ps("acc%d" % i, [128, 512], F32) for i in range(4)]
    ntile = ntok // TT

    def prep(tix):
        b = tix % 2
        t0 = tix * TT
        xT, selT = xTb[b], selTb[b]
        PB = PBP
        for s in range(2):
            hh1 = h1[b][s]
            dma("sp", hh1[:], D["h1_scr"][t0 + s * 128:t0 + (s + 1) * 128, :], [(D["h1_t"], tix * 2 + s)], [hh1])
            act(xs[:], hh1[:], AF.Square, [hh1], [xs, ss], accum_out=ss[:])
            act(rstd[:], ss[:], AF.Sqrt, [ss], [rstd], bias=1e-5, scale=1.0 / 1024)
            p.op("dve", lambda e: e.reciprocal(out=rstd[:], in_=rstd[:]), [rstd], [rstd])
            ts("dve", xs[:], hh1[:], rstd[:, 0:1], None, ALU.mult, None, [hh1, rstd], [xs])
            for hh in range(2):
                pb = PB()
                mms(lambda e, pb=pb, hh=hh: [e.transpose(out=pb[:, j * 128:(j + 1) * 128], in_=xs[:, (hh * 4 + j) * 128:(hh * 4 + j + 1) * 128], identity=ident) for j in range(4)][-1],
                    [xs, cst], [pb])
                cp("act" if hh else "dve", xT[:, hh * 4:hh * 4 + 4, s * 128:(s + 1) * 128], pb[:].rearrange("p (a b) -> p a b", a=4), [pb], [(xT, (s, hh))])
        for c in range(8):
            pb = PB()
            mms(lambda e, pb=pb, c=c, xT=xT: [e.matmul(pb[:, 0:TT], lhsT=Wq[:, dc, c * 128:(c + 1) * 128], rhs=xT[:, dc, :], start=(dc == 0), stop=(dc == 7)) for dc in range(8)][-1],
                [Wq, xT], [pb])
            cp("act" if c % 2 else "dve", qT[:, c, :], pb[:, 0:TT], [pb], [(qT, c)])
        for s in range(0 if "S" in SKIP else 2):
            for par in range(2):
                for half in range(2):
                    pb = PB()
                    mms(lambda e, pb=pb, par=par, half=half, s=s: [e.matmul(pb[:, j * 128:(j + 1) * 128], lhsT=qT[64 * par:64 * par + 64, half * 4 + j, s * 128:(s + 1) * 128],
                                                                        rhs=skT[64 * par:64 * par + 64, half * 4 + j, :], start=True, stop=True) for j in range(4)][-1],
                        [qT, skT], [pb])
                    cp("act", sc[:, 8 * half + par:8 * half + 8:2, :], pb[:].rearrange("p (a b) -> p a b", a=4), [pb], [(sc, 8 * half + par + 2 * j) for j in range(4)])
            tmpA = cand[:].rearrange("p h (a b) -> p (h a) b", a=2)
            for hp in range(16):
                p.op("dve", lambda e, hp=hp: e.max(out=v16[:, hp, 0:8], in_=sc[:, hp, :]), [(sc, hp)], [(v16, hp)])
            for hp in range(16):
                p.op("dve", lambda e, hp=hp, tmpA=tmpA: e.match_replace(out=tmpA[:, hp, :], in_to_replace=v16[:, hp, 0:8], in_values=sc[:, hp, :], imm_value=-1e30), [(sc, hp), (v16, hp)], [(cand, hp)])
            for hp in range(16):
                p.op("dve", lambda e, hp=hp, tmpA=tmpA: e.max(out=v16[:, hp, 8:16], in_=tmpA[:, hp, :]), [(cand, hp)], [(v16, hp)])
            for hp in range(16):
                p.op("dve", lambda e, hp=hp: e.max_index(out=i16u[:, hp, 0:8], in_max=v16[:, hp, 0:8], in_values=sc[:, hp, :]), [(sc, hp), (v16, hp)], [(i16u, hp)])
            for hp in range(16):
                p.op("dve", lambda e, hp=hp: e.max_index(out=i16u[:, hp, 8:16], in_max=v16[:, hp, 8:16], in_values=sc[:, hp, :]), [(sc, hp), (v16, hp)], [(i16u, hp)])
            cp("pool", i16f[:], i16u[:], [i16u], [i16f])
            tt("pool", cand[:].rearrange("p h (a b) -> p h a b", a=16), v16[:, 0:16:2, :].unsqueeze(3).to_broadcast([128, 8, 16, 16]),
               v16[:, 1:16:2, :].unsqueeze(2).to_broadcast([128, 8, 16, 16]), ALU.add, [v16], [cand])
            tmpB = sc[:].rearrange("p (h a) b -> p h (a b)", a=2)
            for hd in range(8):
                p.op("dve", lambda e, hd=hd: e.max(out=tv[:, hd, 0:8], in_=cand[:, hd, :]), [cand], [(tv, hd)])
            for hd in range(8):
                p.op("dve", lambda e, hd=hd, tmpB=tmpB: e.match_replace(out=tmpB[:, hd, :], in_to_replace=tv[:, hd, 0:8], in_values=cand[:, hd, :], imm_value=-1e30), [cand, (tv, hd)], [(sc, 2 * hd), (sc, 2 * hd + 1)])
            for hd in range(8):
                p.op("dve", lambda e, hd=hd, tmpB=tmpB: e.max(out=tv[:, hd, 8:16], in_=tmpB[:, hd, :]), [(sc, 2 * hd), (sc, 2 * hd + 1)], [(tv, hd)])
            for hd in range(8):
                p.op("dve", lambda e, hd=hd: e.max_index(out=posu[:, hd, 0:8], in_max=tv[:, hd, 0:8], in_values=cand[:, hd, :]), [cand, (tv, hd)], [(posu, hd)])
            for hd in range(8):
                p.op("dve", lambda e, hd=hd: e.max_index(out=posu[:, hd, 8:16], in_max=tv[:, hd, 8:16], in_values=cand[:, hd, :]), [cand, (tv, hd)], [(posu, hd)])
            gt = sel[:, 2, :].rearrange("p (h k) -> p h k", h=8)
            tt("pool", gt, tv[:], tv[:, :, 0:1].to_broadcast([128, 8, 16]), ALU.subtract, [tv], [(sel, 2)])
            act(gt, gt, AF.Exp, [(sel, 2)], [(sel, 2)])
            z8 = sm()
            red(z8[:], gt, [(sel, 2)], [z8])
            p.op("dve", lambda e, z8=z8: e.reciprocal(out=z8[:], in_=z8[:]), [z8], [z8])
            tt("dve", gt, gt, z8[:].unsqueeze(2).to_broadcast([128, 8, 16]), ALU.mult, [(sel, 2), z8], [(sel, 2)])
            ts("dve", au[:], posu[:], 4, None, ALU.logical_shift_right, None, [posu], [au])
            ts("dve", bu[:], posu[:], 15, None, ALU.bitwise_and, None, [posu], [bu])
            cp("pool", af[:], au[:], [au], [af])
            cp("pool", bf_[:], bu[:], [bu], [bf_])
            io16 = iota[:, 0:16].unsqueeze(1).unsqueeze(1).to_broadcast([128, 8, 16, 16])
            eq = cand[:].rearrange("p h (a b) -> p h a b", a=16)
            for w, (xf, par) in enumerate(((af, 0), (bf_, 1))):
                tt("dve", eq, io16, xf[:].unsqueeze(3).to_broadcast([128, 8, 16, 16]), ALU.is_equal, [cst, xf], [cand])
                tt("pool", eq, eq, i16f[:, par:16:2, :].unsqueeze(2).to_broadcast([128, 8, 16, 16]), ALU.mult, [cand, i16f], [cand])
                red(sel[:, w, :].rearrange("p (h k) -> p h k", h=8), eq, [cand], [(sel, w)])
            pb = PB()
            mms(lambda e, pb=pb: [e.transpose(out=pb[:, w * 128:(w + 1) * 128], in_=sel[:, w, :], identity=ident) for w in range(3)][-1], [sel, cst], [pb])
            cp("act", selT[:, :, s * 128:(s + 1) * 128], pb[:, 0:384].rearrange("p (a b) -> p a b", a=3), [pb], [(selT, s)])

    def gbuild(tix):
        selT = selTb[tix % 2]
        PB = PBG
        NG = 0 if "G" in SKIP else TT // 16
        bufs = {}

        def onehots(g):
            tk = g * 16
            oa, ob = OA(), OB()
            bufs[g] = (oa, ob)
            io = iota_bf[:, :].unsqueeze(1).to_broadcast([128, 16, 128])
            if "o" in SKIP:
                return
            tt("dve", oa[:], io, selT[:, 0, tk:tk + 16].unsqueeze(2).to_broadcast([128, 16, 128]), ALU.is_equal, [iota_bf, selT], [oa])
            tt("pool", oa[:], oa[:], selT[:, 2, tk:tk + 16].unsqueeze(2).to_broadcast([128, 16, 128]), ALU.mult, [oa, selT], [oa])
            tt("dve", ob[:], io, selT[:, 1, tk:tk + 16].unsqueeze(2).to_broadcast([128, 16, 128]), ALU.is_equal, [iota_bf, selT], [ob])

        def mm_evac(g):
            tk = g * 16
            oa, ob = bufs.pop(g)
            for q4 in range(4):
                pb = PB()
                if "m" not in SKIP:
                  mms(lambda e, pb=pb, q4=q4, oa=oa, ob=ob: [e.matmul(pb[:, j * 128:(j + 1) * 128], lhsT=ob[:, q4 * 4 + j, :], rhs=oa[:, q4 * 4 + j, :], start=True, stop=True) for j in range(4)][-1],
                    [oa, ob], [pb])
                tq = tk + q4 * 4
                if "v" not in SKIP:
                  cp("act" if q4 != 3 else "dve", G[:, :, tq:tq + 4].rearrange("p i t -> p t i"), pb[:].rearrange("p (t i) -> p t i", t=4), [pb], [(G, tq)])

        if NG:
            onehots(0)
        for g in range(NG):
            if g + 1 < NG:
                onehots(g + 1)
            mm_evac(g)

    def expert(tix, nxt):
        xT = xTb[tix % 2]
        LOOK = 2
        grp = {}
        hb = {}

        def emit_H(i):
            ig, ii = divmod(i, 4)
            if ii == 0:
                u2, vt = U2(), Vt()
                grp[ig] = (u2, vt)
                if not ("D" in SKIP and ig >= 2):
                    dma("sp", vt[:], D["vb"][ig * 512:(ig + 1) * 512, :].rearrange("(i p) d -> p i d", p=128), [(D["vb_t"], ig)], [vt])
                    dma("sp", u2[:].rearrange("p i dc e -> p (i dc e)"), D["u2"][:, ig * 4:(ig + 1) * 4, :, :].rearrange("p i dc e -> p (i dc e)"), [(D["u2_t"], ig)], [u2])
            u2, vt = grp[ig]
            pb = PBH()
            mms(lambda e, pb=pb, u2=u2, ii=ii: [e.matmul(pb[:, 0:TT], lhsT=u2[:, ii, dc, :], rhs=xT[:, dc, :], start=(dc == 0), stop=(dc == 7)) for dc in range(8)][-1],
                [u2, xT], [pb])
            hb[i] = pb

        def emit_rest(i):
            ig, ii = divmod(i, 4)
            u2, vt = grp[ig]
            pb = hb.pop(i)
            g_, a_ = gh(), ac()
            if "X" not in SKIP:
                act(g_[:], pb[:, 0:TT], AF.Gelu, [pb], [g_])
                tt("dve", a_[:], g_[:], G[:, i, :], ALU.mult, [g_, G], [a_])
            mms(lambda e, a_=a_, vt=vt, ii=ii, i=i: [e.matmul(ACC[s * 2 + hf][:], lhsT=a_[:, s * 128:(s + 1) * 128], rhs=vt[:, ii, hf * 512:(hf + 1) * 512], start=(i == 0), stop=(i == 127))
                                                   for s in range(2) for hf in range(2)][-1], [a_, vt], ACC)

        NE = 0 if "E" in SKIP else 128
        per = (len(nxt) + 99) // 100 if nxt else 0
        for i in range(min(LOOK, NE)):
            emit_H(i)
        for i in range(NE):
            if i + LOOK < NE:
                emit_H(i + LOOK)
            emit_rest(i)
            if nxt:
                p.drain(nxt, per)
        if nxt:
            p.drain(nxt, len(nxt))

    def epilogue(tix):
        b = tix % 2
        t0 = tix * TT
        for s in range(2):
            hh1 = h1[b][s]
            for hf in range(2):
                tt("dve", hh1[:, hf * 512:(hf + 1) * 512], ACC[s * 2 + hf][:], hh1[:, hf * 512:(hf + 1) * 512], ALU.add, [ACC[s * 2 + hf], hh1], [hh1])
            act(xs[:], hh1[:], AF.Square, [hh1], [xs, ss], accum_out=ss[:])
            act(rstd[:], ss[:], AF.Sqrt, [ss], [rstd], bias=1e-5, scale=1.0 / 1024)
            p.op("dve", lambda e: e.reciprocal(out=rstd[:], in_=rstd[:]), [rstd], [rstd])
            stt("dve", hh1[:], hh1[:], rstd[:, 0:1], fin[:], ALU.mult, ALU.mult, [hh1, rstd, fin], [hh1])
            dma("act", D["out"][t0 + s * 128:t0 + (s + 1) * 128, :], hh1[:], [hh1], [])

    prep(0)
    for tix in range(ntile):
        gbuild(tix)
        nxt = []
        if tix + 1 < ntile:
            p.defer_begin()
            prep(tix + 1)
            nxt = p.defer_end()
        expert(tix, nxt)
        epilogue(tix)


N_CORES = 8


def _build(n_seq, n_tiles, ret_d=False, phases="0123"):
    nc = bass.Bass("TRN2", target_bir_lowering=False, dynamic_dma_scratch_size=2048)
    ntok = n_seq * n_tiles * 128
    D = {}

    def din(name, shape):
        D[name] = nc.dram_tensor(name, list(shape), F32, kind="ExternalInput").ap()

    din("x", (ntok, 1024)); din("w_in", (1024, 4640)); din("vecs", (NVEC,)); din("nwp", (128, 16)); din("cst", (128, NCST))
    din("decay_w2", (64, 512)); din("iclr_a2", (64, 512)); din("gate_g2", (160, 512))
    din("proj_attn", (512, 1024)); din("proj_rwkv", (512, 1024)); din("w_out", (1024, 1024))
    din("peer_wq", (1024, 1024)); din("skT", (128, 8, 128)); din("uT", (1024, 16384)); din("v", (16384, 1024))
    D["y_scr"] = nc.dram_tensor("y_scr", [ntok, 1024], F32, kind="Internal").ap()
    D["h1_scr"] = nc.dram_tensor("h1_scr", [ntok, 1024], F32, kind="Internal").ap()
    D["u2"] = nc.dram_tensor("u2", [128, 128, 8, 128], BF16, kind="Internal").ap()
    D["vb"] = nc.dram_tensor("vb", [16384, 1024], BF16, kind="Internal").ap()
    D["out"] = nc.dram_tensor("out", [ntok, 1024], F32, kind="ExternalOutput").ap()
    p = Prog(nc)
    for nm in ("y_t", "h1_t", "u2_t", "vb_t"):
        D[nm] = p.wrap(None, nm)
    m = p.mark()
    if "1" in phases:
        phase_a1(p, nc, D, n_seq, n_tiles, n_tiles * 128)
        p.release(m)
    if "2" in phases:
        drip = None
        if "0" in phases:
            p.defer_begin()
            phase_b0(p, nc, D)
            drip = p.defer_end()
        phase_a2(p, nc, D, ntok, final=("3" not in phases), drip=drip)
        p.release(m)
    elif "0" in phases:
        phase_b0(p, nc, D)
        p.release(m)
    if "3" in phases:
        phase_b(p, nc, D, ntok)
    p.emit()
    p.close()
    return (nc, D) if ret_d else nc


def _inputs(x, norm_mix_w, w_in, shift_mu, attn_sinks, decay_w0, decay_w2, iclr_a0, iclr_a2, gate_g2, k_k, k_a, r_k,
            ln_x_w, ln_x_b, proj_attn, proj_rwkv, w_out, norm_ffn_w, peer_wq, peer_subkeys, peer_u, peer_v, norm_final_w):
    f = lambda a: np.ascontiguousarray(np.asarray(a, dtype=np.float32))
    v = np.zeros(NVEC, np.float32)
    v[V_MU:V_MU + 1824] = f(shift_mu)[0]; v[V_W0:V_W0 + 512] = f(decay_w0)[0]; v[V_A0:V_A0 + 512] = f(iclr_a0)[0]
    v[V_KK:V_KK + 512] = f(k_k)[0]; v[V_KA:V_KA + 512] = f(k_a)[0]; v[V_LNW:V_LNW + 512] = f(ln_x_w)[0]
    v[V_LNB:V_LNB + 512] = f(ln_x_b)[0]; v[V_RK:V_RK + 512] = f(r_k)[0].reshape(-1); v[V_SINK:V_SINK + 8] = f(attn_sinks)[0]
    v[V_FIN:] = f(norm_final_w)
    nwp = np.ones((128, 16), np.float32)
    nwp[:, 0:8] = f(norm_mix_w)[0].reshape(8, 128).T
    nwp[:, 8:16] = f(norm_ffn_w)[0].reshape(8, 128).T
    sk = f(peer_subkeys)[0]
    skT = np.ascontiguousarray(sk.transpose(1, 3, 0, 2).reshape(128, 8, 128))
    return dict(w_in=f(w_in)[0], vecs=v, nwp=nwp, cst=make_cst(), decay_w2=f(decay_w2)[0], iclr_a2=f(iclr_a2)[0],
                gate_g2=f(gate_g2)[0], proj_attn=f(proj_attn)[0], proj_rwkv=f(proj_rwkv)[0], w_out=f(w_out)[0],
                peer_wq=f(peer_wq)[0], skT=skT, uT=np.ascontiguousarray(f(peer_u)[0].T), v=f(peer_v)[0])


def kernel(**inputs):
    x = np.ascontiguousarray(np.asarray(inputs["x"], dtype=np.float32))
    B, S, Dm = x.shape
    common = _inputs(**inputs)
    spc = B // N_CORES
    nc = _build(spc, S // 128)
    in_maps = []
    for c in range(N_CORES):
        d = dict(common)
        d["x"] = np.ascontiguousarray(x[c * spc:(c + 1) * spc].reshape(spc * S, Dm))
        in_maps.append(d)
    res = run_bass_kernel_spmd(nc, in_maps, core_ids=list(range(N_CORES)))
    out = np.concatenate([r["out"].reshape(spc, S, Dm) for r in res.results], axis=0)
    return out.astype(np.float32)
```

```python
import numpy as np
import concourse.bass as bass
import concourse.mybir as mybir

F32 = mybir.dt.float32
BF16 = mybir.dt.bfloat16
U32 = mybir.dt.uint32
I32 = mybir.dt.int32
ALU = mybir.AluOpType
AF = mybir.ActivationFunctionType
AX = mybir.AxisListType

NDMA_SEMS = 12
import os as _os
NOSELF = tuple(_os.environ.get("NOSELF", "").split(","))


class T:
    def __init__(self, h, name):
        self.h = h
        self.name = name
        self.state = {}

    def __getitem__(self, k):
        return self.h[k]


class Prog:
    def __init__(self, nc):
        self.nc = nc
        self.ops = {e: [] for e in ("pe", "dve", "act", "pool", "sp")}
        self.cms = []
        self.ndma = {e: 0 for e in ("sp", "act", "pool")}
        self._defer = None

    def sb(self, name, shape, dtype):
        cm = self.nc.sbuf_tensor("sb_" + name, list(shape), dtype)
        h = cm.__enter__()
        self.cms.append(cm)
        return T(h, name)

    def ps(self, name, shape, dtype):
        cm = self.nc.psum_tensor("ps_" + name, list(shape), dtype)
        h = cm.__enter__()
        self.cms.append(cm)
        return T(h, name)

    def wrap(self, ap, name):
        return T(ap, name)

    def defer_begin(self):
        self._defer = []

    def defer_end(self):
        lst, self._defer = self._defer, None
        return lst

    def drain(self, lst, k):
        for _ in range(min(k, len(lst))):
            eng, fn, reads, writes, dma, extra = lst.pop(0)
            self.op(eng, fn, reads, writes, dma, extra)

    def mark(self):
        return len(self.cms)

    def release(self, mark):
        lasts = []
        for e in ("pe", "dve", "act", "pool", "sp"):
            for j in range(len(self.ops[e]) - 1, -1, -1):
                if self.ops[e][j][2][0] not in ("dma", "bar"):
                    lasts.append((e, j))
                    break
        dmat = []
        for q in ("sp", "act", "pool"):
            n = self.ndma[q]
            dmat += [("dma", q, i) for i in range(max(0, n - NDMA_SEMS), n)]
        for e in ("pe", "dve", "act", "pool", "sp"):
            self.ops[e].append((None, lasts + dmat, ("bar", e, len(self.ops[e]))))
        while len(self.cms) > mark:
            self.cms.pop().__exit__(None, None, None)

    def _collect(self, t, key, is_write, deps):
        if key is None:
            keys = list(t.state.keys())
        else:
            keys = [k for k in (key, None) if k in t.state]
        for k in keys:
            w, rs = t.state[k]
            if w is not None:
                deps.append(w)
            if is_write:
                deps.extend(rs)

    def _update(self, t, key, is_write, me):
        if is_write:
            if key is None:
                t.state = {None: [me, []]}
            else:
                t.state[key] = [me, []]
        else:
            st = t.state.setdefault(key, [None, []])
            if me[0] != "dma":
                st[1] = [r for r in st[1] if not (r[0] == me[0])]
            st[1].append(me)

    def op(self, eng, fn, reads=(), writes=(), dma=False, extra=()):
        if self._defer is not None:
            self._defer.append((eng, fn, reads, writes, dma, extra))
            return None
        deps = list(extra)
        norm = lambda x: x if isinstance(x, tuple) else (x, None)
        reads = [norm(r) for r in reads]
        writes = [norm(w) for w in writes]
        for t, k in reads:
            self._collect(t, k, False, deps)
        for t, k in writes:
            self._collect(t, k, True, deps)
        idx = len(self.ops[eng])
        if dma:
            n = self.ndma[eng]
            self.ndma[eng] += 1
            me = ("dma", eng, n)
        else:
            me = (eng, idx)
        for t, k in reads:
            self._update(t, k, False, me)
        for t, k in writes:
            self._update(t, k, True, me)
        self.ops[eng].append((fn, deps, me))
        return me

    def emit(self):
        nc = self.nc
        engs = ("pe", "dve", "act", "pool", "sp")
        sem_cms = {}
        sems = {}
        for e in engs:
            cm = nc.semaphore("s_" + e)
            sems[e] = cm.__enter__()
            self.cms.append(cm)
        dsems = {}
        for e in ("sp", "act", "pool"):
            if self.ndma[e]:
                lst = []
                for i in range(NDMA_SEMS):
                    cm = nc.semaphore("d_%s_%d" % (e, i))
                    lst.append(cm.__enter__())
                    self.cms.append(cm)
                dsems[e] = lst
        ops = self.ops

        def run(ename, engine):
            seen = {}
            cnt = 0
            for fn, deps, me in ops[ename]:
                need = {}
                for d in deps:
                    if d[0] == "dma":
                        s = dsems[d[1]][d[2] % NDMA_SEMS]
                        v = 16 * (d[2] // NDMA_SEMS + 1)
                    else:
                        if d[0] == ename:
                            if ename == "pe" or fn is None or ename in NOSELF:
                                continue
                        s = sems[d[0]]
                        v = d[1] + 1 - self.dma_before[d[0]][d[1]]
                    key = s.num if hasattr(s, "num") else id(s)
                    if v > need.get(key, (None, 0))[1]:
                        need[key] = (s, v)
                if me[0] == "dma":
                    n = me[2]
                    s = dsems[ename][n % NDMA_SEMS]
                    if n >= NDMA_SEMS:
                        key = s.num if hasattr(s, "num") else id(s)
                        v = 16 * (n // NDMA_SEMS)
                        if v > need.get(key, (None, 0))[1]:
                            need[key] = (s, v)
                for key, (s, v) in need.items():
                    if seen.get(key, 0) >= v:
                        continue
                    engine.wait_ge(s, v)
                    seen[key] = v
                if fn is None:
                    continue
                ins = fn(engine)
                if me[0] == "dma":
                    ins.then_inc(dsems[ename][me[2] % NDMA_SEMS], 16)
                else:
                    ins.then_inc(sems[ename], 1)

        self.dma_before = {}
        for e in engs:
            c = 0
            lst = []
            for fn, deps, me in ops[e]:
                lst.append(c)
                if me[0] in ("dma", "bar"):
                    c += 1
            self.dma_before[e] = lst

        with nc.Block() as block:
            @block.tensor
            def _(eng):
                run("pe", eng)

            @block.vector
            def _(eng):
                run("dve", eng)

            @block.scalar
            def _(eng):
                run("act", eng)
                self._drain("act", eng, dsems)

            @block.gpsimd
            def _(eng):
                run("pool", eng)
                self._drain("pool", eng, dsems)

            @block.sync
            def _(eng):
                run("sp", eng)
                self._drain("sp", eng, dsems)

    def _drain(self, e, eng, dsems):
        n = self.ndma[e]
        if not n:
            return
        for j in range(min(n, NDMA_SEMS)):
            last = ((n - 1 - j) // NDMA_SEMS) * NDMA_SEMS + j
            eng.wait_ge(dsems[e][j], 16 * (last // NDMA_SEMS + 1))

    def close(self):
        for cm in reversed(self.cms):
            cm.__exit__(None, None, None)
from concourse.bass_utils import run_bass_kernel_spmd

D_MODEL = 1024
C_DEC = 0.6065306597126334
STAGE = 0
SKIP = ""
V_MU, V_W0, V_A0, V_KK, V_KA, V_LNW, V_LNB, V_RK, V_SINK, V_FIN = 0, 1824, 2336, 2848, 3360, 3872, 4384, 4896, 5408, 5416
NVEC = 5416 + 1024
C_ID, C_SU, C_IU, C_SL, C_BD, C_SH, C_CA, C_COS, C_SIN, C_ONE, C_IOTA = 0, 128, 256, 384, 512, 640, 768, 896, 1408, 1920, 1921
NCST = 1921 + 128


def make_cst():
    c = np.zeros((128, NCST), np.float32)
    i = np.arange(128)
    r, q = i[:, None], i[None, :]
    c[:, C_ID:C_ID + 128] = (r == q)
    c[:, C_SU:C_SU + 128] = (r < q)
    c[:, C_IU:C_IU + 128] = (r <= q)
    c[:, C_SL:C_SL + 128] = (r > q)
    c[:, C_BD:C_BD + 128] = ((r // 64) == (q // 64))
    c[:, C_SH:C_SH + 128] = (r == q - 1)
    c[127, C_CA] = 1.0
    inv = 10000.0 ** (-np.arange(0, 64, 2, dtype=np.float32) / 64)
    pos = (np.arange(16)[None, :] * 128 + i[:, None]).astype(np.float32)
    ang = pos[:, :, None] * inv[None, None, :]
    c[:, C_COS:C_COS + 512] = np.cos(ang).reshape(128, 512)
    c[:, C_SIN:C_SIN + 512] = np.sin(ang).reshape(128, 512)
    c[:, C_ONE] = 1.0
    c[:, C_IOTA:C_IOTA + 128] = q
    return c


class Ctx:
    pass


def helpers(p):
    h = Ctx()

    def tt(eng, out, in0, in1, op, r, w):
        p.op(eng, lambda e: e.tensor_tensor(out=out, in0=in0, in1=in1, op=op), r, w)

    def stt(eng, out, in0, scalar, in1, op0, op1, r, w):
        p.op(eng, lambda e: e.scalar_tensor_tensor(out=out, in0=in0, scalar=scalar, in1=in1, op0=op0, op1=op1), r, w)

    def ts(eng, out, in0, s1, s2, op0, op1, r, w):
        if s2 is None:
            p.op(eng, lambda e: e.tensor_scalar(out=out, in0=in0, scalar1=s1, scalar2=None, op0=op0), r, w)
        else:
            p.op(eng, lambda e: e.tensor_scalar(out=out, in0=in0, scalar1=s1, scalar2=s2, op0=op0, op1=op1), r, w)

    def act(out, in_, func, r, w, **kw):
        p.op("act", lambda e: e.activation(out=out, in_=in_, func=func, **kw), r, w)

    def cp(eng, out, in_, r, w):
        if eng == "act":
            p.op("act", lambda e: e.activation(out=out, in_=in_, func=AF.Copy), r, w)
        else:
            p.op(eng, lambda e: e.tensor_copy(out=out, in_=in_), r, w)

    def red(out, in_, r, w, op=ALU.add):
        p.op("dve", lambda e: e.tensor_reduce(out=out, in_=in_, axis=AX.X, op=op), r, w)

    def dma(eng, out, in_, r, w):
        p.op(eng, lambda e: e.dma_start(out=out, in_=in_), r, w, dma=True)

    def mms(fn, r, w):
        p.op("pe", fn, r, w)

    h.tt, h.stt, h.ts, h.act, h.cp, h.red, h.dma, h.mms = tt, stt, ts, act, cp, red, dma, mms
    return h


class Rot:
    def __init__(self, tiles):
        self.tiles = tiles
        self.i = 0

    def __call__(self):
        t = self.tiles[self.i % len(self.tiles)]
        self.i += 1
        return t


def load_w_bf16(p, h, name, dram, K, N, ncol0, ncols, stg, scale_t=None, scale_off=0, dt=BF16):
    kc = K // 128
    wt = p.sb(name, [128, kc, ncols], dt)
    for c in range(kc):
        s = stg()
        h.dma("sp", s[:, 0:ncols], dram[c * 128:(c + 1) * 128, ncol0:ncol0 + ncols], [], [s])
        if scale_t is not None:
            h.act(wt[:, c, :], s[:, 0:ncols], AF.Copy, [s, scale_t], [(wt, c)], scale=scale_t[:, scale_off + c:scale_off + c + 1])
        else:
            h.cp("pool", wt[:, c, :], s[:, 0:ncols], [s], [(wt, c)])
    return wt


def phase_a1(p, nc, D, n_seq, n_tiles, S_TOK):
    h = helpers(p)
    tt, stt, ts, act, cp, red, dma, mms = h.tt, h.stt, h.ts, h.act, h.cp, h.red, h.dma, h.mms
    NZ = 2592
    cst = p.sb("cst", [128, NCST], F32)
    dma("sp", cst[:], D["cst"], [], [cst])
    vec = p.sb("vec", [128, V_SINK + 8], F32)
    dma("sp", vec[:], D["vecs"][0:V_SINK + 8].partition_broadcast(128), [], [vec])
    nwp = p.sb("nwp", [128, 16], F32)
    dma("sp", nwp[:], D["nwp"], [], [nwp])
    ident = cst[:, C_ID:C_ID + 128]
    z = p.sb("z", [128, NZ], F32)
    Wb = load_w_bf16(p, h, "Wb1", D["w_in"], 1024, 4640, 0, NZ, (lambda: z), nwp, 0)
    w2 = p.sb("w2", [128, 512], F32)
    dma("sp", w2[0:64, :], D["decay_w2"], [], [w2])
    dma("sp", w2[64:128, :], D["iclr_a2"], [], [w2])
    g2 = p.sb("g2", [128, 2, 512], F32)
    dma("sp", g2[:, 0, :], D["gate_g2"][0:128, :], [], [g2])
    dma("sp", g2[0:32, 1, :], D["gate_g2"][128:160, :], [], [g2])
    negsink = p.sb("negsink", [128, 8], F32)
    ts("dve", negsink[:], vec[:, V_SINK:V_SINK + 8], -1.0, None, ALU.mult, None, [vec], [negsink])

    xt = Rot([p.sb("xt%d" % i, [128, 1024], F32) for i in range(2)])
    ss = p.sb("ss", [128, 1], F32)
    rstd = p.sb("rstd", [128, 1], F32)
    xs = p.sb("xs", [128, 1024], F32)
    xsT = p.sb("xsT", [128, 8, 128], BF16)
    carry = p.sb("carry", [128, 1824], F32)
    p.op("pool", lambda e: e.memset(carry[:], 0.0), [], [carry])
    zr = p.sb("zr", [128, 1824], F32)
    lin = p.sb("lin", [128, 288], F32)
    linT = p.sb("linT", [128, 3, 128], F32)
    T5 = Rot([p.sb("t5_%d" % i, [128, 512], F32) for i in range(12)])
    gv = p.sb("gv", [128, 512], F32)
    k2 = p.sb("k2", [128, 512], F32)
    M8 = Rot([p.sb("m8_%d" % i, [128, 8, 128], F32) for i in range(8)])
    M8d = [p.sb("m8d_%d" % i, [128, 8, 128], F32) for i in range(3)]
    Tq = Rot([p.sb("tq_%d" % i, [128, 4, 128], F32) for i in range(4)])
    sm = Rot([p.sb("sm_%d" % i, [128, 8], F32) for i in range(12)])
    PB = Rot([p.ps("pb%d" % i, [128, 512], F32) for i in range(6)])
    PV = [p.ps("pv%d" % i, [128, 512], F32) for i in range(2)]
    NDUM = 0
    if NDUM:
        dumb = p.ps("dumb", [128, 512], F32)
        mms0 = mms

        def mms(fn, r, w):
            def fn2(e):
                ins = fn(e)
                for _ in range(NDUM):
                    e.matmul(dumb[:], lhsT=Wb[:, 0, 0:128], rhs=Wb[:, 0, 0:512], start=True, stop=True)
                return ins
            mms0(fn2, r, w)
    ST = p.sb("ST", [128, 4, 128], F32)
    gC = p.sb("gC", [128, 4], F32)
    qk = p.sb("qk", [128, 10, 64], F32)
    kdup = p.sb("kdup", [128, 2, 2, 64], F32)
    qT = p.sb("qT", [128, 4, 128], BF16)
    kT = [p.sb("kT%d" % i, [128, 2, 128], BF16) for i in range(2)]
    va = [p.sb("va%d" % i, [128, 2, 65], BF16) for i in range(2)]
    for i in range(2):
        p.op("pool", lambda e, i=i: e.memset(va[i][:], 1.0), [], [va[i]])
    pT = Rot([p.sb("pT%d" % i, [128, 2, 128], BF16) for i in range(4)])
    yout = Rot([p.sb("yout%d" % i, [128, 1024], F32) for i in range(1)])

    def vb(off, n=512):
        return vec[:, off:off + n]

    order = [(b, n) for b in range(n_seq) for n in range(n_tiles)]
    xq = {}

    def prefetch(i):
        if i < len(order):
            b, n = order[i]
            t0 = b * S_TOK + n * 128
            xq[i] = xt()
            dma("sp", xq[i][:], D["x"][t0:t0 + 128, :], [], [xq[i]])

    prefetch(0)

    def tile_body(i):
            b, n = order[i]
            if n == 0:
                p.op("pool", lambda e: e.memset(ST[:], 0.0), [], [ST])
            tok0 = b * S_TOK + n * 128
            first = (n == 0)
            x_t = xq.pop(i)
            prefetch(i + 1)
            act(xs[:], x_t[:], AF.Square, [x_t], [xs, ss], accum_out=ss[:])
            act(rstd[:], ss[:], AF.Sqrt, [ss], [rstd], bias=1e-5, scale=1.0 / 1024)
            p.op("dve", lambda e: e.reciprocal(out=rstd[:], in_=rstd[:]), [rstd], [rstd])
            ts("dve", xs[:], x_t[:], rstd[:, 0:1], None, ALU.mult, None, [x_t, rstd], [xs])
            for hh in range(2):
                pb = PB()
                mms(lambda e, pb=pb, hh=hh: [e.transpose(out=pb[:, j * 128:(j + 1) * 128], in_=xs[:, (hh * 4 + j) * 128:(hh * 4 + j + 1) * 128], identity=ident) for j in range(4)][-1],
                    [xs, cst], [pb])
                cp("act" if hh else "dve", xsT[:, hh * 4:hh * 4 + 4, :], pb[:].rearrange("p (a b) -> p a b", a=4), [pb], [(xsT, hh)])
            for cc in range(6):
                c0 = cc * 512
                cw = min(512, NZ - c0)
                pb = PB()
                mms(lambda e, pb=pb, c0=c0, cw=cw: [e.matmul(pb[:, 0:cw], lhsT=xsT[:, c, :], rhs=Wb[:, c, c0:c0 + cw], start=(c == 0), stop=(c == 7)) for c in range(8)][-1],
                    [xsT, Wb], [pb])
                cp("act" if cc % 2 else "dve", z[:, c0:c0 + cw], pb[:, 0:cw], [pb], [(z, cc)])
            if STAGE == 1:
                yo = yout()
                cp("dve", yo[:], z[:, 0:1024], [z], [yo])
                dma("sp", D["y_scr"][tok0:tok0 + 128, :], yo[:], [yo], [])
                return
            cosb = cst[:, C_COS + n * 32:C_COS + n * 32 + 32].unsqueeze(1).to_broadcast([128, 10, 32])
            sinb = cst[:, C_SIN + n * 32:C_SIN + n * 32 + 32].unsqueeze(1).to_broadcast([128, 10, 32])
            zq = z[:, 0:640].rearrange("p (h d) -> p h d", h=10)
            x1, x2 = zq[:, :, 0:32], zq[:, :, 32:64]
            ta, tb_ = T5(), T5()
            tav = ta[:, 0:320].rearrange("p (h d) -> p h d", h=10)
            tbv = tb_[:, 0:320].rearrange("p (h d) -> p h d", h=10)
            zk = [(z, 0), (z, 1)]
            tt("dve", tav, x1, cosb, ALU.mult, zk + [cst], [ta])
            tt("pool", tbv, x2, sinb, ALU.mult, zk + [cst], [tb_])
            tt("dve", qk[:, :, 0:32], tav, tbv, ALU.subtract, [ta, tb_], [(qk, 0)])
            tc_, td = T5(), T5()
            tcv = tc_[:, 0:320].rearrange("p (h d) -> p h d", h=10)
            tdv = td[:, 0:320].rearrange("p (h d) -> p h d", h=10)
            tt("pool", tcv, x2, cosb, ALU.mult, zk + [cst], [tc_])
            tt("dve", tdv, x1, sinb, ALU.mult, zk + [cst], [td])
            tt("pool", qk[:, :, 32:64], tcv, tdv, ALU.add, [tc_, td], [(qk, 1)])
            cp("pool", kdup[:], qk[:, 8:10, :].unsqueeze(2).to_broadcast([128, 2, 2, 64]), [qk], [kdup])
            kTc, kTp = kT[n % 2], kT[(n + 1) % 2]
            vac, vap = va[n % 2], va[(n + 1) % 2]
            cp("pool", vac[:, :, 0:64], z[:, 640:768].rearrange("p (g d) -> p g d", g=2), [(z, 1)], [vac])
            pb = PB()
            mms(lambda e, pb=pb: [e.transpose(out=pb[:, j * 128:(j + 1) * 128], in_=qk[:, 2 * j:2 * j + 2, :].rearrange("p a d -> p (a d)"), identity=ident) for j in range(4)][-1],
                [qk, cst], [pb])
            cp("act", qT[:], pb[:].rearrange("p (a b) -> p a b", a=4), [pb], [qT])
            pb = PB()
            mms(lambda e, pb=pb: [e.transpose(out=pb[:, g * 128:(g + 1) * 128], in_=kdup[:, g, :, :].rearrange("p a d -> p (a d)"), identity=ident) for g in range(2)][-1],
                [kdup, cst], [pb])
            cp("dve", kTc[:], pb[:, 0:256].rearrange("p (a b) -> p a b", a=2), [pb], [kTc])
            yo = yout()
            pv = PV
            for hd in range(0 if "a" in SKIP else 8):
                m, base, g = hd // 2, 64 * (hd % 2), hd // 4
                pb = PB()
                if first:
                    mms(lambda e, pb=pb, m=m, base=base, g=g: e.matmul(pb[:, 128:256], lhsT=kTc[base:base + 64, g, :], rhs=qT[base:base + 64, m, :], start=True, stop=True),
                        [kTc, qT], [pb])
                else:
                    mms(lambda e, pb=pb, m=m, base=base, g=g: [e.matmul(pb[:, 0:128], lhsT=kTp[base:base + 64, g, :], rhs=qT[base:base + 64, m, :], start=True, stop=True),
                                                              e.matmul(pb[:, 128:256], lhsT=kTc[base:base + 64, g, :], rhs=qT[base:base + 64, m, :], start=True, stop=True)][-1],
                        [kTc, kTp, qT], [pb])
                pt = pT()
                lo = 1 if first else 0
                act(pt[:, lo:2, :], pb[:, lo * 128:256].rearrange("p (a b) -> p a b", a=2 - lo), AF.Exp, [pb, negsink], [pt],
                    scale=0.125, bias=negsink[:, hd:hd + 1])
                if not first:
                    tt("pool", pt[:, 0, :], pt[:, 0, :], cst[:, C_SL:C_SL + 128], ALU.mult, [pt, cst], [pt])
                tt("dve", pt[:, 1, :], pt[:, 1, :], cst[:, C_IU:C_IU + 128], ALU.mult, [pt, cst], [pt])
                pvb = pv[hd // 4]
                o0 = (hd % 4) * 65
                if first:
                    mms(lambda e, pvb=pvb, pt=pt, g=g, o0=o0: e.matmul(pvb[:, o0:o0 + 65], lhsT=pt[:, 1, :], rhs=vac[:, g, :], start=True, stop=True),
                        [pt, vac], [pvb])
                else:
                    mms(lambda e, pvb=pvb, pt=pt, g=g, o0=o0: [e.matmul(pvb[:, o0:o0 + 65], lhsT=pt[:, 0, :], rhs=vap[:, g, :], start=True, stop=False),
                                                              e.matmul(pvb[:, o0:o0 + 65], lhsT=pt[:, 1, :], rhs=vac[:, g, :], start=False, stop=True)][-1],
                        [pt, vac, vap], [pvb])
            for hf in range(2):
                pvb = pv[hf]
                pvv = pvb[:, 0:260].rearrange("p (h d) -> p h d", h=4)
                den = sm()
                ts("dve", den[:, 0:4], pvv[:, :, 64], 1.0, None, ALU.add, None, [pvb], [den])
                p.op("dve", lambda e, den=den: e.reciprocal(out=den[:, 0:4], in_=den[:, 0:4]), [den], [den])
                tt("dve", yo[:, hf * 256:(hf + 1) * 256].rearrange("p (h d) -> p h d", h=4), pvv[:, :, 0:64],
                   den[:, 0:4].unsqueeze(2).to_broadcast([128, 4, 64]), ALU.mult, [pvb, den], [(yo, hf)])
            if STAGE == 2:
                cp("dve", yo[:, 512:1024], z[:, 0:512], [z], [yo])
                dma("sp", D["y_scr"][tok0:tok0 + 128, :], yo[:], [yo], [])
                return
            if "r" in SKIP:
                dma("sp", D["y_scr"][tok0:tok0 + 128, :], yo[:], [yo], [(D["y_t"], tok0 // 128)])
                return
            for j in range(4):
                c0 = 768 + j * 512
                cw = min(512, NZ - c0)
                pb = PB()
                zkeys = [(z, 1), (z, 2), (z, 3), (z, 4), (z, 5)]
                if first:
                    mms(lambda e, pb=pb, c0=c0, cw=cw: e.matmul(pb[:, 0:cw], lhsT=cst[:, C_SH:C_SH + 128], rhs=z[:, c0:c0 + cw], start=True, stop=True),
                        zkeys + [cst], [pb])
                else:
                    mms(lambda e, pb=pb, c0=c0, cw=cw: [e.matmul(pb[:, 0:cw], lhsT=cst[:, C_SH:C_SH + 128], rhs=z[:, c0:c0 + cw], start=True, stop=False),
                                                       e.matmul(pb[:, 0:cw], lhsT=cst[:, C_CA:C_CA + 128], rhs=carry[:, c0 - 768:c0 - 768 + cw], start=False, stop=True)][-1],
                        zkeys + [cst, carry], [pb])
                r0 = c0 - 768
                tt("dve", zr[:, r0:r0 + cw], pb[:, 0:cw], z[:, c0:c0 + cw], ALU.subtract, [pb] + zkeys, [(zr, j)])
                tt("dve", zr[:, r0:r0 + cw], zr[:, r0:r0 + cw], vec[:, V_MU + r0:V_MU + r0 + cw], ALU.mult, [(zr, j), vec], [(zr, j)])
                tt("pool", zr[:, r0:r0 + cw], zr[:, r0:r0 + cw], z[:, c0:c0 + cw], ALU.add, [(zr, j)] + zkeys, [(zr, j)])
            cp("pool", carry[96:128, :], z[96:128, 768:NZ], [z], [carry])
            r_, k_, v_ = zr[:, 0:512], zr[:, 512:1024], zr[:, 1024:1536]
            if STAGE == 3:
                cp("dve", yo[:, 512:1024], zr[:, 0:512], [], [yo])
                dma("sp", D["y_scr"][tok0:tok0 + 128, :], yo[:], [yo], [])
                return
            act(lin[:, 0:64], zr[:, 1536:1600], AF.Tanh, [zr], [(lin, 0)])
            cp("pool", lin[:, 64:128], zr[:, 1600:1664], [zr], [(lin, 1)])
            act(lin[:, 128:288], zr[:, 1664:1824], AF.Sigmoid, [zr], [(lin, 2)])
            pb = PB()
            mms(lambda e, pb=pb: [e.transpose(out=pb[:, 0:128], in_=lin[:, 0:128], identity=ident),
                                  e.transpose(out=pb[:, 128:256], in_=lin[:, 128:256], identity=ident),
                                  e.transpose(out=pb[0:32, 256:384], in_=lin[:, 256:288], identity=ident)][-1], [lin, cst], [pb])
            cp("dve", linT[:, 0:2, :], pb[:, 0:256].rearrange("p (a b) -> p a b", a=2), [pb], [(linT, 0)])
            cp("dve", linT[0:32, 2, :], pb[0:32, 256:384], [pb], [(linT, 1)])
            pw, pa_, pg = PB(), PB(), PB()
            mms(lambda e, pw=pw: e.matmul(pw[:], lhsT=linT[0:64, 0, :], rhs=w2[0:64, :], start=True, stop=True), [linT, w2], [pw])
            mms(lambda e, pa_=pa_: e.matmul(pa_[:], lhsT=linT[64:128, 0, :], rhs=w2[64:128, :], start=True, stop=True), [linT, w2], [pa_])
            mms(lambda e, pg=pg: [e.matmul(pg[:], lhsT=linT[:, 1, :], rhs=g2[:, 0, :], start=True, stop=False),
                                  e.matmul(pg[:], lhsT=linT[0:32, 2, :], rhs=g2[0:32, 1, :], start=False, stop=True)][-1], [linT, g2], [pg])
            sg, av = T5(), T5()
            tt("dve", sg[:], pw[:], vb(V_W0), ALU.add, [pw, vec], [sg])
            act(sg[:], sg[:], AF.Sigmoid, [sg], [sg])
            tt("dve", av[:], pa_[:], vb(V_A0), ALU.add, [pa_, vec], [av])
            act(av[:], av[:], AF.Sigmoid, [av], [av])
            cp("act", gv[:], pg[:], [pg], [gv])
            if STAGE == 4:
                cp("dve", yo[:, 512:1024], sg[:], [], [yo])
                dma("sp", D["y_scr"][tok0:tok0 + 128, :], yo[:], [yo], [])
                return
            pc = PB()
            mms(lambda e, pc=pc, sg=sg: e.matmul(pc[:], lhsT=cst[:, C_IU:C_IU + 128], rhs=sg[:], start=True, stop=True), [cst, sg], [pc])
            gam, igam, gprev = T5(), T5(), T5()
            act(gam[:], pc[:], AF.Exp, [pc], [gam], scale=-C_DEC)
            act(igam[:], pc[:], AF.Exp, [pc], [igam], scale=C_DEC)
            tt("dve", gprev[:], pc[:], sg[:], ALU.subtract, [pc, sg], [gprev])
            act(gprev[:], gprev[:], AF.Exp, [gprev], [gprev], scale=-C_DEC)
            pgc = PB()
            mms(lambda e, pgc=pgc, sg=sg: [e.matmul(pgc[:, m:m + 1], lhsT=sg[:, m * 128:(m + 1) * 128], rhs=cst[:, C_ONE:C_ONE + 1], start=True, stop=True) for m in range(4)][-1],
                [cst, sg], [pgc])
            act(gC[:], pgc[:, 0:4], AF.Exp, [pgc], [gC], scale=-C_DEC)
            if STAGE == 5:
                cp("dve", yo[:, 512:1024], gprev[:], [], [yo])
                dma("sp", D["y_scr"][tok0:tok0 + 128, :], yo[:], [yo], [])
                return
            kk, sq = T5(), T5()
            tt("dve", kk[:], k_, vb(V_KK), ALU.mult, [zr, vec], [kk])
            tt("pool", sq[:], kk[:], kk[:], ALU.mult, [kk], [sq])
            if STAGE == 51:
                cp("dve", yo[:, 512:1024], sq[:], [], [yo])
                dma("sp", D["y_scr"][tok0:tok0 + 128, :], yo[:], [yo], [])
                return
            s8 = sm()
            red(s8[:], sq[:].rearrange("p (h d) -> p h d", h=8), [sq], [s8])
            if STAGE == 52:
                cp("dve", yo[:, 512:1024], sq[:], [], [yo])
                dma("sp", D["y_scr"][tok0:tok0 + 128, :], yo[:], [yo], [])
                return
            act(s8[:], s8[:], AF.Sqrt, [s8], [s8], bias=1e-24, scale=1.0)
            p.op("dve", lambda e, s8=s8: e.reciprocal(out=s8[:], in_=s8[:]), [s8], [s8])
            if STAGE == 53:
                cp("dve", yo[:, 512:1024], sq[:], [], [yo])
                dma("sp", D["y_scr"][tok0:tok0 + 128, :], yo[:], [yo], [])
                return
            tt("dve", kk[:].rearrange("p (h d) -> p h d", h=8), kk[:].rearrange("p (h d) -> p h d", h=8),
               s8[:].unsqueeze(2).to_broadcast([128, 8, 64]), ALU.mult, [kk, s8], [kk])
            if STAGE == 54:
                cp("dve", yo[:, 512:1024], kk[:], [], [yo])
                dma("sp", D["y_scr"][tok0:tok0 + 128, :], yo[:], [yo], [])
                return
            t1 = T5()
            stt("dve", t1[:], av[:], -1.0, vb(V_KA), ALU.add, ALU.mult, [av, vec], [t1])
            stt("dve", k2[:], t1[:], 1.0, k_, ALU.add, ALU.mult, [t1, zr], [k2])
            if STAGE == 55:
                cp("dve", yo[:, 512:1024], k2[:], [], [yo])
                dma("sp", D["y_scr"][tok0:tok0 + 128, :], yo[:], [yo], [])
                return
            At, Bt, Kt, Rt = T5(), T5(), T5(), T5()
            stt("dve", At[:], kk[:], -1.0, gprev[:], ALU.mult, ALU.mult, [kk, gprev], [At])
            if STAGE == 56:
                cp("dve", yo[:, 512:1024], At[:], [At], [yo])
                dma("sp", D["y_scr"][tok0:tok0 + 128, :], yo[:], [yo], [])
                return
            tt("pool", Bt[:], kk[:], av[:], ALU.mult, [kk, av], [Bt])
            tt("dve", Bt[:], Bt[:], igam[:], ALU.mult, [Bt, igam], [Bt])
            if STAGE == 57:
                cp("dve", yo[:, 512:1024], Bt[:], [Bt], [yo])
                dma("sp", D["y_scr"][tok0:tok0 + 128, :], yo[:], [yo], [])
                return
            tt("dve", Kt[:], k2[:], igam[:], ALU.mult, [k2, igam], [Kt])
            if STAGE == 58:
                cp("dve", yo[:, 512:1024], Kt[:], [Kt], [yo])
                dma("sp", D["y_scr"][tok0:tok0 + 128, :], yo[:], [yo], [])
                return
            tt("pool", Rt[:], r_, gam[:], ALU.mult, [zr, gam], [Rt])
            if STAGE == 6:
                cp("dve", yo[:, 512:1024], Rt[:], [Rt], [yo])
                dma("sp", D["y_scr"][tok0:tok0 + 128, :], yo[:], [yo], [])
                return
            XT = {}
            for nm, src in (("A", At), ("B", Bt), ("K", Kt), ("R", Rt)):
                pb = PB()
                mms(lambda e, pb=pb, src=src: [e.transpose(out=pb[:, j * 128:(j + 1) * 128], in_=src[:, j * 128:(j + 1) * 128], identity=ident) for j in range(4)][-1],
                    [src, cst], [pb])
                dst = Tq()
                cp("act" if nm in ("A", "K") else "dve", dst[:], pb[:].rearrange("p (a b) -> p a b", a=4), [pb], [dst])
                XT[nm] = dst
            AT, BT, KT, RT = XT["A"], XT["B"], XT["K"], XT["R"]

            def pairmat(l, r_op, mask_off, eng2, dst=None):
                dst = dst or M8()
                for par in range(2):
                    pb = PB()
                    mms(lambda e, pb=pb, par=par: [e.matmul(pb[:, j * 128:(j + 1) * 128],
                                                          lhsT=l[64 * par:64 * par + 64, j, :],
                                                          rhs=r_op[64 * par:64 * par + 64, j, :],
                                                          start=True, stop=True) for j in range(4)][-1], [l, r_op], [pb])
                    tt(eng2[par], dst[:, par:8:2, :], pb[:].rearrange("p (a b) -> p a b", a=4),
                       cst[:, mask_off:mask_off + 128].unsqueeze(1).to_broadcast([128, 4, 128]), ALU.mult, [pb, cst], [(dst, par)])
                return dst

            Nm = pairmat(BT, AT, C_SU, ("dve", "dve"))
            Am = pairmat(AT, BT, C_SL, ("dve", "dve"))
            AkT = pairmat(KT, AT, C_SU, ("dve", "dve"), M8d[0])
            RbT = pairmat(BT, RT, C_IU, ("dve", "dve"), M8d[1])
            RkT = pairmat(KT, RT, C_IU, ("dve", "dve"), M8d[2])
            if STAGE == 7:
                cp("dve", yo[:, 512:1024].rearrange("p (a b) -> p a b", a=4), RkT[:, 0:4, :], [RkT], [yo])
                dma("sp", D["y_scr"][tok0:tok0 + 128, :], yo[:], [yo], [])
                return
            X = M8()
            tt("dve", X[:], Nm[:], ident.unsqueeze(1).to_broadcast([128, 8, 128]), ALU.add, [Nm, cst], [X])
            for j in range(0 if "d" in SKIP else 6):
                Nn, An, Xn = M8(), M8(), M8()
                last = (j == 5)
                for hf in range(2):
                    pbn, pba = PB(), PB()
                    if not last:
                        mms(lambda e, pbn=pbn, hf=hf, Am=Am, Nm=Nm: [e.matmul(pbn[:, q * 128:(q + 1) * 128], lhsT=Am[:, hf * 4 + q, :], rhs=Nm[:, hf * 4 + q, :], start=True, stop=True) for q in range(4)][-1],
                            [Am, Nm], [pbn])
                        cp("act", Nn[:, hf * 4:hf * 4 + 4, :], pbn[:].rearrange("p (a b) -> p a b", a=4), [pbn], [(Nn, hf)])
                    mms(lambda e, pba=pba, hf=hf, Am=Am, Nm=Nm: [e.matmul(pba[:, q * 128:(q + 1) * 128], lhsT=Nm[:, hf * 4 + q, :], rhs=Am[:, hf * 4 + q, :], start=True, stop=True) for q in range(4)][-1],
                        [Am, Nm], [pba])
                    cp("dve", An[:, hf * 4:hf * 4 + 4, :], pba[:].rearrange("p (a b) -> p a b", a=4), [pba], [(An, hf)])
                for hf in range(2):
                    pbx = PB()
                    mms(lambda e, pbx=pbx, hf=hf, An=An, X=X: [e.matmul(pbx[:, q * 128:(q + 1) * 128], lhsT=An[:, hf * 4 + q, :], rhs=X[:, hf * 4 + q, :], start=True, stop=True) for q in range(4)][-1],
                        [An, X], [pbx])
                    tt("dve", Xn[:, hf * 4:hf * 4 + 4, :], pbx[:].rearrange("p (a b) -> p a b", a=4), X[:, hf * 4:hf * 4 + 4, :], ALU.add, [pbx, X], [(Xn, hf)])
                Nm, Am, X = Nn, An, Xn
            if STAGE == 8:
                cp("dve", yo[:, 512:1024].rearrange("p (a b) -> p a b", a=4), X[:, 0:4, :], [X], [yo])
                dma("sp", D["y_scr"][tok0:tok0 + 128, :], yo[:], [yo], [])
                return
            pr = PB()

            def f_rhs0(e, pr=pr, AT=AT, AkT=AkT):
                ins = None
                for m in range(4):
                    e.matmul(pr[:, m * 128:(m + 1) * 128], lhsT=AT[:, m, :], rhs=ST[:, m, :], start=True, stop=False)
                    for q in range(2):
                        hd = 2 * m + q
                        ins = e.matmul(pr[:, hd * 64:(hd + 1) * 64], lhsT=AkT[:, hd, :], rhs=zr[:, 1024 + hd * 64:1024 + (hd + 1) * 64], start=False, stop=(q == 1))
                return ins
            mms(f_rhs0, [AT, ST, AkT, zr], [pr])
            rhs0 = T5()
            cp("act", rhs0[:], pr[:], [pr], [rhs0])
            pu = PB()
            mms(lambda e, pu=pu, X=X, rhs0=rhs0: [e.matmul(pu[:, hd * 64:(hd + 1) * 64], lhsT=X[:, hd, :], rhs=rhs0[:, hd * 64:(hd + 1) * 64], start=True, stop=True) for hd in range(8)][-1],
                [X, rhs0], [pu])
            U = T5()
            cp("dve", U[:], pu[:], [pu], [U])
            py = PB()

            def f_y(e, py=py, RT=RT, RbT=RbT, RkT=RkT, U=U):
                ins = None
                for m in range(4):
                    e.matmul(py[:, m * 128:(m + 1) * 128], lhsT=RT[:, m, :], rhs=ST[:, m, :], start=True, stop=False)
                    for q in range(2):
                        hd = 2 * m + q
                        e.matmul(py[:, hd * 64:(hd + 1) * 64], lhsT=RbT[:, hd, :], rhs=U[:, hd * 64:(hd + 1) * 64], start=False, stop=False)
                        ins = e.matmul(py[:, hd * 64:(hd + 1) * 64], lhsT=RkT[:, hd, :], rhs=zr[:, 1024 + hd * 64:1024 + (hd + 1) * 64], start=False, stop=(q == 1))
                return ins
            mms(f_y, [RT, ST, RbT, RkT, U, zr], [py])
            yv = T5()
            cp("act", yv[:], py[:], [py], [yv])
            pst = PB()

            def f_s(e, pst=pst, Bt=Bt, Kt=Kt, U=U):
                ins = None
                for m in range(4):
                    e.matmul(pst[:, m * 128:(m + 1) * 128], lhsT=Bt[:, m * 128:(m + 1) * 128], rhs=U[:, m * 128:(m + 1) * 128], start=True, stop=False)
                    ins = e.matmul(pst[:, m * 128:(m + 1) * 128], lhsT=Kt[:, m * 128:(m + 1) * 128], rhs=zr[:, 1024 + m * 128:1024 + (m + 1) * 128], start=False, stop=True)
                return ins
            mms(f_s, [Bt, Kt, U, zr], [pst])
            tt("dve", ST[:], pst[:].rearrange("p (a b) -> p a b", a=4), ST[:], ALU.add, [pst, ST], [ST])
            tt("dve", ST[:], ST[:], gC[:].unsqueeze(2).to_broadcast([128, 4, 128]), ALU.mult, [ST, gC], [ST])
            tt("dve", ST[:], ST[:], cst[:, C_BD:C_BD + 128].unsqueeze(1).to_broadcast([128, 4, 128]), ALU.mult, [ST, cst], [ST])
            if STAGE == 9:
                cp("dve", yo[:, 512:1024], yv[:], [yv], [yo])
                dma("sp", D["y_scr"][tok0:tok0 + 128, :], yo[:], [yo], [])
                return
            y3 = yv[:].rearrange("p (h d) -> p h d", h=8)
            ysq = T5()
            tt("pool", ysq[:], yv[:], yv[:], ALU.mult, [yv], [ysq])
            s1, s2, mean, var = sm(), sm(), sm(), sm()
            red(s1[:], y3, [yv], [s1])
            red(s2[:], ysq[:].rearrange("p (h d) -> p h d", h=8), [ysq], [s2])
            ts("dve", mean[:], s1[:], 1.0 / 64, None, ALU.mult, None, [s1], [mean])
            tt("dve", var[:], mean[:], mean[:], ALU.mult, [mean], [var])
            stt("dve", var[:], s2[:], 1.0 / 64, var[:], ALU.mult, ALU.subtract, [s2, var], [var])
            act(var[:], var[:], AF.Sqrt, [var], [var], bias=64e-5, scale=1.0)
            p.op("dve", lambda e, var=var: e.reciprocal(out=var[:], in_=var[:]), [var], [var])
            yn = T5()
            yn3 = yn[:].rearrange("p (h d) -> p h d", h=8)
            tt("dve", yn3, y3, mean[:].unsqueeze(2).to_broadcast([128, 8, 64]), ALU.subtract, [yv, mean], [yn])
            tt("dve", yn3, yn3, var[:].unsqueeze(2).to_broadcast([128, 8, 64]), ALU.mult, [yn, var], [yn])
            tt("pool", yn[:], yn[:], vb(V_LNW), ALU.mult, [yn, vec], [yn])
            tt("pool", yn[:], yn[:], vb(V_LNB), ALU.add, [yn, vec], [yn])
            rk = T5()
            tt("pool", rk[:], r_, k2[:], ALU.mult, [zr, k2], [rk])
            tt("pool", rk[:], rk[:], vb(V_RK), ALU.mult, [rk, vec], [rk])
            sb_ = sm()
            red(sb_[:], rk[:].rearrange("p (h d) -> p h d", h=8), [rk], [sb_])
            tt("dve", rk[:].rearrange("p (h d) -> p h d", h=8), v_.rearrange("p (h d) -> p h d", h=8),
               sb_[:].unsqueeze(2).to_broadcast([128, 8, 64]), ALU.mult, [zr, sb_], [rk])
            tt("pool", yn[:], yn[:], rk[:], ALU.add, [yn, rk], [yn])
            tt("pool", yo[:, 512:1024], yn[:], gv[:], ALU.mult, [yn, gv], [(yo, 2)])
            dma("sp", D["y_scr"][tok0:tok0 + 128, :], yo[:], [yo], [(D["y_t"], tok0 // 128)])

    for i in range(len(order)):
        tile_body(i)


def phase_a2(p, nc, D, ntok, final=True, drip=None):
    h = helpers(p)
    tt, stt, ts, act, cp, red, dma, mms = h.tt, h.stt, h.ts, h.act, h.cp, h.red, h.dma, h.mms
    cst = p.sb("cstb", [128, 128], F32)
    dma("sp", cst[:], D["cst"][:, C_ID:C_ID + 128], [], [cst])
    ident = cst[:, 0:128]
    nwp = p.sb("nwpb", [128, 16], F32)
    dma("sp", nwp[:], D["nwp"], [], [nwp])
    fin = p.sb("finw", [128, 1024], F32)
    dma("sp", fin[:], D["vecs"][V_FIN:V_FIN + 1024].partition_broadcast(128), [], [fin])
    stg = Rot([p.sb("stgb%d" % i, [128, 2048], F32) for i in range(2)])
    Wg = load_w_bf16(p, h, "Wg", D["w_in"], 1024, 4640, 2592, 2048, stg, nwp, 0)
    PA = load_w_bf16(p, h, "PAw", D["proj_attn"], 512, 1024, 0, 1024, stg)
    PBw = load_w_bf16(p, h, "PBw", D["proj_rwkv"], 512, 1024, 0, 1024, stg)
    WO = load_w_bf16(p, h, "WOw", D["w_out"], 1024, 1024, 0, 1024, stg)
    nt = ntok // 128
    NS = 2

    class St:
        pass

    streams = []
    for k in range(NS):
        S = St()
        S.xt = Rot([p.sb("xtb%d_%d" % (k, i), [128, 1024], F32) for i in range(2)])
        S.yt = Rot([p.sb("ytb%d_%d" % (k, i), [128, 1024], F32) for i in range(2)])
        S.ss = p.sb("ssb%d" % k, [128, 1], F32)
        S.rstd = p.sb("rstdb%d" % k, [128, 1], F32)
        S.xs = p.sb("xsb%d" % k, [128, 1024], F32)
        S.xsT = p.sb("xsTb%d" % k, [128, 8, 128], BF16)
        S.yT = p.sb("yTb%d" % k, [128, 8, 128], BF16)
        S.sgt = p.sb("sgt%d" % k, [128, 2048], BF16)
        S.mg = p.sb("mg%d" % k, [128, 1024], F32)
        S.m2 = p.sb("m2%d" % k, [128, 1024], F32)
        S.mgT = p.sb("mgT%d" % k, [128, 8, 128], BF16)
        S.h1 = Rot([p.sb("h1b%d_%d" % (k, i), [128, 1024], F32) for i in range(2)])
        S.PB = Rot([p.ps("pq%d_%d" % (k, i), [128, 512], F32) for i in range(4)])
        S.xq, S.yq = {}, {}
        streams.append(S)

    def prefetch(S, i):
        if i < nt:
            S.xq[i] = S.xt()
            dma("sp", S.xq[i][:], D["x"][i * 128:(i + 1) * 128, :], [], [S.xq[i]])
            S.yq[i] = S.yt()
            dma("sp", S.yq[i][:], D["y_scr"][i * 128:(i + 1) * 128, :], [(D["y_t"], i)], [S.yq[i]])

    def tp8(S, src, dst):
        for hh in range(2):
            pb = S.PB()
            mms(lambda e, pb=pb, hh=hh: [e.transpose(out=pb[:, j * 128:(j + 1) * 128], in_=src[:, (hh * 4 + j) * 128:(hh * 4 + j + 1) * 128], identity=ident) for j in range(4)][-1],
                [src, cst], [pb])
            cp("act" if hh else "dve", dst[:, hh * 4:hh * 4 + 4, :], pb[:].rearrange("p (a b) -> p a b", a=4), [pb], [(dst, hh)])

    def body(S, i):
        ss, rstd, xs, xsT, yT, sgt, mg, m2, mgT = S.ss, S.rstd, S.xs, S.xsT, S.yT, S.sgt, S.mg, S.m2, S.mgT
        x_t, y_t = S.xq.pop(i), S.yq.pop(i)
        prefetch(S, i + NS)
        act(xs[:], x_t[:], AF.Square, [x_t], [xs, ss], accum_out=ss[:])
        act(rstd[:], ss[:], AF.Sqrt, [ss], [rstd], bias=1e-5, scale=1.0 / 1024)
        p.op("dve", lambda e: e.reciprocal(out=rstd[:], in_=rstd[:]), [rstd], [rstd])
        ts("dve", xs[:], x_t[:], rstd[:, 0:1], None, ALU.mult, None, [x_t, rstd], [xs])
        tp8(S, xs, xsT)
        for cc in range(4):
            pb = S.PB()
            mms(lambda e, pb=pb, cc=cc: [e.matmul(pb[:], lhsT=xsT[:, c, :], rhs=Wg[:, c, cc * 512:(cc + 1) * 512], start=(c == 0), stop=(c == 7)) for c in range(8)][-1],
                [xsT, Wg], [pb])
            act(sgt[:, cc * 512:(cc + 1) * 512], pb[:], AF.Sigmoid, [pb], [(sgt, cc)])
        tp8(S, y_t, yT)
        for br, (W, dstt) in enumerate(((PA, mg), (PBw, m2))):
            for hf in range(2):
                pb = S.PB()
                mms(lambda e, pb=pb, br=br, hf=hf, W=W: [e.matmul(pb[:], lhsT=yT[:, br * 4 + c, :], rhs=W[:, c, hf * 512:(hf + 1) * 512], start=(c == 0), stop=(c == 3)) for c in range(4)][-1],
                    [yT, W], [pb])
                tt("dve", dstt[:, hf * 512:(hf + 1) * 512], pb[:], sgt[:, br * 1024 + hf * 512:br * 1024 + (hf + 1) * 512], ALU.mult, [pb, sgt], [(dstt, hf)])
        tt("pool", mg[:], mg[:], m2[:], ALU.add, [mg, m2], [mg])
        tp8(S, mg, mgT)
        ho = S.h1()
        for hf in range(2):
            pb = S.PB()
            mms(lambda e, pb=pb, hf=hf: [e.matmul(pb[:], lhsT=mgT[:, c, :], rhs=WO[:, c, hf * 512:(hf + 1) * 512], start=(c == 0), stop=(c == 7)) for c in range(8)][-1],
                [mgT, WO], [pb])
            tt("dve", ho[:, hf * 512:(hf + 1) * 512], pb[:], x_t[:, hf * 512:(hf + 1) * 512], ALU.add, [pb, x_t], [(ho, hf)])
        if final:
            act(xs[:], ho[:], AF.Square, [ho], [xs, ss], accum_out=ss[:])
            act(rstd[:], ss[:], AF.Sqrt, [ss], [rstd], bias=1e-5, scale=1.0 / 1024)
            p.op("dve", lambda e: e.reciprocal(out=rstd[:], in_=rstd[:]), [rstd], [rstd])
            stt("dve", ho[:], ho[:], rstd[:, 0:1], fin[:], ALU.mult, ALU.mult, [ho, rstd, fin], [ho])
            dma("sp", D["out"][i * 128:(i + 1) * 128, :], ho[:], [ho], [])
        else:
            dma("sp", D["h1_scr"][i * 128:(i + 1) * 128, :], ho[:], [ho], [(D["h1_t"], i)])

    for k in range(NS):
        prefetch(streams[k], k)
    per = (len(drip) + max(nt // NS - 1, 1) - 1) // max(nt // NS - 1, 1) if drip else 0
    for i0 in range(0, nt, NS):
        lists = []
        for k in range(NS):
            if i0 + k < nt:
                p.defer_begin()
                body(streams[k], i0 + k)
                lists.append(p.defer_end())
        while any(lists):
            for lst in lists:
                if lst:
                    p.drain(lst, 1)
        if drip:
            p.drain(drip, per)
    if drip:
        p.drain(drip, len(drip))


def phase_b0(p, nc, D, eng_rot=("dve", "pool"), nbuf=2, ldq="sp", stq="act"):
    h = helpers(p)
    dma, cp = h.dma, h.cp
    stg = Rot([p.sb("cs%d" % i, [128, 4096], F32) for i in range(nbuf)])
    ob = Rot([p.sb("co%d" % i, [128, 4096], BF16) for i in range(nbuf)])
    nwp0 = p.sb("nwp0", [128, 16], F32)
    dma("sp", nwp0[:], D["nwp"], [], [nwp0])
    k = 0
    for g in range(32):
        s, o = stg(), ob()
        dma(ldq, s[:].rearrange("p (dc e) -> p dc e", dc=8), D["uT"][:, g * 512:(g + 1) * 512].rearrange("(dc p) e -> p dc e", p=128), [], [s])
        h.tt(eng_rot[k % 2], o[:].rearrange("p (i dc e) -> p dc i e", i=4, dc=8), s[:].rearrange("p (dc i e) -> p dc i e", dc=8, i=4),
             nwp0[:, 8:16].unsqueeze(2).unsqueeze(3).to_broadcast([128, 8, 4, 128]), ALU.mult, [s, nwp0], [o])
        k += 1
        dma(stq, D["u2"][:, g * 4:(g + 1) * 4, :, :].rearrange("p i dc e -> p (i dc e)"), o[:], [o], [(D["u2_t"], g)])
        s, o = stg(), ob()
        dma(ldq, s[:].rearrange("p (i d) -> p i d", i=4), D["v"][g * 512:(g + 1) * 512, :].rearrange("(i p) d -> p i d", p=128), [], [s])
        cp(eng_rot[k % 2], o[:], s[:], [s], [o])
        k += 1
        dma(stq, D["vb"][g * 512:(g + 1) * 512, :].rearrange("(i p) d -> p i d", p=128), o[:].rearrange("p (i d) -> p i d", i=4), [o], [(D["vb_t"], g)])


def phase_b(p, nc, D, ntok):
    h = helpers(p)
    tt, stt, ts, act, cp, red, dma, mms = h.tt, h.stt, h.ts, h.act, h.cp, h.red, h.dma, h.mms
    TT = 256
    cst = p.sb("cstc", [128, 256], F32)
    dma("sp", cst[:, 0:128], D["cst"][:, C_ID:C_ID + 128], [], [cst])
    dma("sp", cst[:, 128:256], D["cst"][:, C_IOTA:C_IOTA + 128], [], [cst])
    ident = cst[:, 0:128]
    iota = cst[:, 128:256]
    iota_bf = p.sb("iota_bf", [128, 128], BF16)
    cp("dve", iota_bf[:], iota, [cst], [iota_bf])
    fin = p.sb("finc", [128, 1024], F32)
    dma("sp", fin[:], D["vecs"][V_FIN:V_FIN + 1024].partition_broadcast(128), [], [fin])
    nwpb = p.sb("nwpc", [128, 16], F32)
    dma("sp", nwpb[:], D["nwp"], [], [nwpb])
    skT = p.sb("skT", [128, 8, 128], F32)
    dma("sp", skT[:], D["skT"], [], [skT])
    G = p.sb("G", [128, TT, 128], BF16)
    xs = p.sb("xsc", [128, 1024], F32)
    Wq = load_w_bf16(p, h, "Wq", D["peer_wq"], 1024, 1024, 0, 1024, (lambda: xs), nwpb, 8)
    U2 = Rot([p.sb("u2t%d" % i, [128, 4, 8, 128], BF16) for i in range(3)])
    Vt = Rot([p.sb("vt%d" % i, [128, 4, 1024], BF16) for i in range(3)])
    h1 = [[p.sb("h1c%d_%d" % (b, i), [128, 1024], F32) for i in range(2)] for b in range(2)]
    ss = p.sb("ssc", [128, 1], F32)
    rstd = p.sb("rstdc", [128, 1], F32)
    xTb = [p.sb("xs2T%d" % b, [128, 8, TT], BF16) for b in range(2)]
    qT = p.sb("qTc", [128, 8, TT], F32)
    sc = p.sb("sc", [128, 16, 128], F32)
    v16 = p.sb("v16", [128, 16, 16], F32)
    i16u = p.sb("i16u", [128, 16, 16], U32)
    i16f = p.sb("i16f", [128, 16, 16], F32)
    cand = p.sb("cand", [128, 8, 256], F32)
    tv = p.sb("tv", [128, 8, 16], F32)
    posu = p.sb("posu", [128, 8, 16], U32)
    au = p.sb("au", [128, 8, 16], U32)
    bu = p.sb("bu", [128, 8, 16], U32)
    af = p.sb("af", [128, 8, 16], F32)
    bf_ = p.sb("bf", [128, 8, 16], F32)
    sel = p.sb("sel", [128, 3, 128], F32)
    sm = Rot([p.sb("smc%d" % i, [128, 8], F32) for i in range(4)])
    selTb = [p.sb("selT%d" % b, [128, 3, TT], F32) for b in range(2)]
    OA = Rot([p.sb("oa%d" % i, [128, 16, 128], BF16) for i in range(2)])
    OB = Rot([p.sb("ob%d" % i, [128, 16, 128], BF16) for i in range(2)])
    gh = Rot([p.sb("gh%d" % i, [128, TT], BF16) for i in range(2)])
    ac = Rot([p.sb("ac%d" % i, [128, TT], BF16) for i in range(3)])
    pbs = [p.ps("pr%d" % i, [128, 512], F32) for i in range(4)]
    PBH = Rot(pbs[0:3])
    PBP = Rot(pbs[3:4])
    PBG = Rot(pbs)
    ACC = [p.ps("acc%d" % i, [128, 512], F32) for i in range(4)]
    ntile = ntok // TT

    def prep(tix):
        b = tix % 2
        t0 = tix * TT
        xT, selT = xTb[b], selTb[b]
        PB = PBP
        for s in range(2):
            hh1 = h1[b][s]
            dma("sp", hh1[:], D["h1_scr"][t0 + s * 128:t0 + (s + 1) * 128, :], [(D["h1_t"], tix * 2 + s)], [hh1])
            act(xs[:], hh1[:], AF.Square, [hh1], [xs, ss], accum_out=ss[:])
            act(rstd[:], ss[:], AF.Sqrt, [ss], [rstd], bias=1e-5, scale=1.0 / 1024)
            p.op("dve", lambda e: e.reciprocal(out=rstd[:], in_=rstd[:]), [rstd], [rstd])
            ts("dve", xs[:], hh1[:], rstd[:, 0:1], None, ALU.mult, None, [hh1, rstd], [xs])
            for hh in range(2):
                pb = PB()
                mms(lambda e, pb=pb, hh=hh: [e.transpose(out=pb[:, j * 128:(j + 1) * 128], in_=xs[:, (hh * 4 + j) * 128:(hh * 4 + j + 1) * 128], identity=ident) for j in range(4)][-1],
                    [xs, cst], [pb])
                cp("act" if hh else "dve", xT[:, hh * 4:hh * 4 + 4, s * 128:(s + 1) * 128], pb[:].rearrange("p (a b) -> p a b", a=4), [pb], [(xT, (s, hh))])
        for c in range(8):
            pb = PB()
            mms(lambda e, pb=pb, c=c, xT=xT: [e.matmul(pb[:, 0:TT], lhsT=Wq[:, dc, c * 128:(c + 1) * 128], rhs=xT[:, dc, :], start=(dc == 0), stop=(dc == 7)) for dc in range(8)][-1],
                [Wq, xT], [pb])
            cp("act" if c % 2 else "dve", qT[:, c, :], pb[:, 0:TT], [pb], [(qT, c)])
        for s in range(0 if "S" in SKIP else 2):
            for par in range(2):
                for half in range(2):
                    pb = PB()
                    mms(lambda e, pb=pb, par=par, half=half, s=s: [e.matmul(pb[:, j * 128:(j + 1) * 128], lhsT=qT[64 * par:64 * par + 64, half * 4 + j, s * 128:(s + 1) * 128],
                                                                        rhs=skT[64 * par:64 * par + 64, half * 4 + j, :], start=True, stop=True) for j in range(4)][-1],
                        [qT, skT], [pb])
                    cp("act", sc[:, 8 * half + par:8 * half + 8:2, :], pb[:].rearrange("p (a b) -> p a b", a=4), [pb], [(sc, 8 * half + par + 2 * j) for j in range(4)])
            tmpA = cand[:].rearrange("p h (a b) -> p (h a) b", a=2)
            for hp in range(16):
                p.op("dve", lambda e, hp=hp: e.max(out=v16[:, hp, 0:8], in_=sc[:, hp, :]), [(sc, hp)], [(v16, hp)])
            for hp in range(16):
                p.op("dve", lambda e, hp=hp, tmpA=tmpA: e.match_replace(out=tmpA[:, hp, :], in_to_replace=v16[:, hp, 0:8], in_values=sc[:, hp, :], imm_value=-1e30), [(sc, hp), (v16, hp)], [(cand, hp)])
            for hp in range(16):
                p.op("dve", lambda e, hp=hp, tmpA=tmpA: e.max(out=v16[:, hp, 8:16], in_=tmpA[:, hp, :]), [(cand, hp)], [(v16, hp)])
            for hp in range(16):
                p.op("dve", lambda e, hp=hp: e.max_index(out=i16u[:, hp, 0:8], in_max=v16[:, hp, 0:8], in_values=sc[:, hp, :]), [(sc, hp), (v16, hp)], [(i16u, hp)])
            for hp in range(16):
                p.op("dve", lambda e, hp=hp: e.max_index(out=i16u[:, hp, 8:16], in_max=v16[:, hp, 8:16], in_values=sc[:, hp, :]), [(sc, hp), (v16, hp)], [(i16u, hp)])
            cp("pool", i16f[:], i16u[:], [i16u], [i16f])
            tt("pool", cand[:].rearrange("p h (a b) -> p h a b", a=16), v16[:, 0:16:2, :].unsqueeze(3).to_broadcast([128, 8, 16, 16]),
               v16[:, 1:16:2, :].unsqueeze(2).to_broadcast([128, 8, 16, 16]), ALU.add, [v16], [cand])
            tmpB = sc[:].rearrange("p (h a) b -> p h (a b)", a=2)
            for hd in range(8):
                p.op("dve", lambda e, hd=hd: e.max(out=tv[:, hd, 0:8], in_=cand[:, hd, :]), [cand], [(tv, hd)])
            for hd in range(8):
                p.op("dve", lambda e, hd=hd, tmpB=tmpB: e.match_replace(out=tmpB[:, hd, :], in_to_replace=tv[:, hd, 0:8], in_values=cand[:, hd, :], imm_value=-1e30), [cand, (tv, hd)], [(sc, 2 * hd), (sc, 2 * hd + 1)])
            for hd in range(8):
                p.op("dve", lambda e, hd=hd, tmpB=tmpB: e.max(out=tv[:, hd, 8:16], in_=tmpB[:, hd, :]), [(sc, 2 * hd), (sc, 2 * hd + 1)], [(tv, hd)])
            for hd in range(8):
                p.op("dve", lambda e, hd=hd: e.max_index(out=posu[:, hd, 0:8], in_max=tv[:, hd, 0:8], in_values=cand[:, hd, :]), [cand, (tv, hd)], [(posu, hd)])
            for hd in range(8):
                p.op("dve", lambda e, hd=hd: e.max_index(out=posu[:, hd, 8:16], in_max=tv[:, hd, 8:16], in_values=cand[:, hd, :]), [cand, (tv, hd)], [(posu, hd)])
            gt = sel[:, 2, :].rearrange("p (h k) -> p h k", h=8)
            tt("pool", gt, tv[:], tv[:, :, 0:1].to_broadcast([128, 8, 16]), ALU.subtract, [tv], [(sel, 2)])
            act(gt, gt, AF.Exp, [(sel, 2)], [(sel, 2)])
            z8 = sm()
            red(z8[:], gt, [(sel, 2)], [z8])
            p.op("dve", lambda e, z8=z8: e.reciprocal(out=z8[:], in_=z8[:]), [z8], [z8])
            tt("dve", gt, gt, z8[:].unsqueeze(2).to_broadcast([128, 8, 16]), ALU.mult, [(sel, 2), z8], [(sel, 2)])
            ts("dve", au[:], posu[:], 4, None, ALU.logical_shift_right, None, [posu], [au])
            ts("dve", bu[:], posu[:], 15, None, ALU.bitwise_and, None, [posu], [bu])
            cp("pool", af[:], au[:], [au], [af])
            cp("pool", bf_[:], bu[:], [bu], [bf_])
            io16 = iota[:, 0:16].unsqueeze(1).unsqueeze(1).to_broadcast([128, 8, 16, 16])
            eq = cand[:].rearrange("p h (a b) -> p h a b", a=16)
            for w, (xf, par) in enumerate(((af, 0), (bf_, 1))):
                tt("dve", eq, io16, xf[:].unsqueeze(3).to_broadcast([128, 8, 16, 16]), ALU.is_equal, [cst, xf], [cand])
                tt("pool", eq, eq, i16f[:, par:16:2, :].unsqueeze(2).to_broadcast([128, 8, 16, 16]), ALU.mult, [cand, i16f], [cand])
                red(sel[:, w, :].rearrange("p (h k) -> p h k", h=8), eq, [cand], [(sel, w)])
            pb = PB()
            mms(lambda e, pb=pb: [e.transpose(out=pb[:, w * 128:(w + 1) * 128], in_=sel[:, w, :], identity=ident) for w in range(3)][-1], [sel, cst], [pb])
            cp("act", selT[:, :, s * 128:(s + 1) * 128], pb[:, 0:384].rearrange("p (a b) -> p a b", a=3), [pb], [(selT, s)])

    def gbuild(tix):
        selT = selTb[tix % 2]
        PB = PBG
        NG = 0 if "G" in SKIP else TT // 16
        bufs = {}

        def onehots(g):
            tk = g * 16
            oa, ob = OA(), OB()
            bufs[g] = (oa, ob)
            io = iota_bf[:, :].unsqueeze(1).to_broadcast([128, 16, 128])
            if "o" in SKIP:
                return
            tt("dve", oa[:], io, selT[:, 0, tk:tk + 16].unsqueeze(2).to_broadcast([128, 16, 128]), ALU.is_equal, [iota_bf, selT], [oa])
            for t in range(16):
                act(oa[:, t, :], oa[:, t, :], AF.Copy, [(oa, t), selT], [(oa, t)], scale=selT[:, 2, tk + t:tk + t + 1])
            tt("dve", ob[:], io, selT[:, 1, tk:tk + 16].unsqueeze(2).to_broadcast([128, 16, 128]), ALU.is_equal, [iota_bf, selT], [ob])

        def mm_evac(g):
            tk = g * 16
            oa, ob = bufs.pop(g)
            for q4 in range(4):
                pb = PB()
                if "m" not in SKIP:
                  mms(lambda e, pb=pb, q4=q4, oa=oa, ob=ob: [e.matmul(pb[:, j * 128:(j + 1) * 128], lhsT=ob[:, q4 * 4 + j, :], rhs=oa[:, q4 * 4 + j, :], start=True, stop=True) for j in range(4)][-1],
                    [oa, ob], [pb])
                tq = tk + q4 * 4
                if "v" not in SKIP:
                  cp("act" if q4 != 3 else "dve", G[:, tq:tq + 4, :], pb[:].rearrange("p (t i) -> p t i", t=4), [pb], [(G, tq)])

        if NG:
            onehots(0)
        for g in range(NG):
            if g + 1 < NG:
                onehots(g + 1)
            mm_evac(g)

    def expert(tix, nxt):
        xT = xTb[tix % 2]
        LOOK = 2
        grp = {}
        hb = {}

        def emit_H(i):
            ig, ii = divmod(i, 4)
            if ii == 0:
                u2, vt = U2(), Vt()
                grp[ig] = (u2, vt)
                if not ("D" in SKIP and ig >= 2):
                    dma("sp", vt[:], D["vb"][ig * 512:(ig + 1) * 512, :].rearrange("(i p) d -> p i d", p=128), [(D["vb_t"], ig)], [vt])
                    dma("sp", u2[:].rearrange("p i dc e -> p (i dc e)"), D["u2"][:, ig * 4:(ig + 1) * 4, :, :].rearrange("p i dc e -> p (i dc e)"), [(D["u2_t"], ig)], [u2])
            u2, vt = grp[ig]
            pb = PBH()
            mms(lambda e, pb=pb, u2=u2, ii=ii: [e.matmul(pb[:, 0:TT], lhsT=u2[:, ii, dc, :], rhs=xT[:, dc, :], start=(dc == 0), stop=(dc == 7)) for dc in range(8)][-1],
                [u2, xT], [pb])
            hb[i] = pb

        def emit_rest(i):
            ig, ii = divmod(i, 4)
            u2, vt = grp[ig]
            pb = hb.pop(i)
            g_, a_ = gh(), ac()
            if "X" not in SKIP:
                act(g_[:], pb[:, 0:TT], AF.Gelu, [pb], [g_])
                tt("dve", a_[:], g_[:], G[:, :, i], ALU.mult, [g_, G], [a_])
            mms(lambda e, a_=a_, vt=vt, ii=ii, i=i: [e.matmul(ACC[s * 2 + hf][:], lhsT=a_[:, s * 128:(s + 1) * 128], rhs=vt[:, ii, hf * 512:(hf + 1) * 512], start=(i == 0), stop=(i == 127))
                                                   for s in range(2) for hf in range(2)][-1], [a_, vt], ACC)

        NE = 0 if "E" in SKIP else 128
        per = (len(nxt) + 99) // 100 if nxt else 0
        for i in range(min(LOOK, NE)):
            emit_H(i)
        for i in range(NE):
            if i + LOOK < NE:
                emit_H(i + LOOK)
            emit_rest(i)
            if nxt:
                p.drain(nxt, per)
        if nxt:
            p.drain(nxt, len(nxt))

    def epilogue(tix):
        b = tix % 2
        t0 = tix * TT
        for s in range(2):
            hh1 = h1[b][s]
            for hf in range(2):
                tt("dve", hh1[:, hf * 512:(hf + 1) * 512], ACC[s * 2 + hf][:], hh1[:, hf * 512:(hf + 1) * 512], ALU.add, [ACC[s * 2 + hf], hh1], [hh1])
            act(xs[:], hh1[:], AF.Square, [hh1], [xs, ss], accum_out=ss[:])
            act(rstd[:], ss[:], AF.Sqrt, [ss], [rstd], bias=1e-5, scale=1.0 / 1024)
            p.op("dve", lambda e: e.reciprocal(out=rstd[:], in_=rstd[:]), [rstd], [rstd])
            stt("dve", hh1[:], hh1[:], rstd[:, 0:1], fin[:], ALU.mult, ALU.mult, [hh1, rstd, fin], [hh1])
            dma("act", D["out"][t0 + s * 128:t0 + (s + 1) * 128, :], hh1[:], [hh1], [])

    prep(0)
    for tix in range(ntile):
        gbuild(tix)
        nxt = []
        if tix + 1 < ntile:
            p.defer_begin()
            prep(tix + 1)
            nxt = p.defer_end()
        expert(tix, nxt)
        epilogue(tix)


N_CORES = 8


def _build(n_seq, n_tiles, ret_d=False, phases="0123"):
    nc = bass.Bass("TRN2", target_bir_lowering=False, dynamic_dma_scratch_size=2048)
    ntok = n_seq * n_tiles * 128
    D = {}

    def din(name, shape):
        D[name] = nc.dram_tensor(name, list(shape), F32, kind="ExternalInput").ap()

    din("x", (ntok, 1024)); din("w_in", (1024, 4640)); din("vecs", (NVEC,)); din("nwp", (128, 16)); din("cst", (128, NCST))
    din("decay_w2", (64, 512)); din("iclr_a2", (64, 512)); din("gate_g2", (160, 512))
    din("proj_attn", (512, 1024)); din("proj_rwkv", (512, 1024)); din("w_out", (1024, 1024))
    din("peer_wq", (1024, 1024)); din("skT", (128, 8, 128)); din("uT", (1024, 16384)); din("v", (16384, 1024))
    D["y_scr"] = nc.dram_tensor("y_scr", [ntok, 1024], F32, kind="Internal").ap()
    D["h1_scr"] = nc.dram_tensor("h1_scr", [ntok, 1024], F32, kind="Internal").ap()
    D["u2"] = nc.dram_tensor("u2", [128, 128, 8, 128], BF16, kind="Internal").ap()
    D["vb"] = nc.dram_tensor("vb", [16384, 1024], BF16, kind="Internal").ap()
    D["out"] = nc.dram_tensor("out", [ntok, 1024], F32, kind="ExternalOutput").ap()
    p = Prog(nc)
    for nm in ("y_t", "h1_t", "u2_t", "vb_t"):
        D[nm] = p.wrap(None, nm)
    m = p.mark()
    if "1" in phases:
        phase_a1(p, nc, D, n_seq, n_tiles, n_tiles * 128)
        p.release(m)
    if "2" in phases:
        drip = None
        if "0" in phases:
            p.defer_begin()
            phase_b0(p, nc, D, eng_rot=("pool", "pool"), nbuf=1, ldq="pool", stq="pool")
            drip = p.defer_end()
        phase_a2(p, nc, D, ntok, final=("3" not in phases), drip=drip)
        p.release(m)
    elif "0" in phases:
        phase_b0(p, nc, D)
        p.release(m)
    if "3" in phases:
        phase_b(p, nc, D, ntok)
    p.emit()
    p.close()
    return (nc, D) if ret_d else nc


def _inputs(x, norm_mix_w, w_in, shift_mu, attn_sinks, decay_w0, decay_w2, iclr_a0, iclr_a2, gate_g2, k_k, k_a, r_k,
            ln_x_w, ln_x_b, proj_attn, proj_rwkv, w_out, norm_ffn_w, peer_wq, peer_subkeys, peer_u, peer_v, norm_final_w):
    f = lambda a: np.ascontiguousarray(np.asarray(a, dtype=np.float32))
    v = np.zeros(NVEC, np.float32)
    v[V_MU:V_MU + 1824] = f(shift_mu)[0]; v[V_W0:V_W0 + 512] = f(decay_w0)[0]; v[V_A0:V_A0 + 512] = f(iclr_a0)[0]
    v[V_KK:V_KK + 512] = f(k_k)[0]; v[V_KA:V_KA + 512] = f(k_a)[0]; v[V_LNW:V_LNW + 512] = f(ln_x_w)[0]
    v[V_LNB:V_LNB + 512] = f(ln_x_b)[0]; v[V_RK:V_RK + 512] = f(r_k)[0].reshape(-1); v[V_SINK:V_SINK + 8] = f(attn_sinks)[0]
    v[V_FIN:] = f(norm_final_w)
    nwp = np.ones((128, 16), np.float32)
    nwp[:, 0:8] = f(norm_mix_w)[0].reshape(8, 128).T
    nwp[:, 8:16] = f(norm_ffn_w)[0].reshape(8, 128).T
    sk = f(peer_subkeys)[0]
    skT = np.ascontiguousarray(sk.transpose(1, 3, 0, 2).reshape(128, 8, 128))
    return dict(w_in=f(w_in)[0], vecs=v, nwp=nwp, cst=make_cst(), decay_w2=f(decay_w2)[0], iclr_a2=f(iclr_a2)[0],
                gate_g2=f(gate_g2)[0], proj_attn=f(proj_attn)[0], proj_rwkv=f(proj_rwkv)[0], w_out=f(w_out)[0],
                peer_wq=f(peer_wq)[0], skT=skT, uT=np.ascontiguousarray(f(peer_u)[0].T), v=f(peer_v)[0])


def kernel(**inputs):
    x = np.ascontiguousarray(np.asarray(inputs["x"], dtype=np.float32))
    B, S, Dm = x.shape
    common = _inputs(**inputs)
    spc = B // N_CORES
    nc = _build(spc, S // 128)
    in_maps = []
    for c in range(N_CORES):
        d = dict(common)
        d["x"] = np.ascontiguousarray(x[c * spc:(c + 1) * spc].reshape(spc * S, Dm))
        in_maps.append(d)
    res = run_bass_kernel_spmd(nc, in_maps, core_ids=list(range(N_CORES)))
    out = np.concatenate([r["out"].reshape(spc, S, Dm) for r in res.results], axis=0)
    return out.astype(np.float32)
```

```python
import numpy as np
import concourse.bass as bass
import concourse.mybir as mybir

F32 = mybir.dt.float32
BF16 = mybir.dt.bfloat16
U32 = mybir.dt.uint32
I32 = mybir.dt.int32
ALU = mybir.AluOpType
AF = mybir.ActivationFunctionType
AX = mybir.AxisListType

NDMA_SEMS = 12
import os as _os
NOSELF = tuple(_os.environ.get("NOSELF", "").split(","))


class T:
    def __init__(self, h, name):
        self.h = h
        self.name = name
        self.state = {}

    def __getitem__(self, k):
        return self.h[k]


class Prog:
    def __init__(self, nc):
        self.nc = nc
        self.ops = {e: [] for e in ("pe", "dve", "act", "pool", "sp")}
        self.cms = []
        self.ndma = {e: 0 for e in ("sp", "act", "pool")}
        self._defer = None

    def sb(self, name, shape, dtype):
        self._uid = getattr(self, "_uid", 0) + 1
        cm = self.nc.sbuf_tensor("sb%d_" % self._uid + name, list(shape), dtype)
        h = cm.__enter__()
        self.cms.append(cm)
        return T(h, name)

    def ps(self, name, shape, dtype):
        self._uid = getattr(self, "_uid", 0) + 1
        cm = self.nc.psum_tensor("ps%d_" % self._uid + name, list(shape), dtype)
        h = cm.__enter__()
        self.cms.append(cm)
        return T(h, name)

    def wrap(self, ap, name):
        return T(ap, name)

    def defer_begin(self):
        self._defer = []

    def defer_end(self):
        lst, self._defer = self._defer, None
        return lst

    def drain(self, lst, k):
        for _ in range(min(k, len(lst))):
            eng, fn, reads, writes, dma, extra = lst.pop(0)
            self.op(eng, fn, reads, writes, dma, extra)

    def mark(self):
        return len(self.cms)

    def release(self, mark):
        lasts = []
        for e in ("pe", "dve", "act", "pool", "sp"):
            for j in range(len(self.ops[e]) - 1, -1, -1):
                if self.ops[e][j][2][0] not in ("dma", "bar"):
                    lasts.append((e, j))
                    break
        dmat = []
        for q in ("sp", "act", "pool"):
            n = self.ndma[q]
            dmat += [("dma", q, i) for i in range(max(0, n - NDMA_SEMS), n)]
        for e in ("pe", "dve", "act", "pool", "sp"):
            self.ops[e].append((None, lasts + dmat, ("bar", e, len(self.ops[e]))))
        while len(self.cms) > mark:
            self.cms.pop().__exit__(None, None, None)

    def _collect(self, t, key, is_write, deps):
        if key is None:
            keys = list(t.state.keys())
        else:
            keys = [k for k in (key, None) if k in t.state]
        for k in keys:
            w, rs = t.state[k]
            if w is not None:
                deps.append(w)
            if is_write:
                deps.extend(rs)

    def _update(self, t, key, is_write, me):
        if is_write:
            if key is None:
                t.state = {None: [me, []]}
            else:
                t.state[key] = [me, []]
        else:
            st = t.state.setdefault(key, [None, []])
            if me[0] != "dma":
                st[1] = [r for r in st[1] if not (r[0] == me[0])]
            st[1].append(me)

    def op(self, eng, fn, reads=(), writes=(), dma=False, extra=()):
        if self._defer is not None:
            self._defer.append((eng, fn, reads, writes, dma, extra))
            return None
        deps = list(extra)
        norm = lambda x: x if isinstance(x, tuple) else (x, None)
        reads = [norm(r) for r in reads]
        writes = [norm(w) for w in writes]
        for t, k in reads:
            self._collect(t, k, False, deps)
        for t, k in writes:
            self._collect(t, k, True, deps)
        idx = len(self.ops[eng])
        if dma:
            n = self.ndma[eng]
            self.ndma[eng] += 1
            me = ("dma", eng, n)
        else:
            me = (eng, idx)
        for t, k in reads:
            self._update(t, k, False, me)
        for t, k in writes:
            self._update(t, k, True, me)
        self.ops[eng].append((fn, deps, me))
        return me

    def emit(self):
        nc = self.nc
        engs = ("pe", "dve", "act", "pool", "sp")
        sem_cms = {}
        sems = {}
        for e in engs:
            cm = nc.semaphore("s_" + e)
            sems[e] = cm.__enter__()
            self.cms.append(cm)
        dsems = {}
        for e in ("sp", "act", "pool"):
            if self.ndma[e]:
                lst = []
                for i in range(NDMA_SEMS):
                    cm = nc.semaphore("d_%s_%d" % (e, i))
                    lst.append(cm.__enter__())
                    self.cms.append(cm)
                dsems[e] = lst
        ops = self.ops

        def run(ename, engine):
            seen = {}
            cnt = 0
            for fn, deps, me in ops[ename]:
                need = {}
                for d in deps:
                    if d[0] == "dma":
                        s = dsems[d[1]][d[2] % NDMA_SEMS]
                        v = 16 * (d[2] // NDMA_SEMS + 1)
                    else:
                        if d[0] == ename:
                            if ename == "pe" or fn is None or ename in NOSELF:
                                continue
                        s = sems[d[0]]
                        v = d[1] + 1 - self.dma_before[d[0]][d[1]]
                    key = s.num if hasattr(s, "num") else id(s)
                    if v > need.get(key, (None, 0))[1]:
                        need[key] = (s, v)
                if me[0] == "dma":
                    n = me[2]
                    s = dsems[ename][n % NDMA_SEMS]
                    if n >= NDMA_SEMS:
                        key = s.num if hasattr(s, "num") else id(s)
                        v = 16 * (n // NDMA_SEMS)
                        if v > need.get(key, (None, 0))[1]:
                            need[key] = (s, v)
                for key, (s, v) in need.items():
                    if seen.get(key, 0) >= v:
                        continue
                    engine.wait_ge(s, v)
                    seen[key] = v
                if fn is None:
                    continue
                ins = fn(engine)
                if me[0] == "dma":
                    ins.then_inc(dsems[ename][me[2] % NDMA_SEMS], 16)
                else:
                    ins.then_inc(sems[ename], 1)

        self.dma_before = {}
        for e in engs:
            c = 0
            lst = []
            for fn, deps, me in ops[e]:
                lst.append(c)
                if me[0] in ("dma", "bar"):
                    c += 1
            self.dma_before[e] = lst

        with nc.Block() as block:
            @block.tensor
            def _(eng):
                run("pe", eng)

            @block.vector
            def _(eng):
                run("dve", eng)

            @block.scalar
            def _(eng):
                run("act", eng)
                self._drain("act", eng, dsems)

            @block.gpsimd
            def _(eng):
                run("pool", eng)
                self._drain("pool", eng, dsems)

            @block.sync
            def _(eng):
                run("sp", eng)
                self._drain("sp", eng, dsems)

    def _drain(self, e, eng, dsems):
        n = self.ndma[e]
        if not n:
            return
        for j in range(min(n, NDMA_SEMS)):
            last = ((n - 1 - j) // NDMA_SEMS) * NDMA_SEMS + j
            eng.wait_ge(dsems[e][j], 16 * (last // NDMA_SEMS + 1))

    def close(self):
        for cm in reversed(self.cms):
            cm.__exit__(None, None, None)
from concourse.bass_utils import run_bass_kernel_spmd

D_MODEL = 1024
C_DEC = 0.6065306597126334
STAGE = 0
SKIP = ""
NSB = 1
V_MU, V_W0, V_A0, V_KK, V_KA, V_LNW, V_LNB, V_RK, V_SINK, V_FIN = 0, 1824, 2336, 2848, 3360, 3872, 4384, 4896, 5408, 5416
NVEC = 5416 + 1024
C_ID, C_SU, C_IU, C_SL, C_BD, C_SH, C_CA, C_COS, C_SIN, C_ONE, C_IOTA = 0, 128, 256, 384, 512, 640, 768, 896, 1408, 1920, 1921
NCST = 1921 + 128


def make_cst():
    c = np.zeros((128, NCST), np.float32)
    i = np.arange(128)
    r, q = i[:, None], i[None, :]
    c[:, C_ID:C_ID + 128] = (r == q)
    c[:, C_SU:C_SU + 128] = (r < q)
    c[:, C_IU:C_IU + 128] = (r <= q)
    c[:, C_SL:C_SL + 128] = (r > q)
    c[:, C_BD:C_BD + 128] = ((r // 64) == (q // 64))
    c[:, C_SH:C_SH + 128] = (r == q - 1)
    c[127, C_CA] = 1.0
    inv = 10000.0 ** (-np.arange(0, 64, 2, dtype=np.float32) / 64)
    pos = (np.arange(16)[None, :] * 128 + i[:, None]).astype(np.float32)
    ang = pos[:, :, None] * inv[None, None, :]
    c[:, C_COS:C_COS + 512] = np.cos(ang).reshape(128, 512)
    c[:, C_SIN:C_SIN + 512] = np.sin(ang).reshape(128, 512)
    c[:, C_ONE] = 1.0
    c[:, C_IOTA:C_IOTA + 128] = q
    return c


class Ctx:
    pass


def helpers(p):
    h = Ctx()

    def tt(eng, out, in0, in1, op, r, w):
        p.op(eng, lambda e: e.tensor_tensor(out=out, in0=in0, in1=in1, op=op), r, w)

    def stt(eng, out, in0, scalar, in1, op0, op1, r, w):
        p.op(eng, lambda e: e.scalar_tensor_tensor(out=out, in0=in0, scalar=scalar, in1=in1, op0=op0, op1=op1), r, w)

    def ts(eng, out, in0, s1, s2, op0, op1, r, w):
        if s2 is None:
            p.op(eng, lambda e: e.tensor_scalar(out=out, in0=in0, scalar1=s1, scalar2=None, op0=op0), r, w)
        else:
            p.op(eng, lambda e: e.tensor_scalar(out=out, in0=in0, scalar1=s1, scalar2=s2, op0=op0, op1=op1), r, w)

    def act(out, in_, func, r, w, **kw):
        p.op("act", lambda e: e.activation(out=out, in_=in_, func=func, **kw), r, w)

    def cp(eng, out, in_, r, w):
        if eng == "act":
            p.op("act", lambda e: e.activation(out=out, in_=in_, func=AF.Copy), r, w)
        else:
            p.op(eng, lambda e: e.tensor_copy(out=out, in_=in_), r, w)

    def red(out, in_, r, w, op=ALU.add):
        p.op("dve", lambda e: e.tensor_reduce(out=out, in_=in_, axis=AX.X, op=op), r, w)

    def dma(eng, out, in_, r, w):
        p.op(eng, lambda e: e.dma_start(out=out, in_=in_), r, w, dma=True)

    def mms(fn, r, w):
        p.op("pe", fn, r, w)

    h.tt, h.stt, h.ts, h.act, h.cp, h.red, h.dma, h.mms = tt, stt, ts, act, cp, red, dma, mms
    return h


class Rot:
    def __init__(self, tiles):
        self.tiles = tiles
        self.i = 0

    def __call__(self):
        t = self.tiles[self.i % len(self.tiles)]
        self.i += 1
        return t


def load_w_bf16(p, h, name, dram, K, N, ncol0, ncols, stg, scale_t=None, scale_off=0, dt=BF16):
    kc = K // 128
    wt = p.sb(name, [128, kc, ncols], dt)
    for c in range(kc):
        s = stg()
        h.dma("sp", s[:, 0:ncols], dram[c * 128:(c + 1) * 128, ncol0:ncol0 + ncols], [], [s])
        if scale_t is not None:
            h.act(wt[:, c, :], s[:, 0:ncols], AF.Copy, [s, scale_t], [(wt, c)], scale=scale_t[:, scale_off + c:scale_off + c + 1])
        else:
            h.cp("pool", wt[:, c, :], s[:, 0:ncols], [s], [(wt, c)])
    return wt


def phase_a1(p, nc, D, n_seq, n_tiles, S_TOK):
    h = helpers(p)
    tt, stt, ts, act, cp, red, dma, mms = h.tt, h.stt, h.ts, h.act, h.cp, h.red, h.dma, h.mms
    NZ = 2592
    cst = p.sb("cst", [128, NCST], F32)
    dma("sp", cst[:], D["cst"], [], [cst])
    vec = p.sb("vec", [128, V_SINK + 8], F32)
    dma("sp", vec[:], D["vecs"][0:V_SINK + 8].partition_broadcast(128), [], [vec])
    nwp = p.sb("nwp", [128, 16], F32)
    dma("sp", nwp[:], D["nwp"], [], [nwp])
    ident = cst[:, C_ID:C_ID + 128]
    z = p.sb("z", [128, NZ], F32)
    Wb = load_w_bf16(p, h, "Wb1", D["w_in"], 1024, 4640, 0, NZ, (lambda: z), nwp, 0)
    w2 = p.sb("w2", [128, 512], F32)
    dma("sp", w2[0:64, :], D["decay_w2"], [], [w2])
    dma("sp", w2[64:128, :], D["iclr_a2"], [], [w2])
    g2 = p.sb("g2", [128, 2, 512], F32)
    dma("sp", g2[:, 0, :], D["gate_g2"][0:128, :], [], [g2])
    dma("sp", g2[0:32, 1, :], D["gate_g2"][128:160, :], [], [g2])
    negsink = p.sb("negsink", [128, 8], F32)
    ts("dve", negsink[:], vec[:, V_SINK:V_SINK + 8], -1.0, None, ALU.mult, None, [vec], [negsink])

    xt = Rot([p.sb("xt%d" % i, [128, 1024], F32) for i in range(2)])
    ss = p.sb("ss", [128, 1], F32)
    rstd = p.sb("rstd", [128, 1], F32)
    xs = p.sb("xs", [128, 1024], F32)
    xsT = p.sb("xsT", [128, 8, 128], BF16)
    carry = p.sb("carry", [128, 1824], F32)
    p.op("pool", lambda e: e.memset(carry[:], 0.0), [], [carry])
    zr = p.sb("zr", [128, 1824], F32)
    lin = p.sb("lin", [128, 288], F32)
    linT = p.sb("linT", [128, 3, 128], F32)
    T5 = Rot([p.sb("t5_%d" % i, [128, 512], F32) for i in range(12)])
    gv = p.sb("gv", [128, 512], F32)
    k2 = p.sb("k2", [128, 512], F32)
    M8 = Rot([p.sb("m8_%d" % i, [128, 8, 128], F32) for i in range(8)])
    M8d = [p.sb("m8d_%d" % i, [128, 8, 128], F32) for i in range(3)]
    Tq = Rot([p.sb("tq_%d" % i, [128, 4, 128], F32) for i in range(4)])
    sm = Rot([p.sb("sm_%d" % i, [128, 8], F32) for i in range(12)])
    PB = Rot([p.ps("pb%d" % i, [128, 512], F32) for i in range(6)])
    PV = [p.ps("pv%d" % i, [128, 512], F32) for i in range(2)]
    NDUM = 0
    if NDUM:
        dumb = p.ps("dumb", [128, 512], F32)
        mms0 = mms

        def mms(fn, r, w):
            def fn2(e):
                ins = fn(e)
                for _ in range(NDUM):
                    e.matmul(dumb[:], lhsT=Wb[:, 0, 0:128], rhs=Wb[:, 0, 0:512], start=True, stop=True)
                return ins
            mms0(fn2, r, w)
    ST = p.sb("ST", [128, 4, 128], F32)
    gC = p.sb("gC", [128, 4], F32)
    qk = p.sb("qk", [128, 10, 64], F32)
    kdup = p.sb("kdup", [128, 2, 2, 64], F32)
    qT = p.sb("qT", [128, 4, 128], BF16)
    kT = [p.sb("kT%d" % i, [128, 2, 128], BF16) for i in range(2)]
    va = [p.sb("va%d" % i, [128, 2, 65], BF16) for i in range(2)]
    for i in range(2):
        p.op("pool", lambda e, i=i: e.memset(va[i][:], 1.0), [], [va[i]])
    pT = Rot([p.sb("pT%d" % i, [128, 2, 128], BF16) for i in range(4)])
    yout = Rot([p.sb("yout%d" % i, [128, 1024], F32) for i in range(1)])

    def vb(off, n=512):
        return vec[:, off:off + n]

    order = [(b, n) for b in range(n_seq) for n in range(n_tiles)]
    xq = {}

    def prefetch(i):
        if i < len(order):
            b, n = order[i]
            t0 = b * S_TOK + n * 128
            xq[i] = xt()
            dma("sp", xq[i][:], D["x"][t0:t0 + 128, :], [], [xq[i]])

    prefetch(0)

    def tile_body(i):
            b, n = order[i]
            if n == 0:
                p.op("pool", lambda e: e.memset(ST[:], 0.0), [], [ST])
            tok0 = b * S_TOK + n * 128
            first = (n == 0)
            x_t = xq.pop(i)
            prefetch(i + 1)
            act(xs[:], x_t[:], AF.Square, [x_t], [xs, ss], accum_out=ss[:])
            act(rstd[:], ss[:], AF.Sqrt, [ss], [rstd], bias=1e-5, scale=1.0 / 1024)
            p.op("dve", lambda e: e.reciprocal(out=rstd[:], in_=rstd[:]), [rstd], [rstd])
            ts("dve", xs[:], x_t[:], rstd[:, 0:1], None, ALU.mult, None, [x_t, rstd], [xs])
            for hh in range(2):
                pb = PB()
                mms(lambda e, pb=pb, hh=hh: [e.transpose(out=pb[:, j * 128:(j + 1) * 128], in_=xs[:, (hh * 4 + j) * 128:(hh * 4 + j + 1) * 128], identity=ident) for j in range(4)][-1],
                    [xs, cst], [pb])
                cp("act" if hh else "dve", xsT[:, hh * 4:hh * 4 + 4, :], pb[:].rearrange("p (a b) -> p a b", a=4), [pb], [(xsT, hh)])
            for cc in range(6):
                c0 = cc * 512
                cw = min(512, NZ - c0)
                pb = PB()
                mms(lambda e, pb=pb, c0=c0, cw=cw: [e.matmul(pb[:, 0:cw], lhsT=xsT[:, c, :], rhs=Wb[:, c, c0:c0 + cw], start=(c == 0), stop=(c == 7)) for c in range(8)][-1],
                    [xsT, Wb], [pb])
                cp("act" if cc % 2 else "dve", z[:, c0:c0 + cw], pb[:, 0:cw], [pb], [(z, cc)])
            if STAGE == 1:
                yo = yout()
                cp("dve", yo[:], z[:, 0:1024], [z], [yo])
                dma("sp", D["y_scr"][tok0:tok0 + 128, :], yo[:], [yo], [])
                return
            cosb = cst[:, C_COS + n * 32:C_COS + n * 32 + 32].unsqueeze(1).to_broadcast([128, 10, 32])
            sinb = cst[:, C_SIN + n * 32:C_SIN + n * 32 + 32].unsqueeze(1).to_broadcast([128, 10, 32])
            zq = z[:, 0:640].rearrange("p (h d) -> p h d", h=10)
            x1, x2 = zq[:, :, 0:32], zq[:, :, 32:64]
            ta, tb_ = T5(), T5()
            tav = ta[:, 0:320].rearrange("p (h d) -> p h d", h=10)
            tbv = tb_[:, 0:320].rearrange("p (h d) -> p h d", h=10)
            zk = [(z, 0), (z, 1)]
            tt("dve", tav, x1, cosb, ALU.mult, zk + [cst], [ta])
            tt("pool", tbv, x2, sinb, ALU.mult, zk + [cst], [tb_])
            tt("dve", qk[:, :, 0:32], tav, tbv, ALU.subtract, [ta, tb_], [(qk, 0)])
            tc_, td = T5(), T5()
            tcv = tc_[:, 0:320].rearrange("p (h d) -> p h d", h=10)
            tdv = td[:, 0:320].rearrange("p (h d) -> p h d", h=10)
            tt("pool", tcv, x2, cosb, ALU.mult, zk + [cst], [tc_])
            tt("dve", tdv, x1, sinb, ALU.mult, zk + [cst], [td])
            tt("pool", qk[:, :, 32:64], tcv, tdv, ALU.add, [tc_, td], [(qk, 1)])
            cp("pool", kdup[:], qk[:, 8:10, :].unsqueeze(2).to_broadcast([128, 2, 2, 64]), [qk], [kdup])
            kTc, kTp = kT[n % 2], kT[(n + 1) % 2]
            vac, vap = va[n % 2], va[(n + 1) % 2]
            cp("pool", vac[:, :, 0:64], z[:, 640:768].rearrange("p (g d) -> p g d", g=2), [(z, 1)], [vac])
            pb = PB()
            mms(lambda e, pb=pb: [e.transpose(out=pb[:, j * 128:(j + 1) * 128], in_=qk[:, 2 * j:2 * j + 2, :].rearrange("p a d -> p (a d)"), identity=ident) for j in range(4)][-1],
                [qk, cst], [pb])
            cp("act", qT[:], pb[:].rearrange("p (a b) -> p a b", a=4), [pb], [qT])
            pb = PB()
            mms(lambda e, pb=pb: [e.transpose(out=pb[:, g * 128:(g + 1) * 128], in_=kdup[:, g, :, :].rearrange("p a d -> p (a d)"), identity=ident) for g in range(2)][-1],
                [kdup, cst], [pb])
            cp("dve", kTc[:], pb[:, 0:256].rearrange("p (a b) -> p a b", a=2), [pb], [kTc])
            yo = yout()
            pv = PV
            for hd in range(0 if "a" in SKIP else 8):
                m, base, g = hd // 2, 64 * (hd % 2), hd // 4
                pb = PB()
                if first:
                    mms(lambda e, pb=pb, m=m, base=base, g=g: e.matmul(pb[:, 128:256], lhsT=kTc[base:base + 64, g, :], rhs=qT[base:base + 64, m, :], start=True, stop=True),
                        [kTc, qT], [pb])
                else:
                    mms(lambda e, pb=pb, m=m, base=base, g=g: [e.matmul(pb[:, 0:128], lhsT=kTp[base:base + 64, g, :], rhs=qT[base:base + 64, m, :], start=True, stop=True),
                                                              e.matmul(pb[:, 128:256], lhsT=kTc[base:base + 64, g, :], rhs=qT[base:base + 64, m, :], start=True, stop=True)][-1],
                        [kTc, kTp, qT], [pb])
                pt = pT()
                lo = 1 if first else 0
                act(pt[:, lo:2, :], pb[:, lo * 128:256].rearrange("p (a b) -> p a b", a=2 - lo), AF.Exp, [pb, negsink], [pt],
                    scale=0.125, bias=negsink[:, hd:hd + 1])
                if not first:
                    tt("pool", pt[:, 0, :], pt[:, 0, :], cst[:, C_SL:C_SL + 128], ALU.mult, [pt, cst], [pt])
                tt("dve", pt[:, 1, :], pt[:, 1, :], cst[:, C_IU:C_IU + 128], ALU.mult, [pt, cst], [pt])
                pvb = pv[hd // 4]
                o0 = (hd % 4) * 65
                if first:
                    mms(lambda e, pvb=pvb, pt=pt, g=g, o0=o0: e.matmul(pvb[:, o0:o0 + 65], lhsT=pt[:, 1, :], rhs=vac[:, g, :], start=True, stop=True),
                        [pt, vac], [pvb])
                else:
                    mms(lambda e, pvb=pvb, pt=pt, g=g, o0=o0: [e.matmul(pvb[:, o0:o0 + 65], lhsT=pt[:, 0, :], rhs=vap[:, g, :], start=True, stop=False),
                                                              e.matmul(pvb[:, o0:o0 + 65], lhsT=pt[:, 1, :], rhs=vac[:, g, :], start=False, stop=True)][-1],
                        [pt, vac, vap], [pvb])
            for hf in range(2):
                pvb = pv[hf]
                pvv = pvb[:, 0:260].rearrange("p (h d) -> p h d", h=4)
                den = sm()
                ts("dve", den[:, 0:4], pvv[:, :, 64], 1.0, None, ALU.add, None, [pvb], [den])
                p.op("dve", lambda e, den=den: e.reciprocal(out=den[:, 0:4], in_=den[:, 0:4]), [den], [den])
                tt("dve", yo[:, hf * 256:(hf + 1) * 256].rearrange("p (h d) -> p h d", h=4), pvv[:, :, 0:64],
                   den[:, 0:4].unsqueeze(2).to_broadcast([128, 4, 64]), ALU.mult, [pvb, den], [(yo, hf)])
            if STAGE == 2:
                cp("dve", yo[:, 512:1024], z[:, 0:512], [z], [yo])
                dma("sp", D["y_scr"][tok0:tok0 + 128, :], yo[:], [yo], [])
                return
            if "r" in SKIP:
                dma("sp", D["y_scr"][tok0:tok0 + 128, :], yo[:], [yo], [(D["y_t"], tok0 // 128)])
                return
            for j in range(4):
                c0 = 768 + j * 512
                cw = min(512, NZ - c0)
                pb = PB()
                zkeys = [(z, 1), (z, 2), (z, 3), (z, 4), (z, 5)]
                if first:
                    mms(lambda e, pb=pb, c0=c0, cw=cw: e.matmul(pb[:, 0:cw], lhsT=cst[:, C_SH:C_SH + 128], rhs=z[:, c0:c0 + cw], start=True, stop=True),
                        zkeys + [cst], [pb])
                else:
                    mms(lambda e, pb=pb, c0=c0, cw=cw: [e.matmul(pb[:, 0:cw], lhsT=cst[:, C_SH:C_SH + 128], rhs=z[:, c0:c0 + cw], start=True, stop=False),
                                                       e.matmul(pb[:, 0:cw], lhsT=cst[:, C_CA:C_CA + 128], rhs=carry[:, c0 - 768:c0 - 768 + cw], start=False, stop=True)][-1],
                        zkeys + [cst, carry], [pb])
                r0 = c0 - 768
                tt("dve", zr[:, r0:r0 + cw], pb[:, 0:cw], z[:, c0:c0 + cw], ALU.subtract, [pb] + zkeys, [(zr, j)])
                tt("dve", zr[:, r0:r0 + cw], zr[:, r0:r0 + cw], vec[:, V_MU + r0:V_MU + r0 + cw], ALU.mult, [(zr, j), vec], [(zr, j)])
                tt("pool", zr[:, r0:r0 + cw], zr[:, r0:r0 + cw], z[:, c0:c0 + cw], ALU.add, [(zr, j)] + zkeys, [(zr, j)])
            cp("pool", carry[96:128, :], z[96:128, 768:NZ], [z], [carry])
            r_, k_, v_ = zr[:, 0:512], zr[:, 512:1024], zr[:, 1024:1536]
            if STAGE == 3:
                cp("dve", yo[:, 512:1024], zr[:, 0:512], [], [yo])
                dma("sp", D["y_scr"][tok0:tok0 + 128, :], yo[:], [yo], [])
                return
            act(lin[:, 0:64], zr[:, 1536:1600], AF.Tanh, [zr], [(lin, 0)])
            cp("pool", lin[:, 64:128], zr[:, 1600:1664], [zr], [(lin, 1)])
            act(lin[:, 128:288], zr[:, 1664:1824], AF.Sigmoid, [zr], [(lin, 2)])
            pb = PB()
            mms(lambda e, pb=pb: [e.transpose(out=pb[:, 0:128], in_=lin[:, 0:128], identity=ident),
                                  e.transpose(out=pb[:, 128:256], in_=lin[:, 128:256], identity=ident),
                                  e.transpose(out=pb[0:32, 256:384], in_=lin[:, 256:288], identity=ident)][-1], [lin, cst], [pb])
            cp("dve", linT[:, 0:2, :], pb[:, 0:256].rearrange("p (a b) -> p a b", a=2), [pb], [(linT, 0)])
            cp("dve", linT[0:32, 2, :], pb[0:32, 256:384], [pb], [(linT, 1)])
            pw, pa_, pg = PB(), PB(), PB()
            mms(lambda e, pw=pw: e.matmul(pw[:], lhsT=linT[0:64, 0, :], rhs=w2[0:64, :], start=True, stop=True), [linT, w2], [pw])
            mms(lambda e, pa_=pa_: e.matmul(pa_[:], lhsT=linT[64:128, 0, :], rhs=w2[64:128, :], start=True, stop=True), [linT, w2], [pa_])
            mms(lambda e, pg=pg: [e.matmul(pg[:], lhsT=linT[:, 1, :], rhs=g2[:, 0, :], start=True, stop=False),
                                  e.matmul(pg[:], lhsT=linT[0:32, 2, :], rhs=g2[0:32, 1, :], start=False, stop=True)][-1], [linT, g2], [pg])
            sg, av = T5(), T5()
            tt("dve", sg[:], pw[:], vb(V_W0), ALU.add, [pw, vec], [sg])
            act(sg[:], sg[:], AF.Sigmoid, [sg], [sg])
            tt("dve", av[:], pa_[:], vb(V_A0), ALU.add, [pa_, vec], [av])
            act(av[:], av[:], AF.Sigmoid, [av], [av])
            cp("act", gv[:], pg[:], [pg], [gv])
            if STAGE == 4:
                cp("dve", yo[:, 512:1024], sg[:], [], [yo])
                dma("sp", D["y_scr"][tok0:tok0 + 128, :], yo[:], [yo], [])
                return
            pc = PB()
            mms(lambda e, pc=pc, sg=sg: e.matmul(pc[:], lhsT=cst[:, C_IU:C_IU + 128], rhs=sg[:], start=True, stop=True), [cst, sg], [pc])
            gam, igam, gprev = T5(), T5(), T5()
            act(gam[:], pc[:], AF.Exp, [pc], [gam], scale=-C_DEC)
            act(igam[:], pc[:], AF.Exp, [pc], [igam], scale=C_DEC)
            tt("dve", gprev[:], pc[:], sg[:], ALU.subtract, [pc, sg], [gprev])
            act(gprev[:], gprev[:], AF.Exp, [gprev], [gprev], scale=-C_DEC)
            pgc = PB()
            mms(lambda e, pgc=pgc, sg=sg: [e.matmul(pgc[:, m:m + 1], lhsT=sg[:, m * 128:(m + 1) * 128], rhs=cst[:, C_ONE:C_ONE + 1], start=True, stop=True) for m in range(4)][-1],
                [cst, sg], [pgc])
            act(gC[:], pgc[:, 0:4], AF.Exp, [pgc], [gC], scale=-C_DEC)
            if STAGE == 5:
                cp("dve", yo[:, 512:1024], gprev[:], [], [yo])
                dma("sp", D["y_scr"][tok0:tok0 + 128, :], yo[:], [yo], [])
                return
            kk, sq = T5(), T5()
            tt("dve", kk[:], k_, vb(V_KK), ALU.mult, [zr, vec], [kk])
            tt("pool", sq[:], kk[:], kk[:], ALU.mult, [kk], [sq])
            if STAGE == 51:
                cp("dve", yo[:, 512:1024], sq[:], [], [yo])
                dma("sp", D["y_scr"][tok0:tok0 + 128, :], yo[:], [yo], [])
                return
            s8 = sm()
            red(s8[:], sq[:].rearrange("p (h d) -> p h d", h=8), [sq], [s8])
            if STAGE == 52:
                cp("dve", yo[:, 512:1024], sq[:], [], [yo])
                dma("sp", D["y_scr"][tok0:tok0 + 128, :], yo[:], [yo], [])
                return
            act(s8[:], s8[:], AF.Sqrt, [s8], [s8], bias=1e-24, scale=1.0)
            p.op("dve", lambda e, s8=s8: e.reciprocal(out=s8[:], in_=s8[:]), [s8], [s8])
            if STAGE == 53:
                cp("dve", yo[:, 512:1024], sq[:], [], [yo])
                dma("sp", D["y_scr"][tok0:tok0 + 128, :], yo[:], [yo], [])
                return
            tt("dve", kk[:].rearrange("p (h d) -> p h d", h=8), kk[:].rearrange("p (h d) -> p h d", h=8),
               s8[:].unsqueeze(2).to_broadcast([128, 8, 64]), ALU.mult, [kk, s8], [kk])
            if STAGE == 54:
                cp("dve", yo[:, 512:1024], kk[:], [], [yo])
                dma("sp", D["y_scr"][tok0:tok0 + 128, :], yo[:], [yo], [])
                return
            t1 = T5()
            stt("dve", t1[:], av[:], -1.0, vb(V_KA), ALU.add, ALU.mult, [av, vec], [t1])
            stt("dve", k2[:], t1[:], 1.0, k_, ALU.add, ALU.mult, [t1, zr], [k2])
            if STAGE == 55:
                cp("dve", yo[:, 512:1024], k2[:], [], [yo])
                dma("sp", D["y_scr"][tok0:tok0 + 128, :], yo[:], [yo], [])
                return
            At, Bt, Kt, Rt = T5(), T5(), T5(), T5()
            stt("dve", At[:], kk[:], -1.0, gprev[:], ALU.mult, ALU.mult, [kk, gprev], [At])
            if STAGE == 56:
                cp("dve", yo[:, 512:1024], At[:], [At], [yo])
                dma("sp", D["y_scr"][tok0:tok0 + 128, :], yo[:], [yo], [])
                return
            tt("pool", Bt[:], kk[:], av[:], ALU.mult, [kk, av], [Bt])
            tt("dve", Bt[:], Bt[:], igam[:], ALU.mult, [Bt, igam], [Bt])
            if STAGE == 57:
                cp("dve", yo[:, 512:1024], Bt[:], [Bt], [yo])
                dma("sp", D["y_scr"][tok0:tok0 + 128, :], yo[:], [yo], [])
                return
            tt("dve", Kt[:], k2[:], igam[:], ALU.mult, [k2, igam], [Kt])
            if STAGE == 58:
                cp("dve", yo[:, 512:1024], Kt[:], [Kt], [yo])
                dma("sp", D["y_scr"][tok0:tok0 + 128, :], yo[:], [yo], [])
                return
            tt("pool", Rt[:], r_, gam[:], ALU.mult, [zr, gam], [Rt])
            if STAGE == 6:
                cp("dve", yo[:, 512:1024], Rt[:], [Rt], [yo])
                dma("sp", D["y_scr"][tok0:tok0 + 128, :], yo[:], [yo], [])
                return
            XT = {}
            for nm, src in (("A", At), ("B", Bt), ("K", Kt), ("R", Rt)):
                pb = PB()
                mms(lambda e, pb=pb, src=src: [e.transpose(out=pb[:, j * 128:(j + 1) * 128], in_=src[:, j * 128:(j + 1) * 128], identity=ident) for j in range(4)][-1],
                    [src, cst], [pb])
                dst = Tq()
                cp("act" if nm in ("A", "K") else "dve", dst[:], pb[:].rearrange("p (a b) -> p a b", a=4), [pb], [dst])
                XT[nm] = dst
            AT, BT, KT, RT = XT["A"], XT["B"], XT["K"], XT["R"]

            def pairmat(l, r_op, mask_off, eng2, dst=None):
                dst = dst or M8()
                for par in range(2):
                    pb = PB()
                    mms(lambda e, pb=pb, par=par: [e.matmul(pb[:, j * 128:(j + 1) * 128],
                                                          lhsT=l[64 * par:64 * par + 64, j, :],
                                                          rhs=r_op[64 * par:64 * par + 64, j, :],
                                                          start=True, stop=True) for j in range(4)][-1], [l, r_op], [pb])
                    tt(eng2[par], dst[:, par:8:2, :], pb[:].rearrange("p (a b) -> p a b", a=4),
                       cst[:, mask_off:mask_off + 128].unsqueeze(1).to_broadcast([128, 4, 128]), ALU.mult, [pb, cst], [(dst, par)])
                return dst

            Nm = pairmat(BT, AT, C_SU, ("dve", "dve"))
            Am = pairmat(AT, BT, C_SL, ("dve", "dve"))
            AkT = pairmat(KT, AT, C_SU, ("dve", "dve"), M8d[0])
            RbT = pairmat(BT, RT, C_IU, ("dve", "dve"), M8d[1])
            RkT = pairmat(KT, RT, C_IU, ("dve", "dve"), M8d[2])
            if STAGE == 7:
                cp("dve", yo[:, 512:1024].rearrange("p (a b) -> p a b", a=4), RkT[:, 0:4, :], [RkT], [yo])
                dma("sp", D["y_scr"][tok0:tok0 + 128, :], yo[:], [yo], [])
                return
            X = M8()
            tt("dve", X[:], Nm[:], ident.unsqueeze(1).to_broadcast([128, 8, 128]), ALU.add, [Nm, cst], [X])
            for j in range(0 if "d" in SKIP else 6):
                Nn, An, Xn = M8(), M8(), M8()
                last = (j == 5)
                for hf in range(2):
                    pbn, pba = PB(), PB()
                    if not last:
                        mms(lambda e, pbn=pbn, hf=hf, Am=Am, Nm=Nm: [e.matmul(pbn[:, q * 128:(q + 1) * 128], lhsT=Am[:, hf * 4 + q, :], rhs=Nm[:, hf * 4 + q, :], start=True, stop=True) for q in range(4)][-1],
                            [Am, Nm], [pbn])
                        cp("act", Nn[:, hf * 4:hf * 4 + 4, :], pbn[:].rearrange("p (a b) -> p a b", a=4), [pbn], [(Nn, hf)])
                    mms(lambda e, pba=pba, hf=hf, Am=Am, Nm=Nm: [e.matmul(pba[:, q * 128:(q + 1) * 128], lhsT=Nm[:, hf * 4 + q, :], rhs=Am[:, hf * 4 + q, :], start=True, stop=True) for q in range(4)][-1],
                        [Am, Nm], [pba])
                    cp("dve", An[:, hf * 4:hf * 4 + 4, :], pba[:].rearrange("p (a b) -> p a b", a=4), [pba], [(An, hf)])
                for hf in range(2):
                    pbx = PB()
                    mms(lambda e, pbx=pbx, hf=hf, An=An, X=X: [e.matmul(pbx[:, q * 128:(q + 1) * 128], lhsT=An[:, hf * 4 + q, :], rhs=X[:, hf * 4 + q, :], start=True, stop=True) for q in range(4)][-1],
                        [An, X], [pbx])
                    tt("dve", Xn[:, hf * 4:hf * 4 + 4, :], pbx[:].rearrange("p (a b) -> p a b", a=4), X[:, hf * 4:hf * 4 + 4, :], ALU.add, [pbx, X], [(Xn, hf)])
                Nm, Am, X = Nn, An, Xn
            if STAGE == 8:
                cp("dve", yo[:, 512:1024].rearrange("p (a b) -> p a b", a=4), X[:, 0:4, :], [X], [yo])
                dma("sp", D["y_scr"][tok0:tok0 + 128, :], yo[:], [yo], [])
                return
            pr = PB()

            def f_rhs0(e, pr=pr, AT=AT, AkT=AkT):
                ins = None
                for m in range(4):
                    e.matmul(pr[:, m * 128:(m + 1) * 128], lhsT=AT[:, m, :], rhs=ST[:, m, :], start=True, stop=False)
                    for q in range(2):
                        hd = 2 * m + q
                        ins = e.matmul(pr[:, hd * 64:(hd + 1) * 64], lhsT=AkT[:, hd, :], rhs=zr[:, 1024 + hd * 64:1024 + (hd + 1) * 64], start=False, stop=(q == 1))
                return ins
            mms(f_rhs0, [AT, ST, AkT, zr], [pr])
            rhs0 = T5()
            cp("act", rhs0[:], pr[:], [pr], [rhs0])
            pu = PB()
            mms(lambda e, pu=pu, X=X, rhs0=rhs0: [e.matmul(pu[:, hd * 64:(hd + 1) * 64], lhsT=X[:, hd, :], rhs=rhs0[:, hd * 64:(hd + 1) * 64], start=True, stop=True) for hd in range(8)][-1],
                [X, rhs0], [pu])
            U = T5()
            cp("dve", U[:], pu[:], [pu], [U])
            py = PB()

            def f_y(e, py=py, RT=RT, RbT=RbT, RkT=RkT, U=U):
                ins = None
                for m in range(4):
                    e.matmul(py[:, m * 128:(m + 1) * 128], lhsT=RT[:, m, :], rhs=ST[:, m, :], start=True, stop=False)
                    for q in range(2):
                        hd = 2 * m + q
                        e.matmul(py[:, hd * 64:(hd + 1) * 64], lhsT=RbT[:, hd, :], rhs=U[:, hd * 64:(hd + 1) * 64], start=False, stop=False)
                        ins = e.matmul(py[:, hd * 64:(hd + 1) * 64], lhsT=RkT[:, hd, :], rhs=zr[:, 1024 + hd * 64:1024 + (hd + 1) * 64], start=False, stop=(q == 1))
                return ins
            mms(f_y, [RT, ST, RbT, RkT, U, zr], [py])
            yv = T5()
            cp("act", yv[:], py[:], [py], [yv])
            pst = PB()

            def f_s(e, pst=pst, Bt=Bt, Kt=Kt, U=U):
                ins = None
                for m in range(4):
                    e.matmul(pst[:, m * 128:(m + 1) * 128], lhsT=Bt[:, m * 128:(m + 1) * 128], rhs=U[:, m * 128:(m + 1) * 128], start=True, stop=False)
                    ins = e.matmul(pst[:, m * 128:(m + 1) * 128], lhsT=Kt[:, m * 128:(m + 1) * 128], rhs=zr[:, 1024 + m * 128:1024 + (m + 1) * 128], start=False, stop=True)
                return ins
            mms(f_s, [Bt, Kt, U, zr], [pst])
            tt("dve", ST[:], pst[:].rearrange("p (a b) -> p a b", a=4), ST[:], ALU.add, [pst, ST], [ST])
            tt("dve", ST[:], ST[:], gC[:].unsqueeze(2).to_broadcast([128, 4, 128]), ALU.mult, [ST, gC], [ST])
            tt("dve", ST[:], ST[:], cst[:, C_BD:C_BD + 128].unsqueeze(1).to_broadcast([128, 4, 128]), ALU.mult, [ST, cst], [ST])
            if STAGE == 9:
                cp("dve", yo[:, 512:1024], yv[:], [yv], [yo])
                dma("sp", D["y_scr"][tok0:tok0 + 128, :], yo[:], [yo], [])
                return
            y3 = yv[:].rearrange("p (h d) -> p h d", h=8)
            ysq = T5()
            tt("pool", ysq[:], yv[:], yv[:], ALU.mult, [yv], [ysq])
            s1, s2, mean, var = sm(), sm(), sm(), sm()
            red(s1[:], y3, [yv], [s1])
            red(s2[:], ysq[:].rearrange("p (h d) -> p h d", h=8), [ysq], [s2])
            ts("dve", mean[:], s1[:], 1.0 / 64, None, ALU.mult, None, [s1], [mean])
            tt("dve", var[:], mean[:], mean[:], ALU.mult, [mean], [var])
            stt("dve", var[:], s2[:], 1.0 / 64, var[:], ALU.mult, ALU.subtract, [s2, var], [var])
            act(var[:], var[:], AF.Sqrt, [var], [var], bias=64e-5, scale=1.0)
            p.op("dve", lambda e, var=var: e.reciprocal(out=var[:], in_=var[:]), [var], [var])
            yn = T5()
            yn3 = yn[:].rearrange("p (h d) -> p h d", h=8)
            tt("dve", yn3, y3, mean[:].unsqueeze(2).to_broadcast([128, 8, 64]), ALU.subtract, [yv, mean], [yn])
            tt("dve", yn3, yn3, var[:].unsqueeze(2).to_broadcast([128, 8, 64]), ALU.mult, [yn, var], [yn])
            tt("pool", yn[:], yn[:], vb(V_LNW), ALU.mult, [yn, vec], [yn])
            tt("pool", yn[:], yn[:], vb(V_LNB), ALU.add, [yn, vec], [yn])
            rk = T5()
            tt("pool", rk[:], r_, k2[:], ALU.mult, [zr, k2], [rk])
            tt("pool", rk[:], rk[:], vb(V_RK), ALU.mult, [rk, vec], [rk])
            sb_ = sm()
            red(sb_[:], rk[:].rearrange("p (h d) -> p h d", h=8), [rk], [sb_])
            tt("dve", rk[:].rearrange("p (h d) -> p h d", h=8), v_.rearrange("p (h d) -> p h d", h=8),
               sb_[:].unsqueeze(2).to_broadcast([128, 8, 64]), ALU.mult, [zr, sb_], [rk])
            tt("pool", yn[:], yn[:], rk[:], ALU.add, [yn, rk], [yn])
            tt("pool", yo[:, 512:1024], yn[:], gv[:], ALU.mult, [yn, gv], [(yo, 2)])
            dma("sp", D["y_scr"][tok0:tok0 + 128, :], yo[:], [yo], [(D["y_t"], tok0 // 128)])

    for i in range(len(order)):
        tile_body(i)


def phase_a1a(p, nc, D, n_seq, n_tiles, S_TOK):
    h = helpers(p)
    tt, stt, ts, act, cp, red, dma, mms = h.tt, h.stt, h.ts, h.act, h.cp, h.red, h.dma, h.mms
    NZ = 2592
    cst = p.sb("cst", [128, NCST], F32)
    dma("sp", cst[:], D["cst"], [], [cst])
    vec = p.sb("vecmu", [128, 1824], F32)
    dma("sp", vec[:], D["vecs"][V_MU:V_MU + 1824].partition_broadcast(128), [], [vec])
    snk = p.sb("snk", [128, 8], F32)
    dma("sp", snk[:], D["vecs"][V_SINK:V_SINK + 8].partition_broadcast(128), [], [snk])
    nwp = p.sb("nwp", [128, 16], F32)
    dma("sp", nwp[:], D["nwp"], [], [nwp])
    ident = cst[:, C_ID:C_ID + 128]
    negsink = p.sb("negsink", [128, 8], F32)
    ts("dve", negsink[:], snk[:], -1.0, None, ALU.mult, None, [snk], [negsink])

    def make_stream(k):
        sf = "_s%d" % k
        xt = Rot([p.sb("xt%d" % i + sf, [128, 1024], F32) for i in range(2)])
        ss = p.sb("ss" + sf, [128, 1], F32)
        rstd = p.sb("rstd" + sf, [128, 1], F32)
        xs = p.sb("xs" + sf, [128, 1024], F32)
        xsT = p.sb("xsT" + sf, [128, 8, 128], BF16)
        z = p.sb("z" + sf, [128, NZ], F32)
        carry = p.sb("carry" + sf, [128, 1824], F32)
        p.op("pool", lambda e: e.memset(carry[:], 0.0), [], [carry])
        ZR = Rot([p.sb("zr%d" % i + sf, [128, 1824], F32) for i in range(2)])
        T5 = Rot([p.sb("t5_%d" % i + sf, [128, 320], F32) for i in range(4)])
        sm = Rot([p.sb("sm_%d" % i + sf, [128, 8], F32) for i in range(4)])
        PB = Rot([p.ps("pb%d" % i + sf, [128, 512], F32) for i in range(2)])
        PV = [p.ps("pv%d" % i + sf, [128, 512], F32) for i in range(2)]
        qk = p.sb("qk" + sf, [128, 10, 64], F32)
        kdup = p.sb("kdup" + sf, [128, 2, 2, 64], F32)
        qT = p.sb("qT" + sf, [128, 4, 128], BF16)
        kT = [p.sb("kT%d" % i + sf, [128, 2, 128], BF16) for i in range(2)]
        va = [p.sb("va%d" % i + sf, [128, 2, 65], BF16) for i in range(2)]
        for i in range(2):
            p.op("pool", lambda e, i=i: e.memset(va[i][:], 1.0), [], [va[i]])
        pT = Rot([p.sb("pT%d" % i + sf, [128, 2, 128], BF16) for i in range(4)])
        yout = Rot([p.sb("yout%d" % i + sf, [128, 512], F32) for i in range(2)])
        xq = {}

        def prefetch(b, n):
            if n < n_tiles:
                t0 = b * S_TOK + n * 128
                xq[(b, n)] = xt()
                dma("sp", xq[(b, n)][:], D["x"][t0:t0 + 128, :], [], [xq[(b, n)]])

        def tile_body(b, n):
            tok0 = b * S_TOK + n * 128
            first = (n == 0)
            x_t = xq.pop((b, n))
            prefetch(b, n + 1)
            zr = ZR()
            act(xs[:], x_t[:], AF.Square, [x_t], [xs, ss], accum_out=ss[:])
            act(rstd[:], ss[:], AF.Sqrt, [ss], [rstd], bias=1e-5, scale=1.0 / 1024)
            p.op("dve", lambda e: e.reciprocal(out=rstd[:], in_=rstd[:]), [rstd], [rstd])
            ts("dve", xs[:], x_t[:], rstd[:, 0:1], None, ALU.mult, None, [x_t, rstd], [xs])
            for hh in range(2):
                pb = PB()
                mms(lambda e, pb=pb, hh=hh: [e.transpose(out=pb[:, j * 128:(j + 1) * 128], in_=xs[:, (hh * 4 + j) * 128:(hh * 4 + j + 1) * 128], identity=ident) for j in range(4)][-1],
                    [xs, cst], [pb])
                cp("act" if hh else "dve", xsT[:, hh * 4:hh * 4 + 4, :], pb[:].rearrange("p (a b) -> p a b", a=4), [pb], [(xsT, hh)])
            for cc in range(6):
                c0 = cc * 512
                cw = min(512, NZ - c0)
                pb = PB()
                mms(lambda e, pb=pb, c0=c0, cw=cw: [e.matmul(pb[:, 0:cw], lhsT=xsT[:, c, :], rhs=Wb[:, c, c0:c0 + cw], start=(c == 0), stop=(c == 7)) for c in range(8)][-1],
                    [xsT, Wb], [pb])
                cp("act" if cc % 2 else "dve", z[:, c0:c0 + cw], pb[:, 0:cw], [pb], [(z, cc)])
            cosb = cst[:, C_COS + n * 32:C_COS + n * 32 + 32].unsqueeze(1).to_broadcast([128, 10, 32])
            sinb = cst[:, C_SIN + n * 32:C_SIN + n * 32 + 32].unsqueeze(1).to_broadcast([128, 10, 32])
            zq = z[:, 0:640].rearrange("p (h d) -> p h d", h=10)
            x1, x2 = zq[:, :, 0:32], zq[:, :, 32:64]
            ta, tb_ = T5(), T5()
            tav = ta[:, 0:320].rearrange("p (h d) -> p h d", h=10)
            tbv = tb_[:, 0:320].rearrange("p (h d) -> p h d", h=10)
            zk = [(z, 0), (z, 1)]
            tt("dve", tav, x1, cosb, ALU.mult, zk + [cst], [ta])
            tt("pool", tbv, x2, sinb, ALU.mult, zk + [cst], [tb_])
            tt("dve", qk[:, :, 0:32], tav, tbv, ALU.subtract, [ta, tb_], [(qk, 0)])
            tc_, td = T5(), T5()
            tcv = tc_[:, 0:320].rearrange("p (h d) -> p h d", h=10)
            tdv = td[:, 0:320].rearrange("p (h d) -> p h d", h=10)
            tt("pool", tcv, x2, cosb, ALU.mult, zk + [cst], [tc_])
            tt("dve", tdv, x1, sinb, ALU.mult, zk + [cst], [td])
            tt("pool", qk[:, :, 32:64], tcv, tdv, ALU.add, [tc_, td], [(qk, 1)])
            cp("pool", kdup[:], qk[:, 8:10, :].unsqueeze(2).to_broadcast([128, 2, 2, 64]), [qk], [kdup])
            kTc, kTp = kT[n % 2], kT[(n + 1) % 2]
            vac, vap = va[n % 2], va[(n + 1) % 2]
            cp("pool", vac[:, :, 0:64], z[:, 640:768].rearrange("p (g d) -> p g d", g=2), [(z, 1)], [vac])
            pb = PB()
            mms(lambda e, pb=pb: [e.transpose(out=pb[:, j * 128:(j + 1) * 128], in_=qk[:, 2 * j:2 * j + 2, :].rearrange("p a d -> p (a d)"), identity=ident) for j in range(4)][-1],
                [qk, cst], [pb])
            cp("act", qT[:], pb[:].rearrange("p (a b) -> p a b", a=4), [pb], [qT])
            pb = PB()
            mms(lambda e, pb=pb: [e.transpose(out=pb[:, g * 128:(g + 1) * 128], in_=kdup[:, g, :, :].rearrange("p a d -> p (a d)"), identity=ident) for g in range(2)][-1],
                [kdup, cst], [pb])
            cp("dve", kTc[:], pb[:, 0:256].rearrange("p (a b) -> p a b", a=2), [pb], [kTc])
            yo = yout()
            pv = PV
            for hd in range(0 if "a" in SKIP else 8):
                m, base, g = hd // 2, 64 * (hd % 2), hd // 4
                pb = PB()
                if first:
                    mms(lambda e, pb=pb, m=m, base=base, g=g: e.matmul(pb[:, 128:256], lhsT=kTc[base:base + 64, g, :], rhs=qT[base:base + 64, m, :], start=True, stop=True),
                        [kTc, qT], [pb])
                else:
                    mms(lambda e, pb=pb, m=m, base=base, g=g: [e.matmul(pb[:, 0:128], lhsT=kTp[base:base + 64, g, :], rhs=qT[base:base + 64, m, :], start=True, stop=True),
                                                              e.matmul(pb[:, 128:256], lhsT=kTc[base:base + 64, g, :], rhs=qT[base:base + 64, m, :], start=True, stop=True)][-1],
                        [kTc, kTp, qT], [pb])
                pt = pT()
                lo = 1 if first else 0
                act(pt[:, lo:2, :], pb[:, lo * 128:256].rearrange("p (a b) -> p a b", a=2 - lo), AF.Exp, [pb, negsink], [pt],
                    scale=0.125, bias=negsink[:, hd:hd + 1])
                if not first:
                    tt("pool", pt[:, 0, :], pt[:, 0, :], cst[:, C_SL:C_SL + 128], ALU.mult, [pt, cst], [pt])
                tt("dve", pt[:, 1, :], pt[:, 1, :], cst[:, C_IU:C_IU + 128], ALU.mult, [pt, cst], [pt])
                pvb = pv[hd // 4]
                o0 = (hd % 4) * 65
                if first:
                    mms(lambda e, pvb=pvb, pt=pt, g=g, o0=o0: e.matmul(pvb[:, o0:o0 + 65], lhsT=pt[:, 1, :], rhs=vac[:, g, :], start=True, stop=True),
                        [pt, vac], [pvb])
                else:
                    mms(lambda e, pvb=pvb, pt=pt, g=g, o0=o0: [e.matmul(pvb[:, o0:o0 + 65], lhsT=pt[:, 0, :], rhs=vap[:, g, :], start=True, stop=False),
                                                              e.matmul(pvb[:, o0:o0 + 65], lhsT=pt[:, 1, :], rhs=vac[:, g, :], start=False, stop=True)][-1],
                        [pt, vac, vap], [pvb])
            for hf in range(2):
                pvb = pv[hf]
                pvv = pvb[:, 0:260].rearrange("p (h d) -> p h d", h=4)
                den = sm()
                ts("dve", den[:, 0:4], pvv[:, :, 64], 1.0, None, ALU.add, None, [pvb], [den])
                p.op("dve", lambda e, den=den: e.reciprocal(out=den[:, 0:4], in_=den[:, 0:4]), [den], [den])
                tt("dve", yo[:, hf * 256:(hf + 1) * 256].rearrange("p (h d) -> p h d", h=4), pvv[:, :, 0:64],
                   den[:, 0:4].unsqueeze(2).to_broadcast([128, 4, 64]), ALU.mult, [pvb, den], [(yo, hf)])
            for j in range(4):
                c0 = 768 + j * 512
                cw = min(512, NZ - c0)
                pb = PB()
                zkeys = [(z, 1), (z, 2), (z, 3), (z, 4), (z, 5)]
                if first:
                    mms(lambda e, pb=pb, c0=c0, cw=cw: e.matmul(pb[:, 0:cw], lhsT=cst[:, C_SH:C_SH + 128], rhs=z[:, c0:c0 + cw], start=True, stop=True),
                        zkeys + [cst], [pb])
                else:
                    mms(lambda e, pb=pb, c0=c0, cw=cw: [e.matmul(pb[:, 0:cw], lhsT=cst[:, C_SH:C_SH + 128], rhs=z[:, c0:c0 + cw], start=True, stop=False),
                                                       e.matmul(pb[:, 0:cw], lhsT=cst[:, C_CA:C_CA + 128], rhs=carry[:, c0 - 768:c0 - 768 + cw], start=False, stop=True)][-1],
                        zkeys + [cst, carry], [pb])
                r0 = c0 - 768
                tt("dve", zr[:, r0:r0 + cw], pb[:, 0:cw], z[:, c0:c0 + cw], ALU.subtract, [pb] + zkeys, [(zr, j)])
                tt("dve", zr[:, r0:r0 + cw], zr[:, r0:r0 + cw], vec[:, V_MU + r0:V_MU + r0 + cw], ALU.mult, [(zr, j), vec], [(zr, j)])
                tt("pool", zr[:, r0:r0 + cw], zr[:, r0:r0 + cw], z[:, c0:c0 + cw], ALU.add, [(zr, j)] + zkeys, [(zr, j)])
            cp("pool", carry[96:128, :], z[96:128, 768:NZ], [z], [carry])
            dma("sp", D["y_scr"][tok0:tok0 + 128, 0:512], yo[:], [yo], [(D["y_t"], (tok0 // 128, 0))])
            dma("sp", D["zr_scr"][tok0:tok0 + 128, :], zr[:], [zr], [(D["zr_t"], tok0 // 128)])

        class S:
            pass
        S.prefetch, S.body, S.z = prefetch, tile_body, z
        return S

    streams = [make_stream(k) for k in range(2)]
    Wb = load_w_bf16(p, h, "Wb1", D["w_in"], 1024, 4640, 0, NZ, (lambda: streams[0].z), nwp, 0)
    zip_streams(p, streams, n_seq, n_tiles)


def phase_a1b(p, nc, D, n_seq, n_tiles, S_TOK):
    h = helpers(p)
    tt, stt, ts, act, cp, red, dma, mms = h.tt, h.stt, h.ts, h.act, h.cp, h.red, h.dma, h.mms
    cst = p.sb("cst", [128, NCST], F32)
    dma("sp", cst[:], D["cst"], [], [cst])
    VOFF = V_W0
    vec = p.sb("vecr", [128, V_SINK - VOFF], F32)
    dma("sp", vec[:], D["vecs"][VOFF:V_SINK].partition_broadcast(128), [], [vec])
    ident = cst[:, C_ID:C_ID + 128]
    w2 = p.sb("w2", [128, 512], F32)
    dma("sp", w2[0:64, :], D["decay_w2"], [], [w2])
    dma("sp", w2[64:128, :], D["iclr_a2"], [], [w2])
    g2 = p.sb("g2", [128, 2, 512], F32)
    dma("sp", g2[:, 0, :], D["gate_g2"][0:128, :], [], [g2])
    dma("sp", g2[0:32, 1, :], D["gate_g2"][128:160, :], [], [g2])

    def vb(off, n=512):
        return vec[:, off - VOFF:off - VOFF + n]

    def make_stream(k):
        sf = "_r%d" % k
        ZR = Rot([p.sb("zr%d" % i + sf, [128, 1824], F32) for i in range(2)])
        lin = p.sb("lin" + sf, [128, 288], F32)
        linT = p.sb("linT" + sf, [128, 3, 128], F32)
        T5 = Rot([p.sb("t5_%d" % i + sf, [128, 512], F32) for i in range(10)])
        gv = p.sb("gv" + sf, [128, 512], F32)
        k2 = p.sb("k2" + sf, [128, 512], F32)
        M8 = Rot([p.sb("m8_%d" % i + sf, [128, 8, 128], F32) for i in range(6)])
        M8d = [p.sb("m8d_%d" % i + sf, [128, 8, 128], F32) for i in range(3)]
        Tq = Rot([p.sb("tq_%d" % i + sf, [128, 4, 128], F32) for i in range(4)])
        sm = Rot([p.sb("sm_%d" % i + sf, [128, 8], F32) for i in range(8)])
        PB = Rot([p.ps("pb%d" % i + sf, [128, 512], F32) for i in range(4)])
        ST = p.sb("ST" + sf, [128, 4, 128], F32)
        gC = p.sb("gC" + sf, [128, 4], F32)
        yout = Rot([p.sb("yout%d" % i + sf, [128, 512], F32) for i in range(1)])
        zq = {}

        def prefetch(b, n):
            if n < n_tiles:
                t0 = b * S_TOK + n * 128
                zq[(b, n)] = ZR()
                dma("sp", zq[(b, n)][:], D["zr_scr"][t0:t0 + 128, :], [(D["zr_t"], t0 // 128)], [zq[(b, n)]])

        def tile_body(b, n):
            if n == 0:
                p.op("pool", lambda e: e.memset(ST[:], 0.0), [], [ST])
            tok0 = b * S_TOK + n * 128
            zr = zq.pop((b, n))
            prefetch(b, n + 1)
            yo = yout()
            r_, k_, v_ = zr[:, 0:512], zr[:, 512:1024], zr[:, 1024:1536]
            act(lin[:, 0:64], zr[:, 1536:1600], AF.Tanh, [zr], [(lin, 0)])
            cp("pool", lin[:, 64:128], zr[:, 1600:1664], [zr], [(lin, 1)])
            act(lin[:, 128:288], zr[:, 1664:1824], AF.Sigmoid, [zr], [(lin, 2)])
            pb = PB()
            mms(lambda e, pb=pb: [e.transpose(out=pb[:, 0:128], in_=lin[:, 0:128], identity=ident),
                                  e.transpose(out=pb[:, 128:256], in_=lin[:, 128:256], identity=ident),
                                  e.transpose(out=pb[0:32, 256:384], in_=lin[:, 256:288], identity=ident)][-1], [lin, cst], [pb])
            cp("dve", linT[:, 0:2, :], pb[:, 0:256].rearrange("p (a b) -> p a b", a=2), [pb], [(linT, 0)])
            cp("dve", linT[0:32, 2, :], pb[0:32, 256:384], [pb], [(linT, 1)])
            pw, pa_, pg = PB(), PB(), PB()
            mms(lambda e, pw=pw: e.matmul(pw[:], lhsT=linT[0:64, 0, :], rhs=w2[0:64, :], start=True, stop=True), [linT, w2], [pw])
            mms(lambda e, pa_=pa_: e.matmul(pa_[:], lhsT=linT[64:128, 0, :], rhs=w2[64:128, :], start=True, stop=True), [linT, w2], [pa_])
            mms(lambda e, pg=pg: [e.matmul(pg[:], lhsT=linT[:, 1, :], rhs=g2[:, 0, :], start=True, stop=False),
                                  e.matmul(pg[:], lhsT=linT[0:32, 2, :], rhs=g2[0:32, 1, :], start=False, stop=True)][-1], [linT, g2], [pg])
            sg, av = T5(), T5()
            tt("dve", sg[:], pw[:], vb(V_W0), ALU.add, [pw, vec], [sg])
            act(sg[:], sg[:], AF.Sigmoid, [sg], [sg])
            tt("dve", av[:], pa_[:], vb(V_A0), ALU.add, [pa_, vec], [av])
            act(av[:], av[:], AF.Sigmoid, [av], [av])
            cp("act", gv[:], pg[:], [pg], [gv])
            pc = PB()
            mms(lambda e, pc=pc, sg=sg: e.matmul(pc[:], lhsT=cst[:, C_IU:C_IU + 128], rhs=sg[:], start=True, stop=True), [cst, sg], [pc])
            gam, igam, gprev = T5(), T5(), T5()
            act(gam[:], pc[:], AF.Exp, [pc], [gam], scale=-C_DEC)
            act(igam[:], pc[:], AF.Exp, [pc], [igam], scale=C_DEC)
            tt("dve", gprev[:], pc[:], sg[:], ALU.subtract, [pc, sg], [gprev])
            act(gprev[:], gprev[:], AF.Exp, [gprev], [gprev], scale=-C_DEC)
            pgc = PB()
            mms(lambda e, pgc=pgc, sg=sg: [e.matmul(pgc[:, m:m + 1], lhsT=sg[:, m * 128:(m + 1) * 128], rhs=cst[:, C_ONE:C_ONE + 1], start=True, stop=True) for m in range(4)][-1],
                [cst, sg], [pgc])
            act(gC[:], pgc[:, 0:4], AF.Exp, [pgc], [gC], scale=-C_DEC)
            kk, sq = T5(), T5()
            tt("dve", kk[:], k_, vb(V_KK), ALU.mult, [zr, vec], [kk])
            tt("pool", sq[:], kk[:], kk[:], ALU.mult, [kk], [sq])
            s8 = sm()
            red(s8[:], sq[:].rearrange("p (h d) -> p h d", h=8), [sq], [s8])
            act(s8[:], s8[:], AF.Sqrt, [s8], [s8], bias=1e-24, scale=1.0)
            p.op("dve", lambda e, s8=s8: e.reciprocal(out=s8[:], in_=s8[:]), [s8], [s8])
            tt("dve", kk[:].rearrange("p (h d) -> p h d", h=8), kk[:].rearrange("p (h d) -> p h d", h=8),
               s8[:].unsqueeze(2).to_broadcast([128, 8, 64]), ALU.mult, [kk, s8], [kk])
            t1 = T5()
            stt("dve", t1[:], av[:], -1.0, vb(V_KA), ALU.add, ALU.mult, [av, vec], [t1])
            stt("dve", k2[:], t1[:], 1.0, k_, ALU.add, ALU.mult, [t1, zr], [k2])
            At, Bt, Kt, Rt = T5(), T5(), T5(), T5()
            stt("dve", At[:], kk[:], -1.0, gprev[:], ALU.mult, ALU.mult, [kk, gprev], [At])
            tt("pool", Bt[:], kk[:], av[:], ALU.mult, [kk, av], [Bt])
            tt("dve", Bt[:], Bt[:], igam[:], ALU.mult, [Bt, igam], [Bt])
            tt("dve", Kt[:], k2[:], igam[:], ALU.mult, [k2, igam], [Kt])
            tt("pool", Rt[:], r_, gam[:], ALU.mult, [zr, gam], [Rt])
            XT = {}
            for nm, src in (("A", At), ("B", Bt), ("K", Kt), ("R", Rt)):
                pb = PB()
                mms(lambda e, pb=pb, src=src: [e.transpose(out=pb[:, j * 128:(j + 1) * 128], in_=src[:, j * 128:(j + 1) * 128], identity=ident) for j in range(4)][-1],
                    [src, cst], [pb])
                dst = Tq()
                cp("act" if nm in ("A", "K") else "dve", dst[:], pb[:].rearrange("p (a b) -> p a b", a=4), [pb], [dst])
                XT[nm] = dst
            AT, BT, KT, RT = XT["A"], XT["B"], XT["K"], XT["R"]

            def pairmat(l, r_op, mask_off, eng2, dst=None):
                dst = dst or M8()
                for par in range(2):
                    pb = PB()
                    mms(lambda e, pb=pb, par=par: [e.matmul(pb[:, j * 128:(j + 1) * 128],
                                                          lhsT=l[64 * par:64 * par + 64, j, :],
                                                          rhs=r_op[64 * par:64 * par + 64, j, :],
                                                          start=True, stop=True) for j in range(4)][-1], [l, r_op], [pb])
                    tt(eng2[par], dst[:, par:8:2, :], pb[:].rearrange("p (a b) -> p a b", a=4),
                       cst[:, mask_off:mask_off + 128].unsqueeze(1).to_broadcast([128, 4, 128]), ALU.mult, [pb, cst], [(dst, par)])
                return dst

            Nm = pairmat(BT, AT, C_SU, ("dve", "dve"))
            Am = pairmat(AT, BT, C_SL, ("dve", "dve"))
            AkT = pairmat(KT, AT, C_SU, ("dve", "dve"), M8d[0])
            RbT = pairmat(BT, RT, C_IU, ("dve", "dve"), M8d[1])
            RkT = pairmat(KT, RT, C_IU, ("dve", "dve"), M8d[2])
            X = M8()
            tt("dve", X[:], Nm[:], ident.unsqueeze(1).to_broadcast([128, 8, 128]), ALU.add, [Nm, cst], [X])
            for j in range(0 if "d" in SKIP else 6):
                Nn, An, Xn = M8(), M8(), M8()
                last = (j == 5)
                for hf in range(2):
                    pbn, pba = PB(), PB()
                    if not last:
                        mms(lambda e, pbn=pbn, hf=hf, Am=Am, Nm=Nm: [e.matmul(pbn[:, q * 128:(q + 1) * 128], lhsT=Am[:, hf * 4 + q, :], rhs=Nm[:, hf * 4 + q, :], start=True, stop=True) for q in range(4)][-1],
                            [Am, Nm], [pbn])
                        cp("act", Nn[:, hf * 4:hf * 4 + 4, :], pbn[:].rearrange("p (a b) -> p a b", a=4), [pbn], [(Nn, hf)])
                    mms(lambda e, pba=pba, hf=hf, Am=Am, Nm=Nm: [e.matmul(pba[:, q * 128:(q + 1) * 128], lhsT=Nm[:, hf * 4 + q, :], rhs=Am[:, hf * 4 + q, :], start=True, stop=True) for q in range(4)][-1],
                        [Am, Nm], [pba])
                    cp("dve", An[:, hf * 4:hf * 4 + 4, :], pba[:].rearrange("p (a b) -> p a b", a=4), [pba], [(An, hf)])
                for hf in range(2):
                    pbx = PB()
                    mms(lambda e, pbx=pbx, hf=hf, An=An, X=X: [e.matmul(pbx[:, q * 128:(q + 1) * 128], lhsT=An[:, hf * 4 + q, :], rhs=X[:, hf * 4 + q, :], start=True, stop=True) for q in range(4)][-1],
                        [An, X], [pbx])
                    tt("dve", Xn[:, hf * 4:hf * 4 + 4, :], pbx[:].rearrange("p (a b) -> p a b", a=4), X[:, hf * 4:hf * 4 + 4, :], ALU.add, [pbx, X], [(Xn, hf)])
                Nm, Am, X = Nn, An, Xn
            pr = PB()

            def f_rhs0(e, pr=pr, AT=AT, AkT=AkT):
                ins = None
                for m in range(4):
                    e.matmul(pr[:, m * 128:(m + 1) * 128], lhsT=AT[:, m, :], rhs=ST[:, m, :], start=True, stop=False)
                    for q in range(2):
                        hd = 2 * m + q
                        ins = e.matmul(pr[:, hd * 64:(hd + 1) * 64], lhsT=AkT[:, hd, :], rhs=zr[:, 1024 + hd * 64:1024 + (hd + 1) * 64], start=False, stop=(q == 1))
                return ins
            mms(f_rhs0, [AT, ST, AkT, zr], [pr])
            rhs0 = T5()
            cp("act", rhs0[:], pr[:], [pr], [rhs0])
            pu = PB()
            mms(lambda e, pu=pu, X=X, rhs0=rhs0: [e.matmul(pu[:, hd * 64:(hd + 1) * 64], lhsT=X[:, hd, :], rhs=rhs0[:, hd * 64:(hd + 1) * 64], start=True, stop=True) for hd in range(8)][-1],
                [X, rhs0], [pu])
            U = T5()
            cp("dve", U[:], pu[:], [pu], [U])
            py = PB()

            def f_y(e, py=py, RT=RT, RbT=RbT, RkT=RkT, U=U):
                ins = None
                for m in range(4):
                    e.matmul(py[:, m * 128:(m + 1) * 128], lhsT=RT[:, m, :], rhs=ST[:, m, :], start=True, stop=False)
                    for q in range(2):
                        hd = 2 * m + q
                        e.matmul(py[:, hd * 64:(hd + 1) * 64], lhsT=RbT[:, hd, :], rhs=U[:, hd * 64:(hd + 1) * 64], start=False, stop=False)
                        ins = e.matmul(py[:, hd * 64:(hd + 1) * 64], lhsT=RkT[:, hd, :], rhs=zr[:, 1024 + hd * 64:1024 + (hd + 1) * 64], start=False, stop=(q == 1))
                return ins
            mms(f_y, [RT, ST, RbT, RkT, U, zr], [py])
            yv = T5()
            cp("act", yv[:], py[:], [py], [yv])
            pst = PB()

            def f_s(e, pst=pst, Bt=Bt, Kt=Kt, U=U):
                ins = None
                for m in range(4):
                    e.matmul(pst[:, m * 128:(m + 1) * 128], lhsT=Bt[:, m * 128:(m + 1) * 128], rhs=U[:, m * 128:(m + 1) * 128], start=True, stop=False)
                    ins = e.matmul(pst[:, m * 128:(m + 1) * 128], lhsT=Kt[:, m * 128:(m + 1) * 128], rhs=zr[:, 1024 + m * 128:1024 + (m + 1) * 128], start=False, stop=True)
                return ins
            mms(f_s, [Bt, Kt, U, zr], [pst])
            tt("dve", ST[:], pst[:].rearrange("p (a b) -> p a b", a=4), ST[:], ALU.add, [pst, ST], [ST])
            tt("dve", ST[:], ST[:], gC[:].unsqueeze(2).to_broadcast([128, 4, 128]), ALU.mult, [ST, gC], [ST])
            tt("dve", ST[:], ST[:], cst[:, C_BD:C_BD + 128].unsqueeze(1).to_broadcast([128, 4, 128]), ALU.mult, [ST, cst], [ST])
            y3 = yv[:].rearrange("p (h d) -> p h d", h=8)
            ysq = T5()
            tt("pool", ysq[:], yv[:], yv[:], ALU.mult, [yv], [ysq])
            s1, s2, mean, var = sm(), sm(), sm(), sm()
            red(s1[:], y3, [yv], [s1])
            red(s2[:], ysq[:].rearrange("p (h d) -> p h d", h=8), [ysq], [s2])
            ts("dve", mean[:], s1[:], 1.0 / 64, None, ALU.mult, None, [s1], [mean])
            tt("dve", var[:], mean[:], mean[:], ALU.mult, [mean], [var])
            stt("dve", var[:], s2[:], 1.0 / 64, var[:], ALU.mult, ALU.subtract, [s2, var], [var])
            act(var[:], var[:], AF.Sqrt, [var], [var], bias=64e-5, scale=1.0)
            p.op("dve", lambda e, var=var: e.reciprocal(out=var[:], in_=var[:]), [var], [var])
            yn = T5()
            yn3 = yn[:].rearrange("p (h d) -> p h d", h=8)
            tt("dve", yn3, y3, mean[:].unsqueeze(2).to_broadcast([128, 8, 64]), ALU.subtract, [yv, mean], [yn])
            tt("dve", yn3, yn3, var[:].unsqueeze(2).to_broadcast([128, 8, 64]), ALU.mult, [yn, var], [yn])
            tt("pool", yn[:], yn[:], vb(V_LNW), ALU.mult, [yn, vec], [yn])
            tt("pool", yn[:], yn[:], vb(V_LNB), ALU.add, [yn, vec], [yn])
            rk = T5()
            tt("pool", rk[:], r_, k2[:], ALU.mult, [zr, k2], [rk])
            tt("pool", rk[:], rk[:], vb(V_RK), ALU.mult, [rk, vec], [rk])
            sb_ = sm()
            red(sb_[:], rk[:].rearrange("p (h d) -> p h d", h=8), [rk], [sb_])
            tt("dve", rk[:].rearrange("p (h d) -> p h d", h=8), v_.rearrange("p (h d) -> p h d", h=8),
               sb_[:].unsqueeze(2).to_broadcast([128, 8, 64]), ALU.mult, [zr, sb_], [rk])
            tt("pool", yn[:], yn[:], rk[:], ALU.add, [yn, rk], [yn])
            tt("pool", yo[:, 0:512], yn[:], gv[:], ALU.mult, [yn, gv], [(yo, 2)])
            dma("sp", D["y_scr"][tok0:tok0 + 128, 512:1024], yo[:], [yo], [(D["y_t"], (tok0 // 128, 1))])

        class S:
            pass
        S.prefetch, S.body = prefetch, tile_body
        return S

    streams = [make_stream(k) for k in range(NSB)]
    zip_streams(p, streams, n_seq, n_tiles)


def zip_streams(p, streams, n_seq, n_tiles):
    NS = len(streams)
    for b0 in range(0, n_seq, NS):
        seqs = list(range(b0, min(b0 + NS, n_seq)))
        for k, b in enumerate(seqs):
            streams[k].prefetch(b, 0)
        for n in range(n_tiles):
            lists = []
            for k, b in enumerate(seqs):
                p.defer_begin()
                streams[k].body(b, n)
                lists.append(p.defer_end())
            while any(lists):
                for lst in lists:
                    if lst:
                        p.drain(lst, 1)


def phase_a2(p, nc, D, ntok, final=True, drip=None):
    h = helpers(p)
    tt, stt, ts, act, cp, red, dma, mms = h.tt, h.stt, h.ts, h.act, h.cp, h.red, h.dma, h.mms
    cst = p.sb("cstb", [128, 128], F32)
    dma("sp", cst[:], D["cst"][:, C_ID:C_ID + 128], [], [cst])
    ident = cst[:, 0:128]
    nwp = p.sb("nwpb", [128, 16], F32)
    dma("sp", nwp[:], D["nwp"], [], [nwp])
    fin = p.sb("finw", [128, 1024], F32)
    dma("sp", fin[:], D["vecs"][V_FIN:V_FIN + 1024].partition_broadcast(128), [], [fin])
    stg = Rot([p.sb("stgb%d" % i, [128, 2048], F32) for i in range(2)])
    Wg = load_w_bf16(p, h, "Wg", D["w_in"], 1024, 4640, 2592, 2048, stg, nwp, 0)
    PA = load_w_bf16(p, h, "PAw", D["proj_attn"], 512, 1024, 0, 1024, stg)
    PBw = load_w_bf16(p, h, "PBw", D["proj_rwkv"], 512, 1024, 0, 1024, stg)
    WO = load_w_bf16(p, h, "WOw", D["w_out"], 1024, 1024, 0, 1024, stg)
    nt = ntok // 128
    NS = 2

    class St:
        pass

    streams = []
    for k in range(NS):
        S = St()
        S.xt = Rot([p.sb("xtb%d_%d" % (k, i), [128, 1024], F32) for i in range(2)])
        S.yt = Rot([p.sb("ytb%d_%d" % (k, i), [128, 1024], F32) for i in range(2)])
        S.ss = p.sb("ssb%d" % k, [128, 1], F32)
        S.rstd = p.sb("rstdb%d" % k, [128, 1], F32)
        S.xs = p.sb("xsb%d" % k, [128, 1024], F32)
        S.xsT = p.sb("xsTb%d" % k, [128, 8, 128], BF16)
        S.yT = p.sb("yTb%d" % k, [128, 8, 128], BF16)
        S.sgt = p.sb("sgt%d" % k, [128, 2048], BF16)
        S.mg = p.sb("mg%d" % k, [128, 1024], F32)
        S.m2 = p.sb("m2%d" % k, [128, 1024], F32)
        S.mgT = p.sb("mgT%d" % k, [128, 8, 128], BF16)
        S.h1 = Rot([p.sb("h1b%d_%d" % (k, i), [128, 1024], F32) for i in range(2)])
        S.PB = Rot([p.ps("pq%d_%d" % (k, i), [128, 512], F32) for i in range(4)])
        S.xq, S.yq = {}, {}
        streams.append(S)

    def prefetch(S, i):
        if i < nt:
            S.xq[i] = S.xt()
            dma("sp", S.xq[i][:], D["x"][i * 128:(i + 1) * 128, :], [], [S.xq[i]])
            S.yq[i] = S.yt()
            dma("sp", S.yq[i][:], D["y_scr"][i * 128:(i + 1) * 128, :], [(D["y_t"], (i, 0)), (D["y_t"], (i, 1))], [S.yq[i]])

    def tp8(S, src, dst):
        for hh in range(2):
            pb = S.PB()
            mms(lambda e, pb=pb, hh=hh: [e.transpose(out=pb[:, j * 128:(j + 1) * 128], in_=src[:, (hh * 4 + j) * 128:(hh * 4 + j + 1) * 128], identity=ident) for j in range(4)][-1],
                [src, cst], [pb])
            cp("act" if hh else "dve", dst[:, hh * 4:hh * 4 + 4, :], pb[:].rearrange("p (a b) -> p a b", a=4), [pb], [(dst, hh)])

    def body(S, i):
        ss, rstd, xs, xsT, yT, sgt, mg, m2, mgT = S.ss, S.rstd, S.xs, S.xsT, S.yT, S.sgt, S.mg, S.m2, S.mgT
        x_t, y_t = S.xq.pop(i), S.yq.pop(i)
        prefetch(S, i + NS)
        act(xs[:], x_t[:], AF.Square, [x_t], [xs, ss], accum_out=ss[:])
        act(rstd[:], ss[:], AF.Sqrt, [ss], [rstd], bias=1e-5, scale=1.0 / 1024)
        p.op("dve", lambda e: e.reciprocal(out=rstd[:], in_=rstd[:]), [rstd], [rstd])
        ts("dve", xs[:], x_t[:], rstd[:, 0:1], None, ALU.mult, None, [x_t, rstd], [xs])
        tp8(S, xs, xsT)
        for cc in range(4):
            pb = S.PB()
            mms(lambda e, pb=pb, cc=cc: [e.matmul(pb[:], lhsT=xsT[:, c, :], rhs=Wg[:, c, cc * 512:(cc + 1) * 512], start=(c == 0), stop=(c == 7)) for c in range(8)][-1],
                [xsT, Wg], [pb])
            act(sgt[:, cc * 512:(cc + 1) * 512], pb[:], AF.Sigmoid, [pb], [(sgt, cc)])
        tp8(S, y_t, yT)
        for br, (W, dstt) in enumerate(((PA, mg), (PBw, m2))):
            for hf in range(2):
                pb = S.PB()
                mms(lambda e, pb=pb, br=br, hf=hf, W=W: [e.matmul(pb[:], lhsT=yT[:, br * 4 + c, :], rhs=W[:, c, hf * 512:(hf + 1) * 512], start=(c == 0), stop=(c == 3)) for c in range(4)][-1],
                    [yT, W], [pb])
                tt("dve", dstt[:, hf * 512:(hf + 1) * 512], pb[:], sgt[:, br * 1024 + hf * 512:br * 1024 + (hf + 1) * 512], ALU.mult, [pb, sgt], [(dstt, hf)])
        tt("pool", mg[:], mg[:], m2[:], ALU.add, [mg, m2], [mg])
        tp8(S, mg, mgT)
        ho = S.h1()
        for hf in range(2):
            pb = S.PB()
            mms(lambda e, pb=pb, hf=hf: [e.matmul(pb[:], lhsT=mgT[:, c, :], rhs=WO[:, c, hf * 512:(hf + 1) * 512], start=(c == 0), stop=(c == 7)) for c in range(8)][-1],
                [mgT, WO], [pb])
            tt("dve", ho[:, hf * 512:(hf + 1) * 512], pb[:], x_t[:, hf * 512:(hf + 1) * 512], ALU.add, [pb, x_t], [(ho, hf)])
        if final:
            act(xs[:], ho[:], AF.Square, [ho], [xs, ss], accum_out=ss[:])
            act(rstd[:], ss[:], AF.Sqrt, [ss], [rstd], bias=1e-5, scale=1.0 / 1024)
            p.op("dve", lambda e: e.reciprocal(out=rstd[:], in_=rstd[:]), [rstd], [rstd])
            stt("dve", ho[:], ho[:], rstd[:, 0:1], fin[:], ALU.mult, ALU.mult, [ho, rstd, fin], [ho])
            dma("sp", D["out"][i * 128:(i + 1) * 128, :], ho[:], [ho], [])
        else:
            dma("sp", D["h1_scr"][i * 128:(i + 1) * 128, :], ho[:], [ho], [(D["h1_t"], i)])

    for k in range(NS):
        prefetch(streams[k], k)
    per = (len(drip) + max(nt // NS - 1, 1) - 1) // max(nt // NS - 1, 1) if drip else 0
    for i0 in range(0, nt, NS):
        lists = []
        for k in range(NS):
            if i0 + k < nt:
                p.defer_begin()
                body(streams[k], i0 + k)
                lists.append(p.defer_end())
        while any(lists):
            for lst in lists:
                if lst:
                    p.drain(lst, 1)
        if drip:
            p.drain(drip, per)
    if drip:
        p.drain(drip, len(drip))


def phase_b0(p, nc, D, eng_rot=("dve", "pool"), nbuf=2, ldq="sp", stq="act"):
    h = helpers(p)
    dma, cp = h.dma, h.cp
    stg = Rot([p.sb("cs%d" % i, [128, 4096], F32) for i in range(nbuf)])
    ob = Rot([p.sb("co%d" % i, [128, 4096], BF16) for i in range(nbuf)])
    nwp0 = p.sb("nwp0", [128, 16], F32)
    dma("sp", nwp0[:], D["nwp"], [], [nwp0])
    k = 0
    for g in range(32):
        s, o = stg(), ob()
        dma(ldq, s[:].rearrange("p (dc e) -> p dc e", dc=8), D["uT"][:, g * 512:(g + 1) * 512].rearrange("(dc p) e -> p dc e", p=128), [], [s])
        h.tt(eng_rot[k % 2], o[:].rearrange("p (i dc e) -> p dc i e", i=4, dc=8), s[:].rearrange("p (dc i e) -> p dc i e", dc=8, i=4),
             nwp0[:, 8:16].unsqueeze(2).unsqueeze(3).to_broadcast([128, 8, 4, 128]), ALU.mult, [s, nwp0], [o])
        k += 1
        dma(stq, D["u2"][:, g * 4:(g + 1) * 4, :, :].rearrange("p i dc e -> p (i dc e)"), o[:], [o], [(D["u2_t"], g)])
        s, o = stg(), ob()
        dma(ldq, s[:].rearrange("p (i d) -> p i d", i=4), D["v"][g * 512:(g + 1) * 512, :].rearrange("(i p) d -> p i d", p=128), [], [s])
        cp(eng_rot[k % 2], o[:], s[:], [s], [o])
        k += 1
        dma(stq, D["vb"][g * 512:(g + 1) * 512, :].rearrange("(i p) d -> p i d", p=128), o[:].rearrange("p (i d) -> p i d", i=4), [o], [(D["vb_t"], g)])


def phase_b(p, nc, D, ntok):
    h = helpers(p)
    tt, stt, ts, act, cp, red, dma, mms = h.tt, h.stt, h.ts, h.act, h.cp, h.red, h.dma, h.mms
    TT = 256
    cst = p.sb("cstc", [128, 256], F32)
    dma("sp", cst[:, 0:128], D["cst"][:, C_ID:C_ID + 128], [], [cst])
    dma("sp", cst[:, 128:256], D["cst"][:, C_IOTA:C_IOTA + 128], [], [cst])
    ident = cst[:, 0:128]
    iota = cst[:, 128:256]
    iota_bf = p.sb("iota_bf", [128, 128], BF16)
    cp("dve", iota_bf[:], iota, [cst], [iota_bf])
    fin = p.sb("finc", [128, 1024], F32)
    dma("sp", fin[:], D["vecs"][V_FIN:V_FIN + 1024].partition_broadcast(128), [], [fin])
    nwpb = p.sb("nwpc", [128, 16], F32)
    dma("sp", nwpb[:], D["nwp"], [], [nwpb])
    skT = p.sb("skT", [128, 8, 128], F32)
    dma("sp", skT[:], D["skT"], [], [skT])
    G = p.sb("G", [128, TT, 128], BF16)
    xs = p.sb("xsc", [128, 1024], F32)
    Wq = load_w_bf16(p, h, "Wq", D["peer_wq"], 1024, 1024, 0, 1024, (lambda: xs), nwpb, 8)
    U2 = Rot([p.sb("u2t%d" % i, [128, 4, 8, 128], BF16) for i in range(3)])
    Vt = Rot([p.sb("vt%d" % i, [128, 4, 1024], BF16) for i in range(3)])
    h1 = [[p.sb("h1c%d_%d" % (b, i), [128, 1024], F32) for i in range(2)] for b in range(2)]
    ss = p.sb("ssc", [128, 1], F32)
    rstd = p.sb("rstdc", [128, 1], F32)
    xTb = [p.sb("xs2T%d" % b, [128, 8, TT], BF16) for b in range(2)]
    qT = p.sb("qTc", [128, 8, TT], F32)
    sc = p.sb("sc", [128, 16, 128], F32)
    v16 = p.sb("v16", [128, 16, 16], F32)
    i16u = p.sb("i16u", [128, 16, 16], U32)
    i16f = p.sb("i16f", [128, 16, 16], F32)
    cand = p.sb("cand", [128, 8, 256], F32)
    tv = p.sb("tv", [128, 8, 16], F32)
    posu = p.sb("posu", [128, 8, 16], U32)
    au = p.sb("au", [128, 8, 16], U32)
    bu = p.sb("bu", [128, 8, 16], U32)
    af = p.sb("af", [128, 8, 16], F32)
    bf_ = p.sb("bf", [128, 8, 16], F32)
    sel = p.sb("sel", [128, 3, 128], F32)
    sm = Rot([p.sb("smc%d" % i, [128, 8], F32) for i in range(4)])
    selTb = [p.sb("selT%d" % b, [128, 3, TT], F32) for b in range(2)]
    OA = Rot([p.sb("oa%d" % i, [128, 16, 128], BF16) for i in range(2)])
    OB = Rot([p.sb("ob%d" % i, [128, 16, 128], BF16) for i in range(2)])
    gh = Rot([p.sb("gh%d" % i, [128, TT], BF16) for i in range(2)])
    ac = Rot([p.sb("ac%d" % i, [128, TT], BF16) for i in range(3)])
    pbs = [p.ps("pr%d" % i, [128, 512], F32) for i in range(4)]
    PBH = Rot(pbs[0:3])
    PBP = Rot(pbs[3:4])
    PBG = Rot(pbs)
    ACC = [p.ps("acc%d" % i, [128, 512], F32) for i in range(4)]
    ntile = ntok // TT

    def prep(tix):
        b = tix % 2
        t0 = tix * TT
        xT, selT = xTb[b], selTb[b]
        PB = PBP
        for s in range(2):
            hh1 = h1[b][s]
            dma("sp", hh1[:], D["h1_scr"][t0 + s * 128:t0 + (s + 1) * 128, :], [(D["h1_t"], tix * 2 + s)], [hh1])
            act(xs[:], hh1[:], AF.Square, [hh1], [xs, ss], accum_out=ss[:])
            act(rstd[:], ss[:], AF.Sqrt, [ss], [rstd], bias=1e-5, scale=1.0 / 1024)
            p.op("dve", lambda e: e.reciprocal(out=rstd[:], in_=rstd[:]), [rstd], [rstd])
            ts("dve", xs[:], hh1[:], rstd[:, 0:1], None, ALU.mult, None, [hh1, rstd], [xs])
            for hh in range(2):
                pb = PB()
                mms(lambda e, pb=pb, hh=hh: [e.transpose(out=pb[:, j * 128:(j + 1) * 128], in_=xs[:, (hh * 4 + j) * 128:(hh * 4 + j + 1) * 128], identity=ident) for j in range(4)][-1],
                    [xs, cst], [pb])
                cp("act" if hh else "dve", xT[:, hh * 4:hh * 4 + 4, s * 128:(s + 1) * 128], pb[:].rearrange("p (a b) -> p a b", a=4), [pb], [(xT, (s, hh))])
        for c in range(8):
            pb = PB()
            mms(lambda e, pb=pb, c=c, xT=xT: [e.matmul(pb[:, 0:TT], lhsT=Wq[:, dc, c * 128:(c + 1) * 128], rhs=xT[:, dc, :], start=(dc == 0), stop=(dc == 7)) for dc in range(8)][-1],
                [Wq, xT], [pb])
            cp("act" if c % 2 else "dve", qT[:, c, :], pb[:, 0:TT], [pb], [(qT, c)])
        for s in range(0 if "S" in SKIP else 2):
            for par in range(2):
                for half in range(2):
                    pb = PB()
                    mms(lambda e, pb=pb, par=par, half=half, s=s: [e.matmul(pb[:, j * 128:(j + 1) * 128], lhsT=qT[64 * par:64 * par + 64, half * 4 + j, s * 128:(s + 1) * 128],
                                                                        rhs=skT[64 * par:64 * par + 64, half * 4 + j, :], start=True, stop=True) for j in range(4)][-1],
                        [qT, skT], [pb])
                    cp("act", sc[:, 8 * half + par:8 * half + 8:2, :], pb[:].rearrange("p (a b) -> p a b", a=4), [pb], [(sc, 8 * half + par + 2 * j) for j in range(4)])
            tmpA = cand[:].rearrange("p h (a b) -> p (h a) b", a=2)
            for hp in range(16):
                p.op("dve", lambda e, hp=hp: e.max(out=v16[:, hp, 0:8], in_=sc[:, hp, :]), [(sc, hp)], [(v16, hp)])
            for hp in range(16):
                p.op("dve", lambda e, hp=hp, tmpA=tmpA: e.match_replace(out=tmpA[:, hp, :], in_to_replace=v16[:, hp, 0:8], in_values=sc[:, hp, :], imm_value=-1e30), [(sc, hp), (v16, hp)], [(cand, hp)])
            for hp in range(16):
                p.op("dve", lambda e, hp=hp, tmpA=tmpA: e.max(out=v16[:, hp, 8:16], in_=tmpA[:, hp, :]), [(cand, hp)], [(v16, hp)])
            for hp in range(16):
                p.op("dve", lambda e, hp=hp: e.max_index(out=i16u[:, hp, 0:8], in_max=v16[:, hp, 0:8], in_values=sc[:, hp, :]), [(sc, hp), (v16, hp)], [(i16u, hp)])
            for hp in range(16):
                p.op("dve", lambda e, hp=hp: e.max_index(out=i16u[:, hp, 8:16], in_max=v16[:, hp, 8:16], in_values=sc[:, hp, :]), [(sc, hp), (v16, hp)], [(i16u, hp)])
            cp("pool", i16f[:], i16u[:], [i16u], [i16f])
            tt("pool", cand[:].rearrange("p h (a b) -> p h a b", a=16), v16[:, 0:16:2, :].unsqueeze(3).to_broadcast([128, 8, 16, 16]),
               v16[:, 1:16:2, :].unsqueeze(2).to_broadcast([128, 8, 16, 16]), ALU.add, [v16], [cand])
            tmpB = sc[:].rearrange("p (h a) b -> p h (a b)", a=2)
            for hd in range(8):
                p.op("dve", lambda e, hd=hd: e.max(out=tv[:, hd, 0:8], in_=cand[:, hd, :]), [cand], [(tv, hd)])
            for hd in range(8):
                p.op("dve", lambda e, hd=hd, tmpB=tmpB: e.match_replace(out=tmpB[:, hd, :], in_to_replace=tv[:, hd, 0:8], in_values=cand[:, hd, :], imm_value=-1e30), [cand, (tv, hd)], [(sc, 2 * hd), (sc, 2 * hd + 1)])
            for hd in range(8):
                p.op("dve", lambda e, hd=hd, tmpB=tmpB: e.max(out=tv[:, hd, 8:16], in_=tmpB[:, hd, :]), [(sc, 2 * hd), (sc, 2 * hd + 1)], [(tv, hd)])
            for hd in range(8):
                p.op("dve", lambda e, hd=hd: e.max_index(out=posu[:, hd, 0:8], in_max=tv[:, hd, 0:8], in_values=cand[:, hd, :]), [cand, (tv, hd)], [(posu, hd)])
            for hd in range(8):
                p.op("dve", lambda e, hd=hd: e.max_index(out=posu[:, hd, 8:16], in_max=tv[:, hd, 8:16], in_values=cand[:, hd, :]), [cand, (tv, hd)], [(posu, hd)])
            gt = sel[:, 2, :].rearrange("p (h k) -> p h k", h=8)
            tt("pool", gt, tv[:], tv[:, :, 0:1].to_broadcast([128, 8, 16]), ALU.subtract, [tv], [(sel, 2)])
            act(gt, gt, AF.Exp, [(sel, 2)], [(sel, 2)])
            z8 = sm()
            red(z8[:], gt, [(sel, 2)], [z8])
            p.op("dve", lambda e, z8=z8: e.reciprocal(out=z8[:], in_=z8[:]), [z8], [z8])
            tt("dve", gt, gt, z8[:].unsqueeze(2).to_broadcast([128, 8, 16]), ALU.mult, [(sel, 2), z8], [(sel, 2)])
            ts("dve", au[:], posu[:], 4, None, ALU.logical_shift_right, None, [posu], [au])
            ts("dve", bu[:], posu[:], 15, None, ALU.bitwise_and, None, [posu], [bu])
            cp("pool", af[:], au[:], [au], [af])
            cp("pool", bf_[:], bu[:], [bu], [bf_])
            io16 = iota[:, 0:16].unsqueeze(1).unsqueeze(1).to_broadcast([128, 8, 16, 16])
            eq = cand[:].rearrange("p h (a b) -> p h a b", a=16)
            for w, (xf, par) in enumerate(((af, 0), (bf_, 1))):
                tt("dve", eq, io16, xf[:].unsqueeze(3).to_broadcast([128, 8, 16, 16]), ALU.is_equal, [cst, xf], [cand])
                tt("pool", eq, eq, i16f[:, par:16:2, :].unsqueeze(2).to_broadcast([128, 8, 16, 16]), ALU.mult, [cand, i16f], [cand])
                red(sel[:, w, :].rearrange("p (h k) -> p h k", h=8), eq, [cand], [(sel, w)])
            pb = PB()
            mms(lambda e, pb=pb: [e.transpose(out=pb[:, w * 128:(w + 1) * 128], in_=sel[:, w, :], identity=ident) for w in range(3)][-1], [sel, cst], [pb])
            cp("act", selT[:, :, s * 128:(s + 1) * 128], pb[:, 0:384].rearrange("p (a b) -> p a b", a=3), [pb], [(selT, s)])

    def gbuild(tix):
        selT = selTb[tix % 2]
        PB = PBG
        NG = 0 if "G" in SKIP else TT // 16
        bufs = {}

        def onehots(g):
            tk = g * 16
            oa, ob = OA(), OB()
            bufs[g] = (oa, ob)
            io = iota_bf[:, :].unsqueeze(1).to_broadcast([128, 16, 128])
            if "o" in SKIP:
                return
            tt("dve", oa[:], io, selT[:, 0, tk:tk + 16].unsqueeze(2).to_broadcast([128, 16, 128]), ALU.is_equal, [iota_bf, selT], [oa])
            for t in range(16):
                act(oa[:, t, :], oa[:, t, :], AF.Copy, [(oa, t), selT], [(oa, t)], scale=selT[:, 2, tk + t:tk + t + 1])
            tt("dve", ob[:], io, selT[:, 1, tk:tk + 16].unsqueeze(2).to_broadcast([128, 16, 128]), ALU.is_equal, [iota_bf, selT], [ob])

        def mm_evac(g):
            tk = g * 16
            oa, ob = bufs.pop(g)
            for q4 in range(4):
                pb = PB()
                if "m" not in SKIP:
                  mms(lambda e, pb=pb, q4=q4, oa=oa, ob=ob: [e.matmul(pb[:, j * 128:(j + 1) * 128], lhsT=ob[:, q4 * 4 + j, :], rhs=oa[:, q4 * 4 + j, :], start=True, stop=True) for j in range(4)][-1],
                    [oa, ob], [pb])
                tq = tk + q4 * 4
                if "v" not in SKIP:
                  cp("act" if q4 != 3 else "dve", G[:, tq:tq + 4, :], pb[:].rearrange("p (t i) -> p t i", t=4), [pb], [(G, tq)])

        if NG:
            onehots(0)
        for g in range(NG):
            if g + 1 < NG:
                onehots(g + 1)
            mm_evac(g)

    def expert(tix, nxt):
        xT = xTb[tix % 2]
        LOOK = 2
        grp = {}
        hb = {}

        def emit_H(i):
            ig, ii = divmod(i, 4)
            if ii == 0:
                u2, vt = U2(), Vt()
                grp[ig] = (u2, vt)
                if not ("D" in SKIP and ig >= 2):
                    dma("sp", vt[:], D["vb"][ig * 512:(ig + 1) * 512, :].rearrange("(i p) d -> p i d", p=128), [(D["vb_t"], ig)], [vt])
                    dma("sp", u2[:].rearrange("p i dc e -> p (i dc e)"), D["u2"][:, ig * 4:(ig + 1) * 4, :, :].rearrange("p i dc e -> p (i dc e)"), [(D["u2_t"], ig)], [u2])
            u2, vt = grp[ig]
            pb = PBH()
            mms(lambda e, pb=pb, u2=u2, ii=ii: [e.matmul(pb[:, 0:TT], lhsT=u2[:, ii, dc, :], rhs=xT[:, dc, :], start=(dc == 0), stop=(dc == 7)) for dc in range(8)][-1],
                [u2, xT], [pb])
            hb[i] = pb

        def emit_rest(i):
            ig, ii = divmod(i, 4)
            u2, vt = grp[ig]
            pb = hb.pop(i)
            g_, a_ = gh(), ac()
            if "X" not in SKIP:
                act(g_[:], pb[:, 0:TT], AF.Gelu, [pb], [g_])
                tt("dve", a_[:], g_[:], G[:, :, i], ALU.mult, [g_, G], [a_])
            mms(lambda e, a_=a_, vt=vt, ii=ii, i=i: [e.matmul(ACC[s * 2 + hf][:], lhsT=a_[:, s * 128:(s + 1) * 128], rhs=vt[:, ii, hf * 512:(hf + 1) * 512], start=(i == 0), stop=(i == 127))
                                                   for s in range(2) for hf in range(2)][-1], [a_, vt], ACC)

        NE = 0 if "E" in SKIP else 128
        per = (len(nxt) + 99) // 100 if nxt else 0
        for i in range(min(LOOK, NE)):
            emit_H(i)
        for i in range(NE):
            if i + LOOK < NE:
                emit_H(i + LOOK)
            emit_rest(i)
            if nxt:
                p.drain(nxt, per)
        if nxt:
            p.drain(nxt, len(nxt))

    def epilogue(tix):
        b = tix % 2
        t0 = tix * TT
        for s in range(2):
            hh1 = h1[b][s]
            for hf in range(2):
                tt("dve", hh1[:, hf * 512:(hf + 1) * 512], ACC[s * 2 + hf][:], hh1[:, hf * 512:(hf + 1) * 512], ALU.add, [ACC[s * 2 + hf], hh1], [hh1])
            act(xs[:], hh1[:], AF.Square, [hh1], [xs, ss], accum_out=ss[:])
            act(rstd[:], ss[:], AF.Sqrt, [ss], [rstd], bias=1e-5, scale=1.0 / 1024)
            p.op("dve", lambda e: e.reciprocal(out=rstd[:], in_=rstd[:]), [rstd], [rstd])
            stt("dve", hh1[:], hh1[:], rstd[:, 0:1], fin[:], ALU.mult, ALU.mult, [hh1, rstd, fin], [hh1])
            dma("act", D["out"][t0 + s * 128:t0 + (s + 1) * 128, :], hh1[:], [hh1], [])

    prep(0)
    for tix in range(ntile):
        gbuild(tix)
        nxt = []
        if tix + 1 < ntile:
            p.defer_begin()
            prep(tix + 1)
            nxt = p.defer_end()
        expert(tix, nxt)
        epilogue(tix)


N_CORES = 8


def _build(n_seq, n_tiles, ret_d=False, phases="0123"):
    nc = bass.Bass("TRN2", target_bir_lowering=False, dynamic_dma_scratch_size=2048)
    ntok = n_seq * n_tiles * 128
    D = {}

    def din(name, shape):
        D[name] = nc.dram_tensor(name, list(shape), F32, kind="ExternalInput").ap()

    din("x", (ntok, 1024)); din("w_in", (1024, 4640)); din("vecs", (NVEC,)); din("nwp", (128, 16)); din("cst", (128, NCST))
    din("decay_w2", (64, 512)); din("iclr_a2", (64, 512)); din("gate_g2", (160, 512))
    din("proj_attn", (512, 1024)); din("proj_rwkv", (512, 1024)); din("w_out", (1024, 1024))
    din("peer_wq", (1024, 1024)); din("skT", (128, 8, 128)); din("uT", (1024, 16384)); din("v", (16384, 1024))
    D["y_scr"] = nc.dram_tensor("y_scr", [ntok, 1024], F32, kind="Internal").ap()
    D["h1_scr"] = D["y_scr"]
    NA = 2 * 16384 * 1024
    arena = nc.dram_tensor("arena", [NA], BF16, kind="Internal").ap()
    assert ntok * 1824 * 2 <= NA
    D["zr_scr"] = arena[0:ntok * 1824 * 2].bitcast(F32).rearrange("(t c) -> t c", c=1824)
    D["u2"] = arena[0:16384 * 1024].rearrange("(p i dc e) -> p i dc e", p=128, i=128, dc=8)
    D["vb"] = arena[16384 * 1024:NA].rearrange("(r c) -> r c", c=1024)
    D["out"] = nc.dram_tensor("out", [ntok, 1024], F32, kind="ExternalOutput").ap()
    p = Prog(nc)
    for nm in ("y_t", "zr_t", "h1_t", "u2_t", "vb_t"):
        D[nm] = p.wrap(None, nm)
    m = p.mark()
    if "1" in phases or "a" in phases:
        phase_a1a(p, nc, D, n_seq, n_tiles, n_tiles * 128)
        p.release(m)
    if "1" in phases or "b" in phases:
        phase_a1b(p, nc, D, n_seq, n_tiles, n_tiles * 128)
        p.release(m)
    if "0" in phases:
        phase_b0(p, nc, D, stq="sp")
        p.release(m)
    if "2" in phases:
        phase_a2(p, nc, D, ntok, final=("3" not in phases))
        p.release(m)
    if "3" in phases:
        phase_b(p, nc, D, ntok)
    p.emit()
    p.close()
    return (nc, D) if ret_d else nc


def _inputs(x, norm_mix_w, w_in, shift_mu, attn_sinks, decay_w0, decay_w2, iclr_a0, iclr_a2, gate_g2, k_k, k_a, r_k,
            ln_x_w, ln_x_b, proj_attn, proj_rwkv, w_out, norm_ffn_w, peer_wq, peer_subkeys, peer_u, peer_v, norm_final_w):
    f = lambda a: np.ascontiguousarray(np.asarray(a, dtype=np.float32))
    v = np.zeros(NVEC, np.float32)
    v[V_MU:V_MU + 1824] = f(shift_mu)[0]; v[V_W0:V_W0 + 512] = f(decay_w0)[0]; v[V_A0:V_A0 + 512] = f(iclr_a0)[0]
    v[V_KK:V_KK + 512] = f(k_k)[0]; v[V_KA:V_KA + 512] = f(k_a)[0]; v[V_LNW:V_LNW + 512] = f(ln_x_w)[0]
    v[V_LNB:V_LNB + 512] = f(ln_x_b)[0]; v[V_RK:V_RK + 512] = f(r_k)[0].reshape(-1); v[V_SINK:V_SINK + 8] = f(attn_sinks)[0]
    v[V_FIN:] = f(norm_final_w)
    nwp = np.ones((128, 16), np.float32)
    nwp[:, 0:8] = f(norm_mix_w)[0].reshape(8, 128).T
    nwp[:, 8:16] = f(norm_ffn_w)[0].reshape(8, 128).T
    sk = f(peer_subkeys)[0]
    skT = np.ascontiguousarray(sk.transpose(1, 3, 0, 2).reshape(128, 8, 128))
    return dict(w_in=f(w_in)[0], vecs=v, nwp=nwp, cst=make_cst(), decay_w2=f(decay_w2)[0], iclr_a2=f(iclr_a2)[0],
                gate_g2=f(gate_g2)[0], proj_attn=f(proj_attn)[0], proj_rwkv=f(proj_rwkv)[0], w_out=f(w_out)[0],
                peer_wq=f(peer_wq)[0], skT=skT, uT=np.ascontiguousarray(f(peer_u)[0].T), v=f(peer_v)[0])


def kernel(**inputs):
    x = np.ascontiguousarray(np.asarray(inputs["x"], dtype=np.float32))
    B, S, Dm = x.shape
    common = _inputs(**inputs)
    spc = B // N_CORES
    nc = _build(spc, S // 128)
    in_maps = []
    for c in range(N_CORES):
        d = dict(common)
        d["x"] = np.ascontiguousarray(x[c * spc:(c + 1) * spc].reshape(spc * S, Dm))
        in_maps.append(d)
    res = run_bass_kernel_spmd(nc, in_maps, core_ids=list(range(N_CORES)))
    out = np.concatenate([r["out"].reshape(spc, S, Dm) for r in res.results], axis=0)
    return out.astype(np.float32)
```

```python
import numpy as np
import concourse.bass as bass
import concourse.mybir as mybir

F32 = mybir.dt.float32
BF16 = mybir.dt.bfloat16
U32 = mybir.dt.uint32
I32 = mybir.dt.int32
ALU = mybir.AluOpType
AF = mybir.ActivationFunctionType
AX = mybir.AxisListType

NDMA_SEMS = 12
import os as _os
NOSELF = tuple(_os.environ.get("NOSELF", "").split(","))


class T:
    def __init__(self, h, name):
        self.h = h
        self.name = name
        self.state = {}

    def __getitem__(self, k):
        return self.h[k]


class Prog:
    def __init__(self, nc):
        self.nc = nc
        self.ops = {e: [] for e in ("pe", "dve", "act", "pool", "sp")}
        self.cms = []
        self.ndma = {e: 0 for e in ("sp", "act", "pool")}
        self._defer = None

    def sb(self, name, shape, dtype):
        self._uid = getattr(self, "_uid", 0) + 1
        cm = self.nc.sbuf_tensor("sb%d_" % self._uid + name, list(shape), dtype)
        h = cm.__enter__()
        self.cms.append(cm)
        return T(h, name)

    def ps(self, name, shape, dtype):
        self._uid = getattr(self, "_uid", 0) + 1
        cm = self.nc.psum_tensor("ps%d_" % self._uid + name, list(shape), dtype)
        h = cm.__enter__()
        self.cms.append(cm)
        return T(h, name)

    def wrap(self, ap, name):
        return T(ap, name)

    def defer_begin(self):
        self._defer = []

    def defer_end(self):
        lst, self._defer = self._defer, None
        return lst

    def drain(self, lst, k):
        for _ in range(min(k, len(lst))):
            eng, fn, reads, writes, dma, extra = lst.pop(0)
            self.op(eng, fn, reads, writes, dma, extra)

    def mark(self):
        return len(self.cms)

    def release(self, mark):
        lasts = []
        for e in ("pe", "dve", "act", "pool", "sp"):
            for j in range(len(self.ops[e]) - 1, -1, -1):
                if self.ops[e][j][2][0] not in ("dma", "bar"):
                    lasts.append((e, j))
                    break
        dmat = []
        for q in ("sp", "act", "pool"):
            n = self.ndma[q]
            dmat += [("dma", q, i) for i in range(max(0, n - NDMA_SEMS), n)]
        for e in ("pe", "dve", "act", "pool", "sp"):
            self.ops[e].append((None, lasts + dmat, ("bar", e, len(self.ops[e]))))
        while len(self.cms) > mark:
            self.cms.pop().__exit__(None, None, None)

    def _collect(self, t, key, is_write, deps):
        if key is None:
            keys = list(t.state.keys())
        else:
            keys = [k for k in (key, None) if k in t.state]
        for k in keys:
            w, rs = t.state[k]
            if w is not None:
                deps.append(w)
            if is_write:
                deps.extend(rs)

    def _update(self, t, key, is_write, me):
        if is_write:
            if key is None:
                t.state = {None: [me, []]}
            else:
                t.state[key] = [me, []]
        else:
            st = t.state.setdefault(key, [None, []])
            if me[0] != "dma":
                st[1] = [r for r in st[1] if not (r[0] == me[0])]
            st[1].append(me)

    def op(self, eng, fn, reads=(), writes=(), dma=False, extra=()):
        if self._defer is not None:
            self._defer.append((eng, fn, reads, writes, dma, extra))
            return None
        deps = list(extra)
        norm = lambda x: x if isinstance(x, tuple) else (x, None)
        reads = [norm(r) for r in reads]
        writes = [norm(w) for w in writes]
        for t, k in reads:
            self._collect(t, k, False, deps)
        for t, k in writes:
            self._collect(t, k, True, deps)
        idx = len(self.ops[eng])
        if dma:
            n = self.ndma[eng]
            self.ndma[eng] += 1
            me = ("dma", eng, n)
        else:
            me = (eng, idx)
        for t, k in reads:
            self._update(t, k, False, me)
        for t, k in writes:
            self._update(t, k, True, me)
        self.ops[eng].append((fn, deps, me))
        return me

    def emit(self):
        nc = self.nc
        engs = ("pe", "dve", "act", "pool", "sp")
        sem_cms = {}
        sems = {}
        for e in engs:
            cm = nc.semaphore("s_" + e)
            sems[e] = cm.__enter__()
            self.cms.append(cm)
        dsems = {}
        for e in ("sp", "act", "pool"):
            if self.ndma[e]:
                lst = []
                for i in range(NDMA_SEMS):
                    cm = nc.semaphore("d_%s_%d" % (e, i))
                    lst.append(cm.__enter__())
                    self.cms.append(cm)
                dsems[e] = lst
        ops = self.ops

        def run(ename, engine):
            seen = {}
            cnt = 0
            for fn, deps, me in ops[ename]:
                need = {}
                for d in deps:
                    if d[0] == "dma":
                        s = dsems[d[1]][d[2] % NDMA_SEMS]
                        v = 16 * (d[2] // NDMA_SEMS + 1)
                    else:
                        if d[0] == ename:
                            if ename == "pe" or fn is None or ename in NOSELF:
                                continue
                        s = sems[d[0]]
                        v = d[1] + 1 - self.dma_before[d[0]][d[1]]
                    key = s.num if hasattr(s, "num") else id(s)
                    if v > need.get(key, (None, 0))[1]:
                        need[key] = (s, v)
                if me[0] == "dma":
                    n = me[2]
                    s = dsems[ename][n % NDMA_SEMS]
                    if n >= NDMA_SEMS:
                        key = s.num if hasattr(s, "num") else id(s)
                        v = 16 * (n // NDMA_SEMS)
                        if v > need.get(key, (None, 0))[1]:
                            need[key] = (s, v)
                for key, (s, v) in need.items():
                    if seen.get(key, 0) >= v:
                        continue
                    engine.wait_ge(s, v)
                    seen[key] = v
                if fn is None:
                    continue
                ins = fn(engine)
                if me[0] == "dma":
                    ins.then_inc(dsems[ename][me[2] % NDMA_SEMS], 16)
                else:
                    ins.then_inc(sems[ename], 1)

        self.dma_before = {}
        for e in engs:
            c = 0
            lst = []
            for fn, deps, me in ops[e]:
                lst.append(c)
                if me[0] in ("dma", "bar"):
                    c += 1
            self.dma_before[e] = lst

        with nc.Block() as block:
            @block.tensor
            def _(eng):
                run("pe", eng)

            @block.vector
            def _(eng):
                run("dve", eng)

            @block.scalar
            def _(eng):
                run("act", eng)
                self._drain("act", eng, dsems)

            @block.gpsimd
            def _(eng):
                run("pool", eng)
                self._drain("pool", eng, dsems)

            @block.sync
            def _(eng):
                run("sp", eng)
                self._drain("sp", eng, dsems)

    def _drain(self, e, eng, dsems):
        n = self.ndma[e]
        if not n:
            return
        for j in range(min(n, NDMA_SEMS)):
            last = ((n - 1 - j) // NDMA_SEMS) * NDMA_SEMS + j
            eng.wait_ge(dsems[e][j], 16 * (last // NDMA_SEMS + 1))

    def close(self):
        for cm in reversed(self.cms):
            cm.__exit__(None, None, None)
from concourse.bass_utils import run_bass_kernel_spmd

D_MODEL = 1024
C_DEC = 0.6065306597126334
STAGE = 0
SKIP = ""
NZRB = 2
NSB = 1
V_MU, V_W0, V_A0, V_KK, V_KA, V_LNW, V_LNB, V_RK, V_SINK, V_FIN = 0, 1824, 2336, 2848, 3360, 3872, 4384, 4896, 5408, 5416
NVEC = 5416 + 1024
C_ID, C_SU, C_IU, C_SL, C_BD, C_SH, C_CA, C_COS, C_SIN, C_ONE, C_IOTA = 0, 128, 256, 384, 512, 640, 768, 896, 1408, 1920, 1921
NCST = 1921 + 128


def make_cst():
    c = np.zeros((128, NCST), np.float32)
    i = np.arange(128)
    r, q = i[:, None], i[None, :]
    c[:, C_ID:C_ID + 128] = (r == q)
    c[:, C_SU:C_SU + 128] = (r < q)
    c[:, C_IU:C_IU + 128] = (r <= q)
    c[:, C_SL:C_SL + 128] = (r > q)
    c[:, C_BD:C_BD + 128] = ((r // 64) == (q // 64))
    c[:, C_SH:C_SH + 128] = (r == q - 1)
    c[127, C_CA] = 1.0
    inv = 10000.0 ** (-np.arange(0, 64, 2, dtype=np.float32) / 64)
    pos = (np.arange(16)[None, :] * 128 + i[:, None]).astype(np.float32)
    ang = pos[:, :, None] * inv[None, None, :]
    c[:, C_COS:C_COS + 512] = np.cos(ang).reshape(128, 512)
    c[:, C_SIN:C_SIN + 512] = np.sin(ang).reshape(128, 512)
    c[:, C_ONE] = 1.0
    c[:, C_IOTA:C_IOTA + 128] = q
    return c


class Ctx:
    pass


def helpers(p):
    h = Ctx()

    def tt(eng, out, in0, in1, op, r, w):
        p.op(eng, lambda e: e.tensor_tensor(out=out, in0=in0, in1=in1, op=op), r, w)

    def stt(eng, out, in0, scalar, in1, op0, op1, r, w):
        p.op(eng, lambda e: e.scalar_tensor_tensor(out=out, in0=in0, scalar=scalar, in1=in1, op0=op0, op1=op1), r, w)

    def ts(eng, out, in0, s1, s2, op0, op1, r, w):
        if s2 is None:
            p.op(eng, lambda e: e.tensor_scalar(out=out, in0=in0, scalar1=s1, scalar2=None, op0=op0), r, w)
        else:
            p.op(eng, lambda e: e.tensor_scalar(out=out, in0=in0, scalar1=s1, scalar2=s2, op0=op0, op1=op1), r, w)

    def act(out, in_, func, r, w, **kw):
        p.op("act", lambda e: e.activation(out=out, in_=in_, func=func, **kw), r, w)

    def cp(eng, out, in_, r, w):
        if eng == "act":
            p.op("act", lambda e: e.activation(out=out, in_=in_, func=AF.Copy), r, w)
        else:
            p.op(eng, lambda e: e.tensor_copy(out=out, in_=in_), r, w)

    def red(out, in_, r, w, op=ALU.add):
        p.op("dve", lambda e: e.tensor_reduce(out=out, in_=in_, axis=AX.X, op=op), r, w)

    def dma(eng, out, in_, r, w):
        p.op(eng, lambda e: e.dma_start(out=out, in_=in_), r, w, dma=True)

    def mms(fn, r, w):
        p.op("pe", fn, r, w)

    h.tt, h.stt, h.ts, h.act, h.cp, h.red, h.dma, h.mms = tt, stt, ts, act, cp, red, dma, mms
    return h


class Rot:
    def __init__(self, tiles):
        self.tiles = tiles
        self.i = 0

    def __call__(self):
        t = self.tiles[self.i % len(self.tiles)]
        self.i += 1
        return t


def load_w_bf16(p, h, name, dram, K, N, ncol0, ncols, stg, scale_t=None, scale_off=0, dt=BF16):
    kc = K // 128
    wt = p.sb(name, [128, kc, ncols], dt)
    for c in range(kc):
        s = stg()
        h.dma("sp", s[:, 0:ncols], dram[c * 128:(c + 1) * 128, ncol0:ncol0 + ncols], [], [s])
        if scale_t is not None:
            h.act(wt[:, c, :], s[:, 0:ncols], AF.Copy, [s, scale_t], [(wt, c)], scale=scale_t[:, scale_off + c:scale_off + c + 1])
        else:
            h.cp("pool", wt[:, c, :], s[:, 0:ncols], [s], [(wt, c)])
    return wt


def phase_a1(p, nc, D, n_seq, n_tiles, S_TOK):
    h = helpers(p)
    tt, stt, ts, act, cp, red, dma, mms = h.tt, h.stt, h.ts, h.act, h.cp, h.red, h.dma, h.mms
    NZ = 2592
    cst = p.sb("cst", [128, NCST], F32)
    dma("sp", cst[:], D["cst"], [], [cst])
    vec = p.sb("vec", [128, V_SINK + 8], F32)
    dma("sp", vec[:], D["vecs"][0:V_SINK + 8].partition_broadcast(128), [], [vec])
    nwp = p.sb("nwp", [128, 16], F32)
    dma("sp", nwp[:], D["nwp"], [], [nwp])
    ident = cst[:, C_ID:C_ID + 128]
    z = p.sb("z", [128, NZ], F32)
    Wb = load_w_bf16(p, h, "Wb1", D["w_in"], 1024, 4640, 0, NZ, (lambda: z), nwp, 0)
    w2 = p.sb("w2", [128, 512], F32)
    dma("sp", w2[0:64, :], D["decay_w2"], [], [w2])
    dma("sp", w2[64:128, :], D["iclr_a2"], [], [w2])
    g2 = p.sb("g2", [128, 2, 512], F32)
    dma("sp", g2[:, 0, :], D["gate_g2"][0:128, :], [], [g2])
    dma("sp", g2[0:32, 1, :], D["gate_g2"][128:160, :], [], [g2])
    negsink = p.sb("negsink", [128, 8], F32)
    ts("dve", negsink[:], vec[:, V_SINK:V_SINK + 8], -1.0, None, ALU.mult, None, [vec], [negsink])

    xt = Rot([p.sb("xt%d" % i, [128, 1024], F32) for i in range(2)])
    ss = p.sb("ss", [128, 1], F32)
    rstd = p.sb("rstd", [128, 1], F32)
    xs = p.sb("xs", [128, 1024], F32)
    xsT = p.sb("xsT", [128, 8, 128], BF16)
    carry = p.sb("carry", [128, 1824], F32)
    p.op("pool", lambda e: e.memset(carry[:], 0.0), [], [carry])
    zr = p.sb("zr", [128, 1824], F32)
    lin = p.sb("lin", [128, 288], F32)
    linT = p.sb("linT", [128, 3, 128], F32)
    T5 = Rot([p.sb("t5_%d" % i, [128, 512], F32) for i in range(12)])
    gv = p.sb("gv", [128, 512], F32)
    k2 = p.sb("k2", [128, 512], F32)
    M8 = Rot([p.sb("m8_%d" % i, [128, 8, 128], F32) for i in range(8)])
    M8d = [p.sb("m8d_%d" % i, [128, 8, 128], F32) for i in range(3)]
    Tq = Rot([p.sb("tq_%d" % i, [128, 4, 128], F32) for i in range(4)])
    sm = Rot([p.sb("sm_%d" % i, [128, 8], F32) for i in range(12)])
    PB = Rot([p.ps("pb%d" % i, [128, 512], F32) for i in range(6)])
    PV = [p.ps("pv%d" % i, [128, 512], F32) for i in range(2)]
    NDUM = 0
    if NDUM:
        dumb = p.ps("dumb", [128, 512], F32)
        mms0 = mms

        def mms(fn, r, w):
            def fn2(e):
                ins = fn(e)
                for _ in range(NDUM):
                    e.matmul(dumb[:], lhsT=Wb[:, 0, 0:128], rhs=Wb[:, 0, 0:512], start=True, stop=True)
                return ins
            mms0(fn2, r, w)
    ST = p.sb("ST", [128, 4, 128], F32)
    gC = p.sb("gC", [128, 4], F32)
    qk = p.sb("qk", [128, 10, 64], F32)
    kdup = p.sb("kdup", [128, 2, 2, 64], F32)
    qT = p.sb("qT", [128, 4, 128], BF16)
    kT = [p.sb("kT%d" % i, [128, 2, 128], BF16) for i in range(2)]
    va = [p.sb("va%d" % i, [128, 2, 65], BF16) for i in range(2)]
    for i in range(2):
        p.op("pool", lambda e, i=i: e.memset(va[i][:], 1.0), [], [va[i]])
    pT = Rot([p.sb("pT%d" % i, [128, 2, 128], BF16) for i in range(4)])
    yout = Rot([p.sb("yout%d" % i, [128, 1024], F32) for i in range(1)])

    def vb(off, n=512):
        return vec[:, off:off + n]

    order = [(b, n) for b in range(n_seq) for n in range(n_tiles)]
    xq = {}

    def prefetch(i):
        if i < len(order):
            b, n = order[i]
            t0 = b * S_TOK + n * 128
            xq[i] = xt()
            dma("sp", xq[i][:], D["x"][t0:t0 + 128, :], [], [xq[i]])

    prefetch(0)

    def tile_body(i):
            b, n = order[i]
            if n == 0:
                p.op("pool", lambda e: e.memset(ST[:], 0.0), [], [ST])
            tok0 = b * S_TOK + n * 128
            first = (n == 0)
            x_t = xq.pop(i)
            prefetch(i + 1)
            act(xs[:], x_t[:], AF.Square, [x_t], [xs, ss], accum_out=ss[:])
            act(rstd[:], ss[:], AF.Sqrt, [ss], [rstd], bias=1e-5, scale=1.0 / 1024)
            p.op("dve", lambda e: e.reciprocal(out=rstd[:], in_=rstd[:]), [rstd], [rstd])
            ts("dve", xs[:], x_t[:], rstd[:, 0:1], None, ALU.mult, None, [x_t, rstd], [xs])
            for hh in range(2):
                pb = PB()
                mms(lambda e, pb=pb, hh=hh: [e.transpose(out=pb[:, j * 128:(j + 1) * 128], in_=xs[:, (hh * 4 + j) * 128:(hh * 4 + j + 1) * 128], identity=ident) for j in range(4)][-1],
                    [xs, cst], [pb])
                cp("act" if hh else "dve", xsT[:, hh * 4:hh * 4 + 4, :], pb[:].rearrange("p (a b) -> p a b", a=4), [pb], [(xsT, hh)])
            for cc in range(6):
                c0 = cc * 512
                cw = min(512, NZ - c0)
                pb = PB()
                mms(lambda e, pb=pb, c0=c0, cw=cw: [e.matmul(pb[:, 0:cw], lhsT=xsT[:, c, :], rhs=Wb[:, c, c0:c0 + cw], start=(c == 0), stop=(c == 7)) for c in range(8)][-1],
                    [xsT, Wb], [pb])
                cp("act" if cc % 2 else "dve", z[:, c0:c0 + cw], pb[:, 0:cw], [pb], [(z, cc)])
            if STAGE == 1:
                yo = yout()
                cp("dve", yo[:], z[:, 0:1024], [z], [yo])
                dma("sp", D["y_scr"][tok0:tok0 + 128, :], yo[:], [yo], [])
                return
            cosb = cst[:, C_COS + n * 32:C_COS + n * 32 + 32].unsqueeze(1).to_broadcast([128, 10, 32])
            sinb = cst[:, C_SIN + n * 32:C_SIN + n * 32 + 32].unsqueeze(1).to_broadcast([128, 10, 32])
            zq = z[:, 0:640].rearrange("p (h d) -> p h d", h=10)
            x1, x2 = zq[:, :, 0:32], zq[:, :, 32:64]
            ta, tb_ = T5(), T5()
            tav = ta[:, 0:320].rearrange("p (h d) -> p h d", h=10)
            tbv = tb_[:, 0:320].rearrange("p (h d) -> p h d", h=10)
            zk = [(z, 0), (z, 1)]
            tt("dve", tav, x1, cosb, ALU.mult, zk + [cst], [ta])
            tt("pool", tbv, x2, sinb, ALU.mult, zk + [cst], [tb_])
            tt("dve", qk[:, :, 0:32], tav, tbv, ALU.subtract, [ta, tb_], [(qk, 0)])
            tc_, td = T5(), T5()
            tcv = tc_[:, 0:320].rearrange("p (h d) -> p h d", h=10)
            tdv = td[:, 0:320].rearrange("p (h d) -> p h d", h=10)
            tt("pool", tcv, x2, cosb, ALU.mult, zk + [cst], [tc_])
            tt("dve", tdv, x1, sinb, ALU.mult, zk + [cst], [td])
            tt("pool", qk[:, :, 32:64], tcv, tdv, ALU.add, [tc_, td], [(qk, 1)])
            cp("pool", kdup[:], qk[:, 8:10, :].unsqueeze(2).to_broadcast([128, 2, 2, 64]), [qk], [kdup])
            kTc, kTp = kT[n % 2], kT[(n + 1) % 2]
            vac, vap = va[n % 2], va[(n + 1) % 2]
            cp("pool", vac[:, :, 0:64], z[:, 640:768].rearrange("p (g d) -> p g d", g=2), [(z, 1)], [vac])
            pb = PB()
            mms(lambda e, pb=pb: [e.transpose(out=pb[:, j * 128:(j + 1) * 128], in_=qk[:, 2 * j:2 * j + 2, :].rearrange("p a d -> p (a d)"), identity=ident) for j in range(4)][-1],
                [qk, cst], [pb])
            cp("act", qT[:], pb[:].rearrange("p (a b) -> p a b", a=4), [pb], [qT])
            pb = PB()
            mms(lambda e, pb=pb: [e.transpose(out=pb[:, g * 128:(g + 1) * 128], in_=kdup[:, g, :, :].rearrange("p a d -> p (a d)"), identity=ident) for g in range(2)][-1],
                [kdup, cst], [pb])
            cp("dve", kTc[:], pb[:, 0:256].rearrange("p (a b) -> p a b", a=2), [pb], [kTc])
            yo = yout()
            pv = PV
            for hd in range(0 if "a" in SKIP else 8):
                m, base, g = hd // 2, 64 * (hd % 2), hd // 4
                pb = PB()
                if first:
                    mms(lambda e, pb=pb, m=m, base=base, g=g: e.matmul(pb[:, 128:256], lhsT=kTc[base:base + 64, g, :], rhs=qT[base:base + 64, m, :], start=True, stop=True),
                        [kTc, qT], [pb])
                else:
                    mms(lambda e, pb=pb, m=m, base=base, g=g: [e.matmul(pb[:, 0:128], lhsT=kTp[base:base + 64, g, :], rhs=qT[base:base + 64, m, :], start=True, stop=True),
                                                              e.matmul(pb[:, 128:256], lhsT=kTc[base:base + 64, g, :], rhs=qT[base:base + 64, m, :], start=True, stop=True)][-1],
                        [kTc, kTp, qT], [pb])
                pt = pT()
                lo = 1 if first else 0
                act(pt[:, lo:2, :], pb[:, lo * 128:256].rearrange("p (a b) -> p a b", a=2 - lo), AF.Exp, [pb, negsink], [pt],
                    scale=0.125, bias=negsink[:, hd:hd + 1])
                if not first:
                    tt("pool", pt[:, 0, :], pt[:, 0, :], cst[:, C_SL:C_SL + 128], ALU.mult, [pt, cst], [pt])
                tt("dve", pt[:, 1, :], pt[:, 1, :], cst[:, C_IU:C_IU + 128], ALU.mult, [pt, cst], [pt])
                pvb = pv[hd // 4]
                o0 = (hd % 4) * 65
                if first:
                    mms(lambda e, pvb=pvb, pt=pt, g=g, o0=o0: e.matmul(pvb[:, o0:o0 + 65], lhsT=pt[:, 1, :], rhs=vac[:, g, :], start=True, stop=True),
                        [pt, vac], [pvb])
                else:
                    mms(lambda e, pvb=pvb, pt=pt, g=g, o0=o0: [e.matmul(pvb[:, o0:o0 + 65], lhsT=pt[:, 0, :], rhs=vap[:, g, :], start=True, stop=False),
                                                              e.matmul(pvb[:, o0:o0 + 65], lhsT=pt[:, 1, :], rhs=vac[:, g, :], start=False, stop=True)][-1],
                        [pt, vac, vap], [pvb])
            for hf in range(2):
                pvb = pv[hf]
                pvv = pvb[:, 0:260].rearrange("p (h d) -> p h d", h=4)
                den = sm()
                ts("dve", den[:, 0:4], pvv[:, :, 64], 1.0, None, ALU.add, None, [pvb], [den])
                p.op("dve", lambda e, den=den: e.reciprocal(out=den[:, 0:4], in_=den[:, 0:4]), [den], [den])
                tt("dve", yo[:, hf * 256:(hf + 1) * 256].rearrange("p (h d) -> p h d", h=4), pvv[:, :, 0:64],
                   den[:, 0:4].unsqueeze(2).to_broadcast([128, 4, 64]), ALU.mult, [pvb, den], [(yo, hf)])
            if STAGE == 2:
                cp("dve", yo[:, 512:1024], z[:, 0:512], [z], [yo])
                dma("sp", D["y_scr"][tok0:tok0 + 128, :], yo[:], [yo], [])
                return
            if "r" in SKIP:
                dma("sp", D["y_scr"][tok0:tok0 + 128, :], yo[:], [yo], [(D["y_t"], tok0 // 128)])
                return
            for j in range(4):
                c0 = 768 + j * 512
                cw = min(512, NZ - c0)
                pb = PB()
                zkeys = [(z, 1), (z, 2), (z, 3), (z, 4), (z, 5)]
                if first:
                    mms(lambda e, pb=pb, c0=c0, cw=cw: e.matmul(pb[:, 0:cw], lhsT=cst[:, C_SH:C_SH + 128], rhs=z[:, c0:c0 + cw], start=True, stop=True),
                        zkeys + [cst], [pb])
                else:
                    mms(lambda e, pb=pb, c0=c0, cw=cw: [e.matmul(pb[:, 0:cw], lhsT=cst[:, C_SH:C_SH + 128], rhs=z[:, c0:c0 + cw], start=True, stop=False),
                                                       e.matmul(pb[:, 0:cw], lhsT=cst[:, C_CA:C_CA + 128], rhs=carry[:, c0 - 768:c0 - 768 + cw], start=False, stop=True)][-1],
                        zkeys + [cst, carry], [pb])
                r0 = c0 - 768
                tt("dve", zr[:, r0:r0 + cw], pb[:, 0:cw], z[:, c0:c0 + cw], ALU.subtract, [pb] + zkeys, [(zr, j)])
                tt("dve", zr[:, r0:r0 + cw], zr[:, r0:r0 + cw], vec[:, V_MU + r0:V_MU + r0 + cw], ALU.mult, [(zr, j), vec], [(zr, j)])
                tt("pool", zr[:, r0:r0 + cw], zr[:, r0:r0 + cw], z[:, c0:c0 + cw], ALU.add, [(zr, j)] + zkeys, [(zr, j)])
            cp("pool", carry[96:128, :], z[96:128, 768:NZ], [z], [carry])
            r_, k_, v_ = zr[:, 0:512], zr[:, 512:1024], zr[:, 1024:1536]
            if STAGE == 3:
                cp("dve", yo[:, 512:1024], zr[:, 0:512], [], [yo])
                dma("sp", D["y_scr"][tok0:tok0 + 128, :], yo[:], [yo], [])
                return
            act(lin[:, 0:64], zr[:, 1536:1600], AF.Tanh, [zr], [(lin, 0)])
            cp("pool", lin[:, 64:128], zr[:, 1600:1664], [zr], [(lin, 1)])
            act(lin[:, 128:288], zr[:, 1664:1824], AF.Sigmoid, [zr], [(lin, 2)])
            pb = PB()
            mms(lambda e, pb=pb: [e.transpose(out=pb[:, 0:128], in_=lin[:, 0:128], identity=ident),
                                  e.transpose(out=pb[:, 128:256], in_=lin[:, 128:256], identity=ident),
                                  e.transpose(out=pb[0:32, 256:384], in_=lin[:, 256:288], identity=ident)][-1], [lin, cst], [pb])
            cp("dve", linT[:, 0:2, :], pb[:, 0:256].rearrange("p (a b) -> p a b", a=2), [pb], [(linT, 0)])
            cp("dve", linT[0:32, 2, :], pb[0:32, 256:384], [pb], [(linT, 1)])
            pw, pa_, pg = PB(), PB(), PB()
            mms(lambda e, pw=pw: e.matmul(pw[:], lhsT=linT[0:64, 0, :], rhs=w2[0:64, :], start=True, stop=True), [linT, w2], [pw])
            mms(lambda e, pa_=pa_: e.matmul(pa_[:], lhsT=linT[64:128, 0, :], rhs=w2[64:128, :], start=True, stop=True), [linT, w2], [pa_])
            mms(lambda e, pg=pg: [e.matmul(pg[:], lhsT=linT[:, 1, :], rhs=g2[:, 0, :], start=True, stop=False),
                                  e.matmul(pg[:], lhsT=linT[0:32, 2, :], rhs=g2[0:32, 1, :], start=False, stop=True)][-1], [linT, g2], [pg])
            sg, av = T5(), T5()
            tt("dve", sg[:], pw[:], vb(V_W0), ALU.add, [pw, vec], [sg])
            act(sg[:], sg[:], AF.Sigmoid, [sg], [sg])
            tt("dve", av[:], pa_[:], vb(V_A0), ALU.add, [pa_, vec], [av])
            act(av[:], av[:], AF.Sigmoid, [av], [av])
            cp("act", gv[:], pg[:], [pg], [gv])
            if STAGE == 4:
                cp("dve", yo[:, 512:1024], sg[:], [], [yo])
                dma("sp", D["y_scr"][tok0:tok0 + 128, :], yo[:], [yo], [])
                return
            pc = PB()
            mms(lambda e, pc=pc, sg=sg: e.matmul(pc[:], lhsT=cst[:, C_IU:C_IU + 128], rhs=sg[:], start=True, stop=True), [cst, sg], [pc])
            gam, igam, gprev = T5(), T5(), T5()
            act(gam[:], pc[:], AF.Exp, [pc], [gam], scale=-C_DEC)
            act(igam[:], pc[:], AF.Exp, [pc], [igam], scale=C_DEC)
            tt("dve", gprev[:], pc[:], sg[:], ALU.subtract, [pc, sg], [gprev])
            act(gprev[:], gprev[:], AF.Exp, [gprev], [gprev], scale=-C_DEC)
            pgc = PB()
            mms(lambda e, pgc=pgc, sg=sg: [e.matmul(pgc[:, m:m + 1], lhsT=sg[:, m * 128:(m + 1) * 128], rhs=cst[:, C_ONE:C_ONE + 1], start=True, stop=True) for m in range(4)][-1],
                [cst, sg], [pgc])
            act(gC[:], pgc[:, 0:4], AF.Exp, [pgc], [gC], scale=-C_DEC)
            if STAGE == 5:
                cp("dve", yo[:, 512:1024], gprev[:], [], [yo])
                dma("sp", D["y_scr"][tok0:tok0 + 128, :], yo[:], [yo], [])
                return
            kk, sq = T5(), T5()
            tt("dve", kk[:], k_, vb(V_KK), ALU.mult, [zr, vec], [kk])
            tt("pool", sq[:], kk[:], kk[:], ALU.mult, [kk], [sq])
            if STAGE == 51:
                cp("dve", yo[:, 512:1024], sq[:], [], [yo])
                dma("sp", D["y_scr"][tok0:tok0 + 128, :], yo[:], [yo], [])
                return
            s8 = sm()
            red(s8[:], sq[:].rearrange("p (h d) -> p h d", h=8), [sq], [s8])
            if STAGE == 52:
                cp("dve", yo[:, 512:1024], sq[:], [], [yo])
                dma("sp", D["y_scr"][tok0:tok0 + 128, :], yo[:], [yo], [])
                return
            act(s8[:], s8[:], AF.Sqrt, [s8], [s8], bias=1e-24, scale=1.0)
            p.op("dve", lambda e, s8=s8: e.reciprocal(out=s8[:], in_=s8[:]), [s8], [s8])
            if STAGE == 53:
                cp("dve", yo[:, 512:1024], sq[:], [], [yo])
                dma("sp", D["y_scr"][tok0:tok0 + 128, :], yo[:], [yo], [])
                return
            tt("dve", kk[:].rearrange("p (h d) -> p h d", h=8), kk[:].rearrange("p (h d) -> p h d", h=8),
               s8[:].unsqueeze(2).to_broadcast([128, 8, 64]), ALU.mult, [kk, s8], [kk])
            if STAGE == 54:
                cp("dve", yo[:, 512:1024], kk[:], [], [yo])
                dma("sp", D["y_scr"][tok0:tok0 + 128, :], yo[:], [yo], [])
                return
            t1 = T5()
            stt("dve", t1[:], av[:], -1.0, vb(V_KA), ALU.add, ALU.mult, [av, vec], [t1])
            stt("dve", k2[:], t1[:], 1.0, k_, ALU.add, ALU.mult, [t1, zr], [k2])
            if STAGE == 55:
                cp("dve", yo[:, 512:1024], k2[:], [], [yo])
                dma("sp", D["y_scr"][tok0:tok0 + 128, :], yo[:], [yo], [])
                return
            At, Bt, Kt, Rt = T5(), T5(), T5(), T5()
            stt("dve", At[:], kk[:], -1.0, gprev[:], ALU.mult, ALU.mult, [kk, gprev], [At])
            if STAGE == 56:
                cp("dve", yo[:, 512:1024], At[:], [At], [yo])
                dma("sp", D["y_scr"][tok0:tok0 + 128, :], yo[:], [yo], [])
                return
            tt("pool", Bt[:], kk[:], av[:], ALU.mult, [kk, av], [Bt])
            tt("dve", Bt[:], Bt[:], igam[:], ALU.mult, [Bt, igam], [Bt])
            if STAGE == 57:
                cp("dve", yo[:, 512:1024], Bt[:], [Bt], [yo])
                dma("sp", D["y_scr"][tok0:tok0 + 128, :], yo[:], [yo], [])
                return
            tt("dve", Kt[:], k2[:], igam[:], ALU.mult, [k2, igam], [Kt])
            if STAGE == 58:
                cp("dve", yo[:, 512:1024], Kt[:], [Kt], [yo])
                dma("sp", D["y_scr"][tok0:tok0 + 128, :], yo[:], [yo], [])
                return
            tt("pool", Rt[:], r_, gam[:], ALU.mult, [zr, gam], [Rt])
            if STAGE == 6:
                cp("dve", yo[:, 512:1024], Rt[:], [Rt], [yo])
                dma("sp", D["y_scr"][tok0:tok0 + 128, :], yo[:], [yo], [])
                return
            XT = {}
            for nm, src in (("A", At), ("B", Bt), ("K", Kt), ("R", Rt)):
                pb = PB()
                mms(lambda e, pb=pb, src=src: [e.transpose(out=pb[:, j * 128:(j + 1) * 128], in_=src[:, j * 128:(j + 1) * 128], identity=ident) for j in range(4)][-1],
                    [src, cst], [pb])
                dst = Tq()
                cp("act" if nm in ("A", "K") else "dve", dst[:], pb[:].rearrange("p (a b) -> p a b", a=4), [pb], [dst])
                XT[nm] = dst
            AT, BT, KT, RT = XT["A"], XT["B"], XT["K"], XT["R"]

            def pairmat(l, r_op, mask_off, eng2, dst=None):
                dst = dst or M8()
                for par in range(2):
                    pb = PB()
                    mms(lambda e, pb=pb, par=par: [e.matmul(pb[:, j * 128:(j + 1) * 128],
                                                          lhsT=l[64 * par:64 * par + 64, j, :],
                                                          rhs=r_op[64 * par:64 * par + 64, j, :],
                                                          start=True, stop=True) for j in range(4)][-1], [l, r_op], [pb])
                    tt(eng2[par], dst[:, par:8:2, :], pb[:].rearrange("p (a b) -> p a b", a=4),
                       cst[:, mask_off:mask_off + 128].unsqueeze(1).to_broadcast([128, 4, 128]), ALU.mult, [pb, cst], [(dst, par)])
                return dst

            Nm = pairmat(BT, AT, C_SU, ("dve", "dve"))
            Am = pairmat(AT, BT, C_SL, ("dve", "dve"))
            AkT = pairmat(KT, AT, C_SU, ("dve", "dve"), M8d[0])
            RbT = pairmat(BT, RT, C_IU, ("dve", "dve"), M8d[1])
            RkT = pairmat(KT, RT, C_IU, ("dve", "dve"), M8d[2])
            if STAGE == 7:
                cp("dve", yo[:, 512:1024].rearrange("p (a b) -> p a b", a=4), RkT[:, 0:4, :], [RkT], [yo])
                dma("sp", D["y_scr"][tok0:tok0 + 128, :], yo[:], [yo], [])
                return
            X = M8()
            tt("dve", X[:], Nm[:], ident.unsqueeze(1).to_broadcast([128, 8, 128]), ALU.add, [Nm, cst], [X])
            for j in range(0 if "d" in SKIP else 6):
                Nn, An, Xn = M8(), M8(), M8()
                last = (j == 5)
                for hf in range(2):
                    pbn, pba = PB(), PB()
                    if not last:
                        mms(lambda e, pbn=pbn, hf=hf, Am=Am, Nm=Nm: [e.matmul(pbn[:, q * 128:(q + 1) * 128], lhsT=Am[:, hf * 4 + q, :], rhs=Nm[:, hf * 4 + q, :], start=True, stop=True) for q in range(4)][-1],
                            [Am, Nm], [pbn])
                        cp("act", Nn[:, hf * 4:hf * 4 + 4, :], pbn[:].rearrange("p (a b) -> p a b", a=4), [pbn], [(Nn, hf)])
                    mms(lambda e, pba=pba, hf=hf, Am=Am, Nm=Nm: [e.matmul(pba[:, q * 128:(q + 1) * 128], lhsT=Nm[:, hf * 4 + q, :], rhs=Am[:, hf * 4 + q, :], start=True, stop=True) for q in range(4)][-1],
                        [Am, Nm], [pba])
                    cp("dve", An[:, hf * 4:hf * 4 + 4, :], pba[:].rearrange("p (a b) -> p a b", a=4), [pba], [(An, hf)])
                for hf in range(2):
                    pbx = PB()
                    mms(lambda e, pbx=pbx, hf=hf, An=An, X=X: [e.matmul(pbx[:, q * 128:(q + 1) * 128], lhsT=An[:, hf * 4 + q, :], rhs=X[:, hf * 4 + q, :], start=True, stop=True) for q in range(4)][-1],
                        [An, X], [pbx])
                    tt("dve", Xn[:, hf * 4:hf * 4 + 4, :], pbx[:].rearrange("p (a b) -> p a b", a=4), X[:, hf * 4:hf * 4 + 4, :], ALU.add, [pbx, X], [(Xn, hf)])
                Nm, Am, X = Nn, An, Xn
            if STAGE == 8:
                cp("dve", yo[:, 512:1024].rearrange("p (a b) -> p a b", a=4), X[:, 0:4, :], [X], [yo])
                dma("sp", D["y_scr"][tok0:tok0 + 128, :], yo[:], [yo], [])
                return
            pr = PB()

            def f_rhs0(e, pr=pr, AT=AT, AkT=AkT):
                ins = None
                for m in range(4):
                    e.matmul(pr[:, m * 128:(m + 1) * 128], lhsT=AT[:, m, :], rhs=ST[:, m, :], start=True, stop=False)
                    for q in range(2):
                        hd = 2 * m + q
                        ins = e.matmul(pr[:, hd * 64:(hd + 1) * 64], lhsT=AkT[:, hd, :], rhs=zr[:, 1024 + hd * 64:1024 + (hd + 1) * 64], start=False, stop=(q == 1))
                return ins
            mms(f_rhs0, [AT, ST, AkT, zr], [pr])
            rhs0 = T5()
            cp("act", rhs0[:], pr[:], [pr], [rhs0])
            pu = PB()
            mms(lambda e, pu=pu, X=X, rhs0=rhs0: [e.matmul(pu[:, hd * 64:(hd + 1) * 64], lhsT=X[:, hd, :], rhs=rhs0[:, hd * 64:(hd + 1) * 64], start=True, stop=True) for hd in range(8)][-1],
                [X, rhs0], [pu])
            U = T5()
            cp("dve", U[:], pu[:], [pu], [U])
            py = PB()

            def f_y(e, py=py, RT=RT, RbT=RbT, RkT=RkT, U=U):
                ins = None
                for m in range(4):
                    e.matmul(py[:, m * 128:(m + 1) * 128], lhsT=RT[:, m, :], rhs=ST[:, m, :], start=True, stop=False)
                    for q in range(2):
                        hd = 2 * m + q
                        e.matmul(py[:, hd * 64:(hd + 1) * 64], lhsT=RbT[:, hd, :], rhs=U[:, hd * 64:(hd + 1) * 64], start=False, stop=False)
                        ins = e.matmul(py[:, hd * 64:(hd + 1) * 64], lhsT=RkT[:, hd, :], rhs=zr[:, 1024 + hd * 64:1024 + (hd + 1) * 64], start=False, stop=(q == 1))
                return ins
            mms(f_y, [RT, ST, RbT, RkT, U, zr], [py])
            yv = T5()
            cp("act", yv[:], py[:], [py], [yv])
            pst = PB()

            def f_s(e, pst=pst, Bt=Bt, Kt=Kt, U=U):
                ins = None
                for m in range(4):
                    e.matmul(pst[:, m * 128:(m + 1) * 128], lhsT=Bt[:, m * 128:(m + 1) * 128], rhs=U[:, m * 128:(m + 1) * 128], start=True, stop=False)
                    ins = e.matmul(pst[:, m * 128:(m + 1) * 128], lhsT=Kt[:, m * 128:(m + 1) * 128], rhs=zr[:, 1024 + m * 128:1024 + (m + 1) * 128], start=False, stop=True)
                return ins
            mms(f_s, [Bt, Kt, U, zr], [pst])
            tt("dve", ST[:], pst[:].rearrange("p (a b) -> p a b", a=4), ST[:], ALU.add, [pst, ST], [ST])
            tt("dve", ST[:], ST[:], gC[:].unsqueeze(2).to_broadcast([128, 4, 128]), ALU.mult, [ST, gC], [ST])
            tt("dve", ST[:], ST[:], cst[:, C_BD:C_BD + 128].unsqueeze(1).to_broadcast([128, 4, 128]), ALU.mult, [ST, cst], [ST])
            if STAGE == 9:
                cp("dve", yo[:, 512:1024], yv[:], [yv], [yo])
                dma("sp", D["y_scr"][tok0:tok0 + 128, :], yo[:], [yo], [])
                return
            y3 = yv[:].rearrange("p (h d) -> p h d", h=8)
            ysq = T5()
            tt("pool", ysq[:], yv[:], yv[:], ALU.mult, [yv], [ysq])
            s1, s2, mean, var = sm(), sm(), sm(), sm()
            red(s1[:], y3, [yv], [s1])
            red(s2[:], ysq[:].rearrange("p (h d) -> p h d", h=8), [ysq], [s2])
            ts("dve", mean[:], s1[:], 1.0 / 64, None, ALU.mult, None, [s1], [mean])
            tt("dve", var[:], mean[:], mean[:], ALU.mult, [mean], [var])
            stt("dve", var[:], s2[:], 1.0 / 64, var[:], ALU.mult, ALU.subtract, [s2, var], [var])
            act(var[:], var[:], AF.Sqrt, [var], [var], bias=64e-5, scale=1.0)
            p.op("dve", lambda e, var=var: e.reciprocal(out=var[:], in_=var[:]), [var], [var])
            yn = T5()
            yn3 = yn[:].rearrange("p (h d) -> p h d", h=8)
            tt("dve", yn3, y3, mean[:].unsqueeze(2).to_broadcast([128, 8, 64]), ALU.subtract, [yv, mean], [yn])
            tt("dve", yn3, yn3, var[:].unsqueeze(2).to_broadcast([128, 8, 64]), ALU.mult, [yn, var], [yn])
            tt("pool", yn[:], yn[:], vb(V_LNW), ALU.mult, [yn, vec], [yn])
            tt("pool", yn[:], yn[:], vb(V_LNB), ALU.add, [yn, vec], [yn])
            rk = T5()
            tt("pool", rk[:], r_, k2[:], ALU.mult, [zr, k2], [rk])
            tt("pool", rk[:], rk[:], vb(V_RK), ALU.mult, [rk, vec], [rk])
            sb_ = sm()
            red(sb_[:], rk[:].rearrange("p (h d) -> p h d", h=8), [rk], [sb_])
            tt("dve", rk[:].rearrange("p (h d) -> p h d", h=8), v_.rearrange("p (h d) -> p h d", h=8),
               sb_[:].unsqueeze(2).to_broadcast([128, 8, 64]), ALU.mult, [zr, sb_], [rk])
            tt("pool", yn[:], yn[:], rk[:], ALU.add, [yn, rk], [yn])
            tt("pool", yo[:, 512:1024], yn[:], gv[:], ALU.mult, [yn, gv], [(yo, 2)])
            dma("sp", D["y_scr"][tok0:tok0 + 128, :], yo[:], [yo], [(D["y_t"], tok0 // 128)])

    for i in range(len(order)):
        tile_body(i)


def phase_a1a(p, nc, D, n_seq, n_tiles, S_TOK):
    h = helpers(p)
    tt, stt, ts, act, cp, red, dma, mms = h.tt, h.stt, h.ts, h.act, h.cp, h.red, h.dma, h.mms
    NZ = 2592
    cst = p.sb("cst", [128, NCST], F32)
    dma("sp", cst[:], D["cst"], [], [cst])
    vec = p.sb("vecmu", [128, 1824], F32)
    dma("sp", vec[:], D["vecs"][V_MU:V_MU + 1824].partition_broadcast(128), [], [vec])
    snk = p.sb("snk", [128, 8], F32)
    dma("sp", snk[:], D["vecs"][V_SINK:V_SINK + 8].partition_broadcast(128), [], [snk])
    nwp = p.sb("nwp", [128, 16], F32)
    dma("sp", nwp[:], D["nwp"], [], [nwp])
    ident = cst[:, C_ID:C_ID + 128]
    negsink = p.sb("negsink", [128, 8], F32)
    ts("dve", negsink[:], snk[:], -1.0, None, ALU.mult, None, [snk], [negsink])

    def make_stream(k):
        sf = "_s%d" % k
        xt = Rot([p.sb("xt%d" % i + sf, [128, 1024], F32) for i in range(2)])
        ss = p.sb("ss" + sf, [128, 1], F32)
        rstd = p.sb("rstd" + sf, [128, 1], F32)
        xs = p.sb("xs" + sf, [128, 1024], F32)
        xsT = p.sb("xsT" + sf, [128, 8, 128], BF16)
        z = p.sb("z" + sf, [128, NZ], F32)
        carry = p.sb("carry" + sf, [128, 1824], F32)
        p.op("pool", lambda e: e.memset(carry[:], 0.0), [], [carry])
        ZR = Rot([p.sb("zr%d" % i + sf, [128, 1824], F32) for i in range(2)])
        T5 = Rot([p.sb("t5_%d" % i + sf, [128, 320], F32) for i in range(4)])
        sm = Rot([p.sb("sm_%d" % i + sf, [128, 8], F32) for i in range(4)])
        PB = Rot([p.ps("pb%d" % i + sf, [128, 512], F32) for i in range(2)])
        PV = [p.ps("pv%d" % i + sf, [128, 512], F32) for i in range(2)]
        qk = p.sb("qk" + sf, [128, 10, 64], F32)
        kdup = p.sb("kdup" + sf, [128, 2, 2, 64], F32)
        qT = p.sb("qT" + sf, [128, 4, 128], BF16)
        kT = [p.sb("kT%d" % i + sf, [128, 2, 128], BF16) for i in range(2)]
        va = [p.sb("va%d" % i + sf, [128, 2, 65], BF16) for i in range(2)]
        for i in range(2):
            p.op("pool", lambda e, i=i: e.memset(va[i][:], 1.0), [], [va[i]])
        pT = Rot([p.sb("pT%d" % i + sf, [128, 2, 128], BF16) for i in range(4)])
        yout = Rot([p.sb("yout%d" % i + sf, [128, 512], F32) for i in range(2)])
        xq = {}

        def prefetch(b, n):
            if n < n_tiles:
                t0 = b * S_TOK + n * 128
                xq[(b, n)] = xt()
                dma("sp", xq[(b, n)][:], D["x"][t0:t0 + 128, :], [], [xq[(b, n)]])

        def tile_body(b, n):
            tok0 = b * S_TOK + n * 128
            first = (n == 0)
            x_t = xq.pop((b, n))
            prefetch(b, n + 1)
            zr = ZR()
            act(xs[:], x_t[:], AF.Square, [x_t], [xs, ss], accum_out=ss[:])
            act(rstd[:], ss[:], AF.Sqrt, [ss], [rstd], bias=1e-5, scale=1.0 / 1024)
            p.op("dve", lambda e: e.reciprocal(out=rstd[:], in_=rstd[:]), [rstd], [rstd])
            ts("dve", xs[:], x_t[:], rstd[:, 0:1], None, ALU.mult, None, [x_t, rstd], [xs])
            for hh in range(2):
                pb = PB()
                mms(lambda e, pb=pb, hh=hh: [e.transpose(out=pb[:, j * 128:(j + 1) * 128], in_=xs[:, (hh * 4 + j) * 128:(hh * 4 + j + 1) * 128], identity=ident) for j in range(4)][-1],
                    [xs, cst], [pb])
                cp("act" if hh else "dve", xsT[:, hh * 4:hh * 4 + 4, :], pb[:].rearrange("p (a b) -> p a b", a=4), [pb], [(xsT, hh)])
            for cc in range(6):
                c0 = cc * 512
                cw = min(512, NZ - c0)
                pb = PB()
                mms(lambda e, pb=pb, c0=c0, cw=cw: [e.matmul(pb[:, 0:cw], lhsT=xsT[:, c, :], rhs=Wb[:, c, c0:c0 + cw], start=(c == 0), stop=(c == 7)) for c in range(8)][-1],
                    [xsT, Wb], [pb])
                cp("act" if cc % 2 else "dve", z[:, c0:c0 + cw], pb[:, 0:cw], [pb], [(z, cc)])
            cosb = cst[:, C_COS + n * 32:C_COS + n * 32 + 32].unsqueeze(1).to_broadcast([128, 10, 32])
            sinb = cst[:, C_SIN + n * 32:C_SIN + n * 32 + 32].unsqueeze(1).to_broadcast([128, 10, 32])
            zq = z[:, 0:640].rearrange("p (h d) -> p h d", h=10)
            x1, x2 = zq[:, :, 0:32], zq[:, :, 32:64]
            ta, tb_ = T5(), T5()
            tav = ta[:, 0:320].rearrange("p (h d) -> p h d", h=10)
            tbv = tb_[:, 0:320].rearrange("p (h d) -> p h d", h=10)
            zk = [(z, 0), (z, 1)]
            tt("dve", tav, x1, cosb, ALU.mult, zk + [cst], [ta])
            tt("pool", tbv, x2, sinb, ALU.mult, zk + [cst], [tb_])
            tt("dve", qk[:, :, 0:32], tav, tbv, ALU.subtract, [ta, tb_], [(qk, 0)])
            tc_, td = T5(), T5()
            tcv = tc_[:, 0:320].rearrange("p (h d) -> p h d", h=10)
            tdv = td[:, 0:320].rearrange("p (h d) -> p h d", h=10)
            tt("pool", tcv, x2, cosb, ALU.mult, zk + [cst], [tc_])
            tt("dve", tdv, x1, sinb, ALU.mult, zk + [cst], [td])
            tt("pool", qk[:, :, 32:64], tcv, tdv, ALU.add, [tc_, td], [(qk, 1)])
            cp("pool", kdup[:], qk[:, 8:10, :].unsqueeze(2).to_broadcast([128, 2, 2, 64]), [qk], [kdup])
            kTc, kTp = kT[n % 2], kT[(n + 1) % 2]
            vac, vap = va[n % 2], va[(n + 1) % 2]
            cp("pool", vac[:, :, 0:64], z[:, 640:768].rearrange("p (g d) -> p g d", g=2), [(z, 1)], [vac])
            pb = PB()
            mms(lambda e, pb=pb: [e.transpose(out=pb[:, j * 128:(j + 1) * 128], in_=qk[:, 2 * j:2 * j + 2, :].rearrange("p a d -> p (a d)"), identity=ident) for j in range(4)][-1],
                [qk, cst], [pb])
            cp("act", qT[:], pb[:].rearrange("p (a b) -> p a b", a=4), [pb], [qT])
            pb = PB()
            mms(lambda e, pb=pb: [e.transpose(out=pb[:, g * 128:(g + 1) * 128], in_=kdup[:, g, :, :].rearrange("p a d -> p (a d)"), identity=ident) for g in range(2)][-1],
                [kdup, cst], [pb])
            cp("dve", kTc[:], pb[:, 0:256].rearrange("p (a b) -> p a b", a=2), [pb], [kTc])
            yo = yout()
            pv = PV
            for hd in range(0 if "a" in SKIP else 8):
                m, base, g = hd // 2, 64 * (hd % 2), hd // 4
                pb = PB()
                if first:
                    mms(lambda e, pb=pb, m=m, base=base, g=g: e.matmul(pb[:, 128:256], lhsT=kTc[base:base + 64, g, :], rhs=qT[base:base + 64, m, :], start=True, stop=True),
                        [kTc, qT], [pb])
                else:
                    mms(lambda e, pb=pb, m=m, base=base, g=g: [e.matmul(pb[:, 0:128], lhsT=kTp[base:base + 64, g, :], rhs=qT[base:base + 64, m, :], start=True, stop=True),
                                                              e.matmul(pb[:, 128:256], lhsT=kTc[base:base + 64, g, :], rhs=qT[base:base + 64, m, :], start=True, stop=True)][-1],
                        [kTc, kTp, qT], [pb])
                pt = pT()
                lo = 1 if first else 0
                act(pt[:, lo:2, :], pb[:, lo * 128:256].rearrange("p (a b) -> p a b", a=2 - lo), AF.Exp, [pb, negsink], [pt],
                    scale=0.125, bias=negsink[:, hd:hd + 1])
                if not first:
                    tt("pool", pt[:, 0, :], pt[:, 0, :], cst[:, C_SL:C_SL + 128], ALU.mult, [pt, cst], [pt])
                tt("dve", pt[:, 1, :], pt[:, 1, :], cst[:, C_IU:C_IU + 128], ALU.mult, [pt, cst], [pt])
                pvb = pv[hd // 4]
                o0 = (hd % 4) * 65
                if first:
                    mms(lambda e, pvb=pvb, pt=pt, g=g, o0=o0: e.matmul(pvb[:, o0:o0 + 65], lhsT=pt[:, 1, :], rhs=vac[:, g, :], start=True, stop=True),
                        [pt, vac], [pvb])
                else:
                    mms(lambda e, pvb=pvb, pt=pt, g=g, o0=o0: [e.matmul(pvb[:, o0:o0 + 65], lhsT=pt[:, 0, :], rhs=vap[:, g, :], start=True, stop=False),
                                                              e.matmul(pvb[:, o0:o0 + 65], lhsT=pt[:, 1, :], rhs=vac[:, g, :], start=False, stop=True)][-1],
                        [pt, vac, vap], [pvb])
            for hf in range(2):
                pvb = pv[hf]
                pvv = pvb[:, 0:260].rearrange("p (h d) -> p h d", h=4)
                den = sm()
                ts("dve", den[:, 0:4], pvv[:, :, 64], 1.0, None, ALU.add, None, [pvb], [den])
                p.op("dve", lambda e, den=den: e.reciprocal(out=den[:, 0:4], in_=den[:, 0:4]), [den], [den])
                tt("dve", yo[:, hf * 256:(hf + 1) * 256].rearrange("p (h d) -> p h d", h=4), pvv[:, :, 0:64],
                   den[:, 0:4].unsqueeze(2).to_broadcast([128, 4, 64]), ALU.mult, [pvb, den], [(yo, hf)])
            for j in range(4):
                c0 = 768 + j * 512
                cw = min(512, NZ - c0)
                pb = PB()
                zkeys = [(z, 1), (z, 2), (z, 3), (z, 4), (z, 5)]
                if first:
                    mms(lambda e, pb=pb, c0=c0, cw=cw: e.matmul(pb[:, 0:cw], lhsT=cst[:, C_SH:C_SH + 128], rhs=z[:, c0:c0 + cw], start=True, stop=True),
                        zkeys + [cst], [pb])
                else:
                    mms(lambda e, pb=pb, c0=c0, cw=cw: [e.matmul(pb[:, 0:cw], lhsT=cst[:, C_SH:C_SH + 128], rhs=z[:, c0:c0 + cw], start=True, stop=False),
                                                       e.matmul(pb[:, 0:cw], lhsT=cst[:, C_CA:C_CA + 128], rhs=carry[:, c0 - 768:c0 - 768 + cw], start=False, stop=True)][-1],
                        zkeys + [cst, carry], [pb])
                r0 = c0 - 768
                tt("dve", zr[:, r0:r0 + cw], pb[:, 0:cw], z[:, c0:c0 + cw], ALU.subtract, [pb] + zkeys, [(zr, j)])
                tt("dve", zr[:, r0:r0 + cw], zr[:, r0:r0 + cw], vec[:, V_MU + r0:V_MU + r0 + cw], ALU.mult, [(zr, j), vec], [(zr, j)])
                tt("pool", zr[:, r0:r0 + cw], zr[:, r0:r0 + cw], z[:, c0:c0 + cw], ALU.add, [(zr, j)] + zkeys, [(zr, j)])
            cp("pool", carry[96:128, :], z[96:128, 768:NZ], [z], [carry])
            dma("sp", D["y_scr"][tok0:tok0 + 128, 0:512], yo[:], [yo], [(D["y_t"], (tok0 // 128, 0))])
            dma("sp", D["zr_scr"][tok0:tok0 + 128, :], zr[:], [zr], [(D["zr_t"], tok0 // 128)])

        class S:
            pass
        S.prefetch, S.body, S.z = prefetch, tile_body, z
        return S

    streams = [make_stream(k) for k in range(2)]
    Wb = load_w_bf16(p, h, "Wb1", D["w_in"], 1024, 4640, 0, NZ, (lambda: streams[0].z), nwp, 0)
    zip_streams(p, streams, n_seq, n_tiles)


def phase_a1b(p, nc, D, n_seq, n_tiles, S_TOK):
    h = helpers(p)
    tt, stt, ts, act, cp, red, dma, mms = h.tt, h.stt, h.ts, h.act, h.cp, h.red, h.dma, h.mms
    cst = p.sb("cst", [128, NCST], F32)
    dma("sp", cst[:], D["cst"], [], [cst])
    VOFF = V_W0
    vec = p.sb("vecr", [128, V_SINK - VOFF], F32)
    dma("sp", vec[:], D["vecs"][VOFF:V_SINK].partition_broadcast(128), [], [vec])
    ident = cst[:, C_ID:C_ID + 128]
    w2 = p.sb("w2", [128, 512], F32)
    dma("sp", w2[0:64, :], D["decay_w2"], [], [w2])
    dma("sp", w2[64:128, :], D["iclr_a2"], [], [w2])
    g2 = p.sb("g2", [128, 2, 512], F32)
    dma("sp", g2[:, 0, :], D["gate_g2"][0:128, :], [], [g2])
    dma("sp", g2[0:32, 1, :], D["gate_g2"][128:160, :], [], [g2])

    def vb(off, n=512):
        return vec[:, off - VOFF:off - VOFF + n]

    def make_stream(k):
        sf = "_r%d" % k
        ZR = Rot([p.sb("zr%d" % i + sf, [128, 1824], F32) for i in range(NZRB)])
        lin = p.sb("lin" + sf, [128, 288], F32)
        linT = p.sb("linT" + sf, [128, 3, 128], F32)
        T5 = Rot([p.sb("t5_%d" % i + sf, [128, 512], F32) for i in range(10)])
        gv = p.sb("gv" + sf, [128, 512], F32)
        k2 = p.sb("k2" + sf, [128, 512], F32)
        M8 = Rot([p.sb("m8_%d" % i + sf, [128, 8, 128], F32) for i in range(6)])
        M8d = [p.sb("m8d_%d" % i + sf, [128, 8, 128], F32) for i in range(3)]
        Tq = Rot([p.sb("tq_%d" % i + sf, [128, 4, 128], F32) for i in range(4)])
        sm = Rot([p.sb("sm_%d" % i + sf, [128, 8], F32) for i in range(8)])
        PB = Rot([p.ps("pb%d" % i + sf, [128, 512], F32) for i in range(8 // NSB)])
        ST = p.sb("ST" + sf, [128, 4, 128], F32)
        gC = p.sb("gC" + sf, [128, 4], F32)
        yout = Rot([p.sb("yout%d" % i + sf, [128, 512], F32) for i in range(1)])
        zq = {}

        def prefetch(b, n):
            if n < n_tiles:
                t0 = b * S_TOK + n * 128
                zq[(b, n)] = ZR()
                dma("sp", zq[(b, n)][:], D["zr_scr"][t0:t0 + 128, :], [(D["zr_t"], t0 // 128)], [zq[(b, n)]])

        def tile_body(b, n):
            if n == 0:
                p.op("pool", lambda e: e.memset(ST[:], 0.0), [], [ST])
            tok0 = b * S_TOK + n * 128
            zr = zq.pop((b, n))
            prefetch(b, n + 1)
            yo = yout()
            r_, k_, v_ = zr[:, 0:512], zr[:, 512:1024], zr[:, 1024:1536]
            act(lin[:, 0:64], zr[:, 1536:1600], AF.Tanh, [zr], [(lin, 0)])
            cp("pool", lin[:, 64:128], zr[:, 1600:1664], [zr], [(lin, 1)])
            act(lin[:, 128:288], zr[:, 1664:1824], AF.Sigmoid, [zr], [(lin, 2)])
            pb = PB()
            mms(lambda e, pb=pb: [e.transpose(out=pb[:, 0:128], in_=lin[:, 0:128], identity=ident),
                                  e.transpose(out=pb[:, 128:256], in_=lin[:, 128:256], identity=ident),
                                  e.transpose(out=pb[0:32, 256:384], in_=lin[:, 256:288], identity=ident)][-1], [lin, cst], [pb])
            cp("dve", linT[:, 0:2, :], pb[:, 0:256].rearrange("p (a b) -> p a b", a=2), [pb], [(linT, 0)])
            cp("dve", linT[0:32, 2, :], pb[0:32, 256:384], [pb], [(linT, 1)])
            pw, pa_, pg = PB(), PB(), PB()
            mms(lambda e, pw=pw: e.matmul(pw[:], lhsT=linT[0:64, 0, :], rhs=w2[0:64, :], start=True, stop=True), [linT, w2], [pw])
            mms(lambda e, pa_=pa_: e.matmul(pa_[:], lhsT=linT[64:128, 0, :], rhs=w2[64:128, :], start=True, stop=True), [linT, w2], [pa_])
            mms(lambda e, pg=pg: [e.matmul(pg[:], lhsT=linT[:, 1, :], rhs=g2[:, 0, :], start=True, stop=False),
                                  e.matmul(pg[:], lhsT=linT[0:32, 2, :], rhs=g2[0:32, 1, :], start=False, stop=True)][-1], [linT, g2], [pg])
            sg, av = T5(), T5()
            tt("dve", sg[:], pw[:], vb(V_W0), ALU.add, [pw, vec], [sg])
            act(sg[:], sg[:], AF.Sigmoid, [sg], [sg])
            tt("dve", av[:], pa_[:], vb(V_A0), ALU.add, [pa_, vec], [av])
            act(av[:], av[:], AF.Sigmoid, [av], [av])
            cp("act", gv[:], pg[:], [pg], [gv])
            pc = PB()
            mms(lambda e, pc=pc, sg=sg: e.matmul(pc[:], lhsT=cst[:, C_IU:C_IU + 128], rhs=sg[:], start=True, stop=True), [cst, sg], [pc])
            gam, igam, gprev = T5(), T5(), T5()
            act(gam[:], pc[:], AF.Exp, [pc], [gam], scale=-C_DEC)
            act(igam[:], pc[:], AF.Exp, [pc], [igam], scale=C_DEC)
            tt("dve", gprev[:], pc[:], sg[:], ALU.subtract, [pc, sg], [gprev])
            act(gprev[:], gprev[:], AF.Exp, [gprev], [gprev], scale=-C_DEC)
            pgc = PB()
            mms(lambda e, pgc=pgc, sg=sg: [e.matmul(pgc[:, m:m + 1], lhsT=sg[:, m * 128:(m + 1) * 128], rhs=cst[:, C_ONE:C_ONE + 1], start=True, stop=True) for m in range(4)][-1],
                [cst, sg], [pgc])
            act(gC[:], pgc[:, 0:4], AF.Exp, [pgc], [gC], scale=-C_DEC)
            kk, sq = T5(), T5()
            tt("dve", kk[:], k_, vb(V_KK), ALU.mult, [zr, vec], [kk])
            tt("pool", sq[:], kk[:], kk[:], ALU.mult, [kk], [sq])
            s8 = sm()
            red(s8[:], sq[:].rearrange("p (h d) -> p h d", h=8), [sq], [s8])
            act(s8[:], s8[:], AF.Sqrt, [s8], [s8], bias=1e-24, scale=1.0)
            p.op("dve", lambda e, s8=s8: e.reciprocal(out=s8[:], in_=s8[:]), [s8], [s8])
            tt("dve", kk[:].rearrange("p (h d) -> p h d", h=8), kk[:].rearrange("p (h d) -> p h d", h=8),
               s8[:].unsqueeze(2).to_broadcast([128, 8, 64]), ALU.mult, [kk, s8], [kk])
            t1 = T5()
            stt("dve", t1[:], av[:], -1.0, vb(V_KA), ALU.add, ALU.mult, [av, vec], [t1])
            stt("dve", k2[:], t1[:], 1.0, k_, ALU.add, ALU.mult, [t1, zr], [k2])
            At, Bt, Kt, Rt = T5(), T5(), T5(), T5()
            stt("dve", At[:], kk[:], -1.0, gprev[:], ALU.mult, ALU.mult, [kk, gprev], [At])
            tt("pool", Bt[:], kk[:], av[:], ALU.mult, [kk, av], [Bt])
            tt("dve", Bt[:], Bt[:], igam[:], ALU.mult, [Bt, igam], [Bt])
            tt("dve", Kt[:], k2[:], igam[:], ALU.mult, [k2, igam], [Kt])
            tt("pool", Rt[:], r_, gam[:], ALU.mult, [zr, gam], [Rt])
            XT = {}
            for nm, src in (("A", At), ("B", Bt), ("K", Kt), ("R", Rt)):
                pb = PB()
                mms(lambda e, pb=pb, src=src: [e.transpose(out=pb[:, j * 128:(j + 1) * 128], in_=src[:, j * 128:(j + 1) * 128], identity=ident) for j in range(4)][-1],
                    [src, cst], [pb])
                dst = Tq()
                cp("act" if nm in ("A", "K") else "dve", dst[:], pb[:].rearrange("p (a b) -> p a b", a=4), [pb], [dst])
                XT[nm] = dst
            AT, BT, KT, RT = XT["A"], XT["B"], XT["K"], XT["R"]

            def pairmat(l, r_op, mask_off, eng2, dst=None):
                dst = dst or M8()
                for par in range(2):
                    pb = PB()
                    mms(lambda e, pb=pb, par=par: [e.matmul(pb[:, j * 128:(j + 1) * 128],
                                                          lhsT=l[64 * par:64 * par + 64, j, :],
                                                          rhs=r_op[64 * par:64 * par + 64, j, :],
                                                          start=True, stop=True) for j in range(4)][-1], [l, r_op], [pb])
                    tt(eng2[par], dst[:, par:8:2, :], pb[:].rearrange("p (a b) -> p a b", a=4),
                       cst[:, mask_off:mask_off + 128].unsqueeze(1).to_broadcast([128, 4, 128]), ALU.mult, [pb, cst], [(dst, par)])
                return dst

            Nm = pairmat(BT, AT, C_SU, ("dve", "dve"))
            Am = pairmat(AT, BT, C_SL, ("dve", "dve"))
            AkT = pairmat(KT, AT, C_SU, ("dve", "dve"), M8d[0])
            RbT = pairmat(BT, RT, C_IU, ("dve", "dve"), M8d[1])
            RkT = pairmat(KT, RT, C_IU, ("dve", "dve"), M8d[2])
            X = M8()
            tt("dve", X[:], Nm[:], ident.unsqueeze(1).to_broadcast([128, 8, 128]), ALU.add, [Nm, cst], [X])
            for j in range(0 if "d" in SKIP else 6):
                Nn, An, Xn = M8(), M8(), M8()
                last = (j == 5)
                for hf in range(2):
                    pbn, pba = PB(), PB()
                    if not last:
                        mms(lambda e, pbn=pbn, hf=hf, Am=Am, Nm=Nm: [e.matmul(pbn[:, q * 128:(q + 1) * 128], lhsT=Am[:, hf * 4 + q, :], rhs=Nm[:, hf * 4 + q, :], start=True, stop=True) for q in range(4)][-1],
                            [Am, Nm], [pbn])
                        cp("act", Nn[:, hf * 4:hf * 4 + 4, :], pbn[:].rearrange("p (a b) -> p a b", a=4), [pbn], [(Nn, hf)])
                    mms(lambda e, pba=pba, hf=hf, Am=Am, Nm=Nm: [e.matmul(pba[:, q * 128:(q + 1) * 128], lhsT=Nm[:, hf * 4 + q, :], rhs=Am[:, hf * 4 + q, :], start=True, stop=True) for q in range(4)][-1],
                        [Am, Nm], [pba])
                    cp("dve", An[:, hf * 4:hf * 4 + 4, :], pba[:].rearrange("p (a b) -> p a b", a=4), [pba], [(An, hf)])
                for hf in range(2):
                    pbx = PB()
                    mms(lambda e, pbx=pbx, hf=hf, An=An, X=X: [e.matmul(pbx[:, q * 128:(q + 1) * 128], lhsT=An[:, hf * 4 + q, :], rhs=X[:, hf * 4 + q, :], start=True, stop=True) for q in range(4)][-1],
                        [An, X], [pbx])
                    tt("dve", Xn[:, hf * 4:hf * 4 + 4, :], pbx[:].rearrange("p (a b) -> p a b", a=4), X[:, hf * 4:hf * 4 + 4, :], ALU.add, [pbx, X], [(Xn, hf)])
                Nm, Am, X = Nn, An, Xn
            pr = PB()

            def f_rhs0(e, pr=pr, AT=AT, AkT=AkT):
                ins = None
                for m in range(4):
                    e.matmul(pr[:, m * 128:(m + 1) * 128], lhsT=AT[:, m, :], rhs=ST[:, m, :], start=True, stop=False)
                    for q in range(2):
                        hd = 2 * m + q
                        ins = e.matmul(pr[:, hd * 64:(hd + 1) * 64], lhsT=AkT[:, hd, :], rhs=zr[:, 1024 + hd * 64:1024 + (hd + 1) * 64], start=False, stop=(q == 1))
                return ins
            mms(f_rhs0, [AT, ST, AkT, zr], [pr])
            rhs0 = T5()
            cp("act", rhs0[:], pr[:], [pr], [rhs0])
            pu = PB()
            mms(lambda e, pu=pu, X=X, rhs0=rhs0: [e.matmul(pu[:, hd * 64:(hd + 1) * 64], lhsT=X[:, hd, :], rhs=rhs0[:, hd * 64:(hd + 1) * 64], start=True, stop=True) for hd in range(8)][-1],
                [X, rhs0], [pu])
            U = T5()
            cp("dve", U[:], pu[:], [pu], [U])
            py = PB()

            def f_y(e, py=py, RT=RT, RbT=RbT, RkT=RkT, U=U):
                ins = None
                for m in range(4):
                    e.matmul(py[:, m * 128:(m + 1) * 128], lhsT=RT[:, m, :], rhs=ST[:, m, :], start=True, stop=False)
                    for q in range(2):
                        hd = 2 * m + q
                        e.matmul(py[:, hd * 64:(hd + 1) * 64], lhsT=RbT[:, hd, :], rhs=U[:, hd * 64:(hd + 1) * 64], start=False, stop=False)
                        ins = e.matmul(py[:, hd * 64:(hd + 1) * 64], lhsT=RkT[:, hd, :], rhs=zr[:, 1024 + hd * 64:1024 + (hd + 1) * 64], start=False, stop=(q == 1))
                return ins
            mms(f_y, [RT, ST, RbT, RkT, U, zr], [py])
            yv = T5()
            cp("act", yv[:], py[:], [py], [yv])
            pst = PB()

            def f_s(e, pst=pst, Bt=Bt, Kt=Kt, U=U):
                ins = None
                for m in range(4):
                    e.matmul(pst[:, m * 128:(m + 1) * 128], lhsT=Bt[:, m * 128:(m + 1) * 128], rhs=U[:, m * 128:(m + 1) * 128], start=True, stop=False)
                    ins = e.matmul(pst[:, m * 128:(m + 1) * 128], lhsT=Kt[:, m * 128:(m + 1) * 128], rhs=zr[:, 1024 + m * 128:1024 + (m + 1) * 128], start=False, stop=True)
                return ins
            mms(f_s, [Bt, Kt, U, zr], [pst])
            tt("dve", ST[:], pst[:].rearrange("p (a b) -> p a b", a=4), ST[:], ALU.add, [pst, ST], [ST])
            tt("dve", ST[:], ST[:], gC[:].unsqueeze(2).to_broadcast([128, 4, 128]), ALU.mult, [ST, gC], [ST])
            tt("dve", ST[:], ST[:], cst[:, C_BD:C_BD + 128].unsqueeze(1).to_broadcast([128, 4, 128]), ALU.mult, [ST, cst], [ST])
            y3 = yv[:].rearrange("p (h d) -> p h d", h=8)
            ysq = T5()
            tt("pool", ysq[:], yv[:], yv[:], ALU.mult, [yv], [ysq])
            s1, s2, mean, var = sm(), sm(), sm(), sm()
            red(s1[:], y3, [yv], [s1])
            red(s2[:], ysq[:].rearrange("p (h d) -> p h d", h=8), [ysq], [s2])
            ts("dve", mean[:], s1[:], 1.0 / 64, None, ALU.mult, None, [s1], [mean])
            tt("dve", var[:], mean[:], mean[:], ALU.mult, [mean], [var])
            stt("dve", var[:], s2[:], 1.0 / 64, var[:], ALU.mult, ALU.subtract, [s2, var], [var])
            act(var[:], var[:], AF.Sqrt, [var], [var], bias=64e-5, scale=1.0)
            p.op("dve", lambda e, var=var: e.reciprocal(out=var[:], in_=var[:]), [var], [var])
            yn = T5()
            yn3 = yn[:].rearrange("p (h d) -> p h d", h=8)
            tt("dve", yn3, y3, mean[:].unsqueeze(2).to_broadcast([128, 8, 64]), ALU.subtract, [yv, mean], [yn])
            tt("dve", yn3, yn3, var[:].unsqueeze(2).to_broadcast([128, 8, 64]), ALU.mult, [yn, var], [yn])
            tt("pool", yn[:], yn[:], vb(V_LNW), ALU.mult, [yn, vec], [yn])
            tt("pool", yn[:], yn[:], vb(V_LNB), ALU.add, [yn, vec], [yn])
            rk = T5()
            tt("pool", rk[:], r_, k2[:], ALU.mult, [zr, k2], [rk])
            tt("pool", rk[:], rk[:], vb(V_RK), ALU.mult, [rk, vec], [rk])
            sb_ = sm()
            red(sb_[:], rk[:].rearrange("p (h d) -> p h d", h=8), [rk], [sb_])
            tt("dve", rk[:].rearrange("p (h d) -> p h d", h=8), v_.rearrange("p (h d) -> p h d", h=8),
               sb_[:].unsqueeze(2).to_broadcast([128, 8, 64]), ALU.mult, [zr, sb_], [rk])
            tt("pool", yn[:], yn[:], rk[:], ALU.add, [yn, rk], [yn])
            tt("pool", yo[:, 0:512], yn[:], gv[:], ALU.mult, [yn, gv], [(yo, 2)])
            dma("sp", D["y_scr"][tok0:tok0 + 128, 512:1024], yo[:], [yo], [(D["y_t"], (tok0 // 128, 1))])

        class S:
            pass
        S.prefetch, S.body = prefetch, tile_body
        return S

    streams = [make_stream(k) for k in range(NSB)]
    zip_streams(p, streams, n_seq, n_tiles)


def zip_streams(p, streams, n_seq, n_tiles):
    NS = len(streams)
    for b0 in range(0, n_seq, NS):
        seqs = list(range(b0, min(b0 + NS, n_seq)))
        for k, b in enumerate(seqs):
            streams[k].prefetch(b, 0)
        for n in range(n_tiles):
            lists = []
            for k, b in enumerate(seqs):
                p.defer_begin()
                streams[k].body(b, n)
                lists.append(p.defer_end())
            while any(lists):
                for lst in lists:
                    if lst:
                        p.drain(lst, 1)


def phase_a2(p, nc, D, ntok, final=True, drip=None):
    h = helpers(p)
    tt, stt, ts, act, cp, red, dma, mms = h.tt, h.stt, h.ts, h.act, h.cp, h.red, h.dma, h.mms
    cst = p.sb("cstb", [128, 128], F32)
    dma("sp", cst[:], D["cst"][:, C_ID:C_ID + 128], [], [cst])
    ident = cst[:, 0:128]
    nwp = p.sb("nwpb", [128, 16], F32)
    dma("sp", nwp[:], D["nwp"], [], [nwp])
    fin = p.sb("finw", [128, 1024], F32)
    dma("sp", fin[:], D["vecs"][V_FIN:V_FIN + 1024].partition_broadcast(128), [], [fin])
    stg = Rot([p.sb("stgb%d" % i, [128, 2048], F32) for i in range(2)])
    Wg = load_w_bf16(p, h, "Wg", D["w_in"], 1024, 4640, 2592, 2048, stg, nwp, 0)
    PA = load_w_bf16(p, h, "PAw", D["proj_attn"], 512, 1024, 0, 1024, stg)
    PBw = load_w_bf16(p, h, "PBw", D["proj_rwkv"], 512, 1024, 0, 1024, stg)
    WO = load_w_bf16(p, h, "WOw", D["w_out"], 1024, 1024, 0, 1024, stg)
    nt = ntok // 128
    NS = 2

    class St:
        pass

    streams = []
    for k in range(NS):
        S = St()
        S.xt = Rot([p.sb("xtb%d_%d" % (k, i), [128, 1024], F32) for i in range(2)])
        S.yt = Rot([p.sb("ytb%d_%d" % (k, i), [128, 1024], F32) for i in range(2)])
        S.ss = p.sb("ssb%d" % k, [128, 1], F32)
        S.rstd = p.sb("rstdb%d" % k, [128, 1], F32)
        S.xs = p.sb("xsb%d" % k, [128, 1024], F32)
        S.xsT = p.sb("xsTb%d" % k, [128, 8, 128], BF16)
        S.yT = p.sb("yTb%d" % k, [128, 8, 128], BF16)
        S.sgt = p.sb("sgt%d" % k, [128, 2048], BF16)
        S.mg = p.sb("mg%d" % k, [128, 1024], F32)
        S.m2 = p.sb("m2%d" % k, [128, 1024], F32)
        S.mgT = p.sb("mgT%d" % k, [128, 8, 128], BF16)
        S.h1 = Rot([p.sb("h1b%d_%d" % (k, i), [128, 1024], F32) for i in range(2)])
        S.PB = Rot([p.ps("pq%d_%d" % (k, i), [128, 512], F32) for i in range(4)])
        S.xq, S.yq = {}, {}
        streams.append(S)

    def prefetch(S, i):
        if i < nt:
            S.xq[i] = S.xt()
            dma("sp", S.xq[i][:], D["x"][i * 128:(i + 1) * 128, :], [], [S.xq[i]])
            S.yq[i] = S.yt()
            dma("sp", S.yq[i][:], D["y_scr"][i * 128:(i + 1) * 128, :], [(D["y_t"], (i, 0)), (D["y_t"], (i, 1))], [S.yq[i]])

    def tp8(S, src, dst):
        for hh in range(2):
            pb = S.PB()
            mms(lambda e, pb=pb, hh=hh: [e.transpose(out=pb[:, j * 128:(j + 1) * 128], in_=src[:, (hh * 4 + j) * 128:(hh * 4 + j + 1) * 128], identity=ident) for j in range(4)][-1],
                [src, cst], [pb])
            cp("act" if hh else "dve", dst[:, hh * 4:hh * 4 + 4, :], pb[:].rearrange("p (a b) -> p a b", a=4), [pb], [(dst, hh)])

    def body(S, i):
        ss, rstd, xs, xsT, yT, sgt, mg, m2, mgT = S.ss, S.rstd, S.xs, S.xsT, S.yT, S.sgt, S.mg, S.m2, S.mgT
        x_t, y_t = S.xq.pop(i), S.yq.pop(i)
        prefetch(S, i + NS)
        act(xs[:], x_t[:], AF.Square, [x_t], [xs, ss], accum_out=ss[:])
        act(rstd[:], ss[:], AF.Sqrt, [ss], [rstd], bias=1e-5, scale=1.0 / 1024)
        p.op("dve", lambda e: e.reciprocal(out=rstd[:], in_=rstd[:]), [rstd], [rstd])
        ts("dve", xs[:], x_t[:], rstd[:, 0:1], None, ALU.mult, None, [x_t, rstd], [xs])
        tp8(S, xs, xsT)
        for cc in range(4):
            pb = S.PB()
            mms(lambda e, pb=pb, cc=cc: [e.matmul(pb[:], lhsT=xsT[:, c, :], rhs=Wg[:, c, cc * 512:(cc + 1) * 512], start=(c == 0), stop=(c == 7)) for c in range(8)][-1],
                [xsT, Wg], [pb])
            act(sgt[:, cc * 512:(cc + 1) * 512], pb[:], AF.Sigmoid, [pb], [(sgt, cc)])
        tp8(S, y_t, yT)
        for br, (W, dstt) in enumerate(((PA, mg), (PBw, m2))):
            for hf in range(2):
                pb = S.PB()
                mms(lambda e, pb=pb, br=br, hf=hf, W=W: [e.matmul(pb[:], lhsT=yT[:, br * 4 + c, :], rhs=W[:, c, hf * 512:(hf + 1) * 512], start=(c == 0), stop=(c == 3)) for c in range(4)][-1],
                    [yT, W], [pb])
                tt("dve", dstt[:, hf * 512:(hf + 1) * 512], pb[:], sgt[:, br * 1024 + hf * 512:br * 1024 + (hf + 1) * 512], ALU.mult, [pb, sgt], [(dstt, hf)])
        tt("pool", mg[:], mg[:], m2[:], ALU.add, [mg, m2], [mg])
        tp8(S, mg, mgT)
        ho = S.h1()
        for hf in range(2):
            pb = S.PB()
            mms(lambda e, pb=pb, hf=hf: [e.matmul(pb[:], lhsT=mgT[:, c, :], rhs=WO[:, c, hf * 512:(hf + 1) * 512], start=(c == 0), stop=(c == 7)) for c in range(8)][-1],
                [mgT, WO], [pb])
            tt("dve", ho[:, hf * 512:(hf + 1) * 512], pb[:], x_t[:, hf * 512:(hf + 1) * 512], ALU.add, [pb, x_t], [(ho, hf)])
        if final:
            act(xs[:], ho[:], AF.Square, [ho], [xs, ss], accum_out=ss[:])
            act(rstd[:], ss[:], AF.Sqrt, [ss], [rstd], bias=1e-5, scale=1.0 / 1024)
            p.op("dve", lambda e: e.reciprocal(out=rstd[:], in_=rstd[:]), [rstd], [rstd])
            stt("dve", ho[:], ho[:], rstd[:, 0:1], fin[:], ALU.mult, ALU.mult, [ho, rstd, fin], [ho])
            dma("sp", D["out"][i * 128:(i + 1) * 128, :], ho[:], [ho], [])
        else:
            dma("sp", D["h1_scr"][i * 128:(i + 1) * 128, :], ho[:], [ho], [(D["h1_t"], i)])

    for k in range(NS):
        prefetch(streams[k], k)
    per = (len(drip) + max(nt // NS - 1, 1) - 1) // max(nt // NS - 1, 1) if drip else 0
    for i0 in range(0, nt, NS):
        lists = []
        for k in range(NS):
            if i0 + k < nt:
                p.defer_begin()
                body(streams[k], i0 + k)
                lists.append(p.defer_end())
        while any(lists):
            for lst in lists:
                if lst:
                    p.drain(lst, 1)
        if drip:
            p.drain(drip, per)
    if drip:
        p.drain(drip, len(drip))


def phase_b0(p, nc, D, eng_rot=("dve", "pool"), nbuf=2, ldq="sp", stq="act"):
    h = helpers(p)
    dma, cp = h.dma, h.cp
    stg = Rot([p.sb("cs%d" % i, [128, 4096], F32) for i in range(nbuf)])
    ob = Rot([p.sb("co%d" % i, [128, 4096], BF16) for i in range(nbuf)])
    nwp0 = p.sb("nwp0", [128, 16], F32)
    dma("sp", nwp0[:], D["nwp"], [], [nwp0])
    k = 0
    for g in range(32):
        s, o = stg(), ob()
        dma(ldq, s[:].rearrange("p (dc e) -> p dc e", dc=8), D["uT"][:, g * 512:(g + 1) * 512].rearrange("(dc p) e -> p dc e", p=128), [], [s])
        h.tt(eng_rot[k % 2], o[:].rearrange("p (i dc e) -> p dc i e", i=4, dc=8), s[:].rearrange("p (dc i e) -> p dc i e", dc=8, i=4),
             nwp0[:, 8:16].unsqueeze(2).unsqueeze(3).to_broadcast([128, 8, 4, 128]), ALU.mult, [s, nwp0], [o])
        k += 1
        dma(stq, D["u2"][:, g * 4:(g + 1) * 4, :, :].rearrange("p i dc e -> p (i dc e)"), o[:], [o], [(D["u2_t"], g)])
        s, o = stg(), ob()
        dma(ldq, s[:].rearrange("p (i d) -> p i d", i=4), D["v"][g * 512:(g + 1) * 512, :].rearrange("(i p) d -> p i d", p=128), [], [s])
        cp(eng_rot[k % 2], o[:], s[:], [s], [o])
        k += 1
        dma(stq, D["vb"][g * 512:(g + 1) * 512, :].rearrange("(i p) d -> p i d", p=128), o[:].rearrange("p (i d) -> p i d", i=4), [o], [(D["vb_t"], g)])


def phase_b(p, nc, D, ntok):
    h = helpers(p)
    tt, stt, ts, act, cp, red, dma, mms = h.tt, h.stt, h.ts, h.act, h.cp, h.red, h.dma, h.mms
    TT = 256
    cst = p.sb("cstc", [128, 256], F32)
    dma("sp", cst[:, 0:128], D["cst"][:, C_ID:C_ID + 128], [], [cst])
    dma("sp", cst[:, 128:256], D["cst"][:, C_IOTA:C_IOTA + 128], [], [cst])
    ident = cst[:, 0:128]
    iota = cst[:, 128:256]
    iota_bf = p.sb("iota_bf", [128, 128], BF16)
    cp("dve", iota_bf[:], iota, [cst], [iota_bf])
    fin = p.sb("finc", [128, 1024], F32)
    dma("sp", fin[:], D["vecs"][V_FIN:V_FIN + 1024].partition_broadcast(128), [], [fin])
    nwpb = p.sb("nwpc", [128, 16], F32)
    dma("sp", nwpb[:], D["nwp"], [], [nwpb])
    skT = p.sb("skT", [128, 8, 128], F32)
    dma("sp", skT[:], D["skT"], [], [skT])
    G = p.sb("G", [128, TT, 128], BF16)
    xs = p.sb("xsc", [128, 1024], F32)
    Wq = load_w_bf16(p, h, "Wq", D["peer_wq"], 1024, 1024, 0, 1024, (lambda: xs), nwpb, 8)
    U2 = Rot([p.sb("u2t%d" % i, [128, 4, 8, 128], BF16) for i in range(3)])
    Vt = Rot([p.sb("vt%d" % i, [128, 4, 1024], BF16) for i in range(3)])
    h1 = [[p.sb("h1c%d_%d" % (b, i), [128, 1024], F32) for i in range(2)] for b in range(2)]
    ss = p.sb("ssc", [128, 1], F32)
    rstd = p.sb("rstdc", [128, 1], F32)
    xTb = [p.sb("xs2T%d" % b, [128, 8, TT], BF16) for b in range(2)]
    qT = p.sb("qTc", [128, 8, TT], F32)
    sc = p.sb("sc", [128, 16, 128], F32)
    v16 = p.sb("v16", [128, 16, 16], F32)
    i16u = p.sb("i16u", [128, 16, 16], U32)
    i16f = p.sb("i16f", [128, 16, 16], F32)
    cand = p.sb("cand", [128, 8, 256], F32)
    tv = p.sb("tv", [128, 8, 16], F32)
    posu = p.sb("posu", [128, 8, 16], U32)
    au = p.sb("au", [128, 8, 16], U32)
    bu = p.sb("bu", [128, 8, 16], U32)
    af = p.sb("af", [128, 8, 16], F32)
    bf_ = p.sb("bf", [128, 8, 16], F32)
    sel = p.sb("sel", [128, 3, 128], F32)
    sm = Rot([p.sb("smc%d" % i, [128, 8], F32) for i in range(4)])
    selTb = [p.sb("selT%d" % b, [128, 3, TT], F32) for b in range(2)]
    OA = Rot([p.sb("oa%d" % i, [128, 16, 128], BF16) for i in range(2)])
    OB = Rot([p.sb("ob%d" % i, [128, 16, 128], BF16) for i in range(2)])
    gh = Rot([p.sb("gh%d" % i, [128, TT], BF16) for i in range(2)])
    ac = Rot([p.sb("ac%d" % i, [128, TT], BF16) for i in range(3)])
    pbs = [p.ps("pr%d" % i, [128, 512], F32) for i in range(4)]
    PBH = Rot(pbs[0:3])
    PBP = Rot(pbs[3:4])
    PBG = Rot(pbs)
    ACC = [p.ps("acc%d" % i, [128, 512], F32) for i in range(4)]
    ntile = ntok // TT

    def prep(tix):
        b = tix % 2
        t0 = tix * TT
        xT, selT = xTb[b], selTb[b]
        PB = PBP
        for s in range(2):
            hh1 = h1[b][s]
            dma("sp", hh1[:], D["h1_scr"][t0 + s * 128:t0 + (s + 1) * 128, :], [(D["h1_t"], tix * 2 + s)], [hh1])
            act(xs[:], hh1[:], AF.Square, [hh1], [xs, ss], accum_out=ss[:])
            act(rstd[:], ss[:], AF.Sqrt, [ss], [rstd], bias=1e-5, scale=1.0 / 1024)
            p.op("dve", lambda e: e.reciprocal(out=rstd[:], in_=rstd[:]), [rstd], [rstd])
            ts("dve", xs[:], hh1[:], rstd[:, 0:1], None, ALU.mult, None, [hh1, rstd], [xs])
            for hh in range(2):
                pb = PB()
                mms(lambda e, pb=pb, hh=hh: [e.transpose(out=pb[:, j * 128:(j + 1) * 128], in_=xs[:, (hh * 4 + j) * 128:(hh * 4 + j + 1) * 128], identity=ident) for j in range(4)][-1],
                    [xs, cst], [pb])
                cp("act" if hh else "dve", xT[:, hh * 4:hh * 4 + 4, s * 128:(s + 1) * 128], pb[:].rearrange("p (a b) -> p a b", a=4), [pb], [(xT, (s, hh))])
        for c in range(8):
            pb = PB()
            mms(lambda e, pb=pb, c=c, xT=xT: [e.matmul(pb[:, 0:TT], lhsT=Wq[:, dc, c * 128:(c + 1) * 128], rhs=xT[:, dc, :], start=(dc == 0), stop=(dc == 7)) for dc in range(8)][-1],
                [Wq, xT], [pb])
            cp("act" if c % 2 else "dve", qT[:, c, :], pb[:, 0:TT], [pb], [(qT, c)])
        for s in range(0 if "S" in SKIP else 2):
            for par in range(2):
                for half in range(2):
                    pb = PB()
                    mms(lambda e, pb=pb, par=par, half=half, s=s: [e.matmul(pb[:, j * 128:(j + 1) * 128], lhsT=qT[64 * par:64 * par + 64, half * 4 + j, s * 128:(s + 1) * 128],
                                                                        rhs=skT[64 * par:64 * par + 64, half * 4 + j, :], start=True, stop=True) for j in range(4)][-1],
                        [qT, skT], [pb])
                    cp("act", sc[:, 8 * half + par:8 * half + 8:2, :], pb[:].rearrange("p (a b) -> p a b", a=4), [pb], [(sc, 8 * half + par + 2 * j) for j in range(4)])
            tmpA = cand[:].rearrange("p h (a b) -> p (h a) b", a=2)
            for hp in range(16):
                p.op("dve", lambda e, hp=hp: e.max(out=v16[:, hp, 0:8], in_=sc[:, hp, :]), [(sc, hp)], [(v16, hp)])
            for hp in range(16):
                p.op("dve", lambda e, hp=hp, tmpA=tmpA: e.match_replace(out=tmpA[:, hp, :], in_to_replace=v16[:, hp, 0:8], in_values=sc[:, hp, :], imm_value=-1e30), [(sc, hp), (v16, hp)], [(cand, hp)])
            for hp in range(16):
                p.op("dve", lambda e, hp=hp, tmpA=tmpA: e.max(out=v16[:, hp, 8:16], in_=tmpA[:, hp, :]), [(cand, hp)], [(v16, hp)])
            for hp in range(16):
                p.op("dve", lambda e, hp=hp: e.max_index(out=i16u[:, hp, 0:8], in_max=v16[:, hp, 0:8], in_values=sc[:, hp, :]), [(sc, hp), (v16, hp)], [(i16u, hp)])
            for hp in range(16):
                p.op("dve", lambda e, hp=hp: e.max_index(out=i16u[:, hp, 8:16], in_max=v16[:, hp, 8:16], in_values=sc[:, hp, :]), [(sc, hp), (v16, hp)], [(i16u, hp)])
            cp("pool", i16f[:], i16u[:], [i16u], [i16f])
            tt("pool", cand[:].rearrange("p h (a b) -> p h a b", a=16), v16[:, 0:16:2, :].unsqueeze(3).to_broadcast([128, 8, 16, 16]),
               v16[:, 1:16:2, :].unsqueeze(2).to_broadcast([128, 8, 16, 16]), ALU.add, [v16], [cand])
            tmpB = sc[:].rearrange("p (h a) b -> p h (a b)", a=2)
            for hd in range(8):
                p.op("dve", lambda e, hd=hd: e.max(out=tv[:, hd, 0:8], in_=cand[:, hd, :]), [cand], [(tv, hd)])
            for hd in range(8):
                p.op("dve", lambda e, hd=hd, tmpB=tmpB: e.match_replace(out=tmpB[:, hd, :], in_to_replace=tv[:, hd, 0:8], in_values=cand[:, hd, :], imm_value=-1e30), [cand, (tv, hd)], [(sc, 2 * hd), (sc, 2 * hd + 1)])
            for hd in range(8):
                p.op("dve", lambda e, hd=hd, tmpB=tmpB: e.max(out=tv[:, hd, 8:16], in_=tmpB[:, hd, :]), [(sc, 2 * hd), (sc, 2 * hd + 1)], [(tv, hd)])
            for hd in range(8):
                p.op("dve", lambda e, hd=hd: e.max_index(out=posu[:, hd, 0:8], in_max=tv[:, hd, 0:8], in_values=cand[:, hd, :]), [cand, (tv, hd)], [(posu, hd)])
            for hd in range(8):
                p.op("dve", lambda e, hd=hd: e.max_index(out=posu[:, hd, 8:16], in_max=tv[:, hd, 8:16], in_values=cand[:, hd, :]), [cand, (tv, hd)], [(posu, hd)])
            gt = sel[:, 2, :].rearrange("p (h k) -> p h k", h=8)
            tt("pool", gt, tv[:], tv[:, :, 0:1].to_broadcast([128, 8, 16]), ALU.subtract, [tv], [(sel, 2)])
            act(gt, gt, AF.Exp, [(sel, 2)], [(sel, 2)])
            z8 = sm()
            red(z8[:], gt, [(sel, 2)], [z8])
            p.op("dve", lambda e, z8=z8: e.reciprocal(out=z8[:], in_=z8[:]), [z8], [z8])
            tt("dve", gt, gt, z8[:].unsqueeze(2).to_broadcast([128, 8, 16]), ALU.mult, [(sel, 2), z8], [(sel, 2)])
            ts("dve", au[:], posu[:], 4, None, ALU.logical_shift_right, None, [posu], [au])
            ts("dve", bu[:], posu[:], 15, None, ALU.bitwise_and, None, [posu], [bu])
            cp("pool", af[:], au[:], [au], [af])
            cp("pool", bf_[:], bu[:], [bu], [bf_])
            io16 = iota[:, 0:16].unsqueeze(1).unsqueeze(1).to_broadcast([128, 8, 16, 16])
            eq = cand[:].rearrange("p h (a b) -> p h a b", a=16)
            for w, (xf, par) in enumerate(((af, 0), (bf_, 1))):
                tt("dve", eq, io16, xf[:].unsqueeze(3).to_broadcast([128, 8, 16, 16]), ALU.is_equal, [cst, xf], [cand])
                tt("pool", eq, eq, i16f[:, par:16:2, :].unsqueeze(2).to_broadcast([128, 8, 16, 16]), ALU.mult, [cand, i16f], [cand])
                red(sel[:, w, :].rearrange("p (h k) -> p h k", h=8), eq, [cand], [(sel, w)])
            pb = PB()
            mms(lambda e, pb=pb: [e.transpose(out=pb[:, w * 128:(w + 1) * 128], in_=sel[:, w, :], identity=ident) for w in range(3)][-1], [sel, cst], [pb])
            cp("act", selT[:, :, s * 128:(s + 1) * 128], pb[:, 0:384].rearrange("p (a b) -> p a b", a=3), [pb], [(selT, s)])

    def gbuild(tix):
        selT = selTb[tix % 2]
        PB = PBG
        NG = 0 if "G" in SKIP else TT // 16
        bufs = {}

        def onehots(g):
            tk = g * 16
            oa, ob = OA(), OB()
            bufs[g] = (oa, ob)
            io = iota_bf[:, :].unsqueeze(1).to_broadcast([128, 16, 128])
            if "o" in SKIP:
                return
            tt("dve", oa[:], io, selT[:, 0, tk:tk + 16].unsqueeze(2).to_broadcast([128, 16, 128]), ALU.is_equal, [iota_bf, selT], [oa])
            for t in range(16):
                act(oa[:, t, :], oa[:, t, :], AF.Copy, [(oa, t), selT], [(oa, t)], scale=selT[:, 2, tk + t:tk + t + 1])
            tt("dve", ob[:], io, selT[:, 1, tk:tk + 16].unsqueeze(2).to_broadcast([128, 16, 128]), ALU.is_equal, [iota_bf, selT], [ob])

        def mm_evac(g):
            tk = g * 16
            oa, ob = bufs.pop(g)
            for q4 in range(4):
                pb = PB()
                if "m" not in SKIP:
                  mms(lambda e, pb=pb, q4=q4, oa=oa, ob=ob: [e.matmul(pb[:, j * 128:(j + 1) * 128], lhsT=ob[:, q4 * 4 + j, :], rhs=oa[:, q4 * 4 + j, :], start=True, stop=True) for j in range(4)][-1],
                    [oa, ob], [pb])
                tq = tk + q4 * 4
                if "v" not in SKIP:
                  cp("act" if q4 != 3 else "dve", G[:, tq:tq + 4, :], pb[:].rearrange("p (t i) -> p t i", t=4), [pb], [(G, tq)])

        if NG:
            onehots(0)
        for g in range(NG):
            if g + 1 < NG:
                onehots(g + 1)
            mm_evac(g)

    def expert(tix, nxt):
        xT = xTb[tix % 2]
        LOOK = 2
        grp = {}
        hb = {}

        def emit_H(i):
            ig, ii = divmod(i, 4)
            if ii == 0:
                u2, vt = U2(), Vt()
                grp[ig] = (u2, vt)
                if not ("D" in SKIP and ig >= 2):
                    dma("sp", vt[:], D["vb"][ig * 512:(ig + 1) * 512, :].rearrange("(i p) d -> p i d", p=128), [(D["vb_t"], ig)], [vt])
                    dma("sp", u2[:].rearrange("p i dc e -> p (i dc e)"), D["u2"][:, ig * 4:(ig + 1) * 4, :, :].rearrange("p i dc e -> p (i dc e)"), [(D["u2_t"], ig)], [u2])
            u2, vt = grp[ig]
            pb = PBH()
            mms(lambda e, pb=pb, u2=u2, ii=ii: [e.matmul(pb[:, 0:TT], lhsT=u2[:, ii, dc, :], rhs=xT[:, dc, :], start=(dc == 0), stop=(dc == 7)) for dc in range(8)][-1],
                [u2, xT], [pb])
            hb[i] = pb

        def emit_rest(i):
            ig, ii = divmod(i, 4)
            u2, vt = grp[ig]
            pb = hb.pop(i)
            g_, a_ = gh(), ac()
            if "X" not in SKIP:
                act(g_[:], pb[:, 0:TT], AF.Gelu, [pb], [g_])
                tt("dve", a_[:], g_[:], G[:, :, i], ALU.mult, [g_, G], [a_])
            mms(lambda e, a_=a_, vt=vt, ii=ii, i=i: [e.matmul(ACC[s * 2 + hf][:], lhsT=a_[:, s * 128:(s + 1) * 128], rhs=vt[:, ii, hf * 512:(hf + 1) * 512], start=(i == 0), stop=(i == 127))
                                                   for s in range(2) for hf in range(2)][-1], [a_, vt], ACC)

        NE = 0 if "E" in SKIP else 128
        per = (len(nxt) + 99) // 100 if nxt else 0
        for i in range(min(LOOK, NE)):
            emit_H(i)
        for i in range(NE):
            if i + LOOK < NE:
                emit_H(i + LOOK)
            emit_rest(i)
            if nxt:
                p.drain(nxt, per)
        if nxt:
            p.drain(nxt, len(nxt))

    def epilogue(tix):
        b = tix % 2
        t0 = tix * TT
        for s in range(2):
            hh1 = h1[b][s]
            for hf in range(2):
                tt("dve", hh1[:, hf * 512:(hf + 1) * 512], ACC[s * 2 + hf][:], hh1[:, hf * 512:(hf + 1) * 512], ALU.add, [ACC[s * 2 + hf], hh1], [hh1])
            act(xs[:], hh1[:], AF.Square, [hh1], [xs, ss], accum_out=ss[:])
            act(rstd[:], ss[:], AF.Sqrt, [ss], [rstd], bias=1e-5, scale=1.0 / 1024)
            p.op("dve", lambda e: e.reciprocal(out=rstd[:], in_=rstd[:]), [rstd], [rstd])
            stt("dve", hh1[:], hh1[:], rstd[:, 0:1], fin[:], ALU.mult, ALU.mult, [hh1, rstd, fin], [hh1])
            dma("act", D["out"][t0 + s * 128:t0 + (s + 1) * 128, :], hh1[:], [hh1], [])

    prep(0)
    for tix in range(ntile):
        gbuild(tix)
        nxt = []
        if tix + 1 < ntile:
            p.defer_begin()
            prep(tix + 1)
            nxt = p.defer_end()
        expert(tix, nxt)
        epilogue(tix)


N_CORES = 8


def _build(n_seq, n_tiles, ret_d=False, phases="0123"):
    nc = bass.Bass("TRN2", target_bir_lowering=False, dynamic_dma_scratch_size=2048)
    ntok = n_seq * n_tiles * 128
    D = {}

    def din(name, shape):
        D[name] = nc.dram_tensor(name, list(shape), F32, kind="ExternalInput").ap()

    din("x", (ntok, 1024)); din("w_in", (1024, 4640)); din("vecs", (NVEC,)); din("nwp", (128, 16)); din("cst", (128, NCST))
    din("decay_w2", (64, 512)); din("iclr_a2", (64, 512)); din("gate_g2", (160, 512))
    din("proj_attn", (512, 1024)); din("proj_rwkv", (512, 1024)); din("w_out", (1024, 1024))
    din("peer_wq", (1024, 1024)); din("skT", (128, 8, 128)); din("uT", (1024, 16384)); din("v", (16384, 1024))
    D["y_scr"] = nc.dram_tensor("y_scr", [ntok, 1024], F32, kind="Internal").ap()
    D["h1_scr"] = D["y_scr"]
    NA = 2 * 16384 * 1024
    arena = nc.dram_tensor("arena", [NA], BF16, kind="Internal").ap()
    assert ntok * 1824 * 2 <= NA
    D["zr_scr"] = arena[0:ntok * 1824 * 2].bitcast(F32).rearrange("(t c) -> t c", c=1824)
    D["u2"] = arena[0:16384 * 1024].rearrange("(p i dc e) -> p i dc e", p=128, i=128, dc=8)
    D["vb"] = arena[16384 * 1024:NA].rearrange("(r c) -> r c", c=1024)
    D["out"] = nc.dram_tensor("out", [ntok, 1024], F32, kind="ExternalOutput").ap()
    p = Prog(nc)
    for nm in ("y_t", "zr_t", "h1_t", "u2_t", "vb_t"):
        D[nm] = p.wrap(None, nm)
    m = p.mark()
    if "1" in phases or "a" in phases:
        phase_a1a(p, nc, D, n_seq, n_tiles, n_tiles * 128)
        p.release(m)
    if "1" in phases or "b" in phases:
        phase_a1b(p, nc, D, n_seq, n_tiles, n_tiles * 128)
        p.release(m)
    if "0" in phases:
        phase_b0(p, nc, D, stq="act")
        p.release(m)
    if "2" in phases:
        phase_a2(p, nc, D, ntok, final=("3" not in phases))
        p.release(m)
    if "3" in phases:
        phase_b(p, nc, D, ntok)
    p.emit()
    p.close()
    return (nc, D) if ret_d else nc


def _inputs(x, norm_mix_w, w_in, shift_mu, attn_sinks, decay_w0, decay_w2, iclr_a0, iclr_a2, gate_g2, k_k, k_a, r_k,
            ln_x_w, ln_x_b, proj_attn, proj_rwkv, w_out, norm_ffn_w, peer_wq, peer_subkeys, peer_u, peer_v, norm_final_w):
    f = lambda a: np.ascontiguousarray(np.asarray(a, dtype=np.float32))
    v = np.zeros(NVEC, np.float32)
    v[V_MU:V_MU + 1824] = f(shift_mu)[0]; v[V_W0:V_W0 + 512] = f(decay_w0)[0]; v[V_A0:V_A0 + 512] = f(iclr_a0)[0]
    v[V_KK:V_KK + 512] = f(k_k)[0]; v[V_KA:V_KA + 512] = f(k_a)[0]; v[V_LNW:V_LNW + 512] = f(ln_x_w)[0]
    v[V_LNB:V_LNB + 512] = f(ln_x_b)[0]; v[V_RK:V_RK + 512] = f(r_k)[0].reshape(-1); v[V_SINK:V_SINK + 8] = f(attn_sinks)[0]
    v[V_FIN:] = f(norm_final_w)
    nwp = np.ones((128, 16), np.float32)
    nwp[:, 0:8] = f(norm_mix_w)[0].reshape(8, 128).T
    nwp[:, 8:16] = f(norm_ffn_w)[0].reshape(8, 128).T
    sk = f(peer_subkeys)[0]
    skT = np.ascontiguousarray(sk.transpose(1, 3, 0, 2).reshape(128, 8, 128))
    return dict(w_in=f(w_in)[0], vecs=v, nwp=nwp, cst=make_cst(), decay_w2=f(decay_w2)[0], iclr_a2=f(iclr_a2)[0],
                gate_g2=f(gate_g2)[0], proj_attn=f(proj_attn)[0], proj_rwkv=f(proj_rwkv)[0], w_out=f(w_out)[0],
                peer_wq=f(peer_wq)[0], skT=skT, uT=np.ascontiguousarray(f(peer_u)[0].T), v=f(peer_v)[0])


def kernel(**inputs):
    x = np.ascontiguousarray(np.asarray(inputs["x"], dtype=np.float32))
    B, S, Dm = x.shape
    common = _inputs(**inputs)
    spc = B // N_CORES
    nc = _build(spc, S // 128)
    in_maps = []
    for c in range(N_CORES):
        d = dict(common)
        d["x"] = np.ascontiguousarray(x[c * spc:(c + 1) * spc].reshape(spc * S, Dm))
        in_maps.append(d)
    res = run_bass_kernel_spmd(nc, in_maps, core_ids=list(range(N_CORES)))
    out = np.concatenate([r["out"].reshape(spc, S, Dm) for r in res.results], axis=0)
    return out.astype(np.float32)
```

```python
import numpy as np
import concourse.bass as bass
import concourse.mybir as mybir

F32 = mybir.dt.float32
BF16 = mybir.dt.bfloat16
U32 = mybir.dt.uint32
I32 = mybir.dt.int32
ALU = mybir.AluOpType
AF = mybir.ActivationFunctionType
AX = mybir.AxisListType

NDMA_SEMS = 12
import os as _os
NOSELF = tuple(_os.environ.get("NOSELF", "").split(","))


class T:
    def __init__(self, h, name):
        self.h = h
        self.name = name
        self.state = {}

    def __getitem__(self, k):
        return self.h[k]


class Prog:
    def __init__(self, nc):
        self.nc = nc
        self.ops = {e: [] for e in ("pe", "dve", "act", "pool", "sp")}
        self.cms = []
        self.ndma = {e: 0 for e in ("sp", "act", "pool")}
        self._defer = None

    def sb(self, name, shape, dtype):
        self._uid = getattr(self, "_uid", 0) + 1
        cm = self.nc.sbuf_tensor("sb%d_" % self._uid + name, list(shape), dtype)
        h = cm.__enter__()
        self.cms.append(cm)
        return T(h, name)

    def ps(self, name, shape, dtype):
        self._uid = getattr(self, "_uid", 0) + 1
        cm = self.nc.psum_tensor("ps%d_" % self._uid + name, list(shape), dtype)
        h = cm.__enter__()
        self.cms.append(cm)
        return T(h, name)

    def wrap(self, ap, name):
        return T(ap, name)

    def defer_begin(self):
        self._defer = []

    def defer_end(self):
        lst, self._defer = self._defer, None
        return lst

    def drain(self, lst, k):
        for _ in range(min(k, len(lst))):
            eng, fn, reads, writes, dma, extra = lst.pop(0)
            self.op(eng, fn, reads, writes, dma, extra)

    def mark(self):
        return len(self.cms)

    def release(self, mark):
        lasts = []
        for e in ("pe", "dve", "act", "pool", "sp"):
            for j in range(len(self.ops[e]) - 1, -1, -1):
                if self.ops[e][j][2][0] not in ("dma", "bar"):
                    lasts.append((e, j))
                    break
        dmat = []
        for q in ("sp", "act", "pool"):
            n = self.ndma[q]
            dmat += [("dma", q, i) for i in range(max(0, n - NDMA_SEMS), n)]
        for e in ("pe", "dve", "act", "pool", "sp"):
            self.ops[e].append((None, lasts + dmat, ("bar", e, len(self.ops[e]))))
        while len(self.cms) > mark:
            self.cms.pop().__exit__(None, None, None)

    def _collect(self, t, key, is_write, deps):
        if key is None:
            keys = list(t.state.keys())
        else:
            keys = [k for k in (key, None) if k in t.state]
        for k in keys:
            w, rs = t.state[k]
            if w is not None:
                deps.append(w)
            if is_write:
                deps.extend(rs)

    def _update(self, t, key, is_write, me):
        if is_write:
            if key is None:
                t.state = {None: [me, []]}
            else:
                t.state[key] = [me, []]
        else:
            st = t.state.setdefault(key, [None, []])
            if me[0] != "dma":
                st[1] = [r for r in st[1] if not (r[0] == me[0])]
            st[1].append(me)

    def op(self, eng, fn, reads=(), writes=(), dma=False, extra=()):
        if self._defer is not None:
            self._defer.append((eng, fn, reads, writes, dma, extra))
            return None
        deps = list(extra)
        norm = lambda x: x if isinstance(x, tuple) else (x, None)
        reads = [norm(r) for r in reads]
        writes = [norm(w) for w in writes]
        for t, k in reads:
            self._collect(t, k, False, deps)
        for t, k in writes:
            self._collect(t, k, True, deps)
        idx = len(self.ops[eng])
        if dma:
            n = self.ndma[eng]
            self.ndma[eng] += 1
            me = ("dma", eng, n)
        else:
            me = (eng, idx)
        for t, k in reads:
            self._update(t, k, False, me)
        for t, k in writes:
            self._update(t, k, True, me)
        self.ops[eng].append((fn, deps, me))
        return me

    def emit(self):
        nc = self.nc
        engs = ("pe", "dve", "act", "pool", "sp")
        sem_cms = {}
        sems = {}
        for e in engs:
            cm = nc.semaphore("s_" + e)
            sems[e] = cm.__enter__()
            self.cms.append(cm)
        dsems = {}
        for e in ("sp", "act", "pool"):
            if self.ndma[e]:
                lst = []
                for i in range(NDMA_SEMS):
                    cm = nc.semaphore("d_%s_%d" % (e, i))
                    lst.append(cm.__enter__())
                    self.cms.append(cm)
                dsems[e] = lst
        ops = self.ops

        def run(ename, engine):
            seen = {}
            cnt = 0
            for fn, deps, me in ops[ename]:
                need = {}
                for d in deps:
                    if d[0] == "dma":
                        s = dsems[d[1]][d[2] % NDMA_SEMS]
                        v = 16 * (d[2] // NDMA_SEMS + 1)
                    else:
                        if d[0] == ename:
                            if ename == "pe" or fn is None or ename in NOSELF:
                                continue
                        s = sems[d[0]]
                        v = d[1] + 1 - self.dma_before[d[0]][d[1]]
                    key = s.num if hasattr(s, "num") else id(s)
                    if v > need.get(key, (None, 0))[1]:
                        need[key] = (s, v)
                if me[0] == "dma":
                    n = me[2]
                    s = dsems[ename][n % NDMA_SEMS]
                    if n >= NDMA_SEMS:
                        key = s.num if hasattr(s, "num") else id(s)
                        v = 16 * (n // NDMA_SEMS)
                        if v > need.get(key, (None, 0))[1]:
                            need[key] = (s, v)
                for key, (s, v) in need.items():
                    if seen.get(key, 0) >= v:
                        continue
                    engine.wait_ge(s, v)
                    seen[key] = v
                if fn is None:
                    continue
                ins = fn(engine)
                if me[0] == "dma":
                    ins.then_inc(dsems[ename][me[2] % NDMA_SEMS], 16)
                else:
                    ins.then_inc(sems[ename], 1)

        self.dma_before = {}
        for e in engs:
            c = 0
            lst = []
            for fn, deps, me in ops[e]:
                lst.append(c)
                if me[0] in ("dma", "bar"):
                    c += 1
            self.dma_before[e] = lst

        with nc.Block() as block:
            @block.tensor
            def _(eng):
                run("pe", eng)

            @block.vector
            def _(eng):
                run("dve", eng)

            @block.scalar
            def _(eng):
                run("act", eng)
                self._drain("act", eng, dsems)

            @block.gpsimd
            def _(eng):
                run("pool", eng)
                self._drain("pool", eng, dsems)

            @block.sync
            def _(eng):
                run("sp", eng)
                self._drain("sp", eng, dsems)

    def _drain(self, e, eng, dsems):
        n = self.ndma[e]
        if not n:
            return
        for j in range(min(n, NDMA_SEMS)):
            last = ((n - 1 - j) // NDMA_SEMS) * NDMA_SEMS + j
            eng.wait_ge(dsems[e][j], 16 * (last // NDMA_SEMS + 1))

    def close(self):
        for cm in reversed(self.cms):
            cm.__exit__(None, None, None)
from concourse.bass_utils import run_bass_kernel_spmd

D_MODEL = 1024
C_DEC = 0.6065306597126334
STAGE = 0
SKIP = ""
NZRB = 2
NSB = 1
V_MU, V_W0, V_A0, V_KK, V_KA, V_LNW, V_LNB, V_RK, V_SINK, V_FIN = 0, 1824, 2336, 2848, 3360, 3872, 4384, 4896, 5408, 5416
NVEC = 5416 + 1024
C_ID, C_SU, C_IU, C_SL, C_BD, C_SH, C_CA, C_COS, C_SIN, C_ONE, C_IOTA = 0, 128, 256, 384, 512, 640, 768, 896, 1408, 1920, 1921
NCST = 1921 + 128


def make_cst():
    c = np.zeros((128, NCST), np.float32)
    i = np.arange(128)
    r, q = i[:, None], i[None, :]
    c[:, C_ID:C_ID + 128] = (r == q)
    c[:, C_SU:C_SU + 128] = (r < q)
    c[:, C_IU:C_IU + 128] = (r <= q)
    c[:, C_SL:C_SL + 128] = (r > q)
    c[:, C_BD:C_BD + 128] = ((r // 64) == (q // 64))
    c[:, C_SH:C_SH + 128] = (r == q - 1)
    c[127, C_CA] = 1.0
    inv = 10000.0 ** (-np.arange(0, 64, 2, dtype=np.float32) / 64)
    pos = (np.arange(16)[None, :] * 128 + i[:, None]).astype(np.float32)
    ang = pos[:, :, None] * inv[None, None, :]
    c[:, C_COS:C_COS + 512] = np.cos(ang).reshape(128, 512)
    c[:, C_SIN:C_SIN + 512] = np.sin(ang).reshape(128, 512)
    c[:, C_ONE] = 1.0
    c[:, C_IOTA:C_IOTA + 128] = q
    return c


class Ctx:
    pass


def helpers(p):
    h = Ctx()

    def tt(eng, out, in0, in1, op, r, w):
        p.op(eng, lambda e: e.tensor_tensor(out=out, in0=in0, in1=in1, op=op), r, w)

    def stt(eng, out, in0, scalar, in1, op0, op1, r, w):
        p.op(eng, lambda e: e.scalar_tensor_tensor(out=out, in0=in0, scalar=scalar, in1=in1, op0=op0, op1=op1), r, w)

    def ts(eng, out, in0, s1, s2, op0, op1, r, w):
        if s2 is None:
            p.op(eng, lambda e: e.tensor_scalar(out=out, in0=in0, scalar1=s1, scalar2=None, op0=op0), r, w)
        else:
            p.op(eng, lambda e: e.tensor_scalar(out=out, in0=in0, scalar1=s1, scalar2=s2, op0=op0, op1=op1), r, w)

    def act(out, in_, func, r, w, **kw):
        p.op("act", lambda e: e.activation(out=out, in_=in_, func=func, **kw), r, w)

    def cp(eng, out, in_, r, w):
        if eng == "act":
            p.op("act", lambda e: e.activation(out=out, in_=in_, func=AF.Copy), r, w)
        else:
            p.op(eng, lambda e: e.tensor_copy(out=out, in_=in_), r, w)

    def red(out, in_, r, w, op=ALU.add):
        p.op("dve", lambda e: e.tensor_reduce(out=out, in_=in_, axis=AX.X, op=op), r, w)

    def dma(eng, out, in_, r, w):
        p.op(eng, lambda e: e.dma_start(out=out, in_=in_), r, w, dma=True)

    def mms(fn, r, w):
        p.op("pe", fn, r, w)

    h.tt, h.stt, h.ts, h.act, h.cp, h.red, h.dma, h.mms = tt, stt, ts, act, cp, red, dma, mms
    return h


class Rot:
    def __init__(self, tiles):
        self.tiles = tiles
        self.i = 0

    def __call__(self):
        t = self.tiles[self.i % len(self.tiles)]
        self.i += 1
        return t


def load_w_bf16(p, h, name, dram, K, N, ncol0, ncols, stg, scale_t=None, scale_off=0, dt=BF16):
    kc = K // 128
    wt = p.sb(name, [128, kc, ncols], dt)
    for c in range(kc):
        s = stg()
        h.dma("sp", s[:, 0:ncols], dram[c * 128:(c + 1) * 128, ncol0:ncol0 + ncols], [], [s])
        if scale_t is not None:
            h.act(wt[:, c, :], s[:, 0:ncols], AF.Copy, [s, scale_t], [(wt, c)], scale=scale_t[:, scale_off + c:scale_off + c + 1])
        else:
            h.cp("pool", wt[:, c, :], s[:, 0:ncols], [s], [(wt, c)])
    return wt


def phase_a1(p, nc, D, n_seq, n_tiles, S_TOK):
    h = helpers(p)
    tt, stt, ts, act, cp, red, dma, mms = h.tt, h.stt, h.ts, h.act, h.cp, h.red, h.dma, h.mms
    NZ = 2592
    cst = p.sb("cst", [128, NCST], F32)
    dma("sp", cst[:], D["cst"], [], [cst])
    vec = p.sb("vec", [128, V_SINK + 8], F32)
    dma("sp", vec[:], D["vecs"][0:V_SINK + 8].partition_broadcast(128), [], [vec])
    nwp = p.sb("nwp", [128, 16], F32)
    dma("sp", nwp[:], D["nwp"], [], [nwp])
    ident = cst[:, C_ID:C_ID + 128]
    z = p.sb("z", [128, NZ], F32)
    Wb = load_w_bf16(p, h, "Wb1", D["w_in"], 1024, 4640, 0, NZ, (lambda: z), nwp, 0)
    w2 = p.sb("w2", [128, 512], F32)
    dma("sp", w2[0:64, :], D["decay_w2"], [], [w2])
    dma("sp", w2[64:128, :], D["iclr_a2"], [], [w2])
    g2 = p.sb("g2", [128, 2, 512], F32)
    dma("sp", g2[:, 0, :], D["gate_g2"][0:128, :], [], [g2])
    dma("sp", g2[0:32, 1, :], D["gate_g2"][128:160, :], [], [g2])
    negsink = p.sb("negsink", [128, 8], F32)
    ts("dve", negsink[:], vec[:, V_SINK:V_SINK + 8], -1.0, None, ALU.mult, None, [vec], [negsink])

    xt = Rot([p.sb("xt%d" % i, [128, 1024], F32) for i in range(2)])
    ss = p.sb("ss", [128, 1], F32)
    rstd = p.sb("rstd", [128, 1], F32)
    xs = p.sb("xs", [128, 1024], F32)
    xsT = p.sb("xsT", [128, 8, 128], BF16)
    carry = p.sb("carry", [128, 1824], F32)
    p.op("pool", lambda e: e.memset(carry[:], 0.0), [], [carry])
    zr = p.sb("zr", [128, 1824], F32)
    lin = p.sb("lin", [128, 288], F32)
    linT = p.sb("linT", [128, 3, 128], F32)
    T5 = Rot([p.sb("t5_%d" % i, [128, 512], F32) for i in range(12)])
    gv = p.sb("gv", [128, 512], F32)
    k2 = p.sb("k2", [128, 512], F32)
    M8 = Rot([p.sb("m8_%d" % i, [128, 8, 128], F32) for i in range(8)])
    M8d = [p.sb("m8d_%d" % i, [128, 8, 128], F32) for i in range(3)]
    Tq = Rot([p.sb("tq_%d" % i, [128, 4, 128], F32) for i in range(4)])
    sm = Rot([p.sb("sm_%d" % i, [128, 8], F32) for i in range(12)])
    PB = Rot([p.ps("pb%d" % i, [128, 512], F32) for i in range(6)])
    PV = [p.ps("pv%d" % i, [128, 512], F32) for i in range(2)]
    NDUM = 0
    if NDUM:
        dumb = p.ps("dumb", [128, 512], F32)
        mms0 = mms

        def mms(fn, r, w):
            def fn2(e):
                ins = fn(e)
                for _ in range(NDUM):
                    e.matmul(dumb[:], lhsT=Wb[:, 0, 0:128], rhs=Wb[:, 0, 0:512], start=True, stop=True)
                return ins
            mms0(fn2, r, w)
    ST = p.sb("ST", [128, 4, 128], F32)
    gC = p.sb("gC", [128, 4], F32)
    qk = p.sb("qk", [128, 10, 64], F32)
    kdup = p.sb("kdup", [128, 2, 2, 64], F32)
    qT = p.sb("qT", [128, 4, 128], BF16)
    kT = [p.sb("kT%d" % i, [128, 2, 128], BF16) for i in range(2)]
    va = [p.sb("va%d" % i, [128, 2, 65], BF16) for i in range(2)]
    for i in range(2):
        p.op("pool", lambda e, i=i: e.memset(va[i][:], 1.0), [], [va[i]])
    pT = Rot([p.sb("pT%d" % i, [128, 2, 128], BF16) for i in range(4)])
    yout = Rot([p.sb("yout%d" % i, [128, 1024], F32) for i in range(1)])

    def vb(off, n=512):
        return vec[:, off:off + n]

    order = [(b, n) for b in range(n_seq) for n in range(n_tiles)]
    xq = {}

    def prefetch(i):
        if i < len(order):
            b, n = order[i]
            t0 = b * S_TOK + n * 128
            xq[i] = xt()
            dma("sp", xq[i][:], D["x"][t0:t0 + 128, :], [], [xq[i]])

    prefetch(0)

    def tile_body(i):
            b, n = order[i]
            if n == 0:
                p.op("pool", lambda e: e.memset(ST[:], 0.0), [], [ST])
            tok0 = b * S_TOK + n * 128
            first = (n == 0)
            x_t = xq.pop(i)
            prefetch(i + 1)
            act(xs[:], x_t[:], AF.Square, [x_t], [xs, ss], accum_out=ss[:])
            act(rstd[:], ss[:], AF.Sqrt, [ss], [rstd], bias=1e-5, scale=1.0 / 1024)
            p.op("dve", lambda e: e.reciprocal(out=rstd[:], in_=rstd[:]), [rstd], [rstd])
            ts("dve", xs[:], x_t[:], rstd[:, 0:1], None, ALU.mult, None, [x_t, rstd], [xs])
            for hh in range(2):
                pb = PB()
                mms(lambda e, pb=pb, hh=hh: [e.transpose(out=pb[:, j * 128:(j + 1) * 128], in_=xs[:, (hh * 4 + j) * 128:(hh * 4 + j + 1) * 128], identity=ident) for j in range(4)][-1],
                    [xs, cst], [pb])
                cp("act" if hh else "dve", xsT[:, hh * 4:hh * 4 + 4, :], pb[:].rearrange("p (a b) -> p a b", a=4), [pb], [(xsT, hh)])
            for cc in range(6):
                c0 = cc * 512
                cw = min(512, NZ - c0)
                pb = PB()
                mms(lambda e, pb=pb, c0=c0, cw=cw: [e.matmul(pb[:, 0:cw], lhsT=xsT[:, c, :], rhs=Wb[:, c, c0:c0 + cw], start=(c == 0), stop=(c == 7)) for c in range(8)][-1],
                    [xsT, Wb], [pb])
                cp("act" if cc % 2 else "dve", z[:, c0:c0 + cw], pb[:, 0:cw], [pb], [(z, cc)])
            if STAGE == 1:
                yo = yout()
                cp("dve", yo[:], z[:, 0:1024], [z], [yo])
                dma("sp", D["y_scr"][tok0:tok0 + 128, :], yo[:], [yo], [])
                return
            cosb = cst[:, C_COS + n * 32:C_COS + n * 32 + 32].unsqueeze(1).to_broadcast([128, 10, 32])
            sinb = cst[:, C_SIN + n * 32:C_SIN + n * 32 + 32].unsqueeze(1).to_broadcast([128, 10, 32])
            zq = z[:, 0:640].rearrange("p (h d) -> p h d", h=10)
            x1, x2 = zq[:, :, 0:32], zq[:, :, 32:64]
            ta, tb_ = T5(), T5()
            tav = ta[:, 0:320].rearrange("p (h d) -> p h d", h=10)
            tbv = tb_[:, 0:320].rearrange("p (h d) -> p h d", h=10)
            zk = [(z, 0), (z, 1)]
            tt("dve", tav, x1, cosb, ALU.mult, zk + [cst], [ta])
            tt("pool", tbv, x2, sinb, ALU.mult, zk + [cst], [tb_])
            tt("dve", qk[:, :, 0:32], tav, tbv, ALU.subtract, [ta, tb_], [(qk, 0)])
            tc_, td = T5(), T5()
            tcv = tc_[:, 0:320].rearrange("p (h d) -> p h d", h=10)
            tdv = td[:, 0:320].rearrange("p (h d) -> p h d", h=10)
            tt("pool", tcv, x2, cosb, ALU.mult, zk + [cst], [tc_])
            tt("dve", tdv, x1, sinb, ALU.mult, zk + [cst], [td])
            tt("pool", qk[:, :, 32:64], tcv, tdv, ALU.add, [tc_, td], [(qk, 1)])
            cp("pool", kdup[:], qk[:, 8:10, :].unsqueeze(2).to_broadcast([128, 2, 2, 64]), [qk], [kdup])
            kTc, kTp = kT[n % 2], kT[(n + 1) % 2]
            vac, vap = va[n % 2], va[(n + 1) % 2]
            cp("pool", vac[:, :, 0:64], z[:, 640:768].rearrange("p (g d) -> p g d", g=2), [(z, 1)], [vac])
            pb = PB()
            mms(lambda e, pb=pb: [e.transpose(out=pb[:, j * 128:(j + 1) * 128], in_=qk[:, 2 * j:2 * j + 2, :].rearrange("p a d -> p (a d)"), identity=ident) for j in range(4)][-1],
                [qk, cst], [pb])
            cp("act", qT[:], pb[:].rearrange("p (a b) -> p a b", a=4), [pb], [qT])
            pb = PB()
            mms(lambda e, pb=pb: [e.transpose(out=pb[:, g * 128:(g + 1) * 128], in_=kdup[:, g, :, :].rearrange("p a d -> p (a d)"), identity=ident) for g in range(2)][-1],
                [kdup, cst], [pb])
            cp("dve", kTc[:], pb[:, 0:256].rearrange("p (a b) -> p a b", a=2), [pb], [kTc])
            yo = yout()
            pv = PV
            for hd in range(0 if "a" in SKIP else 8):
                m, base, g = hd // 2, 64 * (hd % 2), hd // 4
                pb = PB()
                if first:
                    mms(lambda e, pb=pb, m=m, base=base, g=g: e.matmul(pb[:, 128:256], lhsT=kTc[base:base + 64, g, :], rhs=qT[base:base + 64, m, :], start=True, stop=True),
                        [kTc, qT], [pb])
                else:
                    mms(lambda e, pb=pb, m=m, base=base, g=g: [e.matmul(pb[:, 0:128], lhsT=kTp[base:base + 64, g, :], rhs=qT[base:base + 64, m, :], start=True, stop=True),
                                                              e.matmul(pb[:, 128:256], lhsT=kTc[base:base + 64, g, :], rhs=qT[base:base + 64, m, :], start=True, stop=True)][-1],
                        [kTc, kTp, qT], [pb])
                pt = pT()
                lo = 1 if first else 0
                act(pt[:, lo:2, :], pb[:, lo * 128:256].rearrange("p (a b) -> p a b", a=2 - lo), AF.Exp, [pb, negsink], [pt],
                    scale=0.125, bias=negsink[:, hd:hd + 1])
                if not first:
                    tt("pool", pt[:, 0, :], pt[:, 0, :], cst[:, C_SL:C_SL + 128], ALU.mult, [pt, cst], [pt])
                tt("dve", pt[:, 1, :], pt[:, 1, :], cst[:, C_IU:C_IU + 128], ALU.mult, [pt, cst], [pt])
                pvb = pv[hd // 4]
                o0 = (hd % 4) * 65
                if first:
                    mms(lambda e, pvb=pvb, pt=pt, g=g, o0=o0: e.matmul(pvb[:, o0:o0 + 65], lhsT=pt[:, 1, :], rhs=vac[:, g, :], start=True, stop=True),
                        [pt, vac], [pvb])
                else:
                    mms(lambda e, pvb=pvb, pt=pt, g=g, o0=o0: [e.matmul(pvb[:, o0:o0 + 65], lhsT=pt[:, 0, :], rhs=vap[:, g, :], start=True, stop=False),
                                                              e.matmul(pvb[:, o0:o0 + 65], lhsT=pt[:, 1, :], rhs=vac[:, g, :], start=False, stop=True)][-1],
                        [pt, vac, vap], [pvb])
            for hf in range(2):
                pvb = pv[hf]
                pvv = pvb[:, 0:260].rearrange("p (h d) -> p h d", h=4)
                den = sm()
                ts("dve", den[:, 0:4], pvv[:, :, 64], 1.0, None, ALU.add, None, [pvb], [den])
                p.op("dve", lambda e, den=den: e.reciprocal(out=den[:, 0:4], in_=den[:, 0:4]), [den], [den])
                tt("dve", yo[:, hf * 256:(hf + 1) * 256].rearrange("p (h d) -> p h d", h=4), pvv[:, :, 0:64],
                   den[:, 0:4].unsqueeze(2).to_broadcast([128, 4, 64]), ALU.mult, [pvb, den], [(yo, hf)])
            if STAGE == 2:
                cp("dve", yo[:, 512:1024], z[:, 0:512], [z], [yo])
                dma("sp", D["y_scr"][tok0:tok0 + 128, :], yo[:], [yo], [])
                return
            if "r" in SKIP:
                dma("sp", D["y_scr"][tok0:tok0 + 128, :], yo[:], [yo], [(D["y_t"], tok0 // 128)])
                return
            for j in range(4):
                c0 = 768 + j * 512
                cw = min(512, NZ - c0)
                pb = PB()
                zkeys = [(z, 1), (z, 2), (z, 3), (z, 4), (z, 5)]
                if first:
                    mms(lambda e, pb=pb, c0=c0, cw=cw: e.matmul(pb[:, 0:cw], lhsT=cst[:, C_SH:C_SH + 128], rhs=z[:, c0:c0 + cw], start=True, stop=True),
                        zkeys + [cst], [pb])
                else:
                    mms(lambda e, pb=pb, c0=c0, cw=cw: [e.matmul(pb[:, 0:cw], lhsT=cst[:, C_SH:C_SH + 128], rhs=z[:, c0:c0 + cw], start=True, stop=False),
                                                       e.matmul(pb[:, 0:cw], lhsT=cst[:, C_CA:C_CA + 128], rhs=carry[:, c0 - 768:c0 - 768 + cw], start=False, stop=True)][-1],
                        zkeys + [cst, carry], [pb])
                r0 = c0 - 768
                tt("dve", zr[:, r0:r0 + cw], pb[:, 0:cw], z[:, c0:c0 + cw], ALU.subtract, [pb] + zkeys, [(zr, j)])
                tt("dve", zr[:, r0:r0 + cw], zr[:, r0:r0 + cw], vec[:, V_MU + r0:V_MU + r0 + cw], ALU.mult, [(zr, j), vec], [(zr, j)])
                tt("pool", zr[:, r0:r0 + cw], zr[:, r0:r0 + cw], z[:, c0:c0 + cw], ALU.add, [(zr, j)] + zkeys, [(zr, j)])
            cp("pool", carry[96:128, :], z[96:128, 768:NZ], [z], [carry])
            r_, k_, v_ = zr[:, 0:512], zr[:, 512:1024], zr[:, 1024:1536]
            if STAGE == 3:
                cp("dve", yo[:, 512:1024], zr[:, 0:512], [], [yo])
                dma("sp", D["y_scr"][tok0:tok0 + 128, :], yo[:], [yo], [])
                return
            act(lin[:, 0:64], zr[:, 1536:1600], AF.Tanh, [zr], [(lin, 0)])
            cp("pool", lin[:, 64:128], zr[:, 1600:1664], [zr], [(lin, 1)])
            act(lin[:, 128:288], zr[:, 1664:1824], AF.Sigmoid, [zr], [(lin, 2)])
            pb = PB()
            mms(lambda e, pb=pb: [e.transpose(out=pb[:, 0:128], in_=lin[:, 0:128], identity=ident),
                                  e.transpose(out=pb[:, 128:256], in_=lin[:, 128:256], identity=ident),
                                  e.transpose(out=pb[0:32, 256:384], in_=lin[:, 256:288], identity=ident)][-1], [lin, cst], [pb])
            cp("dve", linT[:, 0:2, :], pb[:, 0:256].rearrange("p (a b) -> p a b", a=2), [pb], [(linT, 0)])
            cp("dve", linT[0:32, 2, :], pb[0:32, 256:384], [pb], [(linT, 1)])
            pw, pa_, pg = PB(), PB(), PB()
            mms(lambda e, pw=pw: e.matmul(pw[:], lhsT=linT[0:64, 0, :], rhs=w2[0:64, :], start=True, stop=True), [linT, w2], [pw])
            mms(lambda e, pa_=pa_: e.matmul(pa_[:], lhsT=linT[64:128, 0, :], rhs=w2[64:128, :], start=True, stop=True), [linT, w2], [pa_])
            mms(lambda e, pg=pg: [e.matmul(pg[:], lhsT=linT[:, 1, :], rhs=g2[:, 0, :], start=True, stop=False),
                                  e.matmul(pg[:], lhsT=linT[0:32, 2, :], rhs=g2[0:32, 1, :], start=False, stop=True)][-1], [linT, g2], [pg])
            sg, av = T5(), T5()
            tt("dve", sg[:], pw[:], vb(V_W0), ALU.add, [pw, vec], [sg])
            act(sg[:], sg[:], AF.Sigmoid, [sg], [sg])
            tt("dve", av[:], pa_[:], vb(V_A0), ALU.add, [pa_, vec], [av])
            act(av[:], av[:], AF.Sigmoid, [av], [av])
            cp("act", gv[:], pg[:], [pg], [gv])
            if STAGE == 4:
                cp("dve", yo[:, 512:1024], sg[:], [], [yo])
                dma("sp", D["y_scr"][tok0:tok0 + 128, :], yo[:], [yo], [])
                return
            pc = PB()
            mms(lambda e, pc=pc, sg=sg: e.matmul(pc[:], lhsT=cst[:, C_IU:C_IU + 128], rhs=sg[:], start=True, stop=True), [cst, sg], [pc])
            gam, igam, gprev = T5(), T5(), T5()
            act(gam[:], pc[:], AF.Exp, [pc], [gam], scale=-C_DEC)
            act(igam[:], pc[:], AF.Exp, [pc], [igam], scale=C_DEC)
            tt("dve", gprev[:], pc[:], sg[:], ALU.subtract, [pc, sg], [gprev])
            act(gprev[:], gprev[:], AF.Exp, [gprev], [gprev], scale=-C_DEC)
            pgc = PB()
            mms(lambda e, pgc=pgc, sg=sg: [e.matmul(pgc[:, m:m + 1], lhsT=sg[:, m * 128:(m + 1) * 128], rhs=cst[:, C_ONE:C_ONE + 1], start=True, stop=True) for m in range(4)][-1],
                [cst, sg], [pgc])
            act(gC[:], pgc[:, 0:4], AF.Exp, [pgc], [gC], scale=-C_DEC)
            if STAGE == 5:
                cp("dve", yo[:, 512:1024], gprev[:], [], [yo])
                dma("sp", D["y_scr"][tok0:tok0 + 128, :], yo[:], [yo], [])
                return
            kk, sq = T5(), T5()
            tt("dve", kk[:], k_, vb(V_KK), ALU.mult, [zr, vec], [kk])
            tt("pool", sq[:], kk[:], kk[:], ALU.mult, [kk], [sq])
            if STAGE == 51:
                cp("dve", yo[:, 512:1024], sq[:], [], [yo])
                dma("sp", D["y_scr"][tok0:tok0 + 128, :], yo[:], [yo], [])
                return
            s8 = sm()
            red(s8[:], sq[:].rearrange("p (h d) -> p h d", h=8), [sq], [s8])
            if STAGE == 52:
                cp("dve", yo[:, 512:1024], sq[:], [], [yo])
                dma("sp", D["y_scr"][tok0:tok0 + 128, :], yo[:], [yo], [])
                return
            act(s8[:], s8[:], AF.Sqrt, [s8], [s8], bias=1e-24, scale=1.0)
            p.op("dve", lambda e, s8=s8: e.reciprocal(out=s8[:], in_=s8[:]), [s8], [s8])
            if STAGE == 53:
                cp("dve", yo[:, 512:1024], sq[:], [], [yo])
                dma("sp", D["y_scr"][tok0:tok0 + 128, :], yo[:], [yo], [])
                return
            tt("dve", kk[:].rearrange("p (h d) -> p h d", h=8), kk[:].rearrange("p (h d) -> p h d", h=8),
               s8[:].unsqueeze(2).to_broadcast([128, 8, 64]), ALU.mult, [kk, s8], [kk])
            if STAGE == 54:
                cp("dve", yo[:, 512:1024], kk[:], [], [yo])
                dma("sp", D["y_scr"][tok0:tok0 + 128, :], yo[:], [yo], [])
                return
            t1 = T5()
            stt("dve", t1[:], av[:], -1.0, vb(V_KA), ALU.add, ALU.mult, [av, vec], [t1])
            stt("dve", k2[:], t1[:], 1.0, k_, ALU.add, ALU.mult, [t1, zr], [k2])
            if STAGE == 55:
                cp("dve", yo[:, 512:1024], k2[:], [], [yo])
                dma("sp", D["y_scr"][tok0:tok0 + 128, :], yo[:], [yo], [])
                return
            At, Bt, Kt, Rt = T5(), T5(), T5(), T5()
            stt("dve", At[:], kk[:], -1.0, gprev[:], ALU.mult, ALU.mult, [kk, gprev], [At])
            if STAGE == 56:
                cp("dve", yo[:, 512:1024], At[:], [At], [yo])
                dma("sp", D["y_scr"][tok0:tok0 + 128, :], yo[:], [yo], [])
                return
            tt("pool", Bt[:], kk[:], av[:], ALU.mult, [kk, av], [Bt])
            tt("dve", Bt[:], Bt[:], igam[:], ALU.mult, [Bt, igam], [Bt])
            if STAGE == 57:
                cp("dve", yo[:, 512:1024], Bt[:], [Bt], [yo])
                dma("sp", D["y_scr"][tok0:tok0 + 128, :], yo[:], [yo], [])
                return
            tt("dve", Kt[:], k2[:], igam[:], ALU.mult, [k2, igam], [Kt])
            if STAGE == 58:
                cp("dve", yo[:, 512:1024], Kt[:], [Kt], [yo])
                dma("sp", D["y_scr"][tok0:tok0 + 128, :], yo[:], [yo], [])
                return
            tt("pool", Rt[:], r_, gam[:], ALU.mult, [zr, gam], [Rt])
            if STAGE == 6:
                cp("dve", yo[:, 512:1024], Rt[:], [Rt], [yo])
                dma("sp", D["y_scr"][tok0:tok0 + 128, :], yo[:], [yo], [])
                return
            XT = {}
            for nm, src in (("A", At), ("B", Bt), ("K", Kt), ("R", Rt)):
                pb = PB()
                mms(lambda e, pb=pb, src=src: [e.transpose(out=pb[:, j * 128:(j + 1) * 128], in_=src[:, j * 128:(j + 1) * 128], identity=ident) for j in range(4)][-1],
                    [src, cst], [pb])
                dst = Tq()
                cp("act" if nm in ("A", "K") else "dve", dst[:], pb[:].rearrange("p (a b) -> p a b", a=4), [pb], [dst])
                XT[nm] = dst
            AT, BT, KT, RT = XT["A"], XT["B"], XT["K"], XT["R"]

            def pairmat(l, r_op, mask_off, eng2, dst=None):
                dst = dst or M8()
                for par in range(2):
                    pb = PB()
                    mms(lambda e, pb=pb, par=par: [e.matmul(pb[:, j * 128:(j + 1) * 128],
                                                          lhsT=l[64 * par:64 * par + 64, j, :],
                                                          rhs=r_op[64 * par:64 * par + 64, j, :],
                                                          start=True, stop=True) for j in range(4)][-1], [l, r_op], [pb])
                    tt(eng2[par], dst[:, par:8:2, :], pb[:].rearrange("p (a b) -> p a b", a=4),
                       cst[:, mask_off:mask_off + 128].unsqueeze(1).to_broadcast([128, 4, 128]), ALU.mult, [pb, cst], [(dst, par)])
                return dst

            Nm = pairmat(BT, AT, C_SU, ("dve", "dve"))
            Am = pairmat(AT, BT, C_SL, ("dve", "dve"))
            AkT = pairmat(KT, AT, C_SU, ("dve", "dve"), M8d[0])
            RbT = pairmat(BT, RT, C_IU, ("dve", "dve"), M8d[1])
            RkT = pairmat(KT, RT, C_IU, ("dve", "dve"), M8d[2])
            if STAGE == 7:
                cp("dve", yo[:, 512:1024].rearrange("p (a b) -> p a b", a=4), RkT[:, 0:4, :], [RkT], [yo])
                dma("sp", D["y_scr"][tok0:tok0 + 128, :], yo[:], [yo], [])
                return
            X = M8()
            tt("dve", X[:], Nm[:], ident.unsqueeze(1).to_broadcast([128, 8, 128]), ALU.add, [Nm, cst], [X])
            for j in range(0 if "d" in SKIP else 6):
                Nn, An, Xn = M8(), M8(), M8()
                last = (j == 5)
                for hf in range(2):
                    pbn, pba = PB(), PB()
                    if not last:
                        mms(lambda e, pbn=pbn, hf=hf, Am=Am, Nm=Nm: [e.matmul(pbn[:, q * 128:(q + 1) * 128], lhsT=Am[:, hf * 4 + q, :], rhs=Nm[:, hf * 4 + q, :], start=True, stop=True) for q in range(4)][-1],
                            [Am, Nm], [pbn])
                        cp("act", Nn[:, hf * 4:hf * 4 + 4, :], pbn[:].rearrange("p (a b) -> p a b", a=4), [pbn], [(Nn, hf)])
                    mms(lambda e, pba=pba, hf=hf, Am=Am, Nm=Nm: [e.matmul(pba[:, q * 128:(q + 1) * 128], lhsT=Nm[:, hf * 4 + q, :], rhs=Am[:, hf * 4 + q, :], start=True, stop=True) for q in range(4)][-1],
                        [Am, Nm], [pba])
                    cp("dve", An[:, hf * 4:hf * 4 + 4, :], pba[:].rearrange("p (a b) -> p a b", a=4), [pba], [(An, hf)])
                for hf in range(2):
                    pbx = PB()
                    mms(lambda e, pbx=pbx, hf=hf, An=An, X=X: [e.matmul(pbx[:, q * 128:(q + 1) * 128], lhsT=An[:, hf * 4 + q, :], rhs=X[:, hf * 4 + q, :], start=True, stop=True) for q in range(4)][-1],
                        [An, X], [pbx])
                    tt("dve", Xn[:, hf * 4:hf * 4 + 4, :], pbx[:].rearrange("p (a b) -> p a b", a=4), X[:, hf * 4:hf * 4 + 4, :], ALU.add, [pbx, X], [(Xn, hf)])
                Nm, Am, X = Nn, An, Xn
            if STAGE == 8:
                cp("dve", yo[:, 512:1024].rearrange("p (a b) -> p a b", a=4), X[:, 0:4, :], [X], [yo])
                dma("sp", D["y_scr"][tok0:tok0 + 128, :], yo[:], [yo], [])
                return
            pr = PB()

            def f_rhs0(e, pr=pr, AT=AT, AkT=AkT):
                ins = None
                for m in range(4):
                    e.matmul(pr[:, m * 128:(m + 1) * 128], lhsT=AT[:, m, :], rhs=ST[:, m, :], start=True, stop=False)
                    for q in range(2):
                        hd = 2 * m + q
                        ins = e.matmul(pr[:, hd * 64:(hd + 1) * 64], lhsT=AkT[:, hd, :], rhs=zr[:, 1024 + hd * 64:1024 + (hd + 1) * 64], start=False, stop=(q == 1))
                return ins
            mms(f_rhs0, [AT, ST, AkT, zr], [pr])
            rhs0 = T5()
            cp("act", rhs0[:], pr[:], [pr], [rhs0])
            pu = PB()
            mms(lambda e, pu=pu, X=X, rhs0=rhs0: [e.matmul(pu[:, hd * 64:(hd + 1) * 64], lhsT=X[:, hd, :], rhs=rhs0[:, hd * 64:(hd + 1) * 64], start=True, stop=True) for hd in range(8)][-1],
                [X, rhs0], [pu])
            U = T5()
            cp("dve", U[:], pu[:], [pu], [U])
            py = PB()

            def f_y(e, py=py, RT=RT, RbT=RbT, RkT=RkT, U=U):
                ins = None
                for m in range(4):
                    e.matmul(py[:, m * 128:(m + 1) * 128], lhsT=RT[:, m, :], rhs=ST[:, m, :], start=True, stop=False)
                    for q in range(2):
                        hd = 2 * m + q
                        e.matmul(py[:, hd * 64:(hd + 1) * 64], lhsT=RbT[:, hd, :], rhs=U[:, hd * 64:(hd + 1) * 64], start=False, stop=False)
                        ins = e.matmul(py[:, hd * 64:(hd + 1) * 64], lhsT=RkT[:, hd, :], rhs=zr[:, 1024 + hd * 64:1024 + (hd + 1) * 64], start=False, stop=(q == 1))
                return ins
            mms(f_y, [RT, ST, RbT, RkT, U, zr], [py])
            yv = T5()
            cp("act", yv[:], py[:], [py], [yv])
            pst = PB()

            def f_s(e, pst=pst, Bt=Bt, Kt=Kt, U=U):
                ins = None
                for m in range(4):
                    e.matmul(pst[:, m * 128:(m + 1) * 128], lhsT=Bt[:, m * 128:(m + 1) * 128], rhs=U[:, m * 128:(m + 1) * 128], start=True, stop=False)
                    ins = e.matmul(pst[:, m * 128:(m + 1) * 128], lhsT=Kt[:, m * 128:(m + 1) * 128], rhs=zr[:, 1024 + m * 128:1024 + (m + 1) * 128], start=False, stop=True)
                return ins
            mms(f_s, [Bt, Kt, U, zr], [pst])
            tt("dve", ST[:], pst[:].rearrange("p (a b) -> p a b", a=4), ST[:], ALU.add, [pst, ST], [ST])
            tt("dve", ST[:], ST[:], gC[:].unsqueeze(2).to_broadcast([128, 4, 128]), ALU.mult, [ST, gC], [ST])
            tt("dve", ST[:], ST[:], cst[:, C_BD:C_BD + 128].unsqueeze(1).to_broadcast([128, 4, 128]), ALU.mult, [ST, cst], [ST])
            if STAGE == 9:
                cp("dve", yo[:, 512:1024], yv[:], [yv], [yo])
                dma("sp", D["y_scr"][tok0:tok0 + 128, :], yo[:], [yo], [])
                return
            y3 = yv[:].rearrange("p (h d) -> p h d", h=8)
            ysq = T5()
            tt("pool", ysq[:], yv[:], yv[:], ALU.mult, [yv], [ysq])
            s1, s2, mean, var = sm(), sm(), sm(), sm()
            red(s1[:], y3, [yv], [s1])
            red(s2[:], ysq[:].rearrange("p (h d) -> p h d", h=8), [ysq], [s2])
            ts("dve", mean[:], s1[:], 1.0 / 64, None, ALU.mult, None, [s1], [mean])
            tt("dve", var[:], mean[:], mean[:], ALU.mult, [mean], [var])
            stt("dve", var[:], s2[:], 1.0 / 64, var[:], ALU.mult, ALU.subtract, [s2, var], [var])
            act(var[:], var[:], AF.Sqrt, [var], [var], bias=64e-5, scale=1.0)
            p.op("dve", lambda e, var=var: e.reciprocal(out=var[:], in_=var[:]), [var], [var])
            yn = T5()
            yn3 = yn[:].rearrange("p (h d) -> p h d", h=8)
            tt("dve", yn3, y3, mean[:].unsqueeze(2).to_broadcast([128, 8, 64]), ALU.subtract, [yv, mean], [yn])
            tt("dve", yn3, yn3, var[:].unsqueeze(2).to_broadcast([128, 8, 64]), ALU.mult, [yn, var], [yn])
            tt("pool", yn[:], yn[:], vb(V_LNW), ALU.mult, [yn, vec], [yn])
            tt("pool", yn[:], yn[:], vb(V_LNB), ALU.add, [yn, vec], [yn])
            rk = T5()
            tt("pool", rk[:], r_, k2[:], ALU.mult, [zr, k2], [rk])
            tt("pool", rk[:], rk[:], vb(V_RK), ALU.mult, [rk, vec], [rk])
            sb_ = sm()
            red(sb_[:], rk[:].rearrange("p (h d) -> p h d", h=8), [rk], [sb_])
            tt("dve", rk[:].rearrange("p (h d) -> p h d", h=8), v_.rearrange("p (h d) -> p h d", h=8),
               sb_[:].unsqueeze(2).to_broadcast([128, 8, 64]), ALU.mult, [zr, sb_], [rk])
            tt("pool", yn[:], yn[:], rk[:], ALU.add, [yn, rk], [yn])
            tt("pool", yo[:, 512:1024], yn[:], gv[:], ALU.mult, [yn, gv], [(yo, 2)])
            dma("sp", D["y_scr"][tok0:tok0 + 128, :], yo[:], [yo], [(D["y_t"], tok0 // 128)])

    for i in range(len(order)):
        tile_body(i)


def phase_a1a(p, nc, D, n_seq, n_tiles, S_TOK):
    h = helpers(p)
    tt, stt, ts, act, cp, red, dma, mms = h.tt, h.stt, h.ts, h.act, h.cp, h.red, h.dma, h.mms
    NZ = 2592
    cst = p.sb("cst", [128, NCST], F32)
    dma("sp", cst[:], D["cst"], [], [cst])
    vec = p.sb("vecmu", [128, 1824], F32)
    dma("sp", vec[:], D["vecs"][V_MU:V_MU + 1824].partition_broadcast(128), [], [vec])
    snk = p.sb("snk", [128, 8], F32)
    dma("sp", snk[:], D["vecs"][V_SINK:V_SINK + 8].partition_broadcast(128), [], [snk])
    nwp = p.sb("nwp", [128, 16], F32)
    dma("sp", nwp[:], D["nwp"], [], [nwp])
    ident = cst[:, C_ID:C_ID + 128]
    negsink = p.sb("negsink", [128, 8], F32)
    ts("dve", negsink[:], snk[:], -1.0, None, ALU.mult, None, [snk], [negsink])

    def make_stream(k):
        sf = "_s%d" % k
        xt = Rot([p.sb("xt%d" % i + sf, [128, 1024], F32) for i in range(2)])
        ss = p.sb("ss" + sf, [128, 1], F32)
        rstd = p.sb("rstd" + sf, [128, 1], F32)
        xs = p.sb("xs" + sf, [128, 1024], F32)
        xsT = p.sb("xsT" + sf, [128, 8, 128], BF16)
        z = p.sb("z" + sf, [128, NZ], F32)
        carry = p.sb("carry" + sf, [128, 1824], F32)
        p.op("pool", lambda e: e.memset(carry[:], 0.0), [], [carry])
        ZR = Rot([p.sb("zr%d" % i + sf, [128, 1824], F32) for i in range(2)])
        T5 = Rot([p.sb("t5_%d" % i + sf, [128, 320], F32) for i in range(4)])
        sm = Rot([p.sb("sm_%d" % i + sf, [128, 8], F32) for i in range(4)])
        PB = Rot([p.ps("pb%d" % i + sf, [128, 512], F32) for i in range(2)])
        PV = [p.ps("pv%d" % i + sf, [128, 512], F32) for i in range(2)]
        qk = p.sb("qk" + sf, [128, 10, 64], F32)
        kdup = p.sb("kdup" + sf, [128, 2, 2, 64], F32)
        qT = p.sb("qT" + sf, [128, 4, 128], BF16)
        kT = [p.sb("kT%d" % i + sf, [128, 2, 128], BF16) for i in range(2)]
        va = [p.sb("va%d" % i + sf, [128, 2, 65], BF16) for i in range(2)]
        for i in range(2):
            p.op("pool", lambda e, i=i: e.memset(va[i][:], 1.0), [], [va[i]])
        pT = Rot([p.sb("pT%d" % i + sf, [128, 2, 128], BF16) for i in range(4)])
        yout = Rot([p.sb("yout%d" % i + sf, [128, 512], F32) for i in range(2)])
        xq = {}

        def prefetch(b, n):
            if n < n_tiles:
                t0 = b * S_TOK + n * 128
                xq[(b, n)] = xt()
                dma("sp", xq[(b, n)][:], D["x"][t0:t0 + 128, :], [], [xq[(b, n)]])

        def tile_body(b, n):
            tok0 = b * S_TOK + n * 128
            first = (n == 0)
            x_t = xq.pop((b, n))
            prefetch(b, n + 1)
            zr = ZR()
            act(xs[:], x_t[:], AF.Square, [x_t], [xs, ss], accum_out=ss[:])
            act(rstd[:], ss[:], AF.Sqrt, [ss], [rstd], bias=1e-5, scale=1.0 / 1024)
            p.op("dve", lambda e: e.reciprocal(out=rstd[:], in_=rstd[:]), [rstd], [rstd])
            ts("dve", xs[:], x_t[:], rstd[:, 0:1], None, ALU.mult, None, [x_t, rstd], [xs])
            for hh in range(2):
                pb = PB()
                mms(lambda e, pb=pb, hh=hh: [e.transpose(out=pb[:, j * 128:(j + 1) * 128], in_=xs[:, (hh * 4 + j) * 128:(hh * 4 + j + 1) * 128], identity=ident) for j in range(4)][-1],
                    [xs, cst], [pb])
                cp("act" if hh else "dve", xsT[:, hh * 4:hh * 4 + 4, :], pb[:].rearrange("p (a b) -> p a b", a=4), [pb], [(xsT, hh)])
            for cc in range(6):
                c0 = cc * 512
                cw = min(512, NZ - c0)
                pb = PB()
                mms(lambda e, pb=pb, c0=c0, cw=cw: [e.matmul(pb[:, 0:cw], lhsT=xsT[:, c, :], rhs=Wb[:, c, c0:c0 + cw], start=(c == 0), stop=(c == 7)) for c in range(8)][-1],
                    [xsT, Wb], [pb])
                cp("act" if cc % 2 else "dve", z[:, c0:c0 + cw], pb[:, 0:cw], [pb], [(z, cc)])
            cosb = cst[:, C_COS + n * 32:C_COS + n * 32 + 32].unsqueeze(1).to_broadcast([128, 10, 32])
            sinb = cst[:, C_SIN + n * 32:C_SIN + n * 32 + 32].unsqueeze(1).to_broadcast([128, 10, 32])
            zq = z[:, 0:640].rearrange("p (h d) -> p h d", h=10)
            x1, x2 = zq[:, :, 0:32], zq[:, :, 32:64]
            ta, tb_ = T5(), T5()
            tav = ta[:, 0:320].rearrange("p (h d) -> p h d", h=10)
            tbv = tb_[:, 0:320].rearrange("p (h d) -> p h d", h=10)
            zk = [(z, 0), (z, 1)]
            tt("dve", tav, x1, cosb, ALU.mult, zk + [cst], [ta])
            tt("pool", tbv, x2, sinb, ALU.mult, zk + [cst], [tb_])
            tt("dve", qk[:, :, 0:32], tav, tbv, ALU.subtract, [ta, tb_], [(qk, 0)])
            tc_, td = T5(), T5()
            tcv = tc_[:, 0:320].rearrange("p (h d) -> p h d", h=10)
            tdv = td[:, 0:320].rearrange("p (h d) -> p h d", h=10)
            tt("pool", tcv, x2, cosb, ALU.mult, zk + [cst], [tc_])
            tt("dve", tdv, x1, sinb, ALU.mult, zk + [cst], [td])
            tt("pool", qk[:, :, 32:64], tcv, tdv, ALU.add, [tc_, td], [(qk, 1)])
            cp("pool", kdup[:], qk[:, 8:10, :].unsqueeze(2).to_broadcast([128, 2, 2, 64]), [qk], [kdup])
            kTc, kTp = kT[n % 2], kT[(n + 1) % 2]
            vac, vap = va[n % 2], va[(n + 1) % 2]
            cp("pool", vac[:, :, 0:64], z[:, 640:768].rearrange("p (g d) -> p g d", g=2), [(z, 1)], [vac])
            pb = PB()
            mms(lambda e, pb=pb: [e.transpose(out=pb[:, j * 128:(j + 1) * 128], in_=qk[:, 2 * j:2 * j + 2, :].rearrange("p a d -> p (a d)"), identity=ident) for j in range(4)][-1],
                [qk, cst], [pb])
            cp("act", qT[:], pb[:].rearrange("p (a b) -> p a b", a=4), [pb], [qT])
            pb = PB()
            mms(lambda e, pb=pb: [e.transpose(out=pb[:, g * 128:(g + 1) * 128], in_=kdup[:, g, :, :].rearrange("p a d -> p (a d)"), identity=ident) for g in range(2)][-1],
                [kdup, cst], [pb])
            cp("dve", kTc[:], pb[:, 0:256].rearrange("p (a b) -> p a b", a=2), [pb], [kTc])
            yo = yout()
            pv = PV
            for hd in range(0 if "a" in SKIP else 8):
                m, base, g = hd // 2, 64 * (hd % 2), hd // 4
                pb = PB()
                if first:
                    mms(lambda e, pb=pb, m=m, base=base, g=g: e.matmul(pb[:, 128:256], lhsT=kTc[base:base + 64, g, :], rhs=qT[base:base + 64, m, :], start=True, stop=True),
                        [kTc, qT], [pb])
                else:
                    mms(lambda e, pb=pb, m=m, base=base, g=g: [e.matmul(pb[:, 0:128], lhsT=kTp[base:base + 64, g, :], rhs=qT[base:base + 64, m, :], start=True, stop=True),
                                                              e.matmul(pb[:, 128:256], lhsT=kTc[base:base + 64, g, :], rhs=qT[base:base + 64, m, :], start=True, stop=True)][-1],
                        [kTc, kTp, qT], [pb])
                pt = pT()
                lo = 1 if first else 0
                act(pt[:, lo:2, :], pb[:, lo * 128:256].rearrange("p (a b) -> p a b", a=2 - lo), AF.Exp, [pb, negsink], [pt],
                    scale=0.125, bias=negsink[:, hd:hd + 1])
                if not first:
                    tt("pool", pt[:, 0, :], pt[:, 0, :], cst[:, C_SL:C_SL + 128], ALU.mult, [pt, cst], [pt])
                tt("dve", pt[:, 1, :], pt[:, 1, :], cst[:, C_IU:C_IU + 128], ALU.mult, [pt, cst], [pt])
                pvb = pv[hd // 4]
                o0 = (hd % 4) * 65
                if first:
                    mms(lambda e, pvb=pvb, pt=pt, g=g, o0=o0: e.matmul(pvb[:, o0:o0 + 65], lhsT=pt[:, 1, :], rhs=vac[:, g, :], start=True, stop=True),
                        [pt, vac], [pvb])
                else:
                    mms(lambda e, pvb=pvb, pt=pt, g=g, o0=o0: [e.matmul(pvb[:, o0:o0 + 65], lhsT=pt[:, 0, :], rhs=vap[:, g, :], start=True, stop=False),
                                                              e.matmul(pvb[:, o0:o0 + 65], lhsT=pt[:, 1, :], rhs=vac[:, g, :], start=False, stop=True)][-1],
                        [pt, vac, vap], [pvb])
            for hf in range(2):
                pvb = pv[hf]
                pvv = pvb[:, 0:260].rearrange("p (h d) -> p h d", h=4)
                den = sm()
                ts("dve", den[:, 0:4], pvv[:, :, 64], 1.0, None, ALU.add, None, [pvb], [den])
                p.op("dve", lambda e, den=den: e.reciprocal(out=den[:, 0:4], in_=den[:, 0:4]), [den], [den])
                tt("dve", yo[:, hf * 256:(hf + 1) * 256].rearrange("p (h d) -> p h d", h=4), pvv[:, :, 0:64],
                   den[:, 0:4].unsqueeze(2).to_broadcast([128, 4, 64]), ALU.mult, [pvb, den], [(yo, hf)])
            for j in range(4):
                c0 = 768 + j * 512
                cw = min(512, NZ - c0)
                pb = PB()
                zkeys = [(z, 1), (z, 2), (z, 3), (z, 4), (z, 5)]
                if first:
                    mms(lambda e, pb=pb, c0=c0, cw=cw: e.matmul(pb[:, 0:cw], lhsT=cst[:, C_SH:C_SH + 128], rhs=z[:, c0:c0 + cw], start=True, stop=True),
                        zkeys + [cst], [pb])
                else:
                    mms(lambda e, pb=pb, c0=c0, cw=cw: [e.matmul(pb[:, 0:cw], lhsT=cst[:, C_SH:C_SH + 128], rhs=z[:, c0:c0 + cw], start=True, stop=False),
                                                       e.matmul(pb[:, 0:cw], lhsT=cst[:, C_CA:C_CA + 128], rhs=carry[:, c0 - 768:c0 - 768 + cw], start=False, stop=True)][-1],
                        zkeys + [cst, carry], [pb])
                r0 = c0 - 768
                tt("dve", zr[:, r0:r0 + cw], pb[:, 0:cw], z[:, c0:c0 + cw], ALU.subtract, [pb] + zkeys, [(zr, j)])
                tt("dve", zr[:, r0:r0 + cw], zr[:, r0:r0 + cw], vec[:, V_MU + r0:V_MU + r0 + cw], ALU.mult, [(zr, j), vec], [(zr, j)])
                tt("pool", zr[:, r0:r0 + cw], zr[:, r0:r0 + cw], z[:, c0:c0 + cw], ALU.add, [(zr, j)] + zkeys, [(zr, j)])
            cp("pool", carry[96:128, :], z[96:128, 768:NZ], [z], [carry])
            dma("sp", D["y_scr"][tok0:tok0 + 128, 0:512], yo[:], [yo], [(D["y_t"], (tok0 // 128, 0))])
            dma("sp", D["zr_scr"][tok0:tok0 + 128, :], zr[:], [zr], [(D["zr_t"], tok0 // 128)])

        class S:
            pass
        S.prefetch, S.body, S.z = prefetch, tile_body, z
        return S

    streams = [make_stream(k) for k in range(2)]
    Wb = load_w_bf16(p, h, "Wb1", D["w_in"], 1024, 4640, 0, NZ, (lambda: streams[0].z), nwp, 0)
    zip_streams(p, streams, n_seq, n_tiles)


def phase_a1b(p, nc, D, n_seq, n_tiles, S_TOK):
    h = helpers(p)
    tt, stt, ts, act, cp, red, dma, mms = h.tt, h.stt, h.ts, h.act, h.cp, h.red, h.dma, h.mms
    cst = p.sb("cst", [128, NCST], F32)
    dma("sp", cst[:], D["cst"], [], [cst])
    VOFF = V_W0
    vec = p.sb("vecr", [128, V_SINK - VOFF], F32)
    dma("sp", vec[:], D["vecs"][VOFF:V_SINK].partition_broadcast(128), [], [vec])
    ident = cst[:, C_ID:C_ID + 128]
    w2 = p.sb("w2", [128, 512], F32)
    dma("sp", w2[0:64, :], D["decay_w2"], [], [w2])
    dma("sp", w2[64:128, :], D["iclr_a2"], [], [w2])
    g2 = p.sb("g2", [128, 2, 512], F32)
    dma("sp", g2[:, 0, :], D["gate_g2"][0:128, :], [], [g2])
    dma("sp", g2[0:32, 1, :], D["gate_g2"][128:160, :], [], [g2])

    def vb(off, n=512):
        return vec[:, off - VOFF:off - VOFF + n]

    def make_stream(k):
        sf = "_r%d" % k
        ZR = Rot([p.sb("zr%d" % i + sf, [128, 1824], F32) for i in range(NZRB)])
        lin = p.sb("lin" + sf, [128, 288], F32)
        linT = p.sb("linT" + sf, [128, 3, 128], F32)
        T5 = Rot([p.sb("t5_%d" % i + sf, [128, 512], F32) for i in range(10)])
        gv = p.sb("gv" + sf, [128, 512], F32)
        k2 = p.sb("k2" + sf, [128, 512], F32)
        M8 = Rot([p.sb("m8_%d" % i + sf, [128, 8, 128], F32) for i in range(6)])
        M8d = [p.sb("m8d_%d" % i + sf, [128, 8, 128], F32) for i in range(3)]
        Tq = Rot([p.sb("tq_%d" % i + sf, [128, 4, 128], F32) for i in range(4)])
        sm = Rot([p.sb("sm_%d" % i + sf, [128, 8], F32) for i in range(8)])
        PB = Rot([p.ps("pb%d" % i + sf, [128, 512], F32) for i in range(8 // NSB)])
        ST = p.sb("ST" + sf, [128, 4, 128], F32)
        gC = p.sb("gC" + sf, [128, 4], F32)
        yout = Rot([p.sb("yout%d" % i + sf, [128, 512], F32) for i in range(1)])
        zq = {}

        def prefetch(b, n):
            if n < n_tiles:
                t0 = b * S_TOK + n * 128
                zq[(b, n)] = ZR()
                dma("sp", zq[(b, n)][:], D["zr_scr"][t0:t0 + 128, :], [(D["zr_t"], t0 // 128)], [zq[(b, n)]])

        def tile_body(b, n):
            if n == 0:
                p.op("pool", lambda e: e.memset(ST[:], 0.0), [], [ST])
            tok0 = b * S_TOK + n * 128
            zr = zq.pop((b, n))
            prefetch(b, n + 1)
            yo = yout()
            r_, k_, v_ = zr[:, 0:512], zr[:, 512:1024], zr[:, 1024:1536]
            act(lin[:, 0:64], zr[:, 1536:1600], AF.Tanh, [zr], [(lin, 0)])
            cp("pool", lin[:, 64:128], zr[:, 1600:1664], [zr], [(lin, 1)])
            act(lin[:, 128:288], zr[:, 1664:1824], AF.Sigmoid, [zr], [(lin, 2)])
            pb = PB()
            mms(lambda e, pb=pb: [e.transpose(out=pb[:, 0:128], in_=lin[:, 0:128], identity=ident),
                                  e.transpose(out=pb[:, 128:256], in_=lin[:, 128:256], identity=ident),
                                  e.transpose(out=pb[0:32, 256:384], in_=lin[:, 256:288], identity=ident)][-1], [lin, cst], [pb])
            cp("dve", linT[:, 0:2, :], pb[:, 0:256].rearrange("p (a b) -> p a b", a=2), [pb], [(linT, 0)])
            cp("dve", linT[0:32, 2, :], pb[0:32, 256:384], [pb], [(linT, 1)])
            pw, pa_, pg = PB(), PB(), PB()
            mms(lambda e, pw=pw: e.matmul(pw[:], lhsT=linT[0:64, 0, :], rhs=w2[0:64, :], start=True, stop=True), [linT, w2], [pw])
            mms(lambda e, pa_=pa_: e.matmul(pa_[:], lhsT=linT[64:128, 0, :], rhs=w2[64:128, :], start=True, stop=True), [linT, w2], [pa_])
            mms(lambda e, pg=pg: [e.matmul(pg[:], lhsT=linT[:, 1, :], rhs=g2[:, 0, :], start=True, stop=False),
                                  e.matmul(pg[:], lhsT=linT[0:32, 2, :], rhs=g2[0:32, 1, :], start=False, stop=True)][-1], [linT, g2], [pg])
            sg, av = T5(), T5()
            tt("dve", sg[:], pw[:], vb(V_W0), ALU.add, [pw, vec], [sg])
            act(sg[:], sg[:], AF.Sigmoid, [sg], [sg])
            tt("dve", av[:], pa_[:], vb(V_A0), ALU.add, [pa_, vec], [av])
            act(av[:], av[:], AF.Sigmoid, [av], [av])
            cp("act", gv[:], pg[:], [pg], [gv])
            pc = PB()
            mms(lambda e, pc=pc, sg=sg: e.matmul(pc[:], lhsT=cst[:, C_IU:C_IU + 128], rhs=sg[:], start=True, stop=True), [cst, sg], [pc])
            gam, igam, gprev = T5(), T5(), T5()
            act(gam[:], pc[:], AF.Exp, [pc], [gam], scale=-C_DEC)
            act(igam[:], pc[:], AF.Exp, [pc], [igam], scale=C_DEC)
            tt("dve", gprev[:], pc[:], sg[:], ALU.subtract, [pc, sg], [gprev])
            act(gprev[:], gprev[:], AF.Exp, [gprev], [gprev], scale=-C_DEC)
            pgc = PB()
            mms(lambda e, pgc=pgc, sg=sg: [e.matmul(pgc[:, m:m + 1], lhsT=sg[:, m * 128:(m + 1) * 128], rhs=cst[:, C_ONE:C_ONE + 1], start=True, stop=True) for m in range(4)][-1],
                [cst, sg], [pgc])
            act(gC[:], pgc[:, 0:4], AF.Exp, [pgc], [gC], scale=-C_DEC)
            kk, sq = T5(), T5()
            tt("dve", kk[:], k_, vb(V_KK), ALU.mult, [zr, vec], [kk])
            tt("pool", sq[:], kk[:], kk[:], ALU.mult, [kk], [sq])
            s8 = sm()
            red(s8[:], sq[:].rearrange("p (h d) -> p h d", h=8), [sq], [s8])
            act(s8[:], s8[:], AF.Sqrt, [s8], [s8], bias=1e-24, scale=1.0)
            p.op("dve", lambda e, s8=s8: e.reciprocal(out=s8[:], in_=s8[:]), [s8], [s8])
            tt("dve", kk[:].rearrange("p (h d) -> p h d", h=8), kk[:].rearrange("p (h d) -> p h d", h=8),
               s8[:].unsqueeze(2).to_broadcast([128, 8, 64]), ALU.mult, [kk, s8], [kk])
            t1 = T5()
            stt("dve", t1[:], av[:], -1.0, vb(V_KA), ALU.add, ALU.mult, [av, vec], [t1])
            stt("dve", k2[:], t1[:], 1.0, k_, ALU.add, ALU.mult, [t1, zr], [k2])
            At, Bt, Kt, Rt = T5(), T5(), T5(), T5()
            stt("dve", At[:], kk[:], -1.0, gprev[:], ALU.mult, ALU.mult, [kk, gprev], [At])
            tt("pool", Bt[:], kk[:], av[:], ALU.mult, [kk, av], [Bt])
            tt("dve", Bt[:], Bt[:], igam[:], ALU.mult, [Bt, igam], [Bt])
            tt("dve", Kt[:], k2[:], igam[:], ALU.mult, [k2, igam], [Kt])
            tt("pool", Rt[:], r_, gam[:], ALU.mult, [zr, gam], [Rt])
            XT = {}
            for nm, src in (("A", At), ("B", Bt), ("K", Kt), ("R", Rt)):
                pb = PB()
                mms(lambda e, pb=pb, src=src: [e.transpose(out=pb[:, j * 128:(j + 1) * 128], in_=src[:, j * 128:(j + 1) * 128], identity=ident) for j in range(4)][-1],
                    [src, cst], [pb])
                dst = Tq()
                cp("act" if nm in ("A", "K") else "dve", dst[:], pb[:].rearrange("p (a b) -> p a b", a=4), [pb], [dst])
                XT[nm] = dst
            AT, BT, KT, RT = XT["A"], XT["B"], XT["K"], XT["R"]

            def pairmat(l, r_op, mask_off, eng2, dst=None):
                dst = dst or M8()
                for par in range(2):
                    pb = PB()
                    mms(lambda e, pb=pb, par=par: [e.matmul(pb[:, j * 128:(j + 1) * 128],
                                                          lhsT=l[64 * par:64 * par + 64, j, :],
                                                          rhs=r_op[64 * par:64 * par + 64, j, :],
                                                          start=True, stop=True) for j in range(4)][-1], [l, r_op], [pb])
                    tt(eng2[par], dst[:, par:8:2, :], pb[:].rearrange("p (a b) -> p a b", a=4),
                       cst[:, mask_off:mask_off + 128].unsqueeze(1).to_broadcast([128, 4, 128]), ALU.mult, [pb, cst], [(dst, par)])
                return dst

            Nm = pairmat(BT, AT, C_SU, ("dve", "dve"))
            Am = pairmat(AT, BT, C_SL, ("dve", "dve"))
            AkT = pairmat(KT, AT, C_SU, ("dve", "dve"), M8d[0])
            RbT = pairmat(BT, RT, C_IU, ("dve", "dve"), M8d[1])
            RkT = pairmat(KT, RT, C_IU, ("dve", "dve"), M8d[2])
            X = M8()
            tt("dve", X[:], Nm[:], ident.unsqueeze(1).to_broadcast([128, 8, 128]), ALU.add, [Nm, cst], [X])
            for j in range(0 if "d" in SKIP else 6):
                Nn, An, Xn = M8(), M8(), M8()
                last = (j == 5)
                for hf in range(2):
                    pbn, pba = PB(), PB()
                    if not last:
                        mms(lambda e, pbn=pbn, hf=hf, Am=Am, Nm=Nm: [e.matmul(pbn[:, q * 128:(q + 1) * 128], lhsT=Am[:, hf * 4 + q, :], rhs=Nm[:, hf * 4 + q, :], start=True, stop=True) for q in range(4)][-1],
                            [Am, Nm], [pbn])
                        cp("act", Nn[:, hf * 4:hf * 4 + 4, :], pbn[:].rearrange("p (a b) -> p a b", a=4), [pbn], [(Nn, hf)])
                    mms(lambda e, pba=pba, hf=hf, Am=Am, Nm=Nm: [e.matmul(pba[:, q * 128:(q + 1) * 128], lhsT=Nm[:, hf * 4 + q, :], rhs=Am[:, hf * 4 + q, :], start=True, stop=True) for q in range(4)][-1],
                        [Am, Nm], [pba])
                    cp("dve", An[:, hf * 4:hf * 4 + 4, :], pba[:].rearrange("p (a b) -> p a b", a=4), [pba], [(An, hf)])
                for hf in range(2):
                    pbx = PB()
                    mms(lambda e, pbx=pbx, hf=hf, An=An, X=X: [e.matmul(pbx[:, q * 128:(q + 1) * 128], lhsT=An[:, hf * 4 + q, :], rhs=X[:, hf * 4 + q, :], start=True, stop=True) for q in range(4)][-1],
                        [An, X], [pbx])
                    tt("dve", Xn[:, hf * 4:hf * 4 + 4, :], pbx[:].rearrange("p (a b) -> p a b", a=4), X[:, hf * 4:hf * 4 + 4, :], ALU.add, [pbx, X], [(Xn, hf)])
                Nm, Am, X = Nn, An, Xn
            pr = PB()

            def f_rhs0(e, pr=pr, AT=AT, AkT=AkT):
                ins = None
                for m in range(4):
                    e.matmul(pr[:, m * 128:(m + 1) * 128], lhsT=AT[:, m, :], rhs=ST[:, m, :], start=True, stop=False)
                    for q in range(2):
                        hd = 2 * m + q
                        ins = e.matmul(pr[:, hd * 64:(hd + 1) * 64], lhsT=AkT[:, hd, :], rhs=zr[:, 1024 + hd * 64:1024 + (hd + 1) * 64], start=False, stop=(q == 1))
                return ins
            mms(f_rhs0, [AT, ST, AkT, zr], [pr])
            rhs0 = T5()
            cp("act", rhs0[:], pr[:], [pr], [rhs0])
            pu = PB()
            mms(lambda e, pu=pu, X=X, rhs0=rhs0: [e.matmul(pu[:, hd * 64:(hd + 1) * 64], lhsT=X[:, hd, :], rhs=rhs0[:, hd * 64:(hd + 1) * 64], start=True, stop=True) for hd in range(8)][-1],
                [X, rhs0], [pu])
            U = T5()
            cp("dve", U[:], pu[:], [pu], [U])
            py = PB()

            def f_y(e, py=py, RT=RT, RbT=RbT, RkT=RkT, U=U):
                ins = None
                for m in range(4):
                    e.matmul(py[:, m * 128:(m + 1) * 128], lhsT=RT[:, m, :], rhs=ST[:, m, :], start=True, stop=False)
                    for q in range(2):
                        hd = 2 * m + q
                        e.matmul(py[:, hd * 64:(hd + 1) * 64], lhsT=RbT[:, hd, :], rhs=U[:, hd * 64:(hd + 1) * 64], start=False, stop=False)
                        ins = e.matmul(py[:, hd * 64:(hd + 1) * 64], lhsT=RkT[:, hd, :], rhs=zr[:, 1024 + hd * 64:1024 + (hd + 1) * 64], start=False, stop=(q == 1))
                return ins
            mms(f_y, [RT, ST, RbT, RkT, U, zr], [py])
            yv = T5()
            cp("act", yv[:], py[:], [py], [yv])
            pst = PB()

            def f_s(e, pst=pst, Bt=Bt, Kt=Kt, U=U):
                ins = None
                for m in range(4):
                    e.matmul(pst[:, m * 128:(m + 1) * 128], lhsT=Bt[:, m * 128:(m + 1) * 128], rhs=U[:, m * 128:(m + 1) * 128], start=True, stop=False)
                    ins = e.matmul(pst[:, m * 128:(m + 1) * 128], lhsT=Kt[:, m * 128:(m + 1) * 128], rhs=zr[:, 1024 + m * 128:1024 + (m + 1) * 128], start=False, stop=True)
                return ins
            mms(f_s, [Bt, Kt, U, zr], [pst])
            tt("dve", ST[:], pst[:].rearrange("p (a b) -> p a b", a=4), ST[:], ALU.add, [pst, ST], [ST])
            tt("dve", ST[:], ST[:], gC[:].unsqueeze(2).to_broadcast([128, 4, 128]), ALU.mult, [ST, gC], [ST])
            tt("dve", ST[:], ST[:], cst[:, C_BD:C_BD + 128].unsqueeze(1).to_broadcast([128, 4, 128]), ALU.mult, [ST, cst], [ST])
            y3 = yv[:].rearrange("p (h d) -> p h d", h=8)
            ysq = T5()
            tt("pool", ysq[:], yv[:], yv[:], ALU.mult, [yv], [ysq])
            s1, s2, mean, var = sm(), sm(), sm(), sm()
            red(s1[:], y3, [yv], [s1])
            red(s2[:], ysq[:].rearrange("p (h d) -> p h d", h=8), [ysq], [s2])
            ts("dve", mean[:], s1[:], 1.0 / 64, None, ALU.mult, None, [s1], [mean])
            tt("dve", var[:], mean[:], mean[:], ALU.mult, [mean], [var])
            stt("dve", var[:], s2[:], 1.0 / 64, var[:], ALU.mult, ALU.subtract, [s2, var], [var])
            act(var[:], var[:], AF.Sqrt, [var], [var], bias=64e-5, scale=1.0)
            p.op("dve", lambda e, var=var: e.reciprocal(out=var[:], in_=var[:]), [var], [var])
            yn = T5()
            yn3 = yn[:].rearrange("p (h d) -> p h d", h=8)
            tt("dve", yn3, y3, mean[:].unsqueeze(2).to_broadcast([128, 8, 64]), ALU.subtract, [yv, mean], [yn])
            tt("dve", yn3, yn3, var[:].unsqueeze(2).to_broadcast([128, 8, 64]), ALU.mult, [yn, var], [yn])
            tt("pool", yn[:], yn[:], vb(V_LNW), ALU.mult, [yn, vec], [yn])
            tt("pool", yn[:], yn[:], vb(V_LNB), ALU.add, [yn, vec], [yn])
            rk = T5()
            tt("pool", rk[:], r_, k2[:], ALU.mult, [zr, k2], [rk])
            tt("pool", rk[:], rk[:], vb(V_RK), ALU.mult, [rk, vec], [rk])
            sb_ = sm()
            red(sb_[:], rk[:].rearrange("p (h d) -> p h d", h=8), [rk], [sb_])
            tt("dve", rk[:].rearrange("p (h d) -> p h d", h=8), v_.rearrange("p (h d) -> p h d", h=8),
               sb_[:].unsqueeze(2).to_broadcast([128, 8, 64]), ALU.mult, [zr, sb_], [rk])
            tt("pool", yn[:], yn[:], rk[:], ALU.add, [yn, rk], [yn])
            tt("pool", yo[:, 0:512], yn[:], gv[:], ALU.mult, [yn, gv], [(yo, 2)])
            dma("sp", D["y_scr"][tok0:tok0 + 128, 512:1024], yo[:], [yo], [(D["y_t"], (tok0 // 128, 1))])

        class S:
            pass
        S.prefetch, S.body = prefetch, tile_body
        return S

    streams = [make_stream(k) for k in range(NSB)]
    zip_streams(p, streams, n_seq, n_tiles)


def zip_streams(p, streams, n_seq, n_tiles):
    NS = len(streams)
    for b0 in range(0, n_seq, NS):
        seqs = list(range(b0, min(b0 + NS, n_seq)))
        for k, b in enumerate(seqs):
            streams[k].prefetch(b, 0)
        for n in range(n_tiles):
            lists = []
            for k, b in enumerate(seqs):
                p.defer_begin()
                streams[k].body(b, n)
                lists.append(p.defer_end())
            while any(lists):
                for lst in lists:
                    if lst:
                        p.drain(lst, 1)


def phase_a2(p, nc, D, ntok, final=True, drip=None):
    h = helpers(p)
    tt, stt, ts, act, cp, red, dma, mms = h.tt, h.stt, h.ts, h.act, h.cp, h.red, h.dma, h.mms
    cst = p.sb("cstb", [128, 128], F32)
    dma("sp", cst[:], D["cst"][:, C_ID:C_ID + 128], [], [cst])
    ident = cst[:, 0:128]
    nwp = p.sb("nwpb", [128, 16], F32)
    dma("sp", nwp[:], D["nwp"], [], [nwp])
    fin = p.sb("finw", [128, 1024], F32)
    dma("sp", fin[:], D["vecs"][V_FIN:V_FIN + 1024].partition_broadcast(128), [], [fin])
    stg = Rot([p.sb("stgb%d" % i, [128, 2048], F32) for i in range(2)])
    Wg = load_w_bf16(p, h, "Wg", D["w_in"], 1024, 4640, 2592, 2048, stg, nwp, 0)
    PA = load_w_bf16(p, h, "PAw", D["proj_attn"], 512, 1024, 0, 1024, stg)
    PBw = load_w_bf16(p, h, "PBw", D["proj_rwkv"], 512, 1024, 0, 1024, stg)
    WO = load_w_bf16(p, h, "WOw", D["w_out"], 1024, 1024, 0, 1024, stg)
    nt = ntok // 128
    NS = 2

    class St:
        pass

    streams = []
    for k in range(NS):
        S = St()
        S.xt = Rot([p.sb("xtb%d_%d" % (k, i), [128, 1024], F32) for i in range(2)])
        S.yt = Rot([p.sb("ytb%d_%d" % (k, i), [128, 1024], F32) for i in range(2)])
        S.ss = p.sb("ssb%d" % k, [128, 1], F32)
        S.rstd = p.sb("rstdb%d" % k, [128, 1], F32)
        S.xs = p.sb("xsb%d" % k, [128, 1024], F32)
        S.xsT = p.sb("xsTb%d" % k, [128, 8, 128], BF16)
        S.yT = p.sb("yTb%d" % k, [128, 8, 128], BF16)
        S.sgt = p.sb("sgt%d" % k, [128, 2048], BF16)
        S.mg = p.sb("mg%d" % k, [128, 1024], F32)
        S.m2 = p.sb("m2%d" % k, [128, 1024], F32)
        S.mgT = p.sb("mgT%d" % k, [128, 8, 128], BF16)
        S.h1 = Rot([p.sb("h1b%d_%d" % (k, i), [128, 1024], F32) for i in range(2)])
        S.PB = Rot([p.ps("pq%d_%d" % (k, i), [128, 512], F32) for i in range(4)])
        S.xq, S.yq = {}, {}
        streams.append(S)

    def prefetch(S, i):
        if i < nt:
            S.xq[i] = S.xt()
            dma("sp", S.xq[i][:], D["x"][i * 128:(i + 1) * 128, :], [], [S.xq[i]])
            S.yq[i] = S.yt()
            dma("sp", S.yq[i][:], D["y_scr"][i * 128:(i + 1) * 128, :], [(D["y_t"], (i, 0)), (D["y_t"], (i, 1))], [S.yq[i]])

    def tp8(S, src, dst):
        for hh in range(2):
            pb = S.PB()
            mms(lambda e, pb=pb, hh=hh: [e.transpose(out=pb[:, j * 128:(j + 1) * 128], in_=src[:, (hh * 4 + j) * 128:(hh * 4 + j + 1) * 128], identity=ident) for j in range(4)][-1],
                [src, cst], [pb])
            cp("act" if hh else "dve", dst[:, hh * 4:hh * 4 + 4, :], pb[:].rearrange("p (a b) -> p a b", a=4), [pb], [(dst, hh)])

    def body(S, i):
        ss, rstd, xs, xsT, yT, sgt, mg, m2, mgT = S.ss, S.rstd, S.xs, S.xsT, S.yT, S.sgt, S.mg, S.m2, S.mgT
        x_t, y_t = S.xq.pop(i), S.yq.pop(i)
        prefetch(S, i + NS)
        act(xs[:], x_t[:], AF.Square, [x_t], [xs, ss], accum_out=ss[:])
        act(rstd[:], ss[:], AF.Sqrt, [ss], [rstd], bias=1e-5, scale=1.0 / 1024)
        p.op("dve", lambda e: e.reciprocal(out=rstd[:], in_=rstd[:]), [rstd], [rstd])
        ts("dve", xs[:], x_t[:], rstd[:, 0:1], None, ALU.mult, None, [x_t, rstd], [xs])
        tp8(S, xs, xsT)
        for cc in range(4):
            pb = S.PB()
            mms(lambda e, pb=pb, cc=cc: [e.matmul(pb[:], lhsT=xsT[:, c, :], rhs=Wg[:, c, cc * 512:(cc + 1) * 512], start=(c == 0), stop=(c == 7)) for c in range(8)][-1],
                [xsT, Wg], [pb])
            act(sgt[:, cc * 512:(cc + 1) * 512], pb[:], AF.Sigmoid, [pb], [(sgt, cc)])
        tp8(S, y_t, yT)
        for br, (W, dstt) in enumerate(((PA, mg), (PBw, m2))):
            for hf in range(2):
                pb = S.PB()
                mms(lambda e, pb=pb, br=br, hf=hf, W=W: [e.matmul(pb[:], lhsT=yT[:, br * 4 + c, :], rhs=W[:, c, hf * 512:(hf + 1) * 512], start=(c == 0), stop=(c == 3)) for c in range(4)][-1],
                    [yT, W], [pb])
                tt("dve", dstt[:, hf * 512:(hf + 1) * 512], pb[:], sgt[:, br * 1024 + hf * 512:br * 1024 + (hf + 1) * 512], ALU.mult, [pb, sgt], [(dstt, hf)])
        tt("pool", mg[:], mg[:], m2[:], ALU.add, [mg, m2], [mg])
        tp8(S, mg, mgT)
        ho = S.h1()
        for hf in range(2):
            pb = S.PB()
            mms(lambda e, pb=pb, hf=hf: [e.matmul(pb[:], lhsT=mgT[:, c, :], rhs=WO[:, c, hf * 512:(hf + 1) * 512], start=(c == 0), stop=(c == 7)) for c in range(8)][-1],
                [mgT, WO], [pb])
            tt("dve", ho[:, hf * 512:(hf + 1) * 512], pb[:], x_t[:, hf * 512:(hf + 1) * 512], ALU.add, [pb, x_t], [(ho, hf)])
        if final:
            act(xs[:], ho[:], AF.Square, [ho], [xs, ss], accum_out=ss[:])
            act(rstd[:], ss[:], AF.Sqrt, [ss], [rstd], bias=1e-5, scale=1.0 / 1024)
            p.op("dve", lambda e: e.reciprocal(out=rstd[:], in_=rstd[:]), [rstd], [rstd])
            stt("dve", ho[:], ho[:], rstd[:, 0:1], fin[:], ALU.mult, ALU.mult, [ho, rstd, fin], [ho])
            dma("sp", D["out"][i * 128:(i + 1) * 128, :], ho[:], [ho], [])
        else:
            dma("sp", D["h1_scr"][i * 128:(i + 1) * 128, :], ho[:], [ho], [(D["h1_t"], i)])

    for k in range(NS):
        prefetch(streams[k], k)
    per = (len(drip) + max(nt // NS - 1, 1) - 1) // max(nt // NS - 1, 1) if drip else 0
    for i0 in range(0, nt, NS):
        lists = []
        for k in range(NS):
            if i0 + k < nt:
                p.defer_begin()
                body(streams[k], i0 + k)
                lists.append(p.defer_end())
        while any(lists):
            for lst in lists:
                if lst:
                    p.drain(lst, 1)
        if drip:
            p.drain(drip, per)
    if drip:
        p.drain(drip, len(drip))


def phase_b0(p, nc, D, eng_rot=("dve", "pool"), nbuf=2, ldq="sp", stq="act"):
    h = helpers(p)
    dma, cp = h.dma, h.cp
    stg = Rot([p.sb("cs%d" % i, [128, 4096], F32) for i in range(nbuf)])
    ob = Rot([p.sb("co%d" % i, [128, 4096], BF16) for i in range(nbuf)])
    nwp0 = p.sb("nwp0", [128, 16], F32)
    dma("sp", nwp0[:], D["nwp"], [], [nwp0])
    k = 0
    for g in range(32):
        s, o = stg(), ob()
        dma(ldq, s[:].rearrange("p (dc e) -> p dc e", dc=8), D["uT"][:, g * 512:(g + 1) * 512].rearrange("(dc p) e -> p dc e", p=128), [], [s])
        h.tt(eng_rot[k % 2], o[:].rearrange("p (i dc e) -> p dc i e", i=4, dc=8), s[:].rearrange("p (dc i e) -> p dc i e", dc=8, i=4),
             nwp0[:, 8:16].unsqueeze(2).unsqueeze(3).to_broadcast([128, 8, 4, 128]), ALU.mult, [s, nwp0], [o])
        k += 1
        dma(stq, D["u2"][:, g * 4:(g + 1) * 4, :, :].rearrange("p i dc e -> p (i dc e)"), o[:], [o], [(D["u2_t"], g)])
        s, o = stg(), ob()
        dma(ldq, s[:].rearrange("p (i d) -> p i d", i=4), D["v"][g * 512:(g + 1) * 512, :].rearrange("(i p) d -> p i d", p=128), [], [s])
        cp(eng_rot[k % 2], o[:], s[:], [s], [o])
        k += 1
        dma(stq, D["vb"][g * 512:(g + 1) * 512, :].rearrange("(i p) d -> p i d", p=128), o[:].rearrange("p (i d) -> p i d", i=4), [o], [(D["vb_t"], g)])


def phase_b(p, nc, D, ntok):
    h = helpers(p)
    tt, stt, ts, act, cp, red, dma, mms = h.tt, h.stt, h.ts, h.act, h.cp, h.red, h.dma, h.mms
    TT = 256
    cst = p.sb("cstc", [128, 256], F32)
    dma("sp", cst[:, 0:128], D["cst"][:, C_ID:C_ID + 128], [], [cst])
    dma("sp", cst[:, 128:256], D["cst"][:, C_IOTA:C_IOTA + 128], [], [cst])
    ident = cst[:, 0:128]
    iota = cst[:, 128:256]
    iota_bf = p.sb("iota_bf", [128, 128], BF16)
    cp("dve", iota_bf[:], iota, [cst], [iota_bf])
    fin = p.sb("finc", [128, 1024], F32)
    dma("sp", fin[:], D["vecs"][V_FIN:V_FIN + 1024].partition_broadcast(128), [], [fin])
    nwpb = p.sb("nwpc", [128, 16], F32)
    dma("sp", nwpb[:], D["nwp"], [], [nwpb])
    skT = p.sb("skT", [128, 8, 128], F32)
    dma("sp", skT[:], D["skT"], [], [skT])
    G = p.sb("G", [128, TT, 128], BF16)
    xs = p.sb("xsc", [128, 1024], F32)
    Wq = load_w_bf16(p, h, "Wq", D["peer_wq"], 1024, 1024, 0, 1024, (lambda: xs), nwpb, 8)
    U2 = Rot([p.sb("u2t%d" % i, [128, 4, 8, 128], BF16) for i in range(3)])
    Vt = Rot([p.sb("vt%d" % i, [128, 4, 1024], BF16) for i in range(3)])
    h1 = [[p.sb("h1c%d_%d" % (b, i), [128, 1024], F32) for i in range(2)] for b in range(2)]
    ss = p.sb("ssc", [128, 1], F32)
    rstd = p.sb("rstdc", [128, 1], F32)
    xTb = [p.sb("xs2T%d" % b, [128, 8, TT], BF16) for b in range(2)]
    qT = p.sb("qTc", [128, 8, TT], F32)
    sc = p.sb("sc", [128, 16, 128], F32)
    v16 = p.sb("v16", [128, 16, 16], F32)
    i16u = p.sb("i16u", [128, 16, 16], U32)
    i16f = p.sb("i16f", [128, 16, 16], F32)
    cand = p.sb("cand", [128, 8, 256], F32)
    tv = p.sb("tv", [128, 8, 16], F32)
    posu = p.sb("posu", [128, 8, 16], U32)
    au = p.sb("au", [128, 8, 16], U32)
    bu = p.sb("bu", [128, 8, 16], U32)
    af = p.sb("af", [128, 8, 16], F32)
    bf_ = p.sb("bf", [128, 8, 16], F32)
    sel = p.sb("sel", [128, 3, 128], F32)
    sm = Rot([p.sb("smc%d" % i, [128, 8], F32) for i in range(4)])
    selTb = [p.sb("selT%d" % b, [128, 3, TT], F32) for b in range(2)]
    OA = Rot([p.sb("oa%d" % i, [128, 16, 128], BF16) for i in range(2)])
    OB = Rot([p.sb("ob%d" % i, [128, 16, 128], BF16) for i in range(2)])
    gh = Rot([p.sb("gh%d" % i, [128, TT], BF16) for i in range(2)])
    ac = Rot([p.sb("ac%d" % i, [128, TT], BF16) for i in range(3)])
    pbs = [p.ps("pr%d" % i, [128, 512], F32) for i in range(4)]
    PBH = Rot(pbs[0:3])
    PBP = Rot(pbs[3:4])
    PBG = Rot(pbs)
    ACC = [p.ps("acc%d" % i, [128, 512], F32) for i in range(4)]
    ntile = ntok // TT

    def prep(tix):
        b = tix % 2
        t0 = tix * TT
        xT, selT = xTb[b], selTb[b]
        PB = PBP
        for s in range(2):
            hh1 = h1[b][s]
            dma("sp", hh1[:], D["h1_scr"][t0 + s * 128:t0 + (s + 1) * 128, :], [(D["h1_t"], tix * 2 + s)], [hh1])
            act(xs[:], hh1[:], AF.Square, [hh1], [xs, ss], accum_out=ss[:])
            act(rstd[:], ss[:], AF.Sqrt, [ss], [rstd], bias=1e-5, scale=1.0 / 1024)
            p.op("dve", lambda e: e.reciprocal(out=rstd[:], in_=rstd[:]), [rstd], [rstd])
            ts("dve", xs[:], hh1[:], rstd[:, 0:1], None, ALU.mult, None, [hh1, rstd], [xs])
            for hh in range(2):
                pb = PB()
                mms(lambda e, pb=pb, hh=hh: [e.transpose(out=pb[:, j * 128:(j + 1) * 128], in_=xs[:, (hh * 4 + j) * 128:(hh * 4 + j + 1) * 128], identity=ident) for j in range(4)][-1],
                    [xs, cst], [pb])
                cp("act" if hh else "dve", xT[:, hh * 4:hh * 4 + 4, s * 128:(s + 1) * 128], pb[:].rearrange("p (a b) -> p a b", a=4), [pb], [(xT, (s, hh))])
        for c in range(8):
            pb = PB()
            mms(lambda e, pb=pb, c=c, xT=xT: [e.matmul(pb[:, 0:TT], lhsT=Wq[:, dc, c * 128:(c + 1) * 128], rhs=xT[:, dc, :], start=(dc == 0), stop=(dc == 7)) for dc in range(8)][-1],
                [Wq, xT], [pb])
            cp("act" if c % 2 else "dve", qT[:, c, :], pb[:, 0:TT], [pb], [(qT, c)])
        for s in range(0 if "S" in SKIP else 2):
            for par in range(2):
                for half in range(2):
                    pb = PB()
                    mms(lambda e, pb=pb, par=par, half=half, s=s: [e.matmul(pb[:, j * 128:(j + 1) * 128], lhsT=qT[64 * par:64 * par + 64, half * 4 + j, s * 128:(s + 1) * 128],
                                                                        rhs=skT[64 * par:64 * par + 64, half * 4 + j, :], start=True, stop=True) for j in range(4)][-1],
                        [qT, skT], [pb])
                    cp("act", sc[:, 8 * half + par:8 * half + 8:2, :], pb[:].rearrange("p (a b) -> p a b", a=4), [pb], [(sc, 8 * half + par + 2 * j) for j in range(4)])
            tmpA = cand[:].rearrange("p h (a b) -> p (h a) b", a=2)
            for hp in range(16):
                p.op("dve", lambda e, hp=hp: e.max(out=v16[:, hp, 0:8], in_=sc[:, hp, :]), [(sc, hp)], [(v16, hp)])
            for hp in range(16):
                p.op("dve", lambda e, hp=hp, tmpA=tmpA: e.match_replace(out=tmpA[:, hp, :], in_to_replace=v16[:, hp, 0:8], in_values=sc[:, hp, :], imm_value=-1e30), [(sc, hp), (v16, hp)], [(cand, hp)])
            for hp in range(16):
                p.op("dve", lambda e, hp=hp, tmpA=tmpA: e.max(out=v16[:, hp, 8:16], in_=tmpA[:, hp, :]), [(cand, hp)], [(v16, hp)])
            for hp in range(16):
                p.op("dve", lambda e, hp=hp: e.max_index(out=i16u[:, hp, 0:8], in_max=v16[:, hp, 0:8], in_values=sc[:, hp, :]), [(sc, hp), (v16, hp)], [(i16u, hp)])
            for hp in range(16):
                p.op("dve", lambda e, hp=hp: e.max_index(out=i16u[:, hp, 8:16], in_max=v16[:, hp, 8:16], in_values=sc[:, hp, :]), [(sc, hp), (v16, hp)], [(i16u, hp)])
            cp("pool", i16f[:], i16u[:], [i16u], [i16f])
            tt("pool", cand[:].rearrange("p h (a b) -> p h a b", a=16), v16[:, 0:16:2, :].unsqueeze(3).to_broadcast([128, 8, 16, 16]),
               v16[:, 1:16:2, :].unsqueeze(2).to_broadcast([128, 8, 16, 16]), ALU.add, [v16], [cand])
            tmpB = sc[:].rearrange("p (h a) b -> p h (a b)", a=2)
            for hd in range(8):
                p.op("dve", lambda e, hd=hd: e.max(out=tv[:, hd, 0:8], in_=cand[:, hd, :]), [cand], [(tv, hd)])
            for hd in range(8):
                p.op("dve", lambda e, hd=hd, tmpB=tmpB: e.match_replace(out=tmpB[:, hd, :], in_to_replace=tv[:, hd, 0:8], in_values=cand[:, hd, :], imm_value=-1e30), [cand, (tv, hd)], [(sc, 2 * hd), (sc, 2 * hd + 1)])
            for hd in range(8):
                p.op("dve", lambda e, hd=hd, tmpB=tmpB: e.max(out=tv[:, hd, 8:16], in_=tmpB[:, hd, :]), [(sc, 2 * hd), (sc, 2 * hd + 1)], [(tv, hd)])
            for hd in range(8):
                p.op("dve", lambda e, hd=hd: e.max_index(out=posu[:, hd, 0:8], in_max=tv[:, hd, 0:8], in_values=cand[:, hd, :]), [cand, (tv, hd)], [(posu, hd)])
            for hd in range(8):
                p.op("dve", lambda e, hd=hd: e.max_index(out=posu[:, hd, 8:16], in_max=tv[:, hd, 8:16], in_values=cand[:, hd, :]), [cand, (tv, hd)], [(posu, hd)])
            gt = sel[:, 2, :].rearrange("p (h k) -> p h k", h=8)
            tt("pool", gt, tv[:], tv[:, :, 0:1].to_broadcast([128, 8, 16]), ALU.subtract, [tv], [(sel, 2)])
            act(gt, gt, AF.Exp, [(sel, 2)], [(sel, 2)])
            z8 = sm()
            red(z8[:], gt, [(sel, 2)], [z8])
            p.op("dve", lambda e, z8=z8: e.reciprocal(out=z8[:], in_=z8[:]), [z8], [z8])
            tt("dve", gt, gt, z8[:].unsqueeze(2).to_broadcast([128, 8, 16]), ALU.mult, [(sel, 2), z8], [(sel, 2)])
            ts("dve", au[:], posu[:], 4, None, ALU.logical_shift_right, None, [posu], [au])
            ts("dve", bu[:], posu[:], 15, None, ALU.bitwise_and, None, [posu], [bu])
            cp("pool", af[:], au[:], [au], [af])
            cp("pool", bf_[:], bu[:], [bu], [bf_])
            io16 = iota[:, 0:16].unsqueeze(1).unsqueeze(1).to_broadcast([128, 8, 16, 16])
            eq = cand[:].rearrange("p h (a b) -> p h a b", a=16)
            for w, (xf, par) in enumerate(((af, 0), (bf_, 1))):
                tt("dve", eq, io16, xf[:].unsqueeze(3).to_broadcast([128, 8, 16, 16]), ALU.is_equal, [cst, xf], [cand])
                tt("pool", eq, eq, i16f[:, par:16:2, :].unsqueeze(2).to_broadcast([128, 8, 16, 16]), ALU.mult, [cand, i16f], [cand])
                red(sel[:, w, :].rearrange("p (h k) -> p h k", h=8), eq, [cand], [(sel, w)])
            pb = PB()
            mms(lambda e, pb=pb: [e.transpose(out=pb[:, w * 128:(w + 1) * 128], in_=sel[:, w, :], identity=ident) for w in range(3)][-1], [sel, cst], [pb])
            cp("act", selT[:, :, s * 128:(s + 1) * 128], pb[:, 0:384].rearrange("p (a b) -> p a b", a=3), [pb], [(selT, s)])

    def gbuild(tix):
        selT = selTb[tix % 2]
        PB = PBG
        NG = 0 if "G" in SKIP else TT // 16
        bufs = {}

        def onehots(g):
            tk = g * 16
            oa, ob = OA(), OB()
            bufs[g] = (oa, ob)
            io = iota_bf[:, :].unsqueeze(1).to_broadcast([128, 16, 128])
            if "o" in SKIP:
                return
            tt("dve", oa[:], io, selT[:, 0, tk:tk + 16].unsqueeze(2).to_broadcast([128, 16, 128]), ALU.is_equal, [iota_bf, selT], [oa])
            for t in range(16):
                act(oa[:, t, :], oa[:, t, :], AF.Copy, [(oa, t), selT], [(oa, t)], scale=selT[:, 2, tk + t:tk + t + 1])
            tt("dve", ob[:], io, selT[:, 1, tk:tk + 16].unsqueeze(2).to_broadcast([128, 16, 128]), ALU.is_equal, [iota_bf, selT], [ob])

        def mm_evac(g):
            tk = g * 16
            oa, ob = bufs.pop(g)
            for q4 in range(4):
                pb = PB()
                if "m" not in SKIP:
                  mms(lambda e, pb=pb, q4=q4, oa=oa, ob=ob: [e.matmul(pb[:, j * 128:(j + 1) * 128], lhsT=ob[:, q4 * 4 + j, :], rhs=oa[:, q4 * 4 + j, :], start=True, stop=True) for j in range(4)][-1],
                    [oa, ob], [pb])
                tq = tk + q4 * 4
                if "v" not in SKIP:
                  cp("act" if q4 % 2 == 0 else "dve", G[:, tq:tq + 4, :], pb[:].rearrange("p (t i) -> p t i", t=4), [pb], [(G, tq)])

        if NG:
            onehots(0)
        for g in range(NG):
            if g + 1 < NG:
                onehots(g + 1)
            mm_evac(g)

    def expert(tix, nxt):
        xT = xTb[tix % 2]
        LOOK = 2
        grp = {}
        hb = {}

        def emit_H(i):
            ig, ii = divmod(i, 4)
            if ii == 0:
                u2, vt = U2(), Vt()
                grp[ig] = (u2, vt)
                if not ("D" in SKIP and ig >= 2):
                    dma("sp", vt[:], D["vb"][ig * 512:(ig + 1) * 512, :].rearrange("(i p) d -> p i d", p=128), [(D["vb_t"], ig)], [vt])
                    dma("sp", u2[:].rearrange("p i dc e -> p (i dc e)"), D["u2"][:, ig * 4:(ig + 1) * 4, :, :].rearrange("p i dc e -> p (i dc e)"), [(D["u2_t"], ig)], [u2])
            u2, vt = grp[ig]
            pb = PBH()
            mms(lambda e, pb=pb, u2=u2, ii=ii: [e.matmul(pb[:, 0:TT], lhsT=u2[:, ii, dc, :], rhs=xT[:, dc, :], start=(dc == 0), stop=(dc == 7)) for dc in range(8)][-1],
                [u2, xT], [pb])
            hb[i] = pb

        def emit_rest(i):
            ig, ii = divmod(i, 4)
            u2, vt = grp[ig]
            pb = hb.pop(i)
            g_, a_ = gh(), ac()
            if "X" not in SKIP:
                act(g_[:], pb[:, 0:TT], AF.Gelu, [pb], [g_])
                tt("dve", a_[:], g_[:], G[:, :, i], ALU.mult, [g_, G], [a_])
            mms(lambda e, a_=a_, vt=vt, ii=ii, i=i: [e.matmul(ACC[s * 2 + hf][:], lhsT=a_[:, s * 128:(s + 1) * 128], rhs=vt[:, ii, hf * 512:(hf + 1) * 512], start=(i == 0), stop=(i == 127))
                                                   for s in range(2) for hf in range(2)][-1], [a_, vt], ACC)

        NE = 0 if "E" in SKIP else 128
        per = (len(nxt) + 99) // 100 if nxt else 0
        for i in range(min(LOOK, NE)):
            emit_H(i)
        for i in range(NE):
            if i + LOOK < NE:
                emit_H(i + LOOK)
            emit_rest(i)
            if nxt:
                p.drain(nxt, per)
        if nxt:
            p.drain(nxt, len(nxt))

    def epilogue(tix):
        b = tix % 2
        t0 = tix * TT
        for s in range(2):
            hh1 = h1[b][s]
            for hf in range(2):
                tt("dve", hh1[:, hf * 512:(hf + 1) * 512], ACC[s * 2 + hf][:], hh1[:, hf * 512:(hf + 1) * 512], ALU.add, [ACC[s * 2 + hf], hh1], [hh1])
            act(xs[:], hh1[:], AF.Square, [hh1], [xs, ss], accum_out=ss[:])
            act(rstd[:], ss[:], AF.Sqrt, [ss], [rstd], bias=1e-5, scale=1.0 / 1024)
            p.op("dve", lambda e: e.reciprocal(out=rstd[:], in_=rstd[:]), [rstd], [rstd])
            stt("dve", hh1[:], hh1[:], rstd[:, 0:1], fin[:], ALU.mult, ALU.mult, [hh1, rstd, fin], [hh1])
            dma("act", D["out"][t0 + s * 128:t0 + (s + 1) * 128, :], hh1[:], [hh1], [])

    prep(0)
    for tix in range(ntile):
        gbuild(tix)
        nxt = []
        if tix + 1 < ntile:
            p.defer_begin()
            prep(tix + 1)
            nxt = p.defer_end()
        expert(tix, nxt)
        epilogue(tix)


N_CORES = 8


def _build(n_seq, n_tiles, ret_d=False, phases="0123"):
    nc = bass.Bass("TRN2", target_bir_lowering=False, dynamic_dma_scratch_size=2048)
    ntok = n_seq * n_tiles * 128
    D = {}

    def din(name, shape):
        D[name] = nc.dram_tensor(name, list(shape), F32, kind="ExternalInput").ap()

    din("x", (ntok, 1024)); din("w_in", (1024, 4640)); din("vecs", (NVEC,)); din("nwp", (128, 16)); din("cst", (128, NCST))
    din("decay_w2", (64, 512)); din("iclr_a2", (64, 512)); din("gate_g2", (160, 512))
    din("proj_attn", (512, 1024)); din("proj_rwkv", (512, 1024)); din("w_out", (1024, 1024))
    din("peer_wq", (1024, 1024)); din("skT", (128, 8, 128)); din("uT", (1024, 16384)); din("v", (16384, 1024))
    D["y_scr"] = nc.dram_tensor("y_scr", [ntok, 1024], F32, kind="Internal").ap()
    D["h1_scr"] = D["y_scr"]
    NA = 2 * 16384 * 1024
    arena = nc.dram_tensor("arena", [NA], BF16, kind="Internal").ap()
    assert ntok * 1824 * 2 <= NA
    D["zr_scr"] = arena[0:ntok * 1824 * 2].bitcast(F32).rearrange("(t c) -> t c", c=1824)
    D["u2"] = arena[0:16384 * 1024].rearrange("(p i dc e) -> p i dc e", p=128, i=128, dc=8)
    D["vb"] = arena[16384 * 1024:NA].rearrange("(r c) -> r c", c=1024)
    D["out"] = nc.dram_tensor("out", [ntok, 1024], F32, kind="ExternalOutput").ap()
    p = Prog(nc)
    for nm in ("y_t", "zr_t", "h1_t", "u2_t", "vb_t"):
        D[nm] = p.wrap(None, nm)
    m = p.mark()
    if "1" in phases or "a" in phases:
        phase_a1a(p, nc, D, n_seq, n_tiles, n_tiles * 128)
        p.release(m)
    if "1" in phases or "b" in phases:
        phase_a1b(p, nc, D, n_seq, n_tiles, n_tiles * 128)
        p.release(m)
    if "0" in phases:
        phase_b0(p, nc, D, stq="act")
        p.release(m)
    if "2" in phases:
        phase_a2(p, nc, D, ntok, final=("3" not in phases))
        p.release(m)
    if "3" in phases:
        phase_b(p, nc, D, ntok)
    p.emit()
    p.close()
    return (nc, D) if ret_d else nc


def _inputs(x, norm_mix_w, w_in, shift_mu, attn_sinks, decay_w0, decay_w2, iclr_a0, iclr_a2, gate_g2, k_k, k_a, r_k,
            ln_x_w, ln_x_b, proj_attn, proj_rwkv, w_out, norm_ffn_w, peer_wq, peer_subkeys, peer_u, peer_v, norm_final_w):
    f = lambda a: np.ascontiguousarray(np.asarray(a, dtype=np.float32))
    v = np.zeros(NVEC, np.float32)
    v[V_MU:V_MU + 1824] = f(shift_mu)[0]; v[V_W0:V_W0 + 512] = f(decay_w0)[0]; v[V_A0:V_A0 + 512] = f(iclr_a0)[0]
    v[V_KK:V_KK + 512] = f(k_k)[0]; v[V_KA:V_KA + 512] = f(k_a)[0]; v[V_LNW:V_LNW + 512] = f(ln_x_w)[0]
    v[V_LNB:V_LNB + 512] = f(ln_x_b)[0]; v[V_RK:V_RK + 512] = f(r_k)[0].reshape(-1); v[V_SINK:V_SINK + 8] = f(attn_sinks)[0]
    v[V_FIN:] = f(norm_final_w)
    nwp = np.ones((128, 16), np.float32)
    nwp[:, 0:8] = f(norm_mix_w)[0].reshape(8, 128).T
    nwp[:, 8:16] = f(norm_ffn_w)[0].reshape(8, 128).T
    sk = f(peer_subkeys)[0]
    skT = np.ascontiguousarray(sk.transpose(1, 3, 0, 2).reshape(128, 8, 128))
    return dict(w_in=f(w_in)[0], vecs=v, nwp=nwp, cst=make_cst(), decay_w2=f(decay_w2)[0], iclr_a2=f(iclr_a2)[0],
                gate_g2=f(gate_g2)[0], proj_attn=f(proj_attn)[0], proj_rwkv=f(proj_rwkv)[0], w_out=f(w_out)[0],
                peer_wq=f(peer_wq)[0], skT=skT, uT=np.ascontiguousarray(f(peer_u)[0].T), v=f(peer_v)[0])


def kernel(**inputs):
    x = np.ascontiguousarray(np.asarray(inputs["x"], dtype=np.float32))
    B, S, Dm = x.shape
    common = _inputs(**inputs)
    spc = B // N_CORES
    nc = _build(spc, S // 128)
    in_maps = []
    for c in range(N_CORES):
        d = dict(common)
        d["x"] = np.ascontiguousarray(x[c * spc:(c + 1) * spc].reshape(spc * S, Dm))
        in_maps.append(d)
    res = run_bass_kernel_spmd(nc, in_maps, core_ids=list(range(N_CORES)))
    out = np.concatenate([r["out"].reshape(spc, S, Dm) for r in res.results], axis=0)
    return out.astype(np.float32)
```

```python
import numpy as np
import concourse.bass as bass
import concourse.mybir as mybir

F32 = mybir.dt.float32
BF16 = mybir.dt.bfloat16
U32 = mybir.dt.uint32
I32 = mybir.dt.int32
ALU = mybir.AluOpType
AF = mybir.ActivationFunctionType
AX = mybir.AxisListType

NDMA_SEMS = 12
import os as _os
NOSELF = tuple(_os.environ.get("NOSELF", "").split(","))


class T:
    def __init__(self, h, name):
        self.h = h
        self.name = name
        self.state = {}

    def __getitem__(self, k):
        return self.h[k]


class Prog:
    def __init__(self, nc):
        self.nc = nc
        self.ops = {e: [] for e in ("pe", "dve", "act", "pool", "sp")}
        self.cms = []
        self.ndma = {e: 0 for e in ("sp", "act", "pool")}
        self._defer = None

    def sb(self, name, shape, dtype):
        self._uid = getattr(self, "_uid", 0) + 1
        cm = self.nc.sbuf_tensor("sb%d_" % self._uid + name, list(shape), dtype)
        h = cm.__enter__()
        self.cms.append(cm)
        return T(h, name)

    def ps(self, name, shape, dtype):
        self._uid = getattr(self, "_uid", 0) + 1
        cm = self.nc.psum_tensor("ps%d_" % self._uid + name, list(shape), dtype)
        h = cm.__enter__()
        self.cms.append(cm)
        return T(h, name)

    def wrap(self, ap, name):
        return T(ap, name)

    def defer_begin(self):
        self._defer = []

    def defer_end(self):
        lst, self._defer = self._defer, None
        return lst

    def drain(self, lst, k):
        for _ in range(min(k, len(lst))):
            eng, fn, reads, writes, dma, extra = lst.pop(0)
            self.op(eng, fn, reads, writes, dma, extra)

    def mark(self):
        return len(self.cms)

    def release(self, mark):
        lasts = []
        for e in ("pe", "dve", "act", "pool", "sp"):
            for j in range(len(self.ops[e]) - 1, -1, -1):
                if self.ops[e][j][2][0] not in ("dma", "bar"):
                    lasts.append((e, j))
                    break
        dmat = []
        for q in ("sp", "act", "pool"):
            n = self.ndma[q]
            dmat += [("dma", q, i) for i in range(max(0, n - NDMA_SEMS), n)]
        for e in ("pe", "dve", "act", "pool", "sp"):
            self.ops[e].append((None, lasts + dmat, ("bar", e, len(self.ops[e]))))
        while len(self.cms) > mark:
            self.cms.pop().__exit__(None, None, None)

    def _collect(self, t, key, is_write, deps):
        if key is None:
            keys = list(t.state.keys())
        else:
            keys = [k for k in (key, None) if k in t.state]
        for k in keys:
            w, rs = t.state[k]
            if w is not None:
                deps.append(w)
            if is_write:
                deps.extend(rs)

    def _update(self, t, key, is_write, me):
        if is_write:
            if key is None:
                t.state = {None: [me, []]}
            else:
                t.state[key] = [me, []]
        else:
            st = t.state.setdefault(key, [None, []])
            if me[0] != "dma":
                st[1] = [r for r in st[1] if not (r[0] == me[0])]
            st[1].append(me)

    def op(self, eng, fn, reads=(), writes=(), dma=False, extra=()):
        if self._defer is not None:
            self._defer.append((eng, fn, reads, writes, dma, extra))
            return None
        deps = list(extra)
        norm = lambda x: x if isinstance(x, tuple) else (x, None)
        reads = [norm(r) for r in reads]
        writes = [norm(w) for w in writes]
        for t, k in reads:
            self._collect(t, k, False, deps)
        for t, k in writes:
            self._collect(t, k, True, deps)
        idx = len(self.ops[eng])
        if dma:
            n = self.ndma[eng]
            self.ndma[eng] += 1
            me = ("dma", eng, n)
        else:
            me = (eng, idx)
        for t, k in reads:
            self._update(t, k, False, me)
        for t, k in writes:
            self._update(t, k, True, me)
        self.ops[eng].append((fn, deps, me))
        return me

    def emit(self):
        nc = self.nc
        engs = ("pe", "dve", "act", "pool", "sp")
        sem_cms = {}
        sems = {}
        for e in engs:
            cm = nc.semaphore("s_" + e)
            sems[e] = cm.__enter__()
            self.cms.append(cm)
        dsems = {}
        for e in ("sp", "act", "pool"):
            if self.ndma[e]:
                lst = []
                for i in range(NDMA_SEMS):
                    cm = nc.semaphore("d_%s_%d" % (e, i))
                    lst.append(cm.__enter__())
                    self.cms.append(cm)
                dsems[e] = lst
        ops = self.ops

        def run(ename, engine):
            seen = {}
            cnt = 0
            for fn, deps, me in ops[ename]:
                need = {}
                for d in deps:
                    if d[0] == "dma":
                        s = dsems[d[1]][d[2] % NDMA_SEMS]
                        v = 16 * (d[2] // NDMA_SEMS + 1)
                    else:
                        if d[0] == ename:
                            if ename == "pe" or fn is None or ename in NOSELF:
                                continue
                        s = sems[d[0]]
                        v = d[1] + 1 - self.dma_before[d[0]][d[1]]
                    key = s.num if hasattr(s, "num") else id(s)
                    if v > need.get(key, (None, 0))[1]:
                        need[key] = (s, v)
                if me[0] == "dma":
                    n = me[2]
                    s = dsems[ename][n % NDMA_SEMS]
                    if n >= NDMA_SEMS:
                        key = s.num if hasattr(s, "num") else id(s)
                        v = 16 * (n // NDMA_SEMS)
                        if v > need.get(key, (None, 0))[1]:
                            need[key] = (s, v)
                for key, (s, v) in need.items():
                    if seen.get(key, 0) >= v:
                        continue
                    engine.wait_ge(s, v)
                    seen[key] = v
                if fn is None:
                    continue
                ins = fn(engine)
                if me[0] == "dma":
                    ins.then_inc(dsems[ename][me[2] % NDMA_SEMS], 16)
                else:
                    ins.then_inc(sems[ename], 1)

        self.dma_before = {}
        for e in engs:
            c = 0
            lst = []
            for fn, deps, me in ops[e]:
                lst.append(c)
                if me[0] in ("dma", "bar"):
                    c += 1
            self.dma_before[e] = lst

        with nc.Block() as block:
            @block.tensor
            def _(eng):
                run("pe", eng)

            @block.vector
            def _(eng):
                run("dve", eng)

            @block.scalar
            def _(eng):
                run("act", eng)
                self._drain("act", eng, dsems)

            @block.gpsimd
            def _(eng):
                run("pool", eng)
                self._drain("pool", eng, dsems)

            @block.sync
            def _(eng):
                run("sp", eng)
                self._drain("sp", eng, dsems)

    def _drain(self, e, eng, dsems):
        n = self.ndma[e]
        if not n:
            return
        for j in range(min(n, NDMA_SEMS)):
            last = ((n - 1 - j) // NDMA_SEMS) * NDMA_SEMS + j
            eng.wait_ge(dsems[e][j], 16 * (last // NDMA_SEMS + 1))

    def close(self):
        for cm in reversed(self.cms):
            cm.__exit__(None, None, None)
from concourse.bass_utils import run_bass_kernel_spmd

D_MODEL = 1024
C_DEC = 0.6065306597126334
STAGE = 0
SKIP = ""
NZRB = 2
NSB = 1
V_MU, V_W0, V_A0, V_KK, V_KA, V_LNW, V_LNB, V_RK, V_SINK, V_FIN = 0, 1824, 2336, 2848, 3360, 3872, 4384, 4896, 5408, 5416
NVEC = 5416 + 1024
C_ID, C_SU, C_IU, C_SL, C_BD, C_SH, C_CA, C_COS, C_SIN, C_ONE, C_IOTA = 0, 128, 256, 384, 512, 640, 768, 896, 1408, 1920, 1921
NCST = 1921 + 128


def make_cst():
    c = np.zeros((128, NCST), np.float32)
    i = np.arange(128)
    r, q = i[:, None], i[None, :]
    c[:, C_ID:C_ID + 128] = (r == q)
    c[:, C_SU:C_SU + 128] = (r < q)
    c[:, C_IU:C_IU + 128] = (r <= q)
    c[:, C_SL:C_SL + 128] = (r > q)
    c[:, C_BD:C_BD + 128] = ((r // 64) == (q // 64))
    c[:, C_SH:C_SH + 128] = (r == q - 1)
    c[127, C_CA] = 1.0
    inv = 10000.0 ** (-np.arange(0, 64, 2, dtype=np.float32) / 64)
    pos = (np.arange(16)[None, :] * 128 + i[:, None]).astype(np.float32)
    ang = pos[:, :, None] * inv[None, None, :]
    c[:, C_COS:C_COS + 512] = np.cos(ang).reshape(128, 512)
    c[:, C_SIN:C_SIN + 512] = np.sin(ang).reshape(128, 512)
    c[:, C_ONE] = 1.0
    c[:, C_IOTA:C_IOTA + 128] = q
    return c


class Ctx:
    pass


def helpers(p):
    h = Ctx()

    def tt(eng, out, in0, in1, op, r, w):
        p.op(eng, lambda e: e.tensor_tensor(out=out, in0=in0, in1=in1, op=op), r, w)

    def stt(eng, out, in0, scalar, in1, op0, op1, r, w):
        p.op(eng, lambda e: e.scalar_tensor_tensor(out=out, in0=in0, scalar=scalar, in1=in1, op0=op0, op1=op1), r, w)

    def ts(eng, out, in0, s1, s2, op0, op1, r, w):
        if s2 is None:
            p.op(eng, lambda e: e.tensor_scalar(out=out, in0=in0, scalar1=s1, scalar2=None, op0=op0), r, w)
        else:
            p.op(eng, lambda e: e.tensor_scalar(out=out, in0=in0, scalar1=s1, scalar2=s2, op0=op0, op1=op1), r, w)

    def act(out, in_, func, r, w, **kw):
        p.op("act", lambda e: e.activation(out=out, in_=in_, func=func, **kw), r, w)

    def cp(eng, out, in_, r, w):
        if eng == "act":
            p.op("act", lambda e: e.activation(out=out, in_=in_, func=AF.Copy), r, w)
        else:
            p.op(eng, lambda e: e.tensor_copy(out=out, in_=in_), r, w)

    def red(out, in_, r, w, op=ALU.add):
        p.op("dve", lambda e: e.tensor_reduce(out=out, in_=in_, axis=AX.X, op=op), r, w)

    def dma(eng, out, in_, r, w):
        p.op(eng, lambda e: e.dma_start(out=out, in_=in_), r, w, dma=True)

    def mms(fn, r, w):
        p.op("pe", fn, r, w)

    h.tt, h.stt, h.ts, h.act, h.cp, h.red, h.dma, h.mms = tt, stt, ts, act, cp, red, dma, mms
    return h


class Rot:
    def __init__(self, tiles):
        self.tiles = tiles
        self.i = 0

    def __call__(self):
        t = self.tiles[self.i % len(self.tiles)]
        self.i += 1
        return t


def load_w_bf16(p, h, name, dram, K, N, ncol0, ncols, stg, scale_t=None, scale_off=0, dt=BF16):
    kc = K // 128
    wt = p.sb(name, [128, kc, ncols], dt)
    for c in range(kc):
        s = stg()
        h.dma("sp", s[:, 0:ncols], dram[c * 128:(c + 1) * 128, ncol0:ncol0 + ncols], [], [s])
        if scale_t is not None:
            h.act(wt[:, c, :], s[:, 0:ncols], AF.Copy, [s, scale_t], [(wt, c)], scale=scale_t[:, scale_off + c:scale_off + c + 1])
        else:
            h.cp("pool", wt[:, c, :], s[:, 0:ncols], [s], [(wt, c)])
    return wt


def phase_a1(p, nc, D, n_seq, n_tiles, S_TOK):
    h = helpers(p)
    tt, stt, ts, act, cp, red, dma, mms = h.tt, h.stt, h.ts, h.act, h.cp, h.red, h.dma, h.mms
    NZ = 2592
    cst = p.sb("cst", [128, NCST], F32)
    dma("sp", cst[:], D["cst"], [], [cst])
    vec = p.sb("vec", [128, V_SINK + 8], F32)
    dma("sp", vec[:], D["vecs"][0:V_SINK + 8].partition_broadcast(128), [], [vec])
    nwp = p.sb("nwp", [128, 16], F32)
    dma("sp", nwp[:], D["nwp"], [], [nwp])
    ident = cst[:, C_ID:C_ID + 128]
    z = p.sb("z", [128, NZ], F32)
    Wb = load_w_bf16(p, h, "Wb1", D["w_in"], 1024, 4640, 0, NZ, (lambda: z), nwp, 0)
    w2 = p.sb("w2", [128, 512], F32)
    dma("sp", w2[0:64, :], D["decay_w2"], [], [w2])
    dma("sp", w2[64:128, :], D["iclr_a2"], [], [w2])
    g2 = p.sb("g2", [128, 2, 512], F32)
    dma("sp", g2[:, 0, :], D["gate_g2"][0:128, :], [], [g2])
    dma("sp", g2[0:32, 1, :], D["gate_g2"][128:160, :], [], [g2])
    negsink = p.sb("negsink", [128, 8], F32)
    ts("dve", negsink[:], vec[:, V_SINK:V_SINK + 8], -1.0, None, ALU.mult, None, [vec], [negsink])

    xt = Rot([p.sb("xt%d" % i, [128, 1024], F32) for i in range(2)])
    ss = p.sb("ss", [128, 1], F32)
    rstd = p.sb("rstd", [128, 1], F32)
    xs = p.sb("xs", [128, 1024], F32)
    xsT = p.sb("xsT", [128, 8, 128], BF16)
    carry = p.sb("carry", [128, 1824], F32)
    p.op("pool", lambda e: e.memset(carry[:], 0.0), [], [carry])
    zr = p.sb("zr", [128, 1824], F32)
    lin = p.sb("lin", [128, 288], F32)
    linT = p.sb("linT", [128, 3, 128], F32)
    T5 = Rot([p.sb("t5_%d" % i, [128, 512], F32) for i in range(12)])
    gv = p.sb("gv", [128, 512], F32)
    k2 = p.sb("k2", [128, 512], F32)
    M8 = Rot([p.sb("m8_%d" % i, [128, 8, 128], F32) for i in range(8)])
    M8d = [p.sb("m8d_%d" % i, [128, 8, 128], F32) for i in range(3)]
    Tq = Rot([p.sb("tq_%d" % i, [128, 4, 128], F32) for i in range(4)])
    sm = Rot([p.sb("sm_%d" % i, [128, 8], F32) for i in range(12)])
    PB = Rot([p.ps("pb%d" % i, [128, 512], F32) for i in range(6)])
    PV = [p.ps("pv%d" % i, [128, 512], F32) for i in range(2)]
    NDUM = 0
    if NDUM:
        dumb = p.ps("dumb", [128, 512], F32)
        mms0 = mms

        def mms(fn, r, w):
            def fn2(e):
                ins = fn(e)
                for _ in range(NDUM):
                    e.matmul(dumb[:], lhsT=Wb[:, 0, 0:128], rhs=Wb[:, 0, 0:512], start=True, stop=True)
                return ins
            mms0(fn2, r, w)
    ST = p.sb("ST", [128, 4, 128], F32)
    gC = p.sb("gC", [128, 4], F32)
    qk = p.sb("qk", [128, 10, 64], F32)
    kdup = p.sb("kdup", [128, 2, 2, 64], F32)
    qT = p.sb("qT", [128, 4, 128], BF16)
    kT = [p.sb("kT%d" % i, [128, 2, 128], BF16) for i in range(2)]
    va = [p.sb("va%d" % i, [128, 2, 65], BF16) for i in range(2)]
    for i in range(2):
        p.op("pool", lambda e, i=i: e.memset(va[i][:], 1.0), [], [va[i]])
    pT = Rot([p.sb("pT%d" % i, [128, 2, 128], BF16) for i in range(4)])
    yout = Rot([p.sb("yout%d" % i, [128, 1024], F32) for i in range(1)])

    def vb(off, n=512):
        return vec[:, off:off + n]

    order = [(b, n) for b in range(n_seq) for n in range(n_tiles)]
    xq = {}

    def prefetch(i):
        if i < len(order):
            b, n = order[i]
            t0 = b * S_TOK + n * 128
            xq[i] = xt()
            dma("sp", xq[i][:], D["x"][t0:t0 + 128, :], [], [xq[i]])

    prefetch(0)

    def tile_body(i):
            b, n = order[i]
            if n == 0:
                p.op("pool", lambda e: e.memset(ST[:], 0.0), [], [ST])
            tok0 = b * S_TOK + n * 128
            first = (n == 0)
            x_t = xq.pop(i)
            prefetch(i + 1)
            act(xs[:], x_t[:], AF.Square, [x_t], [xs, ss], accum_out=ss[:])
            act(rstd[:], ss[:], AF.Sqrt, [ss], [rstd], bias=1e-5, scale=1.0 / 1024)
            p.op("dve", lambda e: e.reciprocal(out=rstd[:], in_=rstd[:]), [rstd], [rstd])
            ts("dve", xs[:], x_t[:], rstd[:, 0:1], None, ALU.mult, None, [x_t, rstd], [xs])
            for hh in range(2):
                pb = PB()
                mms(lambda e, pb=pb, hh=hh: [e.transpose(out=pb[:, j * 128:(j + 1) * 128], in_=xs[:, (hh * 4 + j) * 128:(hh * 4 + j + 1) * 128], identity=ident) for j in range(4)][-1],
                    [xs, cst], [pb])
                cp("act" if hh else "dve", xsT[:, hh * 4:hh * 4 + 4, :], pb[:].rearrange("p (a b) -> p a b", a=4), [pb], [(xsT, hh)])
            for cc in range(6):
                c0 = cc * 512
                cw = min(512, NZ - c0)
                pb = PB()
                mms(lambda e, pb=pb, c0=c0, cw=cw: [e.matmul(pb[:, 0:cw], lhsT=xsT[:, c, :], rhs=Wb[:, c, c0:c0 + cw], start=(c == 0), stop=(c == 7)) for c in range(8)][-1],
                    [xsT, Wb], [pb])
                cp("act" if cc % 2 else "dve", z[:, c0:c0 + cw], pb[:, 0:cw], [pb], [(z, cc)])
            if STAGE == 1:
                yo = yout()
                cp("dve", yo[:], z[:, 0:1024], [z], [yo])
                dma("sp", D["y_scr"][tok0:tok0 + 128, :], yo[:], [yo], [])
                return
            cosb = cst[:, C_COS + n * 32:C_COS + n * 32 + 32].unsqueeze(1).to_broadcast([128, 10, 32])
            sinb = cst[:, C_SIN + n * 32:C_SIN + n * 32 + 32].unsqueeze(1).to_broadcast([128, 10, 32])
            zq = z[:, 0:640].rearrange("p (h d) -> p h d", h=10)
            x1, x2 = zq[:, :, 0:32], zq[:, :, 32:64]
            ta, tb_ = T5(), T5()
            tav = ta[:, 0:320].rearrange("p (h d) -> p h d", h=10)
            tbv = tb_[:, 0:320].rearrange("p (h d) -> p h d", h=10)
            zk = [(z, 0), (z, 1)]
            tt("dve", tav, x1, cosb, ALU.mult, zk + [cst], [ta])
            tt("pool", tbv, x2, sinb, ALU.mult, zk + [cst], [tb_])
            tt("dve", qk[:, :, 0:32], tav, tbv, ALU.subtract, [ta, tb_], [(qk, 0)])
            tc_, td = T5(), T5()
            tcv = tc_[:, 0:320].rearrange("p (h d) -> p h d", h=10)
            tdv = td[:, 0:320].rearrange("p (h d) -> p h d", h=10)
            tt("pool", tcv, x2, cosb, ALU.mult, zk + [cst], [tc_])
            tt("dve", tdv, x1, sinb, ALU.mult, zk + [cst], [td])
            tt("pool", qk[:, :, 32:64], tcv, tdv, ALU.add, [tc_, td], [(qk, 1)])
            cp("pool", kdup[:], qk[:, 8:10, :].unsqueeze(2).to_broadcast([128, 2, 2, 64]), [qk], [kdup])
            kTc, kTp = kT[n % 2], kT[(n + 1) % 2]
            vac, vap = va[n % 2], va[(n + 1) % 2]
            cp("pool", vac[:, :, 0:64], z[:, 640:768].rearrange("p (g d) -> p g d", g=2), [(z, 1)], [vac])
            pb = PB()
            mms(lambda e, pb=pb: [e.transpose(out=pb[:, j * 128:(j + 1) * 128], in_=qk[:, 2 * j:2 * j + 2, :].rearrange("p a d -> p (a d)"), identity=ident) for j in range(4)][-1],
                [qk, cst], [pb])
            cp("act", qT[:], pb[:].rearrange("p (a b) -> p a b", a=4), [pb], [qT])
            pb = PB()
            mms(lambda e, pb=pb: [e.transpose(out=pb[:, g * 128:(g + 1) * 128], in_=kdup[:, g, :, :].rearrange("p a d -> p (a d)"), identity=ident) for g in range(2)][-1],
                [kdup, cst], [pb])
            cp("dve", kTc[:], pb[:, 0:256].rearrange("p (a b) -> p a b", a=2), [pb], [kTc])
            yo = yout()
            pv = PV
            for hd in range(0 if "a" in SKIP else 8):
                m, base, g = hd // 2, 64 * (hd % 2), hd // 4
                pb = PB()
                if first:
                    mms(lambda e, pb=pb, m=m, base=base, g=g: e.matmul(pb[:, 128:256], lhsT=kTc[base:base + 64, g, :], rhs=qT[base:base + 64, m, :], start=True, stop=True),
                        [kTc, qT], [pb])
                else:
                    mms(lambda e, pb=pb, m=m, base=base, g=g: [e.matmul(pb[:, 0:128], lhsT=kTp[base:base + 64, g, :], rhs=qT[base:base + 64, m, :], start=True, stop=True),
                                                              e.matmul(pb[:, 128:256], lhsT=kTc[base:base + 64, g, :], rhs=qT[base:base + 64, m, :], start=True, stop=True)][-1],
                        [kTc, kTp, qT], [pb])
                pt = pT()
                lo = 1 if first else 0
                act(pt[:, lo:2, :], pb[:, lo * 128:256].rearrange("p (a b) -> p a b", a=2 - lo), AF.Exp, [pb, negsink], [pt],
                    scale=0.125, bias=negsink[:, hd:hd + 1])
                if not first:
                    tt("pool", pt[:, 0, :], pt[:, 0, :], cst[:, C_SL:C_SL + 128], ALU.mult, [pt, cst], [pt])
                tt("dve", pt[:, 1, :], pt[:, 1, :], cst[:, C_IU:C_IU + 128], ALU.mult, [pt, cst], [pt])
                pvb = pv[hd // 4]
                o0 = (hd % 4) * 65
                if first:
                    mms(lambda e, pvb=pvb, pt=pt, g=g, o0=o0: e.matmul(pvb[:, o0:o0 + 65], lhsT=pt[:, 1, :], rhs=vac[:, g, :], start=True, stop=True),
                        [pt, vac], [pvb])
                else:
                    mms(lambda e, pvb=pvb, pt=pt, g=g, o0=o0: [e.matmul(pvb[:, o0:o0 + 65], lhsT=pt[:, 0, :], rhs=vap[:, g, :], start=True, stop=False),
                                                              e.matmul(pvb[:, o0:o0 + 65], lhsT=pt[:, 1, :], rhs=vac[:, g, :], start=False, stop=True)][-1],
                        [pt, vac, vap], [pvb])
            for hf in range(2):
                pvb = pv[hf]
                pvv = pvb[:, 0:260].rearrange("p (h d) -> p h d", h=4)
                den = sm()
                ts("dve", den[:, 0:4], pvv[:, :, 64], 1.0, None, ALU.add, None, [pvb], [den])
                p.op("dve", lambda e, den=den: e.reciprocal(out=den[:, 0:4], in_=den[:, 0:4]), [den], [den])
                tt("dve", yo[:, hf * 256:(hf + 1) * 256].rearrange("p (h d) -> p h d", h=4), pvv[:, :, 0:64],
                   den[:, 0:4].unsqueeze(2).to_broadcast([128, 4, 64]), ALU.mult, [pvb, den], [(yo, hf)])
            if STAGE == 2:
                cp("dve", yo[:, 512:1024], z[:, 0:512], [z], [yo])
                dma("sp", D["y_scr"][tok0:tok0 + 128, :], yo[:], [yo], [])
                return
            if "r" in SKIP:
                dma("sp", D["y_scr"][tok0:tok0 + 128, :], yo[:], [yo], [(D["y_t"], tok0 // 128)])
                return
            for j in range(4):
                c0 = 768 + j * 512
                cw = min(512, NZ - c0)
                pb = PB()
                zkeys = [(z, 1), (z, 2), (z, 3), (z, 4), (z, 5)]
                if first:
                    mms(lambda e, pb=pb, c0=c0, cw=cw: e.matmul(pb[:, 0:cw], lhsT=cst[:, C_SH:C_SH + 128], rhs=z[:, c0:c0 + cw], start=True, stop=True),
                        zkeys + [cst], [pb])
                else:
                    mms(lambda e, pb=pb, c0=c0, cw=cw: [e.matmul(pb[:, 0:cw], lhsT=cst[:, C_SH:C_SH + 128], rhs=z[:, c0:c0 + cw], start=True, stop=False),
                                                       e.matmul(pb[:, 0:cw], lhsT=cst[:, C_CA:C_CA + 128], rhs=carry[:, c0 - 768:c0 - 768 + cw], start=False, stop=True)][-1],
                        zkeys + [cst, carry], [pb])
                r0 = c0 - 768
                tt("dve", zr[:, r0:r0 + cw], pb[:, 0:cw], z[:, c0:c0 + cw], ALU.subtract, [pb] + zkeys, [(zr, j)])
                tt("dve", zr[:, r0:r0 + cw], zr[:, r0:r0 + cw], vec[:, V_MU + r0:V_MU + r0 + cw], ALU.mult, [(zr, j), vec], [(zr, j)])
                tt("pool", zr[:, r0:r0 + cw], zr[:, r0:r0 + cw], z[:, c0:c0 + cw], ALU.add, [(zr, j)] + zkeys, [(zr, j)])
            cp("pool", carry[96:128, :], z[96:128, 768:NZ], [z], [carry])
            r_, k_, v_ = zr[:, 0:512], zr[:, 512:1024], zr[:, 1024:1536]
            if STAGE == 3:
                cp("dve", yo[:, 512:1024], zr[:, 0:512], [], [yo])
                dma("sp", D["y_scr"][tok0:tok0 + 128, :], yo[:], [yo], [])
                return
            act(lin[:, 0:64], zr[:, 1536:1600], AF.Tanh, [zr], [(lin, 0)])
            cp("pool", lin[:, 64:128], zr[:, 1600:1664], [zr], [(lin, 1)])
            act(lin[:, 128:288], zr[:, 1664:1824], AF.Sigmoid, [zr], [(lin, 2)])
            pb = PB()
            mms(lambda e, pb=pb: [e.transpose(out=pb[:, 0:128], in_=lin[:, 0:128], identity=ident),
                                  e.transpose(out=pb[:, 128:256], in_=lin[:, 128:256], identity=ident),
                                  e.transpose(out=pb[0:32, 256:384], in_=lin[:, 256:288], identity=ident)][-1], [lin, cst], [pb])
            cp("dve", linT[:, 0:2, :], pb[:, 0:256].rearrange("p (a b) -> p a b", a=2), [pb], [(linT, 0)])
            cp("dve", linT[0:32, 2, :], pb[0:32, 256:384], [pb], [(linT, 1)])
            pw, pa_, pg = PB(), PB(), PB()
            mms(lambda e, pw=pw: e.matmul(pw[:], lhsT=linT[0:64, 0, :], rhs=w2[0:64, :], start=True, stop=True), [linT, w2], [pw])
            mms(lambda e, pa_=pa_: e.matmul(pa_[:], lhsT=linT[64:128, 0, :], rhs=w2[64:128, :], start=True, stop=True), [linT, w2], [pa_])
            mms(lambda e, pg=pg: [e.matmul(pg[:], lhsT=linT[:, 1, :], rhs=g2[:, 0, :], start=True, stop=False),
                                  e.matmul(pg[:], lhsT=linT[0:32, 2, :], rhs=g2[0:32, 1, :], start=False, stop=True)][-1], [linT, g2], [pg])
            sg, av = T5(), T5()
            tt("dve", sg[:], pw[:], vb(V_W0), ALU.add, [pw, vec], [sg])
            act(sg[:], sg[:], AF.Sigmoid, [sg], [sg])
            tt("dve", av[:], pa_[:], vb(V_A0), ALU.add, [pa_, vec], [av])
            act(av[:], av[:], AF.Sigmoid, [av], [av])
            cp("act", gv[:], pg[:], [pg], [gv])
            if STAGE == 4:
                cp("dve", yo[:, 512:1024], sg[:], [], [yo])
                dma("sp", D["y_scr"][tok0:tok0 + 128, :], yo[:], [yo], [])
                return
            pc = PB()
            mms(lambda e, pc=pc, sg=sg: e.matmul(pc[:], lhsT=cst[:, C_IU:C_IU + 128], rhs=sg[:], start=True, stop=True), [cst, sg], [pc])
            gam, igam, gprev = T5(), T5(), T5()
            act(gam[:], pc[:], AF.Exp, [pc], [gam], scale=-C_DEC)
            act(igam[:], pc[:], AF.Exp, [pc], [igam], scale=C_DEC)
            tt("dve", gprev[:], pc[:], sg[:], ALU.subtract, [pc, sg], [gprev])
            act(gprev[:], gprev[:], AF.Exp, [gprev], [gprev], scale=-C_DEC)
            pgc = PB()
            mms(lambda e, pgc=pgc, sg=sg: [e.matmul(pgc[:, m:m + 1], lhsT=sg[:, m * 128:(m + 1) * 128], rhs=cst[:, C_ONE:C_ONE + 1], start=True, stop=True) for m in range(4)][-1],
                [cst, sg], [pgc])
            act(gC[:], pgc[:, 0:4], AF.Exp, [pgc], [gC], scale=-C_DEC)
            if STAGE == 5:
                cp("dve", yo[:, 512:1024], gprev[:], [], [yo])
                dma("sp", D["y_scr"][tok0:tok0 + 128, :], yo[:], [yo], [])
                return
            kk, sq = T5(), T5()
            tt("dve", kk[:], k_, vb(V_KK), ALU.mult, [zr, vec], [kk])
            tt("pool", sq[:], kk[:], kk[:], ALU.mult, [kk], [sq])
            if STAGE == 51:
                cp("dve", yo[:, 512:1024], sq[:], [], [yo])
                dma("sp", D["y_scr"][tok0:tok0 + 128, :], yo[:], [yo], [])
                return
            s8 = sm()
            red(s8[:], sq[:].rearrange("p (h d) -> p h d", h=8), [sq], [s8])
            if STAGE == 52:
                cp("dve", yo[:, 512:1024], sq[:], [], [yo])
                dma("sp", D["y_scr"][tok0:tok0 + 128, :], yo[:], [yo], [])
                return
            act(s8[:], s8[:], AF.Sqrt, [s8], [s8], bias=1e-24, scale=1.0)
            p.op("dve", lambda e, s8=s8: e.reciprocal(out=s8[:], in_=s8[:]), [s8], [s8])
            if STAGE == 53:
                cp("dve", yo[:, 512:1024], sq[:], [], [yo])
                dma("sp", D["y_scr"][tok0:tok0 + 128, :], yo[:], [yo], [])
                return
            tt("dve", kk[:].rearrange("p (h d) -> p h d", h=8), kk[:].rearrange("p (h d) -> p h d", h=8),
               s8[:].unsqueeze(2).to_broadcast([128, 8, 64]), ALU.mult, [kk, s8], [kk])
            if STAGE == 54:
                cp("dve", yo[:, 512:1024], kk[:], [], [yo])
                dma("sp", D["y_scr"][tok0:tok0 + 128, :], yo[:], [yo], [])
                return
            t1 = T5()
            stt("dve", t1[:], av[:], -1.0, vb(V_KA), ALU.add, ALU.mult, [av, vec], [t1])
            stt("dve", k2[:], t1[:], 1.0, k_, ALU.add, ALU.mult, [t1, zr], [k2])
            if STAGE == 55:
                cp("dve", yo[:, 512:1024], k2[:], [], [yo])
                dma("sp", D["y_scr"][tok0:tok0 + 128, :], yo[:], [yo], [])
                return
            At, Bt, Kt, Rt = T5(), T5(), T5(), T5()
            stt("dve", At[:], kk[:], -1.0, gprev[:], ALU.mult, ALU.mult, [kk, gprev], [At])
            if STAGE == 56:
                cp("dve", yo[:, 512:1024], At[:], [At], [yo])
                dma("sp", D["y_scr"][tok0:tok0 + 128, :], yo[:], [yo], [])
                return
            tt("pool", Bt[:], kk[:], av[:], ALU.mult, [kk, av], [Bt])
            tt("dve", Bt[:], Bt[:], igam[:], ALU.mult, [Bt, igam], [Bt])
            if STAGE == 57:
                cp("dve", yo[:, 512:1024], Bt[:], [Bt], [yo])
                dma("sp", D["y_scr"][tok0:tok0 + 128, :], yo[:], [yo], [])
                return
            tt("dve", Kt[:], k2[:], igam[:], ALU.mult, [k2, igam], [Kt])
            if STAGE == 58:
                cp("dve", yo[:, 512:1024], Kt[:], [Kt], [yo])
                dma("sp", D["y_scr"][tok0:tok0 + 128, :], yo[:], [yo], [])
                return
            tt("pool", Rt[:], r_, gam[:], ALU.mult, [zr, gam], [Rt])
            if STAGE == 6:
                cp("dve", yo[:, 512:1024], Rt[:], [Rt], [yo])
                dma("sp", D["y_scr"][tok0:tok0 + 128, :], yo[:], [yo], [])
                return
            XT = {}
            for nm, src in (("A", At), ("B", Bt), ("K", Kt), ("R", Rt)):
                pb = PB()
                mms(lambda e, pb=pb, src=src: [e.transpose(out=pb[:, j * 128:(j + 1) * 128], in_=src[:, j * 128:(j + 1) * 128], identity=ident) for j in range(4)][-1],
                    [src, cst], [pb])
                dst = Tq()
                cp("act" if nm in ("A", "K") else "dve", dst[:], pb[:].rearrange("p (a b) -> p a b", a=4), [pb], [dst])
                XT[nm] = dst
            AT, BT, KT, RT = XT["A"], XT["B"], XT["K"], XT["R"]

            def pairmat(l, r_op, mask_off, eng2, dst=None):
                dst = dst or M8()
                for par in range(2):
                    pb = PB()
                    mms(lambda e, pb=pb, par=par: [e.matmul(pb[:, j * 128:(j + 1) * 128],
                                                          lhsT=l[64 * par:64 * par + 64, j, :],
                                                          rhs=r_op[64 * par:64 * par + 64, j, :],
                                                          start=True, stop=True) for j in range(4)][-1], [l, r_op], [pb])
                    tt(eng2[par], dst[:, par:8:2, :], pb[:].rearrange("p (a b) -> p a b", a=4),
                       cst[:, mask_off:mask_off + 128].unsqueeze(1).to_broadcast([128, 4, 128]), ALU.mult, [pb, cst], [(dst, par)])
                return dst

            Nm = pairmat(BT, AT, C_SU, ("dve", "dve"))
            Am = pairmat(AT, BT, C_SL, ("dve", "dve"))
            AkT = pairmat(KT, AT, C_SU, ("dve", "dve"), M8d[0])
            RbT = pairmat(BT, RT, C_IU, ("dve", "dve"), M8d[1])
            RkT = pairmat(KT, RT, C_IU, ("dve", "dve"), M8d[2])
            if STAGE == 7:
                cp("dve", yo[:, 512:1024].rearrange("p (a b) -> p a b", a=4), RkT[:, 0:4, :], [RkT], [yo])
                dma("sp", D["y_scr"][tok0:tok0 + 128, :], yo[:], [yo], [])
                return
            X = M8()
            tt("dve", X[:], Nm[:], ident.unsqueeze(1).to_broadcast([128, 8, 128]), ALU.add, [Nm, cst], [X])
            for j in range(0 if "d" in SKIP else 6):
                Nn, An, Xn = M8(), M8(), M8()
                last = (j == 5)
                for hf in range(2):
                    pbn, pba = PB(), PB()
                    if not last:
                        mms(lambda e, pbn=pbn, hf=hf, Am=Am, Nm=Nm: [e.matmul(pbn[:, q * 128:(q + 1) * 128], lhsT=Am[:, hf * 4 + q, :], rhs=Nm[:, hf * 4 + q, :], start=True, stop=True) for q in range(4)][-1],
                            [Am, Nm], [pbn])
                        cp("act", Nn[:, hf * 4:hf * 4 + 4, :], pbn[:].rearrange("p (a b) -> p a b", a=4), [pbn], [(Nn, hf)])
                    mms(lambda e, pba=pba, hf=hf, Am=Am, Nm=Nm: [e.matmul(pba[:, q * 128:(q + 1) * 128], lhsT=Nm[:, hf * 4 + q, :], rhs=Am[:, hf * 4 + q, :], start=True, stop=True) for q in range(4)][-1],
                        [Am, Nm], [pba])
                    cp("dve", An[:, hf * 4:hf * 4 + 4, :], pba[:].rearrange("p (a b) -> p a b", a=4), [pba], [(An, hf)])
                for hf in range(2):
                    pbx = PB()
                    mms(lambda e, pbx=pbx, hf=hf, An=An, X=X: [e.matmul(pbx[:, q * 128:(q + 1) * 128], lhsT=An[:, hf * 4 + q, :], rhs=X[:, hf * 4 + q, :], start=True, stop=True) for q in range(4)][-1],
                        [An, X], [pbx])
                    tt("dve", Xn[:, hf * 4:hf * 4 + 4, :], pbx[:].rearrange("p (a b) -> p a b", a=4), X[:, hf * 4:hf * 4 + 4, :], ALU.add, [pbx, X], [(Xn, hf)])
                Nm, Am, X = Nn, An, Xn
            if STAGE == 8:
                cp("dve", yo[:, 512:1024].rearrange("p (a b) -> p a b", a=4), X[:, 0:4, :], [X], [yo])
                dma("sp", D["y_scr"][tok0:tok0 + 128, :], yo[:], [yo], [])
                return
            pr = PB()

            def f_rhs0(e, pr=pr, AT=AT, AkT=AkT):
                ins = None
                for m in range(4):
                    e.matmul(pr[:, m * 128:(m + 1) * 128], lhsT=AT[:, m, :], rhs=ST[:, m, :], start=True, stop=False)
                    for q in range(2):
                        hd = 2 * m + q
                        ins = e.matmul(pr[:, hd * 64:(hd + 1) * 64], lhsT=AkT[:, hd, :], rhs=zr[:, 1024 + hd * 64:1024 + (hd + 1) * 64], start=False, stop=(q == 1))
                return ins
            mms(f_rhs0, [AT, ST, AkT, zr], [pr])
            rhs0 = T5()
            cp("act", rhs0[:], pr[:], [pr], [rhs0])
            pu = PB()
            mms(lambda e, pu=pu, X=X, rhs0=rhs0: [e.matmul(pu[:, hd * 64:(hd + 1) * 64], lhsT=X[:, hd, :], rhs=rhs0[:, hd * 64:(hd + 1) * 64], start=True, stop=True) for hd in range(8)][-1],
                [X, rhs0], [pu])
            U = T5()
            cp("dve", U[:], pu[:], [pu], [U])
            py = PB()

            def f_y(e, py=py, RT=RT, RbT=RbT, RkT=RkT, U=U):
                ins = None
                for m in range(4):
                    e.matmul(py[:, m * 128:(m + 1) * 128], lhsT=RT[:, m, :], rhs=ST[:, m, :], start=True, stop=False)
                    for q in range(2):
                        hd = 2 * m + q
                        e.matmul(py[:, hd * 64:(hd + 1) * 64], lhsT=RbT[:, hd, :], rhs=U[:, hd * 64:(hd + 1) * 64], start=False, stop=False)
                        ins = e.matmul(py[:, hd * 64:(hd + 1) * 64], lhsT=RkT[:, hd, :], rhs=zr[:, 1024 + hd * 64:1024 + (hd + 1) * 64], start=False, stop=(q == 1))
                return ins
            mms(f_y, [RT, ST, RbT, RkT, U, zr], [py])
            yv = T5()
            cp("act", yv[:], py[:], [py], [yv])
            pst = PB()

            def f_s(e, pst=pst, Bt=Bt, Kt=Kt, U=U):
                ins = None
                for m in range(4):
                    e.matmul(pst[:, m * 128:(m + 1) * 128], lhsT=Bt[:, m * 128:(m + 1) * 128], rhs=U[:, m * 128:(m + 1) * 128], start=True, stop=False)
                    ins = e.matmul(pst[:, m * 128:(m + 1) * 128], lhsT=Kt[:, m * 128:(m + 1) * 128], rhs=zr[:, 1024 + m * 128:1024 + (m + 1) * 128], start=False, stop=True)
                return ins
            mms(f_s, [Bt, Kt, U, zr], [pst])
            tt("dve", ST[:], pst[:].rearrange("p (a b) -> p a b", a=4), ST[:], ALU.add, [pst, ST], [ST])
            tt("dve", ST[:], ST[:], gC[:].unsqueeze(2).to_broadcast([128, 4, 128]), ALU.mult, [ST, gC], [ST])
            tt("dve", ST[:], ST[:], cst[:, C_BD:C_BD + 128].unsqueeze(1).to_broadcast([128, 4, 128]), ALU.mult, [ST, cst], [ST])
            if STAGE == 9:
                cp("dve", yo[:, 512:1024], yv[:], [yv], [yo])
                dma("sp", D["y_scr"][tok0:tok0 + 128, :], yo[:], [yo], [])
                return
            y3 = yv[:].rearrange("p (h d) -> p h d", h=8)
            ysq = T5()
            tt("pool", ysq[:], yv[:], yv[:], ALU.mult, [yv], [ysq])
            s1, s2, mean, var = sm(), sm(), sm(), sm()
            red(s1[:], y3, [yv], [s1])
            red(s2[:], ysq[:].rearrange("p (h d) -> p h d", h=8), [ysq], [s2])
            ts("dve", mean[:], s1[:], 1.0 / 64, None, ALU.mult, None, [s1], [mean])
            tt("dve", var[:], mean[:], mean[:], ALU.mult, [mean], [var])
            stt("dve", var[:], s2[:], 1.0 / 64, var[:], ALU.mult, ALU.subtract, [s2, var], [var])
            act(var[:], var[:], AF.Sqrt, [var], [var], bias=64e-5, scale=1.0)
            p.op("dve", lambda e, var=var: e.reciprocal(out=var[:], in_=var[:]), [var], [var])
            yn = T5()
            yn3 = yn[:].rearrange("p (h d) -> p h d", h=8)
            tt("dve", yn3, y3, mean[:].unsqueeze(2).to_broadcast([128, 8, 64]), ALU.subtract, [yv, mean], [yn])
            tt("dve", yn3, yn3, var[:].unsqueeze(2).to_broadcast([128, 8, 64]), ALU.mult, [yn, var], [yn])
            tt("pool", yn[:], yn[:], vb(V_LNW), ALU.mult, [yn, vec], [yn])
            tt("pool", yn[:], yn[:], vb(V_LNB), ALU.add, [yn, vec], [yn])
            rk = T5()
            tt("pool", rk[:], r_, k2[:], ALU.mult, [zr, k2], [rk])
            tt("pool", rk[:], rk[:], vb(V_RK), ALU.mult, [rk, vec], [rk])
            sb_ = sm()
            red(sb_[:], rk[:].rearrange("p (h d) -> p h d", h=8), [rk], [sb_])
            tt("dve", rk[:].rearrange("p (h d) -> p h d", h=8), v_.rearrange("p (h d) -> p h d", h=8),
               sb_[:].unsqueeze(2).to_broadcast([128, 8, 64]), ALU.mult, [zr, sb_], [rk])
            tt("pool", yn[:], yn[:], rk[:], ALU.add, [yn, rk], [yn])
            tt("pool", yo[:, 512:1024], yn[:], gv[:], ALU.mult, [yn, gv], [(yo, 2)])
            dma("sp", D["y_scr"][tok0:tok0 + 128, :], yo[:], [yo], [(D["y_t"], tok0 // 128)])

    for i in range(len(order)):
        tile_body(i)


def phase_a1a(p, nc, D, n_seq, n_tiles, S_TOK):
    h = helpers(p)
    tt, stt, ts, act, cp, red, dma, mms = h.tt, h.stt, h.ts, h.act, h.cp, h.red, h.dma, h.mms
    NZ = 2592
    cst = p.sb("cst", [128, NCST], F32)
    dma("sp", cst[:], D["cst"], [], [cst])
    vec = p.sb("vecmu", [128, 1824], F32)
    dma("sp", vec[:], D["vecs"][V_MU:V_MU + 1824].partition_broadcast(128), [], [vec])
    snk = p.sb("snk", [128, 8], F32)
    dma("sp", snk[:], D["vecs"][V_SINK:V_SINK + 8].partition_broadcast(128), [], [snk])
    nwp = p.sb("nwp", [128, 16], F32)
    dma("sp", nwp[:], D["nwp"], [], [nwp])
    ident = cst[:, C_ID:C_ID + 128]
    negsink = p.sb("negsink", [128, 8], F32)
    ts("dve", negsink[:], snk[:], -1.0, None, ALU.mult, None, [snk], [negsink])

    def make_stream(k):
        sf = "_s%d" % k
        xt = Rot([p.sb("xt%d" % i + sf, [128, 1024], F32) for i in range(2)])
        ss = p.sb("ss" + sf, [128, 1], F32)
        rstd = p.sb("rstd" + sf, [128, 1], F32)
        xs = p.sb("xs" + sf, [128, 1024], F32)
        xsT = p.sb("xsT" + sf, [128, 8, 128], BF16)
        z = p.sb("z" + sf, [128, NZ], F32)
        carry = p.sb("carry" + sf, [128, 1824], F32)
        p.op("pool", lambda e: e.memset(carry[:], 0.0), [], [carry])
        ZR = Rot([p.sb("zr%d" % i + sf, [128, 1824], F32) for i in range(2)])
        T5 = Rot([p.sb("t5_%d" % i + sf, [128, 320], F32) for i in range(4)])
        sm = Rot([p.sb("sm_%d" % i + sf, [128, 8], F32) for i in range(4)])
        PB = Rot([p.ps("pb%d" % i + sf, [128, 512], F32) for i in range(2)])
        PV = [p.ps("pv%d" % i + sf, [128, 512], F32) for i in range(2)]
        qk = p.sb("qk" + sf, [128, 10, 64], F32)
        kdup = p.sb("kdup" + sf, [128, 2, 2, 64], F32)
        qT = p.sb("qT" + sf, [128, 4, 128], BF16)
        kT = [p.sb("kT%d" % i + sf, [128, 2, 128], BF16) for i in range(2)]
        va = [p.sb("va%d" % i + sf, [128, 2, 65], BF16) for i in range(2)]
        for i in range(2):
            p.op("pool", lambda e, i=i: e.memset(va[i][:], 1.0), [], [va[i]])
        pT = Rot([p.sb("pT%d" % i + sf, [128, 2, 128], BF16) for i in range(4)])
        yout = Rot([p.sb("yout%d" % i + sf, [128, 512], F32) for i in range(2)])
        xq = {}

        def prefetch(b, n):
            if n < n_tiles:
                t0 = b * S_TOK + n * 128
                xq[(b, n)] = xt()
                dma("sp", xq[(b, n)][:], D["x"][t0:t0 + 128, :], [], [xq[(b, n)]])

        def tile_body(b, n):
            tok0 = b * S_TOK + n * 128
            first = (n == 0)
            x_t = xq.pop((b, n))
            prefetch(b, n + 1)
            zr = ZR()
            act(xs[:], x_t[:], AF.Square, [x_t], [xs, ss], accum_out=ss[:])
            act(rstd[:], ss[:], AF.Sqrt, [ss], [rstd], bias=1e-5, scale=1.0 / 1024)
            p.op("dve", lambda e: e.reciprocal(out=rstd[:], in_=rstd[:]), [rstd], [rstd])
            ts("dve", xs[:], x_t[:], rstd[:, 0:1], None, ALU.mult, None, [x_t, rstd], [xs])
            for hh in range(2):
                pb = PB()
                mms(lambda e, pb=pb, hh=hh: [e.transpose(out=pb[:, j * 128:(j + 1) * 128], in_=xs[:, (hh * 4 + j) * 128:(hh * 4 + j + 1) * 128], identity=ident) for j in range(4)][-1],
                    [xs, cst], [pb])
                cp("act" if hh else "dve", xsT[:, hh * 4:hh * 4 + 4, :], pb[:].rearrange("p (a b) -> p a b", a=4), [pb], [(xsT, hh)])
            for cc in range(6):
                c0 = cc * 512
                cw = min(512, NZ - c0)
                pb = PB()
                mms(lambda e, pb=pb, c0=c0, cw=cw: [e.matmul(pb[:, 0:cw], lhsT=xsT[:, c, :], rhs=Wb[:, c, c0:c0 + cw], start=(c == 0), stop=(c == 7)) for c in range(8)][-1],
                    [xsT, Wb], [pb])
                cp("act" if cc % 2 else "dve", z[:, c0:c0 + cw], pb[:, 0:cw], [pb], [(z, cc)])
            cosb = cst[:, C_COS + n * 32:C_COS + n * 32 + 32].unsqueeze(1).to_broadcast([128, 10, 32])
            sinb = cst[:, C_SIN + n * 32:C_SIN + n * 32 + 32].unsqueeze(1).to_broadcast([128, 10, 32])
            zq = z[:, 0:640].rearrange("p (h d) -> p h d", h=10)
            x1, x2 = zq[:, :, 0:32], zq[:, :, 32:64]
            ta, tb_ = T5(), T5()
            tav = ta[:, 0:320].rearrange("p (h d) -> p h d", h=10)
            tbv = tb_[:, 0:320].rearrange("p (h d) -> p h d", h=10)
            zk = [(z, 0), (z, 1)]
            tt("dve", tav, x1, cosb, ALU.mult, zk + [cst], [ta])
            tt("pool", tbv, x2, sinb, ALU.mult, zk + [cst], [tb_])
            tt("dve", qk[:, :, 0:32], tav, tbv, ALU.subtract, [ta, tb_], [(qk, 0)])
            tc_, td = T5(), T5()
            tcv = tc_[:, 0:320].rearrange("p (h d) -> p h d", h=10)
            tdv = td[:, 0:320].rearrange("p (h d) -> p h d", h=10)
            tt("pool", tcv, x2, cosb, ALU.mult, zk + [cst], [tc_])
            tt("dve", tdv, x1, sinb, ALU.mult, zk + [cst], [td])
            tt("pool", qk[:, :, 32:64], tcv, tdv, ALU.add, [tc_, td], [(qk, 1)])
            cp("pool", kdup[:], qk[:, 8:10, :].unsqueeze(2).to_broadcast([128, 2, 2, 64]), [qk], [kdup])
            kTc, kTp = kT[n % 2], kT[(n + 1) % 2]
            vac, vap = va[n % 2], va[(n + 1) % 2]
            cp("pool", vac[:, :, 0:64], z[:, 640:768].rearrange("p (g d) -> p g d", g=2), [(z, 1)], [vac])
            pb = PB()
            mms(lambda e, pb=pb: [e.transpose(out=pb[:, j * 128:(j + 1) * 128], in_=qk[:, 2 * j:2 * j + 2, :].rearrange("p a d -> p (a d)"), identity=ident) for j in range(4)][-1],
                [qk, cst], [pb])
            cp("act", qT[:], pb[:].rearrange("p (a b) -> p a b", a=4), [pb], [qT])
            pb = PB()
            mms(lambda e, pb=pb: [e.transpose(out=pb[:, g * 128:(g + 1) * 128], in_=kdup[:, g, :, :].rearrange("p a d -> p (a d)"), identity=ident) for g in range(2)][-1],
                [kdup, cst], [pb])
            cp("dve", kTc[:], pb[:, 0:256].rearrange("p (a b) -> p a b", a=2), [pb], [kTc])
            yo = yout()
            pv = PV
            for hd in range(0 if "a" in SKIP else 8):
                m, base, g = hd // 2, 64 * (hd % 2), hd // 4
                pb = PB()
                if first:
                    mms(lambda e, pb=pb, m=m, base=base, g=g: e.matmul(pb[:, 128:256], lhsT=kTc[base:base + 64, g, :], rhs=qT[base:base + 64, m, :], start=True, stop=True),
                        [kTc, qT], [pb])
                else:
                    mms(lambda e, pb=pb, m=m, base=base, g=g: [e.matmul(pb[:, 0:128], lhsT=kTp[base:base + 64, g, :], rhs=qT[base:base + 64, m, :], start=True, stop=True),
                                                              e.matmul(pb[:, 128:256], lhsT=kTc[base:base + 64, g, :], rhs=qT[base:base + 64, m, :], start=True, stop=True)][-1],
                        [kTc, kTp, qT], [pb])
                pt = pT()
                lo = 1 if first else 0
                act(pt[:, lo:2, :], pb[:, lo * 128:256].rearrange("p (a b) -> p a b", a=2 - lo), AF.Exp, [pb, negsink], [pt],
                    scale=0.125, bias=negsink[:, hd:hd + 1])
                if not first:
                    tt("pool", pt[:, 0, :], pt[:, 0, :], cst[:, C_SL:C_SL + 128], ALU.mult, [pt, cst], [pt])
                tt("dve", pt[:, 1, :], pt[:, 1, :], cst[:, C_IU:C_IU + 128], ALU.mult, [pt, cst], [pt])
                pvb = pv[hd // 4]
                o0 = (hd % 4) * 65
                if first:
                    mms(lambda e, pvb=pvb, pt=pt, g=g, o0=o0: e.matmul(pvb[:, o0:o0 + 65], lhsT=pt[:, 1, :], rhs=vac[:, g, :], start=True, stop=True),
                        [pt, vac], [pvb])
                else:
                    mms(lambda e, pvb=pvb, pt=pt, g=g, o0=o0: [e.matmul(pvb[:, o0:o0 + 65], lhsT=pt[:, 0, :], rhs=vap[:, g, :], start=True, stop=False),
                                                              e.matmul(pvb[:, o0:o0 + 65], lhsT=pt[:, 1, :], rhs=vac[:, g, :], start=False, stop=True)][-1],
                        [pt, vac, vap], [pvb])
            for hf in range(2):
                pvb = pv[hf]
                pvv = pvb[:, 0:260].rearrange("p (h d) -> p h d", h=4)
                den = sm()
                ts("dve", den[:, 0:4], pvv[:, :, 64], 1.0, None, ALU.add, None, [pvb], [den])
                p.op("dve", lambda e, den=den: e.reciprocal(out=den[:, 0:4], in_=den[:, 0:4]), [den], [den])
                tt("dve", yo[:, hf * 256:(hf + 1) * 256].rearrange("p (h d) -> p h d", h=4), pvv[:, :, 0:64],
                   den[:, 0:4].unsqueeze(2).to_broadcast([128, 4, 64]), ALU.mult, [pvb, den], [(yo, hf)])
            for j in range(4):
                c0 = 768 + j * 512
                cw = min(512, NZ - c0)
                pb = PB()
                zkeys = [(z, 1), (z, 2), (z, 3), (z, 4), (z, 5)]
                if first:
                    mms(lambda e, pb=pb, c0=c0, cw=cw: e.matmul(pb[:, 0:cw], lhsT=cst[:, C_SH:C_SH + 128], rhs=z[:, c0:c0 + cw], start=True, stop=True),
                        zkeys + [cst], [pb])
                else:
                    mms(lambda e, pb=pb, c0=c0, cw=cw: [e.matmul(pb[:, 0:cw], lhsT=cst[:, C_SH:C_SH + 128], rhs=z[:, c0:c0 + cw], start=True, stop=False),
                                                       e.matmul(pb[:, 0:cw], lhsT=cst[:, C_CA:C_CA + 128], rhs=carry[:, c0 - 768:c0 - 768 + cw], start=False, stop=True)][-1],
                        zkeys + [cst, carry], [pb])
                r0 = c0 - 768
                tt("dve", zr[:, r0:r0 + cw], pb[:, 0:cw], z[:, c0:c0 + cw], ALU.subtract, [pb] + zkeys, [(zr, j)])
                tt("dve", zr[:, r0:r0 + cw], zr[:, r0:r0 + cw], vec[:, V_MU + r0:V_MU + r0 + cw], ALU.mult, [(zr, j), vec], [(zr, j)])
                tt("pool", zr[:, r0:r0 + cw], zr[:, r0:r0 + cw], z[:, c0:c0 + cw], ALU.add, [(zr, j)] + zkeys, [(zr, j)])
            cp("pool", carry[96:128, :], z[96:128, 768:NZ], [z], [carry])
            dma("sp", D["y_scr"][tok0:tok0 + 128, 0:512], yo[:], [yo], [(D["y_t"], (tok0 // 128, 0))])
            dma("sp", D["zr_scr"][tok0:tok0 + 128, :], zr[:], [zr], [(D["zr_t"], tok0 // 128)])

        class S:
            pass
        S.prefetch, S.body, S.z = prefetch, tile_body, z
        return S

    streams = [make_stream(k) for k in range(2)]
    Wb = load_w_bf16(p, h, "Wb1", D["w_in"], 1024, 4640, 0, NZ, (lambda: streams[0].z), nwp, 0)
    zip_streams(p, streams, n_seq, n_tiles)


def phase_a1b(p, nc, D, n_seq, n_tiles, S_TOK):
    h = helpers(p)
    tt, stt, ts, act, cp, red, dma, mms = h.tt, h.stt, h.ts, h.act, h.cp, h.red, h.dma, h.mms
    cst = p.sb("cst", [128, NCST], F32)
    dma("sp", cst[:], D["cst"], [], [cst])
    VOFF = V_W0
    vec = p.sb("vecr", [128, V_SINK - VOFF], F32)
    dma("sp", vec[:], D["vecs"][VOFF:V_SINK].partition_broadcast(128), [], [vec])
    ident = cst[:, C_ID:C_ID + 128]
    w2 = p.sb("w2", [128, 512], F32)
    dma("sp", w2[0:64, :], D["decay_w2"], [], [w2])
    dma("sp", w2[64:128, :], D["iclr_a2"], [], [w2])
    g2 = p.sb("g2", [128, 2, 512], F32)
    dma("sp", g2[:, 0, :], D["gate_g2"][0:128, :], [], [g2])
    dma("sp", g2[0:32, 1, :], D["gate_g2"][128:160, :], [], [g2])

    def vb(off, n=512):
        return vec[:, off - VOFF:off - VOFF + n]

    def make_stream(k):
        sf = "_r%d" % k
        ZR = Rot([p.sb("zr%d" % i + sf, [128, 1824], F32) for i in range(NZRB)])
        lin = p.sb("lin" + sf, [128, 288], F32)
        linT = p.sb("linT" + sf, [128, 3, 128], F32)
        T5 = Rot([p.sb("t5_%d" % i + sf, [128, 512], F32) for i in range(10)])
        gv = p.sb("gv" + sf, [128, 512], F32)
        k2 = p.sb("k2" + sf, [128, 512], F32)
        M8 = Rot([p.sb("m8_%d" % i + sf, [128, 8, 128], F32) for i in range(6)])
        M8d = [p.sb("m8d_%d" % i + sf, [128, 8, 128], F32) for i in range(3)]
        Tq = Rot([p.sb("tq_%d" % i + sf, [128, 4, 128], F32) for i in range(4)])
        sm = Rot([p.sb("sm_%d" % i + sf, [128, 8], F32) for i in range(8)])
        PB = Rot([p.ps("pb%d" % i + sf, [128, 512], F32) for i in range(8 // NSB)])
        ST = p.sb("ST" + sf, [128, 4, 128], F32)
        gC = p.sb("gC" + sf, [128, 4], F32)
        yout = Rot([p.sb("yout%d" % i + sf, [128, 512], F32) for i in range(1)])
        zq = {}

        def prefetch(b, n):
            if n < n_tiles:
                t0 = b * S_TOK + n * 128
                zq[(b, n)] = ZR()
                dma("sp", zq[(b, n)][:], D["zr_scr"][t0:t0 + 128, :], [(D["zr_t"], t0 // 128)], [zq[(b, n)]])

        def tile_body(b, n):
            if n == 0:
                p.op("pool", lambda e: e.memset(ST[:], 0.0), [], [ST])
            tok0 = b * S_TOK + n * 128
            zr = zq.pop((b, n))
            prefetch(b, n + 1)
            yo = yout()
            r_, k_, v_ = zr[:, 0:512], zr[:, 512:1024], zr[:, 1024:1536]
            act(lin[:, 0:64], zr[:, 1536:1600], AF.Tanh, [zr], [(lin, 0)])
            cp("pool", lin[:, 64:128], zr[:, 1600:1664], [zr], [(lin, 1)])
            act(lin[:, 128:288], zr[:, 1664:1824], AF.Sigmoid, [zr], [(lin, 2)])
            pb = PB()
            mms(lambda e, pb=pb: [e.transpose(out=pb[:, 0:128], in_=lin[:, 0:128], identity=ident),
                                  e.transpose(out=pb[:, 128:256], in_=lin[:, 128:256], identity=ident),
                                  e.transpose(out=pb[0:32, 256:384], in_=lin[:, 256:288], identity=ident)][-1], [lin, cst], [pb])
            cp("dve", linT[:, 0:2, :], pb[:, 0:256].rearrange("p (a b) -> p a b", a=2), [pb], [(linT, 0)])
            cp("dve", linT[0:32, 2, :], pb[0:32, 256:384], [pb], [(linT, 1)])
            pw, pa_, pg = PB(), PB(), PB()
            mms(lambda e, pw=pw: e.matmul(pw[:], lhsT=linT[0:64, 0, :], rhs=w2[0:64, :], start=True, stop=True), [linT, w2], [pw])
            mms(lambda e, pa_=pa_: e.matmul(pa_[:], lhsT=linT[64:128, 0, :], rhs=w2[64:128, :], start=True, stop=True), [linT, w2], [pa_])
            mms(lambda e, pg=pg: [e.matmul(pg[:], lhsT=linT[:, 1, :], rhs=g2[:, 0, :], start=True, stop=False),
                                  e.matmul(pg[:], lhsT=linT[0:32, 2, :], rhs=g2[0:32, 1, :], start=False, stop=True)][-1], [linT, g2], [pg])
            sg, av = T5(), T5()
            tt("dve", sg[:], pw[:], vb(V_W0), ALU.add, [pw, vec], [sg])
            act(sg[:], sg[:], AF.Sigmoid, [sg], [sg])
            tt("dve", av[:], pa_[:], vb(V_A0), ALU.add, [pa_, vec], [av])
            act(av[:], av[:], AF.Sigmoid, [av], [av])
            cp("act", gv[:], pg[:], [pg], [gv])
            pc = PB()
            mms(lambda e, pc=pc, sg=sg: e.matmul(pc[:], lhsT=cst[:, C_IU:C_IU + 128], rhs=sg[:], start=True, stop=True), [cst, sg], [pc])
            gam, igam, gprev = T5(), T5(), T5()
            act(gam[:], pc[:], AF.Exp, [pc], [gam], scale=-C_DEC)
            act(igam[:], pc[:], AF.Exp, [pc], [igam], scale=C_DEC)
            tt("dve", gprev[:], pc[:], sg[:], ALU.subtract, [pc, sg], [gprev])
            act(gprev[:], gprev[:], AF.Exp, [gprev], [gprev], scale=-C_DEC)
            pgc = PB()
            mms(lambda e, pgc=pgc, sg=sg: [e.matmul(pgc[:, m:m + 1], lhsT=sg[:, m * 128:(m + 1) * 128], rhs=cst[:, C_ONE:C_ONE + 1], start=True, stop=True) for m in range(4)][-1],
                [cst, sg], [pgc])
            act(gC[:], pgc[:, 0:4], AF.Exp, [pgc], [gC], scale=-C_DEC)
            kk, sq = T5(), T5()
            tt("dve", kk[:], k_, vb(V_KK), ALU.mult, [zr, vec], [kk])
            tt("pool", sq[:], kk[:], kk[:], ALU.mult, [kk], [sq])
            s8 = sm()
            red(s8[:], sq[:].rearrange("p (h d) -> p h d", h=8), [sq], [s8])
            act(s8[:], s8[:], AF.Sqrt, [s8], [s8], bias=1e-24, scale=1.0)
            p.op("dve", lambda e, s8=s8: e.reciprocal(out=s8[:], in_=s8[:]), [s8], [s8])
            tt("dve", kk[:].rearrange("p (h d) -> p h d", h=8), kk[:].rearrange("p (h d) -> p h d", h=8),
               s8[:].unsqueeze(2).to_broadcast([128, 8, 64]), ALU.mult, [kk, s8], [kk])
            t1 = T5()
            stt("dve", t1[:], av[:], -1.0, vb(V_KA), ALU.add, ALU.mult, [av, vec], [t1])
            stt("dve", k2[:], t1[:], 1.0, k_, ALU.add, ALU.mult, [t1, zr], [k2])
            At, Bt, Kt, Rt = T5(), T5(), T5(), T5()
            stt("dve", At[:], kk[:], -1.0, gprev[:], ALU.mult, ALU.mult, [kk, gprev], [At])
            tt("pool", Bt[:], kk[:], av[:], ALU.mult, [kk, av], [Bt])
            tt("dve", Bt[:], Bt[:], igam[:], ALU.mult, [Bt, igam], [Bt])
            tt("dve", Kt[:], k2[:], igam[:], ALU.mult, [k2, igam], [Kt])
            tt("pool", Rt[:], r_, gam[:], ALU.mult, [zr, gam], [Rt])
            XT = {}
            for nm, src in (("A", At), ("B", Bt), ("K", Kt), ("R", Rt)):
                pb = PB()
                mms(lambda e, pb=pb, src=src: [e.transpose(out=pb[:, j * 128:(j + 1) * 128], in_=src[:, j * 128:(j + 1) * 128], identity=ident) for j in range(4)][-1],
                    [src, cst], [pb])
                dst = Tq()
                cp("act" if nm in ("A", "K") else "dve", dst[:], pb[:].rearrange("p (a b) -> p a b", a=4), [pb], [dst])
                XT[nm] = dst
            AT, BT, KT, RT = XT["A"], XT["B"], XT["K"], XT["R"]

            def pairmat(l, r_op, mask_off, eng2, dst=None):
                dst = dst or M8()
                for par in range(2):
                    pb = PB()
                    mms(lambda e, pb=pb, par=par: [e.matmul(pb[:, j * 128:(j + 1) * 128],
                                                          lhsT=l[64 * par:64 * par + 64, j, :],
                                                          rhs=r_op[64 * par:64 * par + 64, j, :],
                                                          start=True, stop=True) for j in range(4)][-1], [l, r_op], [pb])
                    tt(eng2[par], dst[:, par:8:2, :], pb[:].rearrange("p (a b) -> p a b", a=4),
                       cst[:, mask_off:mask_off + 128].unsqueeze(1).to_broadcast([128, 4, 128]), ALU.mult, [pb, cst], [(dst, par)])
                return dst

            Nm = pairmat(BT, AT, C_SU, ("dve", "dve"))
            Am = pairmat(AT, BT, C_SL, ("dve", "dve"))
            AkT = pairmat(KT, AT, C_SU, ("dve", "dve"), M8d[0])
            RbT = pairmat(BT, RT, C_IU, ("dve", "dve"), M8d[1])
            RkT = pairmat(KT, RT, C_IU, ("dve", "dve"), M8d[2])
            X = M8()
            tt("dve", X[:], Nm[:], ident.unsqueeze(1).to_broadcast([128, 8, 128]), ALU.add, [Nm, cst], [X])
            for j in range(0 if "d" in SKIP else 6):
                Nn, An, Xn = M8(), M8(), M8()
                last = (j == 5)
                for hf in range(2):
                    pbn, pba = PB(), PB()
                    if not last:
                        mms(lambda e, pbn=pbn, hf=hf, Am=Am, Nm=Nm: [e.matmul(pbn[:, q * 128:(q + 1) * 128], lhsT=Am[:, hf * 4 + q, :], rhs=Nm[:, hf * 4 + q, :], start=True, stop=True) for q in range(4)][-1],
                            [Am, Nm], [pbn])
                        cp("act", Nn[:, hf * 4:hf * 4 + 4, :], pbn[:].rearrange("p (a b) -> p a b", a=4), [pbn], [(Nn, hf)])
                    mms(lambda e, pba=pba, hf=hf, Am=Am, Nm=Nm: [e.matmul(pba[:, q * 128:(q + 1) * 128], lhsT=Nm[:, hf * 4 + q, :], rhs=Am[:, hf * 4 + q, :], start=True, stop=True) for q in range(4)][-1],
                        [Am, Nm], [pba])
                    cp("dve", An[:, hf * 4:hf * 4 + 4, :], pba[:].rearrange("p (a b) -> p a b", a=4), [pba], [(An, hf)])
                for hf in range(2):
                    pbx = PB()
                    mms(lambda e, pbx=pbx, hf=hf, An=An, X=X: [e.matmul(pbx[:, q * 128:(q + 1) * 128], lhsT=An[:, hf * 4 + q, :], rhs=X[:, hf * 4 + q, :], start=True, stop=True) for q in range(4)][-1],
                        [An, X], [pbx])
                    tt("dve", Xn[:, hf * 4:hf * 4 + 4, :], pbx[:].rearrange("p (a b) -> p a b", a=4), X[:, hf * 4:hf * 4 + 4, :], ALU.add, [pbx, X], [(Xn, hf)])
                Nm, Am, X = Nn, An, Xn
            pr = PB()

            def f_rhs0(e, pr=pr, AT=AT, AkT=AkT):
                ins = None
                for m in range(4):
                    e.matmul(pr[:, m * 128:(m + 1) * 128], lhsT=AT[:, m, :], rhs=ST[:, m, :], start=True, stop=False)
                    for q in range(2):
                        hd = 2 * m + q
                        ins = e.matmul(pr[:, hd * 64:(hd + 1) * 64], lhsT=AkT[:, hd, :], rhs=zr[:, 1024 + hd * 64:1024 + (hd + 1) * 64], start=False, stop=(q == 1))
                return ins
            mms(f_rhs0, [AT, ST, AkT, zr], [pr])
            rhs0 = T5()
            cp("act", rhs0[:], pr[:], [pr], [rhs0])
            pu = PB()
            mms(lambda e, pu=pu, X=X, rhs0=rhs0: [e.matmul(pu[:, hd * 64:(hd + 1) * 64], lhsT=X[:, hd, :], rhs=rhs0[:, hd * 64:(hd + 1) * 64], start=True, stop=True) for hd in range(8)][-1],
                [X, rhs0], [pu])
            U = T5()
            cp("dve", U[:], pu[:], [pu], [U])
            py = PB()

            def f_y(e, py=py, RT=RT, RbT=RbT, RkT=RkT, U=U):
                ins = None
                for m in range(4):
                    e.matmul(py[:, m * 128:(m + 1) * 128], lhsT=RT[:, m, :], rhs=ST[:, m, :], start=True, stop=False)
                    for q in range(2):
                        hd = 2 * m + q
                        e.matmul(py[:, hd * 64:(hd + 1) * 64], lhsT=RbT[:, hd, :], rhs=U[:, hd * 64:(hd + 1) * 64], start=False, stop=False)
                        ins = e.matmul(py[:, hd * 64:(hd + 1) * 64], lhsT=RkT[:, hd, :], rhs=zr[:, 1024 + hd * 64:1024 + (hd + 1) * 64], start=False, stop=(q == 1))
                return ins
            mms(f_y, [RT, ST, RbT, RkT, U, zr], [py])
            yv = T5()
            cp("act", yv[:], py[:], [py], [yv])
            pst = PB()

            def f_s(e, pst=pst, Bt=Bt, Kt=Kt, U=U):
                ins = None
                for m in range(4):
                    e.matmul(pst[:, m * 128:(m + 1) * 128], lhsT=Bt[:, m * 128:(m + 1) * 128], rhs=U[:, m * 128:(m + 1) * 128], start=True, stop=False)
                    ins = e.matmul(pst[:, m * 128:(m + 1) * 128], lhsT=Kt[:, m * 128:(m + 1) * 128], rhs=zr[:, 1024 + m * 128:1024 + (m + 1) * 128], start=False, stop=True)
                return ins
            mms(f_s, [Bt, Kt, U, zr], [pst])
            tt("dve", ST[:], pst[:].rearrange("p (a b) -> p a b", a=4), ST[:], ALU.add, [pst, ST], [ST])
            tt("dve", ST[:], ST[:], gC[:].unsqueeze(2).to_broadcast([128, 4, 128]), ALU.mult, [ST, gC], [ST])
            tt("dve", ST[:], ST[:], cst[:, C_BD:C_BD + 128].unsqueeze(1).to_broadcast([128, 4, 128]), ALU.mult, [ST, cst], [ST])
            y3 = yv[:].rearrange("p (h d) -> p h d", h=8)
            ysq = T5()
            tt("pool", ysq[:], yv[:], yv[:], ALU.mult, [yv], [ysq])
            s1, s2, mean, var = sm(), sm(), sm(), sm()
            red(s1[:], y3, [yv], [s1])
            red(s2[:], ysq[:].rearrange("p (h d) -> p h d", h=8), [ysq], [s2])
            ts("dve", mean[:], s1[:], 1.0 / 64, None, ALU.mult, None, [s1], [mean])
            tt("dve", var[:], mean[:], mean[:], ALU.mult, [mean], [var])
            stt("dve", var[:], s2[:], 1.0 / 64, var[:], ALU.mult, ALU.subtract, [s2, var], [var])
            act(var[:], var[:], AF.Sqrt, [var], [var], bias=64e-5, scale=1.0)
            p.op("dve", lambda e, var=var: e.reciprocal(out=var[:], in_=var[:]), [var], [var])
            yn = T5()
            yn3 = yn[:].rearrange("p (h d) -> p h d", h=8)
            tt("dve", yn3, y3, mean[:].unsqueeze(2).to_broadcast([128, 8, 64]), ALU.subtract, [yv, mean], [yn])
            tt("dve", yn3, yn3, var[:].unsqueeze(2).to_broadcast([128, 8, 64]), ALU.mult, [yn, var], [yn])
            tt("pool", yn[:], yn[:], vb(V_LNW), ALU.mult, [yn, vec], [yn])
            tt("pool", yn[:], yn[:], vb(V_LNB), ALU.add, [yn, vec], [yn])
            rk = T5()
            tt("pool", rk[:], r_, k2[:], ALU.mult, [zr, k2], [rk])
            tt("pool", rk[:], rk[:], vb(V_RK), ALU.mult, [rk, vec], [rk])
            sb_ = sm()
            red(sb_[:], rk[:].rearrange("p (h d) -> p h d", h=8), [rk], [sb_])
            tt("dve", rk[:].rearrange("p (h d) -> p h d", h=8), v_.rearrange("p (h d) -> p h d", h=8),
               sb_[:].unsqueeze(2).to_broadcast([128, 8, 64]), ALU.mult, [zr, sb_], [rk])
            tt("pool", yn[:], yn[:], rk[:], ALU.add, [yn, rk], [yn])
            tt("pool", yo[:, 0:512], yn[:], gv[:], ALU.mult, [yn, gv], [(yo, 2)])
            dma("sp", D["y_scr"][tok0:tok0 + 128, 512:1024], yo[:], [yo], [(D["y_t"], (tok0 // 128, 1))])

        class S:
            pass
        S.prefetch, S.body = prefetch, tile_body
        return S

    streams = [make_stream(k) for k in range(NSB)]
    zip_streams(p, streams, n_seq, n_tiles)


def zip_streams(p, streams, n_seq, n_tiles):
    NS = len(streams)
    for b0 in range(0, n_seq, NS):
        seqs = list(range(b0, min(b0 + NS, n_seq)))
        for k, b in enumerate(seqs):
            streams[k].prefetch(b, 0)
        for n in range(n_tiles):
            lists = []
            for k, b in enumerate(seqs):
                p.defer_begin()
                streams[k].body(b, n)
                lists.append(p.defer_end())
            while any(lists):
                for lst in lists:
                    if lst:
                        p.drain(lst, 1)


def phase_a2(p, nc, D, ntok, final=True, drip=None):
    h = helpers(p)
    tt, stt, ts, act, cp, red, dma, mms = h.tt, h.stt, h.ts, h.act, h.cp, h.red, h.dma, h.mms
    cst = p.sb("cstb", [128, 128], F32)
    dma("sp", cst[:], D["cst"][:, C_ID:C_ID + 128], [], [cst])
    ident = cst[:, 0:128]
    nwp = p.sb("nwpb", [128, 16], F32)
    dma("sp", nwp[:], D["nwp"], [], [nwp])
    fin = p.sb("finw", [128, 1024], F32)
    dma("sp", fin[:], D["vecs"][V_FIN:V_FIN + 1024].partition_broadcast(128), [], [fin])
    stg = Rot([p.sb("stgb%d" % i, [128, 2048], F32) for i in range(2)])
    Wg = load_w_bf16(p, h, "Wg", D["w_in"], 1024, 4640, 2592, 2048, stg, nwp, 0)
    PA = load_w_bf16(p, h, "PAw", D["proj_attn"], 512, 1024, 0, 1024, stg)
    PBw = load_w_bf16(p, h, "PBw", D["proj_rwkv"], 512, 1024, 0, 1024, stg)
    WO = load_w_bf16(p, h, "WOw", D["w_out"], 1024, 1024, 0, 1024, stg)
    nt = ntok // 128
    NS = 2

    class St:
        pass

    streams = []
    for k in range(NS):
        S = St()
        S.xt = Rot([p.sb("xtb%d_%d" % (k, i), [128, 1024], F32) for i in range(2)])
        S.yt = Rot([p.sb("ytb%d_%d" % (k, i), [128, 1024], F32) for i in range(2)])
        S.ss = p.sb("ssb%d" % k, [128, 1], F32)
        S.rstd = p.sb("rstdb%d" % k, [128, 1], F32)
        S.xs = p.sb("xsb%d" % k, [128, 1024], F32)
        S.xsT = p.sb("xsTb%d" % k, [128, 8, 128], BF16)
        S.yT = p.sb("yTb%d" % k, [128, 8, 128], BF16)
        S.sgt = p.sb("sgt%d" % k, [128, 2048], BF16)
        S.mg = p.sb("mg%d" % k, [128, 1024], F32)
        S.m2 = p.sb("m2%d" % k, [128, 1024], F32)
        S.mgT = p.sb("mgT%d" % k, [128, 8, 128], BF16)
        S.h1 = Rot([p.sb("h1b%d_%d" % (k, i), [128, 1024], F32) for i in range(2)])
        S.PB = Rot([p.ps("pq%d_%d" % (k, i), [128, 512], F32) for i in range(4)])
        S.xq, S.yq = {}, {}
        streams.append(S)

    def prefetch(S, i):
        if i < nt:
            S.xq[i] = S.xt()
            dma("sp", S.xq[i][:], D["x"][i * 128:(i + 1) * 128, :], [], [S.xq[i]])
            S.yq[i] = S.yt()
            dma("sp", S.yq[i][:], D["y_scr"][i * 128:(i + 1) * 128, :], [(D["y_t"], (i, 0)), (D["y_t"], (i, 1))], [S.yq[i]])

    def tp8(S, src, dst):
        for hh in range(2):
            pb = S.PB()
            mms(lambda e, pb=pb, hh=hh: [e.transpose(out=pb[:, j * 128:(j + 1) * 128], in_=src[:, (hh * 4 + j) * 128:(hh * 4 + j + 1) * 128], identity=ident) for j in range(4)][-1],
                [src, cst], [pb])
            cp("act" if hh else "dve", dst[:, hh * 4:hh * 4 + 4, :], pb[:].rearrange("p (a b) -> p a b", a=4), [pb], [(dst, hh)])

    def body(S, i):
        ss, rstd, xs, xsT, yT, sgt, mg, m2, mgT = S.ss, S.rstd, S.xs, S.xsT, S.yT, S.sgt, S.mg, S.m2, S.mgT
        x_t, y_t = S.xq.pop(i), S.yq.pop(i)
        prefetch(S, i + NS)
        act(xs[:], x_t[:], AF.Square, [x_t], [xs, ss], accum_out=ss[:])
        act(rstd[:], ss[:], AF.Sqrt, [ss], [rstd], bias=1e-5, scale=1.0 / 1024)
        p.op("dve", lambda e: e.reciprocal(out=rstd[:], in_=rstd[:]), [rstd], [rstd])
        ts("dve", xs[:], x_t[:], rstd[:, 0:1], None, ALU.mult, None, [x_t, rstd], [xs])
        tp8(S, xs, xsT)
        for cc in range(4):
            pb = S.PB()
            mms(lambda e, pb=pb, cc=cc: [e.matmul(pb[:], lhsT=xsT[:, c, :], rhs=Wg[:, c, cc * 512:(cc + 1) * 512], start=(c == 0), stop=(c == 7)) for c in range(8)][-1],
                [xsT, Wg], [pb])
            act(sgt[:, cc * 512:(cc + 1) * 512], pb[:], AF.Sigmoid, [pb], [(sgt, cc)])
        tp8(S, y_t, yT)
        for br, (W, dstt) in enumerate(((PA, mg), (PBw, m2))):
            for hf in range(2):
                pb = S.PB()
                mms(lambda e, pb=pb, br=br, hf=hf, W=W: [e.matmul(pb[:], lhsT=yT[:, br * 4 + c, :], rhs=W[:, c, hf * 512:(hf + 1) * 512], start=(c == 0), stop=(c == 3)) for c in range(4)][-1],
                    [yT, W], [pb])
                tt("dve", dstt[:, hf * 512:(hf + 1) * 512], pb[:], sgt[:, br * 1024 + hf * 512:br * 1024 + (hf + 1) * 512], ALU.mult, [pb, sgt], [(dstt, hf)])
        tt("pool", mg[:], mg[:], m2[:], ALU.add, [mg, m2], [mg])
        tp8(S, mg, mgT)
        ho = S.h1()
        for hf in range(2):
            pb = S.PB()
            mms(lambda e, pb=pb, hf=hf: [e.matmul(pb[:], lhsT=mgT[:, c, :], rhs=WO[:, c, hf * 512:(hf + 1) * 512], start=(c == 0), stop=(c == 7)) for c in range(8)][-1],
                [mgT, WO], [pb])
            tt("dve", ho[:, hf * 512:(hf + 1) * 512], pb[:], x_t[:, hf * 512:(hf + 1) * 512], ALU.add, [pb, x_t], [(ho, hf)])
        if final:
            act(xs[:], ho[:], AF.Square, [ho], [xs, ss], accum_out=ss[:])
            act(rstd[:], ss[:], AF.Sqrt, [ss], [rstd], bias=1e-5, scale=1.0 / 1024)
            p.op("dve", lambda e: e.reciprocal(out=rstd[:], in_=rstd[:]), [rstd], [rstd])
            stt("dve", ho[:], ho[:], rstd[:, 0:1], fin[:], ALU.mult, ALU.mult, [ho, rstd, fin], [ho])
            dma("sp", D["out"][i * 128:(i + 1) * 128, :], ho[:], [ho], [])
        else:
            dma("sp", D["h1_scr"][i * 128:(i + 1) * 128, :], ho[:], [ho], [(D["h1_t"], i)])

    for k in range(NS):
        prefetch(streams[k], k)
    per = (len(drip) + max(nt // NS - 1, 1) - 1) // max(nt // NS - 1, 1) if drip else 0
    for i0 in range(0, nt, NS):
        lists = []
        for k in range(NS):
            if i0 + k < nt:
                p.defer_begin()
                body(streams[k], i0 + k)
                lists.append(p.defer_end())
        while any(lists):
            for lst in lists:
                if lst:
                    p.drain(lst, 1)
        if drip:
            p.drain(drip, per)
    if drip:
        p.drain(drip, len(drip))


def phase_b0(p, nc, D, eng_rot=("dve", "pool"), nbuf=2, ldq="sp", stq="act"):
    h = helpers(p)
    dma, cp = h.dma, h.cp
    stg = Rot([p.sb("cs%d" % i, [128, 4096], F32) for i in range(nbuf)])
    ob = Rot([p.sb("co%d" % i, [128, 4096], BF16) for i in range(nbuf)])
    nwp0 = p.sb("nwp0", [128, 16], F32)
    dma("sp", nwp0[:], D["nwp"], [], [nwp0])
    k = 0
    for g in range(32):
        s, o = stg(), ob()
        dma(ldq, s[:].rearrange("p (dc e) -> p dc e", dc=8), D["uT"][:, g * 512:(g + 1) * 512].rearrange("(dc p) e -> p dc e", p=128), [], [s])
        h.tt(eng_rot[k % 2], o[:].rearrange("p (i dc e) -> p dc i e", i=4, dc=8), s[:].rearrange("p (dc i e) -> p dc i e", dc=8, i=4),
             nwp0[:, 8:16].unsqueeze(2).unsqueeze(3).to_broadcast([128, 8, 4, 128]), ALU.mult, [s, nwp0], [o])
        k += 1
        dma(stq, D["u2"][:, g * 4:(g + 1) * 4, :, :].rearrange("p i dc e -> p (i dc e)"), o[:], [o], [(D["u2_t"], g)])
        s, o = stg(), ob()
        dma(ldq, s[:].rearrange("p (i d) -> p i d", i=4), D["v"][g * 512:(g + 1) * 512, :].rearrange("(i p) d -> p i d", p=128), [], [s])
        cp(eng_rot[k % 2], o[:], s[:], [s], [o])
        k += 1
        dma(stq, D["vb"][g * 512:(g + 1) * 512, :].rearrange("(i p) d -> p i d", p=128), o[:].rearrange("p (i d) -> p i d", i=4), [o], [(D["vb_t"], g)])


def phase_b(p, nc, D, ntok):
    h = helpers(p)
    tt, stt, ts, act, cp, red, dma, mms = h.tt, h.stt, h.ts, h.act, h.cp, h.red, h.dma, h.mms
    TT = 256
    cst = p.sb("cstc", [128, 256], F32)
    dma("sp", cst[:, 0:128], D["cst"][:, C_ID:C_ID + 128], [], [cst])
    dma("sp", cst[:, 128:256], D["cst"][:, C_IOTA:C_IOTA + 128], [], [cst])
    ident = cst[:, 0:128]
    iota = cst[:, 128:256]
    iota_bf = p.sb("iota_bf", [128, 128], BF16)
    cp("dve", iota_bf[:], iota, [cst], [iota_bf])
    fin = p.sb("finc", [128, 1024], F32)
    dma("sp", fin[:], D["vecs"][V_FIN:V_FIN + 1024].partition_broadcast(128), [], [fin])
    nwpb = p.sb("nwpc", [128, 16], F32)
    dma("sp", nwpb[:], D["nwp"], [], [nwpb])
    skT = p.sb("skT", [128, 8, 128], F32)
    dma("sp", skT[:], D["skT"], [], [skT])
    G = p.sb("G", [128, TT, 128], BF16)
    xs = p.sb("xsc", [128, 1024], F32)
    Wq = load_w_bf16(p, h, "Wq", D["peer_wq"], 1024, 1024, 0, 1024, (lambda: xs), nwpb, 8)
    U2 = Rot([p.sb("u2t%d" % i, [128, 4, 8, 128], BF16) for i in range(3)])
    Vt = Rot([p.sb("vt%d" % i, [128, 4, 1024], BF16) for i in range(3)])
    h1 = [[p.sb("h1c%d_%d" % (b, i), [128, 1024], F32) for i in range(2)] for b in range(2)]
    ss = p.sb("ssc", [128, 1], F32)
    rstd = p.sb("rstdc", [128, 1], F32)
    xTb = [p.sb("xs2T%d" % b, [128, 8, TT], BF16) for b in range(2)]
    qT = p.sb("qTc", [128, 8, TT], F32)
    sc = p.sb("sc", [128, 16, 128], F32)
    v16 = p.sb("v16", [128, 16, 16], F32)
    i16u = p.sb("i16u", [128, 16, 16], U32)
    i16f = p.sb("i16f", [128, 16, 16], F32)
    cand = p.sb("cand", [128, 8, 256], F32)
    tv = p.sb("tv", [128, 8, 16], F32)
    posu = p.sb("posu", [128, 8, 16], U32)
    au = p.sb("au", [128, 8, 16], U32)
    bu = p.sb("bu", [128, 8, 16], U32)
    af = p.sb("af", [128, 8, 16], F32)
    bf_ = p.sb("bf", [128, 8, 16], F32)
    sel = p.sb("sel", [128, 3, 128], F32)
    sm = Rot([p.sb("smc%d" % i, [128, 8], F32) for i in range(4)])
    selTb = [p.sb("selT%d" % b, [128, 3, TT], F32) for b in range(2)]
    OA = Rot([p.sb("oa%d" % i, [128, 16, 128], BF16) for i in range(2)])
    OB = Rot([p.sb("ob%d" % i, [128, 16, 128], BF16) for i in range(2)])
    gh = Rot([p.sb("gh%d" % i, [128, TT], BF16) for i in range(2)])
    ac = Rot([p.sb("ac%d" % i, [128, TT], BF16) for i in range(3)])
    pbs = [p.ps("pr%d" % i, [128, 512], F32) for i in range(4)]
    PBH = Rot(pbs[0:3])
    PBP = Rot(pbs[3:4])
    PBG = Rot(pbs)
    ACC = [p.ps("acc%d" % i, [128, 512], F32) for i in range(4)]
    ntile = ntok // TT

    def prep(tix):
        b = tix % 2
        t0 = tix * TT
        xT, selT = xTb[b], selTb[b]
        PB = PBP
        for s in range(2):
            hh1 = h1[b][s]
            dma("sp", hh1[:], D["h1_scr"][t0 + s * 128:t0 + (s + 1) * 128, :], [(D["h1_t"], tix * 2 + s)], [hh1])
            act(xs[:], hh1[:], AF.Square, [hh1], [xs, ss], accum_out=ss[:])
            act(rstd[:], ss[:], AF.Sqrt, [ss], [rstd], bias=1e-5, scale=1.0 / 1024)
            p.op("dve", lambda e: e.reciprocal(out=rstd[:], in_=rstd[:]), [rstd], [rstd])
            ts("dve", xs[:], hh1[:], rstd[:, 0:1], None, ALU.mult, None, [hh1, rstd], [xs])
            for hh in range(2):
                pb = PB()
                mms(lambda e, pb=pb, hh=hh: [e.transpose(out=pb[:, j * 128:(j + 1) * 128], in_=xs[:, (hh * 4 + j) * 128:(hh * 4 + j + 1) * 128], identity=ident) for j in range(4)][-1],
                    [xs, cst], [pb])
                cp("act" if hh else "dve", xT[:, hh * 4:hh * 4 + 4, s * 128:(s + 1) * 128], pb[:].rearrange("p (a b) -> p a b", a=4), [pb], [(xT, (s, hh))])
        for c in range(8):
            pb = PB()
            mms(lambda e, pb=pb, c=c, xT=xT: [e.matmul(pb[:, 0:TT], lhsT=Wq[:, dc, c * 128:(c + 1) * 128], rhs=xT[:, dc, :], start=(dc == 0), stop=(dc == 7)) for dc in range(8)][-1],
                [Wq, xT], [pb])
            cp("act" if c % 2 else "dve", qT[:, c, :], pb[:, 0:TT], [pb], [(qT, c)])
        for s in range(0 if "S" in SKIP else 2):
            for par in range(2):
                for half in range(2):
                    pb = PB()
                    mms(lambda e, pb=pb, par=par, half=half, s=s: [e.matmul(pb[:, j * 128:(j + 1) * 128], lhsT=qT[64 * par:64 * par + 64, half * 4 + j, s * 128:(s + 1) * 128],
                                                                        rhs=skT[64 * par:64 * par + 64, half * 4 + j, :], start=True, stop=True) for j in range(4)][-1],
                        [qT, skT], [pb])
                    cp("act", sc[:, 8 * half + par:8 * half + 8:2, :], pb[:].rearrange("p (a b) -> p a b", a=4), [pb], [(sc, 8 * half + par + 2 * j) for j in range(4)])
            tmpA = cand[:].rearrange("p h (a b) -> p (h a) b", a=2)
            for hp in range(16):
                p.op("dve", lambda e, hp=hp: e.max(out=v16[:, hp, 0:8], in_=sc[:, hp, :]), [(sc, hp)], [(v16, hp)])
            for hp in range(16):
                p.op("dve", lambda e, hp=hp, tmpA=tmpA: e.match_replace(out=tmpA[:, hp, :], in_to_replace=v16[:, hp, 0:8], in_values=sc[:, hp, :], imm_value=-1e30), [(sc, hp), (v16, hp)], [(cand, hp)])
            for hp in range(16):
                p.op("dve", lambda e, hp=hp, tmpA=tmpA: e.max(out=v16[:, hp, 8:16], in_=tmpA[:, hp, :]), [(cand, hp)], [(v16, hp)])
            for hp in range(16):
                p.op("dve", lambda e, hp=hp: e.max_index(out=i16u[:, hp, 0:8], in_max=v16[:, hp, 0:8], in_values=sc[:, hp, :]), [(sc, hp), (v16, hp)], [(i16u, hp)])
            for hp in range(16):
                p.op("dve", lambda e, hp=hp: e.max_index(out=i16u[:, hp, 8:16], in_max=v16[:, hp, 8:16], in_values=sc[:, hp, :]), [(sc, hp), (v16, hp)], [(i16u, hp)])
            cp("pool", i16f[:], i16u[:], [i16u], [i16f])
            tt("pool", cand[:].rearrange("p h (a b) -> p h a b", a=16), v16[:, 0:16:2, :].unsqueeze(3).to_broadcast([128, 8, 16, 16]),
               v16[:, 1:16:2, :].unsqueeze(2).to_broadcast([128, 8, 16, 16]), ALU.add, [v16], [cand])
            tmpB = sc[:].rearrange("p (h a) b -> p h (a b)", a=2)
            for hd in range(8):
                p.op("dve", lambda e, hd=hd: e.max(out=tv[:, hd, 0:8], in_=cand[:, hd, :]), [cand], [(tv, hd)])
            for hd in range(8):
                p.op("dve", lambda e, hd=hd, tmpB=tmpB: e.match_replace(out=tmpB[:, hd, :], in_to_replace=tv[:, hd, 0:8], in_values=cand[:, hd, :], imm_value=-1e30), [cand, (tv, hd)], [(sc, 2 * hd), (sc, 2 * hd + 1)])
            for hd in range(8):
                p.op("dve", lambda e, hd=hd, tmpB=tmpB: e.max(out=tv[:, hd, 8:16], in_=tmpB[:, hd, :]), [(sc, 2 * hd), (sc, 2 * hd + 1)], [(tv, hd)])
            for hd in range(8):
                p.op("dve", lambda e, hd=hd: e.max_index(out=posu[:, hd, 0:8], in_max=tv[:, hd, 0:8], in_values=cand[:, hd, :]), [cand, (tv, hd)], [(posu, hd)])
            for hd in range(8):
                p.op("dve", lambda e, hd=hd: e.max_index(out=posu[:, hd, 8:16], in_max=tv[:, hd, 8:16], in_values=cand[:, hd, :]), [cand, (tv, hd)], [(posu, hd)])
            gt = sel[:, 2, :].rearrange("p (h k) -> p h k", h=8)
            tt("pool", gt, tv[:], tv[:, :, 0:1].to_broadcast([128, 8, 16]), ALU.subtract, [tv], [(sel, 2)])
            act(gt, gt, AF.Exp, [(sel, 2)], [(sel, 2)])
            z8 = sm()
            red(z8[:], gt, [(sel, 2)], [z8])
            p.op("dve", lambda e, z8=z8: e.reciprocal(out=z8[:], in_=z8[:]), [z8], [z8])
            tt("dve", gt, gt, z8[:].unsqueeze(2).to_broadcast([128, 8, 16]), ALU.mult, [(sel, 2), z8], [(sel, 2)])
            ts("dve", au[:], posu[:], 4, None, ALU.logical_shift_right, None, [posu], [au])
            ts("dve", bu[:], posu[:], 15, None, ALU.bitwise_and, None, [posu], [bu])
            cp("pool", af[:], au[:], [au], [af])
            cp("pool", bf_[:], bu[:], [bu], [bf_])
            io16 = iota[:, 0:16].unsqueeze(1).unsqueeze(1).to_broadcast([128, 8, 16, 16])
            eq = cand[:].rearrange("p h (a b) -> p h a b", a=16)
            for w, (xf, par) in enumerate(((af, 0), (bf_, 1))):
                tt("dve", eq, io16, xf[:].unsqueeze(3).to_broadcast([128, 8, 16, 16]), ALU.is_equal, [cst, xf], [cand])
                tt("pool", eq, eq, i16f[:, par:16:2, :].unsqueeze(2).to_broadcast([128, 8, 16, 16]), ALU.mult, [cand, i16f], [cand])
                red(sel[:, w, :].rearrange("p (h k) -> p h k", h=8), eq, [cand], [(sel, w)])
            pb = PB()
            mms(lambda e, pb=pb: [e.transpose(out=pb[:, w * 128:(w + 1) * 128], in_=sel[:, w, :], identity=ident) for w in range(3)][-1], [sel, cst], [pb])
            cp("act", selT[:, :, s * 128:(s + 1) * 128], pb[:, 0:384].rearrange("p (a b) -> p a b", a=3), [pb], [(selT, s)])

    def gbuild(tix):
        selT = selTb[tix % 2]
        PB = PBG
        NG = 0 if "G" in SKIP else TT // 16
        bufs = {}

        def onehots(g):
            tk = g * 16
            oa, ob = OA(), OB()
            bufs[g] = (oa, ob)
            io = iota_bf[:, :].unsqueeze(1).to_broadcast([128, 16, 128])
            if "o" in SKIP:
                return
            tt("dve", oa[:], io, selT[:, 0, tk:tk + 16].unsqueeze(2).to_broadcast([128, 16, 128]), ALU.is_equal, [iota_bf, selT], [oa])
            for t in range(16):
                act(oa[:, t, :], oa[:, t, :], AF.Copy, [(oa, t), selT], [(oa, t)], scale=selT[:, 2, tk + t:tk + t + 1])
            tt("dve", ob[:], io, selT[:, 1, tk:tk + 16].unsqueeze(2).to_broadcast([128, 16, 128]), ALU.is_equal, [iota_bf, selT], [ob])

        def mm_evac(g):
            tk = g * 16
            oa, ob = bufs.pop(g)
            for q4 in range(4):
                pb = PB()
                if "m" not in SKIP:
                  mms(lambda e, pb=pb, q4=q4, oa=oa, ob=ob: [e.matmul(pb[:, j * 128:(j + 1) * 128], lhsT=ob[:, q4 * 4 + j, :], rhs=oa[:, q4 * 4 + j, :], start=True, stop=True) for j in range(4)][-1],
                    [oa, ob], [pb])
                tq = tk + q4 * 4
                if "v" not in SKIP:
                  cp("act" if q4 == 0 else "dve", G[:, tq:tq + 4, :], pb[:].rearrange("p (t i) -> p t i", t=4), [pb], [(G, tq)])

        if NG:
            onehots(0)
        for g in range(NG):
            if g + 1 < NG:
                onehots(g + 1)
            mm_evac(g)

    def expert(tix, nxt):
        xT = xTb[tix % 2]
        LOOK = 2
        grp = {}
        hb = {}

        def emit_H(i):
            ig, ii = divmod(i, 4)
            if ii == 0:
                u2, vt = U2(), Vt()
                grp[ig] = (u2, vt)
                if not ("D" in SKIP and ig >= 2):
                    dma("sp", vt[:], D["vb"][ig * 512:(ig + 1) * 512, :].rearrange("(i p) d -> p i d", p=128), [(D["vb_t"], ig)], [vt])
                    dma("sp", u2[:].rearrange("p i dc e -> p (i dc e)"), D["u2"][:, ig * 4:(ig + 1) * 4, :, :].rearrange("p i dc e -> p (i dc e)"), [(D["u2_t"], ig)], [u2])
            u2, vt = grp[ig]
            pb = PBH()
            mms(lambda e, pb=pb, u2=u2, ii=ii: [e.matmul(pb[:, 0:TT], lhsT=u2[:, ii, dc, :], rhs=xT[:, dc, :], start=(dc == 0), stop=(dc == 7)) for dc in range(8)][-1],
                [u2, xT], [pb])
            hb[i] = pb

        def emit_rest(i):
            ig, ii = divmod(i, 4)
            u2, vt = grp[ig]
            pb = hb.pop(i)
            g_, a_ = gh(), ac()
            if "X" not in SKIP:
                act(g_[:], pb[:, 0:TT], AF.Gelu, [pb], [g_])
                tt("dve", a_[:], g_[:], G[:, :, i], ALU.mult, [g_, G], [a_])
            mms(lambda e, a_=a_, vt=vt, ii=ii, i=i: [e.matmul(ACC[s * 2 + hf][:], lhsT=a_[:, s * 128:(s + 1) * 128], rhs=vt[:, ii, hf * 512:(hf + 1) * 512], start=(i == 0), stop=(i == 127))
                                                   for s in range(2) for hf in range(2)][-1], [a_, vt], ACC)

        NE = 0 if "E" in SKIP else 128
        per = (len(nxt) + 99) // 100 if nxt else 0
        for i in range(min(LOOK, NE)):
            emit_H(i)
        for i in range(NE):
            if i + LOOK < NE:
                emit_H(i + LOOK)
            emit_rest(i)
            if nxt:
                p.drain(nxt, per)
        if nxt:
            p.drain(nxt, len(nxt))

    def epilogue(tix):
        b = tix % 2
        t0 = tix * TT
        for s in range(2):
            hh1 = h1[b][s]
            for hf in range(2):
                tt("dve", hh1[:, hf * 512:(hf + 1) * 512], ACC[s * 2 + hf][:], hh1[:, hf * 512:(hf + 1) * 512], ALU.add, [ACC[s * 2 + hf], hh1], [hh1])
            act(xs[:], hh1[:], AF.Square, [hh1], [xs, ss], accum_out=ss[:])
            act(rstd[:], ss[:], AF.Sqrt, [ss], [rstd], bias=1e-5, scale=1.0 / 1024)
            p.op("dve", lambda e: e.reciprocal(out=rstd[:], in_=rstd[:]), [rstd], [rstd])
            stt("dve", hh1[:], hh1[:], rstd[:, 0:1], fin[:], ALU.mult, ALU.mult, [hh1, rstd, fin], [hh1])
            dma("act", D["out"][t0 + s * 128:t0 + (s + 1) * 128, :], hh1[:], [hh1], [])

    prep(0)
    for tix in range(ntile):
        gbuild(tix)
        nxt = []
        if tix + 1 < ntile:
            p.defer_begin()
            prep(tix + 1)
            nxt = p.defer_end()
        expert(tix, nxt)
        epilogue(tix)


N_CORES = 8


def _build(n_seq, n_tiles, ret_d=False, phases="0123"):
    nc = bass.Bass("TRN2", target_bir_lowering=False, dynamic_dma_scratch_size=2048)
    ntok = n_seq * n_tiles * 128
    D = {}

    def din(name, shape):
        D[name] = nc.dram_tensor(name, list(shape), F32, kind="ExternalInput").ap()

    din("x", (ntok, 1024)); din("w_in", (1024, 4640)); din("vecs", (NVEC,)); din("nwp", (128, 16)); din("cst", (128, NCST))
    din("decay_w2", (64, 512)); din("iclr_a2", (64, 512)); din("gate_g2", (160, 512))
    din("proj_attn", (512, 1024)); din("proj_rwkv", (512, 1024)); din("w_out", (1024, 1024))
    din("peer_wq", (1024, 1024)); din("skT", (128, 8, 128)); din("uT", (1024, 16384)); din("v", (16384, 1024))
    D["y_scr"] = nc.dram_tensor("y_scr", [ntok, 1024], F32, kind="Internal").ap()
    D["h1_scr"] = D["y_scr"]
    NA = 2 * 16384 * 1024
    arena = nc.dram_tensor("arena", [NA], BF16, kind="Internal").ap()
    assert ntok * 1824 * 2 <= NA
    D["zr_scr"] = arena[0:ntok * 1824 * 2].bitcast(F32).rearrange("(t c) -> t c", c=1824)
    D["u2"] = arena[0:16384 * 1024].rearrange("(p i dc e) -> p i dc e", p=128, i=128, dc=8)
    D["vb"] = arena[16384 * 1024:NA].rearrange("(r c) -> r c", c=1024)
    D["out"] = nc.dram_tensor("out", [ntok, 1024], F32, kind="ExternalOutput").ap()
    p = Prog(nc)
    for nm in ("y_t", "zr_t", "h1_t", "u2_t", "vb_t"):
        D[nm] = p.wrap(None, nm)
    m = p.mark()
    if "1" in phases or "a" in phases:
        phase_a1a(p, nc, D, n_seq, n_tiles, n_tiles * 128)
        p.release(m)
    if "1" in phases or "b" in phases:
        phase_a1b(p, nc, D, n_seq, n_tiles, n_tiles * 128)
        p.release(m)
    if "0" in phases:
        phase_b0(p, nc, D, stq="act")
        p.release(m)
    if "2" in phases:
        phase_a2(p, nc, D, ntok, final=("3" not in phases))
        p.release(m)
    if "3" in phases:
        phase_b(p, nc, D, ntok)
    p.emit()
    p.close()
    return (nc, D) if ret_d else nc


def _inputs(x, norm_mix_w, w_in, shift_mu, attn_sinks, decay_w0, decay_w2, iclr_a0, iclr_a2, gate_g2, k_k, k_a, r_k,
            ln_x_w, ln_x_b, proj_attn, proj_rwkv, w_out, norm_ffn_w, peer_wq, peer_subkeys, peer_u, peer_v, norm_final_w):
    f = lambda a: np.ascontiguousarray(np.asarray(a, dtype=np.float32))
    v = np.zeros(NVEC, np.float32)
    v[V_MU:V_MU + 1824] = f(shift_mu)[0]; v[V_W0:V_W0 + 512] = f(decay_w0)[0]; v[V_A0:V_A0 + 512] = f(iclr_a0)[0]
    v[V_KK:V_KK + 512] = f(k_k)[0]; v[V_KA:V_KA + 512] = f(k_a)[0]; v[V_LNW:V_LNW + 512] = f(ln_x_w)[0]
    v[V_LNB:V_LNB + 512] = f(ln_x_b)[0]; v[V_RK:V_RK + 512] = f(r_k)[0].reshape(-1); v[V_SINK:V_SINK + 8] = f(attn_sinks)[0]
    v[V_FIN:] = f(norm_final_w)
    nwp = np.ones((128, 16), np.float32)
    nwp[:, 0:8] = f(norm_mix_w)[0].reshape(8, 128).T
    nwp[:, 8:16] = f(norm_ffn_w)[0].reshape(8, 128).T
    sk = f(peer_subkeys)[0]
    skT = np.ascontiguousarray(sk.transpose(1, 3, 0, 2).reshape(128, 8, 128))
    return dict(w_in=f(w_in)[0], vecs=v, nwp=nwp, cst=make_cst(), decay_w2=f(decay_w2)[0], iclr_a2=f(iclr_a2)[0],
                gate_g2=f(gate_g2)[0], proj_attn=f(proj_attn)[0], proj_rwkv=f(proj_rwkv)[0], w_out=f(w_out)[0],
                peer_wq=f(peer_wq)[0], skT=skT, uT=np.ascontiguousarray(f(peer_u)[0].T), v=f(peer_v)[0])


def kernel(**inputs):
    x = np.ascontiguousarray(np.asarray(inputs["x"], dtype=np.float32))
    B, S, Dm = x.shape
    common = _inputs(**inputs)
    spc = B // N_CORES
    nc = _build(spc, S // 128)
    in_maps = []
    for c in range(N_CORES):
        d = dict(common)
        d["x"] = np.ascontiguousarray(x[c * spc:(c + 1) * spc].reshape(spc * S, Dm))
        in_maps.append(d)
    res = run_bass_kernel_spmd(nc, in_maps, core_ids=list(range(N_CORES)))
    out = np.concatenate([r["out"].reshape(spc, S, Dm) for r in res.results], axis=0)
    return out.astype(np.float32)
```
